# Optimizing a Trainium2 kernel written in Bass

```python
import jax, jax.numpy as jnp
from jax import lax
import numpy as np


D_MODEL = 1024
BATCH = 16
SEQ = 4096
DEPTH = 2

GRID_W = 64
CTX_LEN = 256
ROPE_THETA = 10000.0
EPS = 1e-6
Q_BLOCK = 128

MLA_HEADS = 4
MLA_NOPE = 64
MLA_ROPE = 32
MLA_V = 64
MLA_Q_LORA = 256
MLA_KV_LORA = 128
MLA_SCALE = (MLA_NOPE + MLA_ROPE) ** -0.5
GQA_HEADS = 4
GQA_KV_HEADS = 2
GQA_HD = 64
NA_HEADS = 4
NA_HD = 64
NA_WIN_R = 8
NA_WIN_C = 16
RET_HEADS = 4
RET_DK = 64
RET_DV = 64
RET_CHUNK = 128
N_BRANCH = 4
BRANCH_W = 256
PEER_HEADS = 8
PEER_N_KEYS = 128
PEER_N_EXPERTS = PEER_N_KEYS * PEER_N_KEYS
PEER_TOPK = 16
PEER_DK = 128
PEER_BLOCK = 128

IN_SIZES = (MLA_Q_LORA, MLA_KV_LORA, MLA_ROPE,
            GQA_HEADS * GQA_HD, GQA_KV_HEADS * GQA_HD, GQA_KV_HEADS * GQA_HD,
            NA_HEADS * NA_HD, NA_HEADS * NA_HD, NA_HEADS * NA_HD,
            RET_HEADS * RET_DK, RET_HEADS * RET_DK, RET_HEADS * RET_DV,
            RET_HEADS * RET_DV, RET_HEADS * RET_DV,
            N_BRANCH * D_MODEL)
IN_COLS = sum(IN_SIZES)
IN_OFFSETS = tuple(int(v) for v in np.cumsum(IN_SIZES)[:-1])

kernel_name = 'hybrid_parallel_mla_gqa_natten_retention_peer'


def rmsnorm(x, w):
    xf = x.astype(jnp.float32)
    y = xf * lax.rsqrt(jnp.mean(xf * xf, axis=-1, keepdims=True) + EPS)
    return y.astype(x.dtype) * w


def rope_1d(x, pos):
    half = x.shape[-1] // 2
    freqs = ROPE_THETA ** (-jnp.arange(half, dtype=jnp.float32) / half)
    ang = pos[:, None] * freqs[None, :]
    cos = jnp.cos(ang)[:, None, :].astype(x.dtype)
    sin = jnp.sin(ang)[:, None, :].astype(x.dtype)
    x1, x2 = x[..., :half], x[..., half:]
    return jnp.concatenate([x1 * cos - x2 * sin, x1 * sin + x2 * cos], axis=-1)


def axial_rope(x, row, col):
    half = x.shape[-1] // 2
    return jnp.concatenate([rope_1d(x[..., :half], row), rope_1d(x[..., half:], col)], axis=-1)


def attend(q, k, v, scale):
    s = jnp.einsum('bqhgd,bkhd->bhgqk', q, k).astype(jnp.float32) * scale
    p = jax.nn.softmax(s, axis=-1).astype(v.dtype)
    return jnp.einsum('bhgqk,bkhe->bqhge', p, v)


def block_attend(q, k, v, scale):
    B, S = q.shape[:2]
    nb = S // Q_BLOCK
    qb = jnp.moveaxis(q.reshape((B, nb, Q_BLOCK) + q.shape[2:]), 1, 0)
    o = lax.map(lambda qi: attend(qi, k, v, scale), qb)
    return jnp.moveaxis(o, 0, 1).reshape((B, S) + o.shape[3:])


def neighbourhood_attend(q, k, v, k_ctx, v_ctx, bias):
    B, S, H, d = q.shape
    rows = S // GRID_W
    wr = min(NA_WIN_R, rows)
    wc = NA_WIN_C
    scale = d ** -0.5
    qg = q.reshape(B, rows, GRID_W, H, d)
    kg = k.reshape(B, rows, GRID_W, H, d)
    vg = v.reshape(B, rows, GRID_W, H, d)
    cols = np.arange(GRID_W)
    col_start = np.clip(cols - wc // 2, 0, GRID_W - wc)
    col_idx = col_start[:, None] + np.arange(wc)[None, :]
    rel_c = col_idx - cols[:, None] + (NA_WIN_C - 1)

    def one_row(args):
        r, q_r = args
        rs = jnp.clip(r - wr // 2, 0, rows - wr)
        k_rows = lax.dynamic_slice_in_dim(kg, rs, wr, axis=1)
        v_rows = lax.dynamic_slice_in_dim(vg, rs, wr, axis=1)
        k_win = k_rows[:, :, col_idx]
        v_win = v_rows[:, :, col_idx]
        rel_r = rs + jnp.arange(wr) - r + (NA_WIN_R - 1)
        b = bias[:, rel_r[None, :, None], rel_c[:, None, :]]
        s_win = jnp.einsum('bchd,brcwhd->bhcrw', q_r, k_win).astype(jnp.float32) * scale + b.astype(jnp.float32)
        s_ctx = jnp.einsum('bchd,bkhd->bhck', q_r, k_ctx).astype(jnp.float32) * scale
        s = jnp.concatenate([s_win.reshape(B, H, GRID_W, wr * wc), s_ctx], axis=-1)
        p = jax.nn.softmax(s, axis=-1).astype(v.dtype)
        p_win = p[..., :wr * wc].reshape(B, H, GRID_W, wr, wc)
        p_ctx = p[..., wr * wc:]
        return (jnp.einsum('bhcrw,brcwhd->bchd', p_win, v_win)
                + jnp.einsum('bhck,bkhd->bchd', p_ctx, v_ctx))

    o = lax.map(one_row, (jnp.arange(rows), jnp.moveaxis(qg, 1, 0)))
    return jnp.moveaxis(o, 0, 1).reshape(B, S, H * d)


def retention_scan(q, k, v, log_gamma, s0):
    B, H, L, _ = q.shape
    C = RET_CHUNK
    nc = L // C
    idx = jnp.arange(C, dtype=jnp.float32)
    diff = idx[:, None] - idx[None, :]
    decay = jnp.where(diff >= 0, jnp.exp(log_gamma[:, None, None] * jnp.maximum(diff, 0.0)), 0.0)
    q_decay = jnp.exp(log_gamma[:, None] * (idx + 1.0))[None, :, :, None]
    k_decay = jnp.exp(log_gamma[:, None] * (C - 1.0 - idx))[None, :, :, None]
    chunk_decay = jnp.exp(log_gamma * C)[None, :, None, None]

    def chunks(t):
        return jnp.moveaxis(t.reshape(B, H, nc, C, t.shape[-1]), 2, 0)

    def step(state, qkv):
        qc, kc, vc = qkv
        inner = jnp.einsum('bhnd,bhmd->bhnm', qc, kc) * decay
        o = (jnp.einsum('bhnm,bhme->bhne', inner, vc)
             + jnp.einsum('bhnd,bhde->bhne', qc, state) * q_decay)
        state = state * chunk_decay + jnp.einsum('bhmd,bhme->bhde', kc * k_decay, vc)
        return state, o

    state, o = lax.scan(step, s0, (chunks(q), chunks(k), chunks(v)))
    return jnp.moveaxis(o, 0, 2).reshape(B, H, L, -1), state


def head_groupnorm(o, w):
    B, H, L, dv = o.shape
    mu = jnp.mean(o, axis=-1, keepdims=True)
    var = jnp.mean(jnp.square(o - mu), axis=-1, keepdims=True)
    y = (o - mu) * lax.rsqrt(var + EPS)
    return jnp.moveaxis(y, 1, 2).reshape(B, L, H * dv) * w.astype(jnp.float32)


def retention_mixer(ret_x, ret_c, decay_logit, gn_w, need_ctx):
    def heads(t, d):
        B, L, _ = t.shape
        return jnp.moveaxis(t.reshape(B, L, RET_HEADS, d), 1, 2).astype(jnp.float32)

    def prep(r):
        q, k, v, gf, gb = r
        return heads(q, RET_DK), heads(k, RET_DK) * (RET_DK ** -0.5), heads(v, RET_DV), gf, gb

    qx, kx, vx, gfx, gbx = prep(ret_x)
    qc, kc, vc, gfc, gbc = prep(ret_c)
    log_g = jax.nn.log_sigmoid(decay_logit.astype(jnp.float32))
    flip = lambda t: jnp.flip(t, axis=2)
    s0 = jnp.zeros((qc.shape[0], RET_HEADS, RET_DK, RET_DV), jnp.float32)
    oc_f, st_f = retention_scan(qc, kc, vc, log_g[0], s0)
    oc_b, st_b = retention_scan(flip(qc), flip(kc), flip(vc), log_g[1], s0)
    ox_f, _ = retention_scan(qx, kx, vx, log_g[0], st_f)
    ox_b, _ = retention_scan(flip(qx), flip(kx), flip(vx), log_g[1], st_b)

    def combine(of, ob, gf, gb):
        y = (head_groupnorm(of, gn_w) * jax.nn.silu(gf.astype(jnp.float32))
             + head_groupnorm(flip(ob), gn_w) * jax.nn.silu(gb.astype(jnp.float32)))
        return y.astype(gf.dtype)

    yx = combine(ox_f, ox_b, gfx, gbx)
    yc = combine(oc_f, oc_b, gfc, gbc) if need_ctx else None
    return yx, yc


def stream_heads(proj, pos, mla_qn, mla_wuq, mla_kvn, mla_wukv, gqa_qn, gqa_kn):
    (cq, ckv, kpe, gq, gk, gv, nq, nk, nv, rq, rk, rv, rgf, rgb, gates) = jnp.split(proj, IN_OFFSETS, axis=-1)
    B, L = proj.shape[:2]
    qa = (rmsnorm(cq, mla_qn) @ mla_wuq).reshape(B, L, MLA_HEADS, MLA_NOPE + MLA_ROPE)
    kva = (rmsnorm(ckv, mla_kvn) @ mla_wukv).reshape(B, L, MLA_HEADS, MLA_NOPE + MLA_V)
    q_nope, q_pe = qa[..., :MLA_NOPE], qa[..., MLA_NOPE:]
    k_nope, v_a = kva[..., :MLA_NOPE], kva[..., MLA_NOPE:]
    k_pe = kpe[:, :, None, :]
    gq = rmsnorm(gq.reshape(B, L, GQA_HEADS, GQA_HD), gqa_qn)
    gk = rmsnorm(gk.reshape(B, L, GQA_KV_HEADS, GQA_HD), gqa_kn)
    if pos is not None:
        row, col = pos
        q_pe = axial_rope(q_pe, row, col)
        k_pe = axial_rope(k_pe, row, col)
        gq = axial_rope(gq, row, col)
        gk = axial_rope(gk, row, col)
    q_a = jnp.concatenate([q_nope, q_pe], axis=-1)[:, :, :, None, :]
    k_a = jnp.concatenate([k_nope, jnp.broadcast_to(k_pe, (B, L, MLA_HEADS, MLA_ROPE))], axis=-1)
    mla = (q_a, k_a, v_a)
    gqa = (gq.reshape(B, L, GQA_KV_HEADS, GQA_HEADS // GQA_KV_HEADS, GQA_HD), gk,
           gv.reshape(B, L, GQA_KV_HEADS, GQA_HD))
    na = (nq.reshape(B, L, NA_HEADS, NA_HD), nk.reshape(B, L, NA_HEADS, NA_HD),
          nv.reshape(B, L, NA_HEADS, NA_HD))
    ret = (rq, rk, rv, rgf, rgb)
    return mla, gqa, na, ret, gates


def merge_branches(outs, gates, w_branch, w_out):
    g = jnp.split(gates, N_BRANCH, axis=-1)
    acc = jax.nn.sigmoid(g[0]) * (outs[0] @ w_branch[0])
    for i in range(1, N_BRANCH):
        acc = acc + jax.nn.sigmoid(g[i]) * (outs[i] @ w_branch[i])
    return acc @ w_out


def hybrid_mixer(hx, hc, pos, w_in, mla_qn, mla_wuq, mla_kvn, mla_wukv, gqa_qn, gqa_kn,
                 na_bias, ret_decay_logit, ret_gn_w, w_branch, w_out, need_ctx):
    wts = (mla_qn, mla_wuq, mla_kvn, mla_wukv, gqa_qn, gqa_kn)
    mla_x, gqa_x, na_x, ret_x, gates_x = stream_heads(hx @ w_in, pos, *wts)
    mla_c, gqa_c, na_c, ret_c, gates_c = stream_heads(hc @ w_in, None, *wts)
    B, S = hx.shape[:2]
    cat = lambda a, b: jnp.concatenate([a, b], axis=1)
    oa = block_attend(mla_x[0], cat(mla_c[1], mla_x[1]), cat(mla_c[2], mla_x[2]), MLA_SCALE)
    ob = block_attend(gqa_x[0], cat(gqa_c[1], gqa_x[1]), cat(gqa_c[2], gqa_x[2]), GQA_HD ** -0.5)
    oc = neighbourhood_attend(na_x[0], na_x[1], na_x[2], na_c[1], na_c[2], na_bias)
    od, od_c = retention_mixer(ret_x, ret_c, ret_decay_logit, ret_gn_w, need_ctx)
    yx = merge_branches((oa.reshape(B, S, -1), ob.reshape(B, S, -1), oc, od), gates_x, w_branch, w_out)
    yc = None
    if need_ctx:
        L = hc.shape[1]
        ca = block_attend(mla_c[0], mla_c[1], mla_c[2], MLA_SCALE).reshape(B, L, -1)
        cb = block_attend(gqa_c[0], gqa_c[1], gqa_c[2], GQA_HD ** -0.5).reshape(B, L, -1)
        cc = block_attend(na_c[0][:, :, :, None, :], na_c[1], na_c[2], NA_HD ** -0.5).reshape(B, L, -1)
        yc = merge_branches((ca, cb, cc, od_c), gates_c, w_branch, w_out)
    return yx, yc


def peer_ffn(h, w_q, keys, u, v):
    B, L, D = h.shape
    nb = (B * L) // PEER_BLOCK
    xb = h.reshape(nb, PEER_BLOCK, D)

    def one_block(xt):
        q = (xt @ w_q).reshape(PEER_BLOCK, PEER_HEADS, 2, PEER_DK // 2)
        s = jnp.einsum('thpd,hpkd->thpk', q, keys).astype(jnp.float32)
        s1, i1 = lax.top_k(s[:, :, 0], PEER_TOPK)
        s2, i2 = lax.top_k(s[:, :, 1], PEER_TOPK)
        cand = (s1[..., :, None] + s2[..., None, :]).reshape(PEER_BLOCK, PEER_HEADS, PEER_TOPK * PEER_TOPK)
        cand_idx = (i1[..., :, None] * PEER_N_KEYS + i2[..., None, :]).reshape(PEER_BLOCK, PEER_HEADS, PEER_TOPK * PEER_TOPK)
        top_s, top_pos = lax.top_k(cand, PEER_TOPK)
        eidx = jnp.take_along_axis(cand_idx, top_pos, axis=-1)
        g = jax.nn.softmax(top_s, axis=-1)
        act = jax.nn.gelu(jnp.einsum('td,thkd->thk', xt, u[eidx]).astype(jnp.float32), approximate=False)
        return jnp.einsum('thk,thkd->td', (g * act).astype(xt.dtype), v[eidx])

    return lax.map(one_block, xb).reshape(B, L, D)


def setup_inputs(seed: int = 0) -> dict:
    key = jax.random.key(seed)
    ks = jax.random.split(key, 26)
    f32 = jnp.float32
    D = D_MODEL

    def nrm(k, shape, scale):
        return jax.random.normal(k, shape, f32) * scale

    gam = 1.0 - 2.0 ** (-5.0 - jnp.arange(RET_HEADS, dtype=f32))
    base_logit = jnp.log(gam) - jnp.log1p(-gam)
    return {
        'x': nrm(ks[0], (BATCH, SEQ, D), 1.0),
        'c': nrm(ks[1], (BATCH, D), 1.0),
        'ctx': nrm(ks[2], (BATCH, CTX_LEN, D), 1.0),
        'c_ctx': nrm(ks[3], (D,), 1.0),
        'mod_w': nrm(ks[4], (DEPTH, D, 6 * D), 0.5 * D ** -0.5),
        'mod_b': nrm(ks[5], (DEPTH, 6 * D), 0.01),
        'norm1_w': 1.0 + nrm(ks[6], (DEPTH, D), 0.02),
        'norm2_w': 1.0 + nrm(ks[7], (DEPTH, D), 0.02),
        'w_in': nrm(ks[8], (DEPTH, D, IN_COLS), D ** -0.5),
        'mla_q_norm': 1.0 + nrm(ks[9], (DEPTH, MLA_Q_LORA), 0.02),
        'mla_w_uq': nrm(ks[10], (DEPTH, MLA_Q_LORA, MLA_HEADS * (MLA_NOPE + MLA_ROPE)), MLA_Q_LORA ** -0.5),
        'mla_kv_norm': 1.0 + nrm(ks[11], (DEPTH, MLA_KV_LORA), 0.02),
        'mla_w_ukv': nrm(ks[12], (DEPTH, MLA_KV_LORA, MLA_HEADS * (MLA_NOPE + MLA_V)), MLA_KV_LORA ** -0.5),
        'gqa_q_norm': 1.0 + nrm(ks[13], (DEPTH, GQA_HD), 0.02),
        'gqa_k_norm': 1.0 + nrm(ks[14], (DEPTH, GQA_HD), 0.02),
        'na_bias': nrm(ks[15], (DEPTH, NA_HEADS, 2 * NA_WIN_R - 1, 2 * NA_WIN_C - 1), 0.1),
        'ret_decay_logit': base_logit[None, None, :] + nrm(ks[16], (DEPTH, 2, RET_HEADS), 0.1),
        'ret_gn_w': 1.0 + nrm(ks[17], (DEPTH, RET_HEADS * RET_DV), 0.02),
        'w_branch': nrm(ks[18], (DEPTH, N_BRANCH, BRANCH_W, D), BRANCH_W ** -0.5),
        'w_out': nrm(ks[19], (DEPTH, D, D), D ** -0.5),
        'peer_w_q': nrm(ks[20], (DEPTH, D, PEER_HEADS * PEER_DK), D ** -0.5),
        'peer_keys': nrm(ks[21], (DEPTH, PEER_HEADS, 2, PEER_N_KEYS, PEER_DK // 2), (PEER_DK // 2) ** -0.5),
        'peer_u': nrm(ks[22], (DEPTH, PEER_N_EXPERTS, D), D ** -0.5),
        'peer_v': nrm(ks[23], (DEPTH, PEER_N_EXPERTS, D), PEER_HEADS ** -0.5),
        'final_norm_w': 1.0 + nrm(ks[24], (D,), 0.02),
    }


def reference(x, c, ctx, c_ctx, mod_w, mod_b, norm1_w, norm2_w, w_in, mla_q_norm, mla_w_uq,
              mla_kv_norm, mla_w_ukv, gqa_q_norm, gqa_k_norm, na_bias, ret_decay_logit, ret_gn_w,
              w_branch, w_out, peer_w_q, peer_keys, peer_u, peer_v, final_norm_w):
    S = x.shape[1]
    t = jnp.arange(S)
    pos = ((t // GRID_W).astype(jnp.float32), (t % GRID_W).astype(jnp.float32))
    for l in range(DEPTH):
        need_ctx = l < DEPTH - 1
        mx = (jax.nn.silu(c) @ mod_w[l] + mod_b[l])[:, None, :]
        mc = (jax.nn.silu(c_ctx) @ mod_w[l] + mod_b[l])[None, None, :]
        sh1x, sc1x, g1x, sh2x, sc2x, g2x = jnp.split(mx, 6, axis=-1)
        sh1c, sc1c, g1c, sh2c, sc2c, g2c = jnp.split(mc, 6, axis=-1)
        hx = rmsnorm(x, norm1_w[l]) * (1.0 + sc1x) + sh1x
        hc = rmsnorm(ctx, norm1_w[l]) * (1.0 + sc1c) + sh1c
        yx, yc = hybrid_mixer(hx, hc, pos, w_in[l], mla_q_norm[l], mla_w_uq[l], mla_kv_norm[l],
                              mla_w_ukv[l], gqa_q_norm[l], gqa_k_norm[l], na_bias[l],
                              ret_decay_logit[l], ret_gn_w[l], w_branch[l], w_out[l], need_ctx)
        x = x + g1x * yx
        hx2 = rmsnorm(x, norm2_w[l]) * (1.0 + sc2x) + sh2x
        x = x + g2x * peer_ffn(hx2, peer_w_q[l], peer_keys[l], peer_u[l], peer_v[l])
        if need_ctx:
            ctx = ctx + g1c * yc
            hc2 = rmsnorm(ctx, norm2_w[l]) * (1.0 + sc2c) + sh2c
            ctx = ctx + g2c * peer_ffn(hc2, peer_w_q[l], peer_keys[l], peer_u[l], peer_v[l])
    return rmsnorm(x, final_norm_w)
```

```python
import ml_dtypes
from concourse.bass_utils import run_bass_kernel_spmd
import contextlib
import numpy as np
import concourse.bass as bass
import concourse.mybir as mybir

F32 = mybir.dt.float32
BF16 = mybir.dt.bfloat16
I32 = mybir.dt.int32
U32 = mybir.dt.uint32
ALU = mybir.AluOpType
AF = mybir.ActivationFunctionType
AX = mybir.AxisListType


class Dep:
    __slots__ = ("w", "r")

    def __init__(self):
        self.w = None
        self.r = []


class View:
    __slots__ = ("t", "key", "ap")

    def __init__(self, t, key, ap):
        self.t = t
        self.key = key
        self.ap = ap


class _Sub:
    def __init__(self, t, key):
        self.t = t
        self.key = key

    def __getitem__(self, idx):
        return View(self.t, self.key, self.t.base[idx])


class T:
    def __init__(self, name, base, space):
        self.name = name
        self.base = base
        self.space = space
        self.whole = Dep()
        self.subs = {}

    def __getitem__(self, idx):
        return View(self, None, self.base[idx])

    def k(self, key):
        return _Sub(self, key)

    def v(self, ap, key=None):
        return View(self, key, ap)

    def deps(self, key):
        if key is None:
            return [self.whole] + list(self.subs.values())
        if key not in self.subs:
            self.subs[key] = Dep()
        return [self.whole, self.subs[key]]

    def own(self, key):
        if key is None:
            return self.whole
        if key not in self.subs:
            self.subs[key] = Dep()
        return self.subs[key]


class K:
    ENG = ("pe", "act", "dve", "pool", "sp")

    def __init__(self, n_dma_sems=24):
        self.nc = bass.Bass("TRN2", target_bir_lowering=False)
        self.es = contextlib.ExitStack()
        self.stack = [self.es]
        nc = self.nc
        self.eng = {"pe": nc.tensor, "act": nc.scalar, "dve": nc.vector, "pool": nc.gpsimd, "sp": nc.sync}
        self.sems = []
        self.semval = []
        self.esem = {}
        for e in ("pe", "act", "dve", "pool"):
            self.esem[e] = self._newsem("s_" + e)
        self.dsem = {}
        self.dnext = {}
        for q in ("sp", "pool", "act"):
            n = n_dma_sems if q != "act" else 8
            self.dsem[q] = [self._newsem(f"d_{q}{i}") for i in range(n)]
            self.dnext[q] = 0
        self.known = {e: {} for e in self.ENG}
        self.n_inst = 0
        self.embed = True
        self.n_wait = 0
        self._uid = 0

    def _newsem(self, name):
        h = self.es.enter_context(self.nc.semaphore(name))
        self.sems.append(h)
        self.semval.append(0)
        return len(self.sems) - 1

    def uid(self, p):
        self._uid += 1
        return f"{p}{self._uid}"

    def sb(self, name, shape, dtype):
        h = self.stack[-1].enter_context(self.nc.sbuf_tensor(self.uid("s_" + name + "_"), list(shape), dtype))
        return T(name, h, "sb")

    def ps(self, name, shape, dtype=F32):
        h = self.stack[-1].enter_context(self.nc.psum_tensor(self.uid("p_" + name + "_"), list(shape), dtype))
        return T(name, h, "ps")

    def push(self):
        self.stack.append(contextlib.ExitStack())

    def pop(self):
        self.barrier()
        self.stack.pop().close()

    def barrier(self, engines=("pe", "act", "dve", "pool", "sp")):
        for e in engines:
            for s in range(len(self.sems)):
                if self.semval[s] > 0:
                    self._wait(e, (s, self.semval[s]))

    def dram(self, name, shape, dtype, kind="Internal"):
        h = self.nc.dram_tensor(name, list(shape), dtype, kind=kind)
        return T(name, h.ap(), "dram")

    def _wait(self, e, ev):
        if ev is None:
            return
        s, v = ev
        if self.known[e].get(s, 0) >= v:
            return
        self.eng[e].wait_ge(self.sems[s], v)
        self.known[e][s] = v
        self.n_wait += 1

    def _sync_in(self, e, r, w):
        need = {}

        def add(ev):
            if ev is None:
                return
            s, v = ev
            if e == "pe" and s == self.esem["pe"]:
                return
            if need.get(s, 0) < v:
                need[s] = v

        for v in r:
            for d in v.t.deps(v.key):
                add(d.w)
        for v in w:
            for d in v.t.deps(v.key):
                add(d.w)
                for rv in d.r:
                    add(rv)
        pend = [(s, v) for s, v in need.items() if self.known[e].get(s, 0) < v]
        if self.embed and pend:
            for ev in pend[:-1]:
                self._wait(e, ev)
            return pend[-1]
        for ev in pend:
            self._wait(e, ev)
        return None

    def _sync_out(self, ev, r, w):
        for v in r:
            v.t.own(v.key).r.append(ev)
            if len(v.t.own(v.key).r) > 64:
                mx = {}
                for s, val in v.t.own(v.key).r:
                    mx[s] = max(mx.get(s, 0), val)
                v.t.own(v.key).r = list(mx.items())
        for v in w:
            if v.key is None:
                v.t.subs = {}
            d = v.t.own(v.key)
            d.w = ev
            d.r = []

    def op(self, e, fn, r=(), w=()):
        r = [x for x in r if isinstance(x, View)]
        w = [x for x in w if isinstance(x, View)]
        emb = self._sync_in(e, r, w)
        ins = fn(self.eng[e])
        if emb is not None:
            ins._wait_ge(self.sems[emb[0]], emb[1])
            self.known[e][emb[0]] = emb[1]
        s = self.esem[e]
        self.semval[s] += 1
        ins.then_inc(self.sems[s], 1)
        ev = (s, self.semval[s])
        self._sync_out(ev, r, w)
        self.n_inst += 1
        return ins

    def dma(self, out, in_, q="sp", fn=None, extra_r=()):
        r = [in_] + list(extra_r)
        w = [out]
        pool = self.dsem[q]
        i = self.dnext[q]
        self.dnext[q] = (i + 1) % len(pool)
        s = pool[i]
        self._wait(q, (s, self.semval[s]))
        emb = self._sync_in(q, r, w)
        if fn is None:
            ins = self.eng[q].dma_start(out=out.ap, in_=in_.ap)
        else:
            ins = fn(self.eng[q])
        if emb is not None:
            ins._wait_ge(self.sems[emb[0]], emb[1])
            self.known[q][emb[0]] = emb[1]
        self.semval[s] += 16
        ins.then_inc(self.sems[s], 16)
        ev = (s, self.semval[s])
        self._sync_out(ev, r, w)
        self.n_inst += 1
        return ins

    def wait_all(self, e, views):
        for v in views:
            for d in v.t.deps(v.key):
                self._wait(e, d.w)

    def mm(self, out, lhsT, rhs, start=True, stop=True, **kw):
        return self.op("pe", lambda e: e.matmul(out.ap, lhsT.ap, rhs.ap, start=start, stop=stop, **kw),
                       r=[lhsT, rhs] + ([] if start else [out]), w=[out])

    def tr(self, out, in_, ident):
        return self.op("pe", lambda e: e.transpose(out.ap, in_.ap, ident.ap), r=[in_, ident], w=[out])

    def act(self, out, in_, func, bias=None, scale=None, accum=None, e="act"):
        kw = {}
        r = [in_]
        w = [out]
        if bias is not None:
            kw["bias"] = bias.ap if isinstance(bias, View) else bias
            r.append(bias)
        if scale is not None:
            kw["scale"] = scale.ap if isinstance(scale, View) else scale
            r.append(scale)
        if accum is not None:
            kw["accum_out"] = accum.ap
            w.append(accum)
        return self.op(e, lambda en: en.activation(out=out.ap, in_=in_.ap, func=func, **kw), r=r, w=w)

    def tt(self, out, a, b, op, e="dve"):
        return self.op(e, lambda en: en.tensor_tensor(out.ap, a.ap, b.ap, op), r=[a, b], w=[out])

    def ts(self, out, a, s1, op0, s2=None, op1=None, e="dve", accum=None):
        r = [a, s1, s2]
        w = [out]
        kw = {}
        if accum is not None:
            kw["accum_out"] = accum.ap
            w.append(accum)
        g = lambda s: s.ap if isinstance(s, View) else s
        if op1 is None:
            return self.op(e, lambda en: en.tensor_scalar(out.ap, a.ap, g(s1), None, op0, **kw), r=r, w=w)
        return self.op(e, lambda en: en.tensor_scalar(out.ap, a.ap, g(s1), g(s2), op0, op1, **kw), r=r, w=w)

    def stt(self, out, a, s, b, op0, op1, e="dve", accum=None):
        g = lambda x: x.ap if isinstance(x, View) else x
        w = [out]
        kw = {}
        if accum is not None:
            kw["accum_out"] = accum.ap
            w.append(accum)
        return self.op(e, lambda en: en.scalar_tensor_tensor(out.ap, a.ap, g(s), b.ap, op0, op1, **kw),
                       r=[a, s, b], w=w)

    def copy(self, out, in_, e="dve"):
        if e == "act":
            return self.op(e, lambda en: en.copy(out.ap, in_.ap), r=[in_], w=[out])
        return self.op(e, lambda en: en.tensor_copy(out.ap, in_.ap), r=[in_], w=[out])

    def recip(self, out, in_):
        return self.op("dve", lambda en: en.reciprocal(out.ap, in_.ap), r=[in_], w=[out])

    def memset(self, out, val, e="dve"):
        return self.op(e, lambda en: en.memset(out.ap, val), r=[], w=[out])

    def reduce(self, out, in_, op, axis=AX.X, e="dve"):
        return self.op(e, lambda en: en.tensor_reduce(out.ap, in_.ap, axis, op), r=[in_], w=[out])

    def finish(self):
        self.barrier(engines=("sp",))

D = 1024
S = 4096
LC = 256
TT = S + LC
NL = 2
GW = 64
EPS = 1e-6
INC = 7072
NCORES = 8
BF = ml_dtypes.bfloat16
import os
PEXP = os.environ.get('PEXP', '')


class Pool:
    def __init__(self, k, name, shape, dtype, n, space="sb"):
        mk = k.sb if space == "sb" else k.ps
        self.t = [mk(f"{name}{i}", shape, dtype) for i in range(n)]
        self.i = 0

    def nxt(self):
        t = self.t[self.i]
        self.i = (self.i + 1) % len(self.t)
        return t


def rope_tables():
    t = np.arange(S)
    row = (t // GW).astype(np.float32)
    col = (t % GW).astype(np.float32)

    def tab(hd):
        half = hd // 2
        h = half // 2
        freqs = (np.float32(10000.0) ** (-np.arange(h, dtype=np.float32) / np.float32(h))).astype(np.float32)
        C = np.zeros((hd, S), np.float32)
        Sn = np.zeros((hd, S), np.float32)
        for ax, pos in enumerate((row, col)):
            ang = (pos[:, None] * freqs[None, :]).astype(np.float32)
            c = np.cos(ang).astype(np.float32).T
            s = np.sin(ang).astype(np.float32).T
            o = ax * half
            C[o:o + h] = c
            C[o + h:o + 2 * h] = c
            Sn[o:o + h] = s
            Sn[o + h:o + 2 * h] = s
        return C, Sn

    def rmat(hd):
        half = hd // 2
        h = half // 2
        Rm = np.zeros((hd, hd), np.float32)
        for ax in range(2):
            o = ax * half
            for i in range(h):
                Rm[o + i, o + h + i] = -1.0
                Rm[o + h + i, o + i] = 1.0
        return Rm.T.copy()

    Cg, Sg = tab(64)
    Cm, Sm = tab(32)
    out = {}
    out["ropeCg"] = np.concatenate([Cg, Cg], 0)
    out["ropeSg"] = np.concatenate([Sg, Sg], 0)
    z = np.zeros((32, S), np.float32)
    out["ropeCm"] = np.concatenate([Cm, z, Cm], 0)
    out["ropeSm"] = np.concatenate([Sm, z, Sm], 0)
    Rg = np.zeros((128, 128), np.float32)
    Rg[0:64, 0:64] = rmat(64)
    Rg[64:128, 64:128] = rmat(64)
    Rq = np.zeros((96, 96), np.float32)
    Rq[64:96, 64:96] = rmat(32)
    Rq[0:32, 0:32] = rmat(32)
    out["Rg"] = Rg.astype(BF)
    out["Rq"] = Rq.astype(BF)
    return out


def misc_consts():
    out = {}
    out["ident_bf"] = np.eye(128, dtype=np.float32).astype(BF)
    out["ident_f"] = np.eye(128, dtype=np.float32)
    out["ones_f"] = np.ones((128, 128), np.float32)
    ob = np.zeros((128, 128), np.float32)
    ob[0:64, 0:64] = 1.0
    ob[64:, 64:] = 1.0
    out["onesblk_f"] = ob
    io = np.zeros((128, 8, 16, 16), np.float32)
    io[:] = np.arange(16, dtype=np.float32)[None, None, None, :]
    out["iota4"] = io
    m = np.arange(128)[:, None]
    n = np.arange(128)[None, :]
    Df = np.where(n >= m, (n - m), 100000).astype(np.float32)
    Db = np.where(n <= m, (m - n), 100000).astype(np.float32)
    out["retD"] = np.stack([Df, Db], 0)
    qa = np.zeros((2, 128, 128), np.float32)
    qa[0] = (np.arange(128) + 1)[None, :]
    qa[1] = (128 - np.arange(128))[None, :]
    out["retQA"] = qa
    ka = np.zeros((128, 2), np.float32)
    ka[:, 0] = 127 - np.arange(128)
    ka[:, 1] = np.arange(128)
    out["retKA"] = ka
    return out


def na_biasmask(na_bias):
    L = na_bias.shape[0]
    out = np.full((L, 4, 3, 8, 128, 512), -30000.0, np.float32)
    for pat, qg in enumerate((0, 3, 7)):
        r0 = 8 * qg
        kr0 = int(np.clip(r0 - 4, 0, 48))
        r = r0 + np.arange(8)
        rs = np.clip(r - 4, 0, 56)
        c = np.arange(64)
        cs = np.clip(c - 8, 0, 48)
        for kc in range(8):
            kr = kr0 + 2 * kc + np.arange(2)
            KR = kr[:, None, None, None]
            CP = c[None, :, None, None]
            R = r[None, None, :, None]
            RS = rs[None, None, :, None]
            C = c[None, None, None, :]
            CS = cs[None, None, None, :]
            valid = (KR >= RS) & (KR < RS + 8) & (CP >= CS) & (CP < CS + 16)
            rel_r = np.clip(KR - R + 7, 0, 14)
            rel_c = np.clip(CP - C + 15, 0, 30)
            rel_r, rel_c, valid = np.broadcast_arrays(rel_r, rel_c, valid)
            for l in range(L):
                for h in range(4):
                    vals = na_bias[l, h][rel_r, rel_c]
                    out[l, h, pat, kc] = np.where(valid, vals, np.float32(-30000.0)).reshape(128, 512)
    return out


def build(NB=2, layers=(0, 1), dbg=(), stop=None):
    k = K()
    nc = k.nc
    dbg = set(dbg)
    IN = lambda name, shape, dt=F32: k.dram(name, shape, dt, kind="ExternalInput")

    def SCR(name, shape, dt=BF16):
        return k.dram(name, shape, dt, kind=("ExternalOutput" if name in dbg else "Internal"))

    x_in = IN("x", [NB, S, D])
    ctx_in = IN("ctx", [NB, LC, D])
    cT3 = IN("cT3", [128, 8, NB + 1])
    mod_w = IN("mod_w", [NL, D, 6 * D])
    mod_bT = IN("mod_bT", [NL, 128, 48])
    mod_b = IN("mod_b", [NL, 6 * D])
    n1wT = IN("n1wT", [NL, 128, 8])
    n2wT = IN("n2wT", [NL, 128, 8])
    w_in = IN("w_in", [NL, D, INC])
    qnT = IN("qnT", [NL, 128, 2])
    kvnT = IN("kvnT", [NL, 128, 1])
    gqnT = IN("gqnT", [NL, 128, 1])
    gknT = IN("gknT", [NL, 128, 1])
    wuq = IN("mla_w_uq", [NL, 256, 384])
    wukv = IN("mla_w_ukv", [NL, 128, 512])
    nabm = IN("nabm", [NL, 4, 3, 8, 128, 512])
    rdl = IN("ret_decay_logit", [NL, 8])
    gnw = IN("ret_gn_w", [NL, 256])
    w_branch = IN("w_branch", [NL, 4, 256, D])
    w_out = IN("w_out", [NL, D, D])
    peer_wq = IN("peer_w_q", [NL, D, D])
    keysbd = IN("keysbd", [NL, 8, 128, 256])
    peer_u = [IN("peer_u%d" % i, [16384, D]) for i in range(NL)]
    peer_v = [IN("peer_v%d" % i, [16384, D]) for i in range(NL)]
    fnw = IN("final_norm_w", [D])
    c_ident_bf = IN("ident_bf", [128, 128], BF16)
    c_ident_f = IN("ident_f", [128, 128])
    c_ones_f = IN("ones_f", [128, 128])
    c_onesblk = IN("onesblk_f", [128, 128])
    c_iota4 = IN("iota4", [128, 8, 16, 16])
    c_retD = IN("retD", [2, 128, 128])
    c_retQA = IN("retQA", [2, 128, 128])
    c_retKA = IN("retKA", [128, 2])
    c_Cg = IN("ropeCg", [128, S])
    c_Sg = IN("ropeSg", [128, S])
    c_Cm = IN("ropeCm", [96, S])
    c_Sm = IN("ropeSm", [96, S])
    c_Rg = IN("Rg", [128, 128], BF16)
    c_Rq = IN("Rq", [96, 96], BF16)
    out_d = k.dram("out", [NB, S, D], F32, kind="ExternalOutput")

    xres = SCR("xres", [NB, S, D], F32)
    cres = SCR("cres", [NB, LC, D], F32)
    wb_in = SCR("wb_in", [NL, D, INC])
    wb_uq = SCR("wb_uq", [NL, 256, 384])
    wb_ukv = SCR("wb_ukv", [NL, 128, 512])
    wb_br = SCR("wb_br", [NL, 4, 256, D])
    wb_out = SCR("wb_out", [NL, D, D])
    wb_pq = SCR("wb_pq", [NL, D, D])
    wb_keys = SCR("wb_keys", [NL, 8, 128, 256])
    uvb = [SCR("uvb%d" % i, [16384, 2 * D]) for i in range(NL)]
    nabb = SCR("nabb", [NL, 4, 3, 8, 128, 512])
    mqT = SCR("mqT", [384, NB, TT])
    mknT = SCR("mknT", [256, NB, TT])
    mkpT = SCR("mkpT", [32, NB, TT])
    mv = SCR("mv", [NB, TT, 256])
    gqT = SCR("gqT", [256, NB, TT])
    gkT = SCR("gkT", [128, NB, TT])
    gv = SCR("gv", [NB, TT, 128])
    nqT = SCR("nqT", [256, NB, TT])
    nkT = SCR("nkT", [256, NB, TT])
    nv = SCR("nv", [NB, TT, 256])
    rqT = SCR("rqT", [256, NB, TT])
    rkT = SCR("rkT", [256, NB, TT])
    rkv = SCR("rkv", [NB, TT, 512])
    rg = SCR("rg", [NB, TT, 512])
    gT = SCR("gT", [4096, NB, TT])
    oT = SCR("oT", [1024, NB, TT])

    ident_bf = k.sb("ident_bf", [128, 128], BF16)
    ident_f = k.sb("ident_f", [128, 128], F32)
    ones_f = k.sb("ones_f", [128, 128], F32)
    onesblk = k.sb("onesblk", [128, 128], F32)
    Rg = k.sb("Rg", [128, 128], BF16)
    Rq = k.sb("Rq", [96, 96], BF16)
    for sbt, dr in ((ident_bf, c_ident_bf), (ident_f, c_ident_f), (ones_f, c_ones_f), (onesblk, c_onesblk), (Rg, c_Rg)):
        k.dma(sbt[:], dr[:, :])
    k.dma(Rq[:], c_Rq[:, :])

    psA = Pool(k, "psA", [128, 512], F32, 6, space="ps")
    psB = Pool(k, "psB", [128, 512], F32, 2, space="ps")
    P = psA.nxt

    def bfview(pt, a, b, p0=0, p1=128):
        return pt.v(pt.base.bitcast(BF16)[p0:p1, a:b])

    evn = [0]

    def evac(out, in_, scale=None, func=None):
        evn[0] += 1
        if func is not None:
            return k.act(out, in_, func, scale=scale)
        if evn[0] % 2 == 0:
            return k.act(out, in_, AF.Copy, scale=scale)
        if scale is None:
            return k.copy(out, in_)
        return k.ts(out, in_, scale, ALU.mult)

    k.push()
    cvp_f = Pool(k, "cvf", [128, 2048], F32, 3)
    cvp_b = Pool(k, "cvb", [128, 2048], BF16, 3)
    cvn = [0]

    def conv_all(l):
        def flat(ap, pat, **kw):
            return ap.rearrange(pat, **kw)
        convert(flat(w_in.base[l], "(p a) c -> p (a c)", p=128), wb_in, flat(wb_in.base[l], "(p a) c -> p (a c)", p=128), 8 * INC)
        convert(flat(wuq.base[l], "(p a) c -> p (a c)", p=128), wb_uq, flat(wb_uq.base[l], "(p a) c -> p (a c)", p=128), 2 * 384)
        convert(wukv.base[l], wb_ukv, wb_ukv.base[l], 512)
        for i in range(4):
            convert(flat(w_branch.base[l, i], "(p a) c -> p (a c)", p=128), wb_br, flat(wb_br.base[l, i], "(p a) c -> p (a c)", p=128), 2 * D)
        convert(flat(w_out.base[l], "(p a) c -> p (a c)", p=128), wb_out, flat(wb_out.base[l], "(p a) c -> p (a c)", p=128), 8 * D)
        convert(flat(peer_wq.base[l], "(p a) c -> p (a c)", p=128), wb_pq, flat(wb_pq.base[l], "(p a) c -> p (a c)", p=128), 8 * D)
        for h in range(8):
            convert(keysbd.base[l, h], wb_keys, wb_keys.base[l, h], 256)
        for h in range(4):
            for t_ in range(3):
                for c in range(8):
                    convert(nabm.base[l, h, t_, c], nabb, nabb.base[l, h, t_, c], 512)
        for a in range(128):
            convert(peer_u[l].base.rearrange("(p a) c -> p a c", p=128)[:, a, :], uvb[l], uvb[l].base.rearrange("(p a) c -> p a c", p=128)[:, a, 0:D], D)
            convert(peer_v[l].base.rearrange("(p a) c -> p a c", p=128)[:, a, :], uvb[l], uvb[l].base.rearrange("(p a) c -> p a c", p=128)[:, a, D:2 * D], D)

    ro = T("ro", None, "dram")

    def RO(ap):
        return View(ro, "r", ap)

    def convert(src_ap, dst_t, dst_ap, nper):
        for c0 in range(0, nper, 2048):
            n = min(2048, nper - c0)
            f = cvp_f.nxt()
            b = cvp_b.nxt()
            k.dma(f[:, 0:n], RO(src_ap[:, c0:c0 + n]))
            e = ("dve", "pool", "act")[cvn[0] % 3]
            cvn[0] += 1
            k.copy(b[:, 0:n], f[:, 0:n], e=e)
            k.dma(dst_t.v(dst_ap[:, c0:c0 + n]), b[:, 0:n], q="pool")

    for l in layers:
        conv_all(l)
    k.pop()

    modF = k.sb("modF", [128, 48, NB + 1], F32)
    A1 = k.sb("A1", [128, 8, NB + 1], F32)
    A2 = k.sb("A2", [128, 8, NB + 1], F32)
    grep = k.sb("grep", [128, 2, NB + 1, D], F32)
    cT = k.sb("cT", [128, 8, NB + 1], F32)
    k.dma(cT[:], cT3[:, :, :])
    k.act(cT[:], cT[:], AF.Silu)
    mbT = k.sb("mbT", [128, 48], F32)
    nwT = k.sb("nwT", [128, 2, 8], F32)

    def phase_mod(l):
        k.push()
        mwp = Pool(k, "mw", [128, 8, 512], F32, 2)
        mbrep = k.sb("mbrep", [128, 512], F32)
        cTb = k.sb("cTb", [128, NB + 1, 8, 128], F32)
        for col in range(NB + 1):
            for j in range(8):
                k.copy(cTb[:, col, j, :], cT.v(cT.base[:, j, col:col + 1].to_broadcast([128, 128])))
        k.dma(mbT[:], mod_bT[l])
        k.dma(nwT[:, 0, :], n1wT[l])
        k.dma(nwT[:, 1, :], n2wT[l])
        mwv = mod_w.base[l].rearrange("(j p) c -> p j c", p=128)
        fm_blocks = {0: 0, 1: 4, 2: 8, 3: 12, 6: 24, 7: 28, 8: 32, 9: 36}
        g_blocks = {4: (0, 0), 5: (0, 512), 10: (1, 0), 11: (1, 512)}
        for cb in range(12):
            mw = mwp.nxt()
            k.dma(mw[:], RO(mwv[:, :, cb * 512:(cb + 1) * 512]))
            if cb in fm_blocks:
                for o4 in range(4):
                    oc = cb * 4 + o4
                    p = P()
                    for j in range(8):
                        k.mm(p[:, 0:NB + 1], mw[:, j, o4 * 128:(o4 + 1) * 128], cT[:, j, :], start=(j == 0), stop=(j == 7))
                    k.ts(modF[:, oc, :], p[:, 0:NB + 1], mbT[:, oc:oc + 1], ALU.add)
            else:
                gi, off = g_blocks[cb]
                k.dma(mbrep[:], RO(mod_b.base[l, cb * 512:(cb + 1) * 512].partition_broadcast(128)))
                for col in range(NB + 1):
                    p = P()
                    for j in range(8):
                        k.mm(p[:, :], cTb[:, col, j, :], mw[:, j, :], start=(j == 0), stop=(j == 7))
                    k.tt(grep[:, gi, col, off:off + 512], p[:, :], mbrep[:], ALU.add)
        for j in range(8):
            k.ts(A1[:, j, :], modF[:, 8 + j, :], 1.0, ALU.add)
            k.ts(A1[:, j, :], A1[:, j, :], nwT[:, 0, j:j + 1], ALU.mult)
            k.ts(A2[:, j, :], modF[:, 32 + j, :], 1.0, ALU.add)
            k.ts(A2[:, j, :], A2[:, j, :], nwT[:, 1, j:j + 1], ALU.mult)
        k.pop()

    NP = {}

    def norm_pools():
        NP["junk"] = Pool(k, "junk", [128, D], BF16, 2)
        NP["xn"] = Pool(k, "xn", [128, D], BF16, 2)
    ssp = Pool(k, "ss", [128, 2], F32, 4)

    def norm_T(xt, hxT, i, Am, Bm, col):
        ss = ssp.nxt()
        jk = NP["junk"].nxt()
        k.act(jk[:], xt[:], AF.Square, accum=ss[:, 0:1])
        k.act(ss[:, 1:2], ss[:, 0:1], AF.Sqrt, bias=EPS, scale=1.0 / D)
        k.recip(ss[:, 1:2], ss[:, 1:2])
        xn = NP["xn"].nxt()
        k.ts(xn[:], xt[:], ss[:, 1:2], ALU.mult)
        pt = P()
        for j in range(8):
            k.tr(bfview(pt, j * 128, (j + 1) * 128), xn[:, j * 128:(j + 1) * 128], ident_bf[:])
        for j in range(8):
            o = hxT[:, j, i * 128:(i + 1) * 128]
            src = bfview(pt, j * 128, (j + 1) * 128)
            if j % 2 == 0:
                k.act(o, src, AF.Identity, bias=Bm(j, col), scale=Am[:, j, col:col + 1])
            else:
                k.ts(o, src, Am[:, j, col:col + 1], ALU.mult, Bm(j, col), ALU.add)

    wuq_sb = k.sb("wuq_sb", [128, 2, 384], BF16)
    wkn_sb = k.sb("wkn_sb", [128, 4, 64], BF16)
    wkv_sb = k.sb("wkv_sb", [128, 4, 64], BF16)
    nrmw = k.sb("nrmw", [128, 5], F32)

    def fm_rstd(rsp, sqs, ones_t, nfeat, n):
        p = P()
        for c, sq in enumerate(sqs):
            k.mm(p[:, 0:n], ones_t[:], sq, start=(c == 0), stop=(c == len(sqs) - 1))
        rs = rsp.nxt()
        k.act(rs[:, 0:n], p[:, 0:n], AF.Sqrt, bias=EPS, scale=1.0 / nfeat)
        k.recip(rs[:, 0:n], rs[:, 0:n])
        return rs

    def load_layer_small(l):
        k.dma(wuq_sb[:], wb_uq.v(wb_uq.base[l].rearrange("(c p) m -> p c m", p=128)))
        wv = wb_ukv.base[l].rearrange("p (h t e) -> p h t e", h=4, t=2)
        k.dma(wkn_sb[:], wb_ukv.v(wv[:, :, 0, :]))
        k.dma(wkv_sb[:], wb_ukv.v(wv[:, :, 1, :]))
        k.dma(nrmw[:, 0:2], qnT[l])
        k.dma(nrmw[:, 2:3], kvnT[l])
        k.dma(nrmw[:, 3:4], gqnT[l])
        k.dma(nrmw[:, 4:5], gknT[l])

    def groups_for(l, with_ctx=True):
        gs = []
        for b in range(NB):
            gs.append(dict(b=b, t0=0, n=LC, lat=False, col=NB, s0=0))
            for g in range(S // 512):
                gs.append(dict(b=b, t0=LC + g * 512, n=512, lat=True, col=b, s0=g * 512))
        return gs

    def src_rows(l, g, i):
        b = g["b"]
        if g["lat"]:
            r0 = g["s0"] + i * 128
            return (RO(x_in.base[b, r0:r0 + 128, :]) if l == layers[0] and l == 0 else xres[b, r0:r0 + 128, :])
        r0 = i * 128
        return (RO(ctx_in.base[b, r0:r0 + 128, :]) if l == 0 else cres[b, r0:r0 + 128, :])

    def phase_proj(l):
        k.push()
        norm_pools()
        xpool = Pool(k, "xt", [128, D], F32, 3)
        hxTp = Pool(k, "hxT", [128, 8, 512], BF16, 2)
        wpool = Pool(k, "wblk", [128, 8, 512], BF16, 3)
        sqp = Pool(k, "sq", [128, 2, 512], F32, 2)
        rsp = Pool(k, "rs", [128, 512], F32, 2)
        nrm = Pool(k, "nrm", [128, 2, 512], BF16, 2)
        stg = Pool(k, "stg", [128, 512], BF16, 6)
        tmpf = Pool(k, "tmpf", [128, 512], F32, 4)
        ropeT = Pool(k, "ropeT", [128, 4, 512], F32, 2)
        wv = wb_in.base[l].rearrange("(j p) c -> p j c", p=128)
        blocks = [(0, 416), (416, 512), (928, 512), (1440, 512), (1952, 512), (2464, 512)] + [(2976 + 512 * i, 512) for i in range(8)]
        for g in groups_for(l):
            b, t0, n, lat, col, s0 = g["b"], g["t0"], g["n"], g["lat"], g["col"], g["s0"]
            nt = n // 128
            hxT = hxTp.nxt()
            for i in range(nt):
                xt = xpool.nxt()
                k.dma(xt[:], src_rows(l, g, i))
                norm_T(xt, hxT, i, A1, lambda j, c: modF[:, j, c:c + 1], col)
            if lat:
                rt = ropeT.nxt()
                k.dma(rt[:, 0, 0:n], RO(c_Cg.base[:, s0:s0 + n]))
                k.dma(rt[:, 1, 0:n], RO(c_Sg.base[:, s0:s0 + n]))
                k.dma(rt[0:96, 2, 0:n], RO(c_Cm.base[:, s0:s0 + n]))
                k.dma(rt[0:96, 3, 0:n], RO(c_Sm.base[:, s0:s0 + n]))

            def loadw(bi):
                c0, ncol = blocks[bi]
                wt = wpool.nxt()
                k.dma(wt[:, :, 0:ncol], wb_in.v(wv[:, :, c0:c0 + ncol]))
                return wt

            def fm(wt, c0, ncol):
                p = P()
                for j in range(8):
                    k.mm(p[0:ncol, 0:n], wt[:, j, c0:c0 + ncol], hxT[:, j, 0:n], start=(j == 0), stop=(j == 7))
                return p

            def tm(wt, c0, ncol, i):
                p = P()
                for j in range(8):
                    k.mm(p[:, 0:ncol], hxT[:, j, i * 128:(i + 1) * 128], wt[:, j, c0:c0 + ncol], start=(j == 0), stop=(j == 7))
                return p

            def store_fm(dst, r0, nr, src):
                k.dma(dst.k((b, t0, r0))[r0:r0 + nr, b, t0:t0 + n], src, q="pool")

            def store_tm(dst, i, src):
                k.dma(dst.k((b, t0, i))[b, t0 + i * 128:t0 + (i + 1) * 128, :], src, q="pool")

            def rope(xs, p0, p1, Rt, ci, si):
                R = Rt.base.shape[0]
                p2 = P()
                k.mm(p2[0:R, 0:n], Rt[:, :], xs[0:R, 0:n])
                t1 = tmpf.nxt()
                t2 = tmpf.nxt()
                k.tt(t1[p0:p1, 0:n], p2[p0:p1, 0:n], rt[p0:p1, si, 0:n], ALU.mult)
                k.tt(t2[p0:p1, 0:n], xs[p0:p1, 0:n], rt[p0:p1, ci, 0:n], ALU.mult)
                k.tt(xs[p0:p1, 0:n], t1[p0:p1, 0:n], t2[p0:p1, 0:n], ALU.add)

            w0 = loadw(0)
            w1 = loadw(1)
            pc = [fm(w0, 0, 128), fm(w0, 128, 128)]
            sq = sqp.nxt()
            for c in range(2):
                k.act(sq[:, c, 0:n], pc[c][:, 0:n], AF.Square)
            rs = fm_rstd(rsp, [sq[:, 0, 0:n], sq[:, 1, 0:n]], ones_f, 256, n)
            cqn = nrm.nxt()
            for c in range(2):
                k.stt(cqn[:, c, 0:n], pc[c][:, 0:n], nrmw[:, c:c + 1], rs[:, 0:n], ALU.mult, ALU.mult)
            for h in range(4):
                p = P()
                for c in range(2):
                    k.mm(p[0:96, 0:n], wuq_sb[:, c, h * 96:(h + 1) * 96], cqn[:, c, 0:n], start=(c == 0), stop=(c == 1))
                qa = stg.nxt()
                evac(qa[0:96, 0:n], p[0:96, 0:n])
                if lat:
                    rope(qa, 64, 96, Rq, 2, 3)
                store_fm(mqT, h * 96, 96, qa[0:96, 0:n])
            pk = fm(w0, 256, 128)
            sq = sqp.nxt()
            k.act(sq[:, 0, 0:n], pk[:, 0:n], AF.Square)
            rs = fm_rstd(rsp, [sq[:, 0, 0:n]], ones_f, 128, n)
            ckn = nrm.nxt()
            k.stt(ckn[:, 0, 0:n], pk[:, 0:n], nrmw[:, 2:3], rs[:, 0:n], ALU.mult, ALU.mult)
            for h in range(4):
                p = P()
                k.mm(p[0:64, 0:n], wkn_sb[:, h, :], ckn[:, 0, 0:n])
                kn = stg.nxt()
                evac(kn[0:64, 0:n], p[0:64, 0:n])
                store_fm(mknT, h * 64, 64, kn[0:64, 0:n])
            for i in range(nt):
                p = P()
                k.mm(p[:, 0:256], ckn[:, 0, i * 128:(i + 1) * 128], wkv_sb.v(wkv_sb.base[:, :, :].rearrange("p h e -> p (h e)")))
                vt = stg.nxt()
                evac(vt[:, 0:256], p[:, 0:256])
                store_tm(mv, i, vt[:, 0:256])
            pp = fm(w0, 384, 32)
            kp = stg.nxt()
            evac(kp[0:32, 0:n], pp[0:32, 0:n])
            if lat:
                rope(kp, 0, 32, Rq.v(Rq.base[0:32, 0:32]) if False else Rq32, 2, 3)
            store_fm(mkpT, 0, 32, kp[0:32, 0:n])
            w2 = loadw(2)
            for (c0, dst, r0, nw) in ((0, gqT, 0, 3), (128, gqT, 128, 3), (256, gkT, 0, 4)):
                p = fm(w1, c0, 128)
                sq = sqp.nxt()
                k.act(sq[:, 0, 0:n], p[:, 0:n], AF.Square)
                rs = fm_rstd(rsp, [sq[:, 0, 0:n]], onesblk, 64, n)
                xo = stg.nxt()
                k.stt(xo[:, 0:n], p[:, 0:n], nrmw[:, nw:nw + 1], rs[:, 0:n], ALU.mult, ALU.mult)
                if lat:
                    rope(xo, 0, 128, Rg, 0, 1)
                store_fm(dst, r0, 128, xo[:, 0:n])
            for i in range(nt):
                p = tm(w1, 384, 128, i)
                vt = stg.nxt()
                evac(vt[:, 0:128], p[:, 0:128])
                store_tm(gv, i, vt[:, 0:128])
            w3 = loadw(3)
            for c in range(4):
                p = fm(w2, c * 128, 128)
                xo = stg.nxt()
                evac(xo[:, 0:n], p[:, 0:n], scale=(0.125 if c < 2 else None))
                store_fm(nqT if c < 2 else nkT, (c % 2) * 128, 128, xo[:, 0:n])
            w4 = loadw(4)
            for i in range(nt):
                p = tm(w3, 0, 256, i)
                vt = stg.nxt()
                evac(vt[:, 0:256], p[:, 0:256])
                store_tm(nv, i, vt[:, 0:256])
            for c in range(2):
                p = fm(w3, 256 + c * 128, 128)
                xo = stg.nxt()
                evac(xo[:, 0:n], p[:, 0:n])
                store_fm(rqT, c * 128, 128, xo[:, 0:n])
            w5 = loadw(5)
            for c in range(2):
                p = fm(w4, c * 128, 128)
                xo = stg.nxt()
                evac(xo[:, 0:n], p[:, 0:n], scale=0.125)
                store_fm(rkT, c * 128, 128, xo[:, 0:n])
            for i in range(nt):
                p = tm(w4, 0, 512, i)
                vt = stg.nxt()
                evac(vt[:, 0:256], p[:, 0:256], scale=0.125)
                evac(vt[:, 256:512], p[:, 256:512])
                store_tm(rkv, i, vt[:, 0:512])
            wn = loadw(6)
            for i in range(nt):
                p = tm(w5, 0, 512, i)
                vt = stg.nxt()
                evac(vt[:, 0:512], p[:, 0:512])
                store_tm(rg, i, vt[:, 0:512])
            for gb in range(8):
                wc = wn
                if gb < 7:
                    wn = loadw(7 + gb)
                for c in range(4):
                    p = fm(wc, c * 128, 128)
                    xo = stg.nxt()
                    k.act(xo[:, 0:n], p[:, 0:n], AF.Sigmoid)
                    store_fm(gT, gb * 512 + c * 128, 128, xo[:, 0:n])
        k.pop()

    pend_epi = []

    def attn_flush():
        while pend_epi:
            pend_epi.pop(0)()

    def attn_core(pools, KT, dk, V, kchunks, q_src, nq, scale, o_dst, bias_fn=None):
        qpool, ptp, osp, obp = pools
        qt = q_src
        po = psB.nxt()
        n_ = len(kchunks)
        pss = {}

        def qk(ci):
            kc = kchunks[ci]
            ps = P()
            bm = bias_fn(kc) if bias_fn is not None else None
            k.mm(ps[:, 0:nq], KT[0:dk, kc * 128:(kc + 1) * 128], qt[0:dk, 0:nq], start=True, stop=(bm is None))
            if bm is not None:
                k.mm(ps[:, 0:nq], ident_bf[:], bm, start=False, stop=True)
            pss[ci] = ps

        qk(0)
        if n_ > 1:
            qk(1)
        attn_flush()
        for ci in range(n_):
            pt = ptp.nxt()
            k.act(pt[:, 0:nq], pss.pop(ci)[:, 0:nq], AF.Exp, scale=scale)
            if ci + 2 < n_:
                qk(ci + 2)
            k.mm(po[0:65, 0:nq], V[:, kchunks[ci], 0:65], pt[:, 0:nq], start=(ci == 0), stop=(ci == n_ - 1))

        def epi():
            os_ = osp.nxt()
            k.copy(os_[0:65, 0:nq], po[0:65, 0:nq])
            k.recip(os_[64:65, 0:nq], os_[64:65, 0:nq])
            pb = P()
            k.mm(pb[0:64, 0:nq], ones_f[64:65, 0:64], os_[64:65, 0:nq])
            ob = obp.nxt()
            k.tt(ob[0:64, 0:nq], os_[0:64, 0:nq], pb[0:64, 0:nq], ALU.mult)
            k.dma(o_dst, ob[0:64, 0:nq], q="pool")
        pend_epi.append(epi)

    def attn_pools():
        qpool = Pool(k, "aq", [96, 512], BF16, 2)
        qpoolB = Pool(k, "aqB", [128, 512], BF16, 2)
        for t_ in qpoolB.t:
            k.memset(t_[:], 0.0)
        qpool.padded = qpoolB
        ptp = Pool(k, "apt", [128, 512], BF16, 4)
        osp = Pool(k, "aos", [65, 512], F32, 2)
        obp = Pool(k, "aob", [64, 512], BF16, 2)
        return (qpool, ptp, osp, obp)

    NKC = TT // 128

    def load_V(Vt, src_t, b, c0):
        k.dma(Vt[:, :, 0:64], src_t.v(src_t.base[b, :, c0:c0 + 64].rearrange("(c p) e -> p c e", p=128)))

    def run_attn_jobs(pools, jobs, KTp, Vp, bmp=None):
        qpool = pools[0]
        kvbuf = {}

        def ensure_kv(key, loader):
            if key not in kvbuf:
                KT, Vt = (KTp[1] if key[0] == "g" else KTp[0]).nxt() if isinstance(KTp, tuple) else KTp.nxt(), Vp.nxt()
                loader(KT, Vt)
                kvbuf[key] = (KT, Vt)

        def prefetch(j):
            qt = (qpool.padded if j.get("pad") else qpool).nxt()
            k.dma(qt[0:j["dk"], 0:j["nq"]], j["qsrc"])
            j["qt"] = qt
            if j.get("bmsrc") is not None:
                bm = bmp.nxt()
                k.dma(bm[:], j["bmsrc"])
                j["bm"] = bm

        ensure_kv(jobs[0]["kv"], jobs[0]["kvload"])
        prefetch(jobs[0])
        for i, j in enumerate(jobs):
            if i + 1 < len(jobs):
                prefetch(jobs[i + 1])
            if i == 0 or jobs[i - 1]["kv"] != j["kv"]:
                for j2 in jobs[i + 1:]:
                    if j2["kv"] != j["kv"]:
                        ensure_kv(j2["kv"], j2["kvload"])
                        break
                for key in [kk for kk in kvbuf if kk != j["kv"] and all(kk != j2["kv"] for j2 in jobs[i:])]:
                    del kvbuf[key]
            KT, Vt = kvbuf[j["kv"]]
            bfn = (j["bias"](j["bm"]) if j.get("bias") is not None else None)
            attn_core(pools, KT, (128 if j.get("pad") else j["dk"]), Vt, j["chunks"], j["qt"], j["nq"], j["scale"], j["odst"], bias_fn=bfn)
        attn_flush()

    def phase_attn(l, need_ctx):
        k.push()
        pools = attn_pools()
        KTp = Pool(k, "aKT", [96, TT], BF16, 2)
        KTg = Pool(k, "aKTg", [128, TT], BF16, 2)
        for t_ in KTg.t:
            k.memset(t_[64:128, :], 0.0)
        Vp = Pool(k, "aV", [128, NKC, 65], BF16, 2)
        for Vt in Vp.t:
            k.memset(Vt[:, :, 64:65], 1.0)
        allk = list(range(NKC))
        jobs = []
        for b in range(NB):
            for h in range(4):
                def kvl(KT, Vt, b=b, h=h):
                    k.dma(KT[0:64, :], mknT[h * 64:(h + 1) * 64, b, :])
                    k.dma(KT[64:96, :], mkpT[:, b, :])
                    load_V(Vt, mv, b, h * 64)
                for qg in range(8):
                    t0 = LC + qg * 512
                    jobs.append(dict(kv=("m", b, h), kvload=kvl, dk=96, chunks=allk, qsrc=mqT[h * 96:(h + 1) * 96, b, t0:t0 + 512], nq=512,
                                     scale=96 ** -0.5, odst=oT.k((b, t0, h))[h * 64:(h + 1) * 64, b, t0:t0 + 512]))
                if need_ctx:
                    jobs.append(dict(kv=("m", b, h), kvload=kvl, dk=96, chunks=[0, 1], qsrc=mqT[h * 96:(h + 1) * 96, b, 0:LC], nq=LC,
                                     scale=96 ** -0.5, odst=oT.k((b, 0, h))[h * 64:(h + 1) * 64, b, 0:LC]))
            for kv in range(2):
                def kvl(KT, Vt, b=b, kv=kv):
                    k.dma(KT[0:64, :], gkT[kv * 64:(kv + 1) * 64, b, :])
                    load_V(Vt, gv, b, kv * 64)
                for gq in range(2):
                    h = kv * 2 + gq
                    for qg in range(8):
                        t0 = LC + qg * 512
                        jobs.append(dict(kv=("g", b, kv), kvload=kvl, pad=True, dk=64, chunks=allk, qsrc=gqT[h * 64:(h + 1) * 64, b, t0:t0 + 512], nq=512,
                                         scale=0.125, odst=oT.k((b, t0, 4 + h))[256 + h * 64:256 + (h + 1) * 64, b, t0:t0 + 512]))
                    if need_ctx:
                        jobs.append(dict(kv=("g", b, kv), kvload=kvl, pad=True, dk=64, chunks=[0, 1], qsrc=gqT[h * 64:(h + 1) * 64, b, 0:LC], nq=LC,
                                         scale=0.125, odst=oT.k((b, 0, 4 + h))[256 + h * 64:256 + (h + 1) * 64, b, 0:LC]))
        run_attn_jobs(pools, jobs, (KTp, KTg), Vp)
        k.pop()

    def phase_na(l, need_ctx):
        k.push()
        pools = attn_pools()
        KTp = Pool(k, "nKT", [128, TT], BF16, 2)
        for t_ in KTp.t:
            k.memset(t_[64:128, :], 0.0)
        Vp = Pool(k, "nV", [128, NKC, 65], BF16, 2)
        bmp = Pool(k, "nbm", [128, 8, 512], BF16, 3)
        for Vt in Vp.t:
            k.memset(Vt[:, :, 64:65], 1.0)
        jobs = []
        for b in range(NB):
            for h in range(4):
                def kvl(KT, Vt, b=b, h=h):
                    k.dma(KT[0:64, :], nkT[h * 64:(h + 1) * 64, b, :])
                    load_V(Vt, nv, b, h * 64)
                for qg in range(8):
                    t0 = LC + qg * 512
                    pat = 0 if qg == 0 else (2 if qg == 7 else 1)
                    kr0 = min(max(8 * qg - 4, 0), 48)
                    kc0 = 2 + kr0 // 2
                    jobs.append(dict(kv=("n", b, h), kvload=kvl, pad=True, dk=64, chunks=[0, 1] + [kc0 + j for j in range(8)],
                                     qsrc=nqT[h * 64:(h + 1) * 64, b, t0:t0 + 512], nq=512, scale=1.0,
                                     odst=oT.k((b, t0, 8 + h))[512 + h * 64:512 + (h + 1) * 64, b, t0:t0 + 512],
                                     bmsrc=nabb.v(nabb.base[l, h, pat].rearrange("c p q -> p c q")),
                                     bias=lambda bm, kc0=kc0: (lambda kc: (bm[:, kc - kc0, :] if kc >= 2 else None))))
                if need_ctx:
                    jobs.append(dict(kv=("n", b, h), kvload=kvl, pad=True, dk=64, chunks=[0, 1], qsrc=nqT[h * 64:(h + 1) * 64, b, 0:LC], nq=LC, scale=1.0,
                                     odst=oT.k((b, 0, 8 + h))[512 + h * 64:512 + (h + 1) * 64, b, 0:LC]))
        run_attn_jobs(pools, jobs, KTp, Vp, bmp)
        k.pop()

    def phase_ret(l, need_ctx):
        k.push()
        lg = k.sb("lg", [128, 8], F32)
        k.dma(lg[:], RO(rdl.base[l].partition_broadcast(128)))
        k.act(lg[:], lg[:], AF.Exp, scale=-1.0)
        k.ts(lg[:], lg[:], 1.0, ALU.add)
        k.act(lg[:], lg[:], AF.Ln)
        k.ts(lg[:], lg[:], -1.0, ALU.mult)
        Dd = k.sb("Dd", [128, 2, 128], F32)
        QA = k.sb("QA", [128, 2, 128], F32)
        KA = k.sb("KA", [128, 2], F32)
        k.dma(Dd[:], RO(c_retD.base.rearrange("d p n -> p d n")))
        k.dma(QA[:], RO(c_retQA.base.rearrange("d p n -> p d n")))
        k.dma(KA[:], RO(c_retKA.base))
        decT = k.sb("decT", [128, 2, 4, 128], F32)
        qdec = k.sb("qdec", [128, 2, 4, 128], F32)
        kdec = k.sb("kdec", [128, 2, 4], F32)
        cdec = k.sb("cdec", [128, 8], F32)
        gnr = k.sb("gnr", [128, 256], F32)
        k.dma(gnr[:], RO(gnw.base[l].partition_broadcast(128)))
        for d in range(2):
            for h in range(4):
                k.act(decT[:, d, h, :], Dd[:, d, :], AF.Exp, scale=lg[:, d * 4 + h:d * 4 + h + 1])
                k.act(qdec[:, d, h, :], QA[:, d, :], AF.Exp, scale=lg[:, d * 4 + h:d * 4 + h + 1])
            k.act(kdec[:, d, :], lg[:, d * 4:(d + 1) * 4], AF.Exp, scale=KA[:, d:d + 1])
        k.act(cdec[:], lg[:], AF.Exp, scale=128.0)
        QT = k.sb("rQT", [128, 2, TT], BF16)
        KT = k.sb("rKT", [128, 2, TT], BF16)
        KVt = k.sb("rKV", [128, NKC, 512], BF16)
        G = k.sb("rG", [128, NKC, 512], BF16)
        Ys = [k.sb("rY", [128, NKC, 256], F32), k.sb("rYb", [128, NKC, 256], BF16)]
        Sts = [k.sb("rS", [128, 2, 64], F32) for _ in range(2)]
        Sbs = [k.sb("rSb", [128, 2, 64], BF16) for _ in range(2)]
        kdp = Pool(k, "rkd", [128, 256], BF16, 2)
        idp = Pool(k, "rid", [128, 128], BF16, 3)
        qdp = Pool(k, "rqd", [128, 128], BF16, 3)
        osb = Pool(k, "ros", [128, 256], F32, 2)
        sgp = Pool(k, "rsg", [128, 256], F32, 2)
        stp = Pool(k, "rst", [128, 4], F32, 4)
        ybp = Pool(k, "ryb", [128, 256], BF16, 2)
        ysp = Pool(k, "rys", [128, 2, 128], BF16, 2)
        b3 = lambda t_, ap: t_.v(ap)
        for b in range(NB):
            for c in range(2):
                k.dma(QT[:, c, :], rqT[c * 128:(c + 1) * 128, b, :])
                k.dma(KT[:, c, :], rkT[c * 128:(c + 1) * 128, b, :])
            k.dma(KVt[:], rkv.v(rkv.base[b].rearrange("(c p) f -> p c f", p=128)))
            k.dma(G[:], rg.v(rg.base[b].rearrange("(c p) f -> p c f", p=128)))
            for d in range(2):
                k.memset(Sts[d][:], 0.0)
                k.memset(Sbs[d][:], 0.0)
            orders = [list(range(NKC)), [1, 0] + list(range(NKC - 1, 1, -1))]
            for step in range(2 * NKC):
                for _once in (0,):
                    d = step % 2
                    ci = orders[d][step // 2]
                    St, Sb, Y = Sts[d], Sbs[d], Ys[d]
                    tk = slice(ci * 128, (ci + 1) * 128)
                    Kd = kdp.nxt()
                    k.tt(b3(Kd, Kd.base[:, :].rearrange("p (h e) -> p h e", h=4)),
                         b3(KVt, KVt.base[:, ci, 0:256].rearrange("p (h e) -> p h e", h=4)),
                         b3(kdec, kdec.base[:, d, :].unsqueeze(2).to_broadcast([128, 4, 64])), ALU.mult)
                    pso = psB.nxt()
                    for h in range(4):
                        c2, po = h // 2, (h % 2) * 64
                        ps1 = P()
                        k.mm(ps1[:, 0:128], KT[po:po + 64, c2, tk], QT[po:po + 64, c2, tk])
                        idT = idp.nxt()
                        k.tt(idT[:], ps1[:, 0:128], decT[:, d, h, :], ALU.mult)
                        Qd = qdp.nxt()
                        k.tt(Qd[po:po + 64, :], QT[po:po + 64, c2, tk], qdec[po:po + 64, d, h, :], ALU.mult)
                        k.mm(pso[:, h * 64:(h + 1) * 64], idT[:], KVt[:, ci, 256 + h * 64:256 + (h + 1) * 64], start=True, stop=False)
                        k.mm(pso[:, h * 64:(h + 1) * 64], Qd[po:po + 64, :], Sb[po:po + 64, c2, :], start=False, stop=True)
                    for c2 in range(2):
                        ps2 = P()
                        k.mm(ps2[:, 0:128], Kd[:, c2 * 128:(c2 + 1) * 128], KVt[:, ci, 256 + c2 * 128:256 + (c2 + 1) * 128])
                        for hh in range(2):
                            po = hh * 64
                            h = c2 * 2 + hh
                            k.stt(St[po:po + 64, c2, :], St[po:po + 64, c2, :], cdec[po:po + 64, d * 4 + h:d * 4 + h + 1],
                                  ps2[po:po + 64, po:po + 64], ALU.mult, ALU.add)
                    k.copy(Sb[:], St[:])
                    if ci < 2 and not need_ctx:
                        continue
                    o = osb.nxt()
                    o3 = b3(o, o.base[:, :].rearrange("p (h e) -> p h e", h=4))
                    k.copy(o[:], pso[:, 0:256])
                    st = stp.nxt()
                    k.reduce(st[:], o3, ALU.add)
                    k.ts(st[:], st[:], 1.0 / 64, ALU.mult)
                    k.tt(o3, o3, b3(st, st.base[:, :].unsqueeze(2).to_broadcast([128, 4, 64])), ALU.subtract)
                    sq = sgp.nxt()
                    k.tt(sq[:], o[:], o[:], ALU.mult)
                    st2 = stp.nxt()
                    k.reduce(st2[:], b3(sq, sq.base[:, :].rearrange("p (h e) -> p h e", h=4)), ALU.add)
                    k.act(st2[:], st2[:], AF.Sqrt, bias=EPS, scale=1.0 / 64)
                    k.recip(st2[:], st2[:])
                    k.tt(o3, o3, b3(st2, st2.base[:, :].unsqueeze(2).to_broadcast([128, 4, 64])), ALU.mult)
                    k.tt(o[:], o[:], gnr[:], ALU.mult)
                    sg = sgp.nxt()
                    k.act(sg[:], G[:, ci, d * 256:(d + 1) * 256], AF.Silu)
                    k.tt(Y[:, ci, :], o[:], sg[:], ALU.mult)
            for ci in (range(NKC) if need_ctx else range(2, NKC)):
                yb = ybp.nxt()
                k.tt(yb[:], Ys[0][:, ci, :], Ys[1][:, ci, :], ALU.add)
                pt = P()
                for c2 in range(2):
                    k.tr(bfview(pt, c2 * 128, (c2 + 1) * 128), yb[:, c2 * 128:(c2 + 1) * 128], ident_bf[:])
                ys = ysp.nxt()
                k.copy(b3(ys, ys.base[:, :, :].rearrange("p c t -> p (c t)")), bfview(pt, 0, 256))
                for c2 in range(2):
                    k.dma(oT.k((b, ci, 12 + c2))[768 + c2 * 128:768 + (c2 + 1) * 128, b, ci * 128:(ci + 1) * 128], ys[:, c2, :], q="pool")
        k.pop()

    def phase_merge(l, need_ctx):
        k.push()
        wbr = k.sb("wbr", [128, 4, 2, D], BF16)
        wo = k.sb("wo", [128, 8, D], BF16)
        for i in range(4):
            k.dma(wbr[:, i, :, :], wb_br.v(wb_br.base[l, i].rearrange("(kk p) m -> p kk m", p=128)))
        k.dma(wo[:], wb_out.v(wb_out.base[l].rearrange("(j p) m -> p j m", p=128)))
        osp = Pool(k, "mo", [128, 8, 512], BF16, 2)
        mgl = Pool(k, "mgl", [128, 512], BF16, 6)
        accp = Pool(k, "macc", [128, 512], F32, 2)
        tmpp = Pool(k, "mtmp", [128, 512], F32, 2)
        accT = Pool(k, "maT", [128, 8, 512], BF16, 2)
        xp = Pool(k, "mx", [128, D], F32, 3)
        for g in groups_for(l):
            b, t0, n, lat, col = g["b"], g["t0"], g["n"], g["lat"], g["col"]
            if not lat and not need_ctx:
                continue
            nt = n // 128
            o_sb = osp.nxt()
            k.dma(o_sb[:, :, 0:n], oT.v(oT.base[:, b, t0:t0 + n].rearrange("(kk p) t -> p kk t", p=128)))
            aT = accT.nxt()
            for m in range(8):
                acc = accp.nxt()
                for i in range(4):
                    p = P()
                    for kk in range(2):
                        k.mm(p[:, 0:n], wbr[:, i, kk, m * 128:(m + 1) * 128], o_sb[:, 2 * i + kk, 0:n], start=(kk == 0), stop=(kk == 1))
                    gl = mgl.nxt()
                    k.dma(gl[:, 0:n], gT[i * 1024 + m * 128:i * 1024 + (m + 1) * 128, b, t0:t0 + n])
                    if i == 0:
                        k.tt(acc[:, 0:n], p[:, 0:n], gl[:, 0:n], ALU.mult)
                    else:
                        tp_ = tmpp.nxt()
                        k.tt(tp_[:, 0:n], p[:, 0:n], gl[:, 0:n], ALU.mult)
                        k.tt(acc[:, 0:n], acc[:, 0:n], tp_[:, 0:n], ALU.add, e="pool")
                k.copy(aT[:, m, 0:n], acc[:, 0:n], e="act")
            for i in range(nt):
                xt = xp.nxt()
                k.dma(xt[:], src_rows(l, g, i))
                for hf in range(2):
                    p = P()
                    for m in range(8):
                        k.mm(p[:, :], aT[:, m, i * 128:(i + 1) * 128], wo[:, m, hf * 512:(hf + 1) * 512], start=(m == 0), stop=(m == 7))
                    tp_ = tmpp.nxt()
                    k.tt(tp_[:], p[:, :], grep[:, 0, col, hf * 512:(hf + 1) * 512], ALU.mult)
                    k.tt(xt[:, hf * 512:(hf + 1) * 512], xt[:, hf * 512:(hf + 1) * 512], tp_[:], ALU.add, e="pool")
                dst = xres.k((b, g["s0"] + i * 128))[b, g["s0"] + i * 128:g["s0"] + (i + 1) * 128, :] if lat else cres.k((b, i))[b, i * 128:(i + 1) * 128, :]
                k.dma(dst, xt[:], q="pool")
        k.pop()

    def phase_peer(l, need_ctx):
        k.push()
        norm_pools()
        wq = k.sb("pwq", [128, 8, D], BF16)
        kb = k.sb("pkb", [128, 8, 256], BF16)
        iota16 = k.sb("piota", [128, 16], F32)
        k.dma(wq[:], wb_pq.v(wb_pq.base[l].rearrange("(j p) m -> p j m", p=128)))
        k.dma(kb[:], wb_keys.v(wb_keys.base[l].rearrange("h p c -> p h c")))
        k.dma(iota16[:], RO(c_iota4.base[:, 0, 0, :]))
        xp = Pool(k, "px", [128, D], F32, 2)
        hTp = Pool(k, "phT", [128, 8, 128], BF16, 2)
        qTp = Pool(k, "pqT", [128, 128], BF16, 3)
        wkp = Pool(k, "pwk", [128, 256], F32, 2)
        up = Pool(k, "pu", [128, 2 * D], BF16, 16)
        accp = Pool(k, "pacc", [128, D], F32, 1)
        jkp = Pool(k, "pjk", [128, D], BF16, 2)
        dgp = Pool(k, "pdg", [128, 128], BF16, 4)
        b3 = lambda t_, ap: t_.v(ap)
        B2 = lambda j, c: modF[:, 24 + j, c:c + 1]
        if not breg_box:
            breg_box.append(nc.gpsimd.to_reg(16383))
        breg = breg_box[0]

        class St:
            pass
        sets = []
        for i in range(2):
            s = St()
            s.h2 = k.sb("ph2", [128, D], BF16)
            s.s_sb = k.sb("ps_sb", [128, 8, 256], F32)
            s.V1 = k.sb("pV1", [128, 8, 2, 16], F32)
            s.I1 = k.sb("pI1", [128, 8, 2, 16], U32)
            s.I1f = k.sb("pI1f", [128, 8, 2, 16], F32)
            s.cand = k.sb("pcand", [128, 8, 256], F32)
            s.CS = k.sb("pCS", [128, 8, 16], F32)
            s.POS = k.sb("pPOS", [128, 8, 16], U32)
            s.AB = k.sb("pAB", [128, 2, 8, 16], U32)
            s.ABf = k.sb("pABf", [128, 2, 8, 16], F32)
            s.isel = k.sb("pisel", [128, 2, 8, 16], F32)
            s.EI = k.sb("pEI", [128, 128], I32)
            s.gsm = k.sb("pgsm", [128, 8, 16], F32)
            s.zs = k.sb("pzs", [128, 8], F32)
            s.dots = k.sb("pdots", [128, 128], F32)
            s.wg = k.sb("pwg", [128, 128], F32)
            sets.append(s)

        def top16(vals, idx, src):
            wk = wkp.nxt()
            n_ = src.ap.shape[-1]
            v0, v1 = vals
            i0_, i1_ = idx
            k.op("dve", lambda e: e.max(out=v0.ap, in_=src.ap), r=[src], w=[v0])
            k.op("dve", lambda e: e.max_index(out=i0_.ap, in_max=v0.ap, in_values=src.ap), r=[v0, src], w=[i0_])
            k.op("dve", lambda e: e.match_replace(out=wk[:, 0:n_].ap, in_to_replace=v0.ap, in_values=src.ap, imm_value=-1e30), r=[v0, src], w=[wk[:, 0:n_]])
            k.op("dve", lambda e: e.max(out=v1.ap, in_=wk[:, 0:n_].ap), r=[wk[:, 0:n_]], w=[v1])
            k.op("dve", lambda e: e.max_index(out=i1_.ap, in_max=v1.ap, in_values=wk[:, 0:n_].ap), r=[v1, wk[:, 0:n_]], w=[i1_])

        def front(s, b, ci):
            lat = ci >= 2
            s.col = b if lat else NB
            s.rv = (xres.k((b, (ci - 2) * 128)), (b, slice((ci - 2) * 128, (ci - 1) * 128), slice(None))) if lat else (cres.k((b, ci)), (b, slice(ci * 128, (ci + 1) * 128), slice(None)))
            s.xt = xp.nxt()
            k.dma(s.xt[:], s.rv[0][s.rv[1]])
            hT = hTp.nxt()
            norm_T(s.xt, hT, 0, A2, B2, s.col)
            yield
            pt = P()
            for j in range(8):
                k.tr(bfview(pt, j * 128, (j + 1) * 128), hT[:, j, :], ident_bf[:])
            k.copy(s.h2[:], bfview(pt, 0, 1024), e="act")
            yield
            for h in range(8):
                p = P()
                for j in range(8):
                    k.mm(p[:, 0:128], wq[:, j, h * 128:(h + 1) * 128], hT[:, j, :], start=(j == 0), stop=(j == 7))
                qT = qTp.nxt()
                k.copy(qT[:], p[:, 0:128], e="act")
                p2 = P()
                k.mm(p2[:, 0:256], qT[:], kb[:, h, :])
                k.copy(s.s_sb[:, h, :], p2[:, 0:256], e="act")
                yield
            for h in range(8):
                for p_ in range(2):
                    top16((s.V1[:, h, p_, 0:8], s.V1[:, h, p_, 8:16]), (s.I1[:, h, p_, 0:8], s.I1[:, h, p_, 8:16]), s.s_sb[:, h, p_ * 128:(p_ + 1) * 128])
                yield
            k.tt(b3(s.cand, s.cand.base[:, :, :].rearrange("p h (a c) -> p h a c", a=16)),
                 b3(s.V1, s.V1.base[:, :, 0, :].unsqueeze(3).to_broadcast([128, 8, 16, 16])),
                 b3(s.V1, s.V1.base[:, :, 1, :].unsqueeze(2).to_broadcast([128, 8, 16, 16])), ALU.add)
            yield
            for h in range(8):
                top16((s.CS[:, h, 0:8], s.CS[:, h, 8:16]), (s.POS[:, h, 0:8], s.POS[:, h, 8:16]), s.cand[:, h, :])
                yield
            k.op("dve", lambda e: e.tensor_single_scalar(s.AB[:, 0, :, :].ap, s.POS[:].ap, 4, ALU.logical_shift_right), r=[s.POS[:]], w=[s.AB[:]])
            k.op("dve", lambda e: e.tensor_single_scalar(s.AB[:, 1, :, :].ap, s.POS[:].ap, 15, ALU.bitwise_and), r=[s.POS[:]], w=[s.AB[:]])
            k.copy(s.ABf[:], s.AB[:])
            k.copy(s.I1f[:], s.I1[:])
            yield
            E4 = b3(s.cand, s.cand.base[:, :, :].rearrange("p h (a c) -> p h a c", a=16))
            io = b3(iota16, iota16.base[:, :].unsqueeze(1).unsqueeze(1).to_broadcast([128, 8, 16, 16]))
            for s_ in range(2):
                k.tt(E4, b3(s.ABf, s.ABf.base[:, s_, :, :].unsqueeze(3).to_broadcast([128, 8, 16, 16])), io, ALU.is_equal)
                k.tt(E4, E4, b3(s.I1f, s.I1f.base[:, :, s_, :].unsqueeze(2).to_broadcast([128, 8, 16, 16])), ALU.mult)
                k.reduce(s.isel[:, s_, :, :], E4, ALU.add)
                yield
            k.stt(s.isel[:, 0, :, :], s.isel[:, 0, :, :], 128.0, s.isel[:, 1, :, :], ALU.mult, ALU.add)
            k.copy(b3(s.EI, s.EI.base[:, :].rearrange("p (h c) -> p h c", h=8)), s.isel[:, 0, :, :])
            k.tt(s.gsm[:], s.CS[:], b3(s.CS, s.CS.base[:, :, 0:1].to_broadcast([128, 8, 16])), ALU.subtract)
            k.act(s.gsm[:], s.gsm[:], AF.Exp)
            k.reduce(s.zs[:], s.gsm[:], ALU.add)
            k.recip(s.zs[:], s.zs[:])
            k.tt(s.gsm[:], s.gsm[:], b3(s.zs, s.zs.base[:, :].unsqueeze(2).to_broadcast([128, 8, 16])), ALU.mult)
            yield

        def back(s, nxt_gen):
            acc = accp.nxt()
            pacc = (psB.nxt(), psB.nxt())
            gflat = s.gsm.base[:, :, :].rearrange("p h c -> p (h c)")
            for g4 in range(32):
                bufs = []
                for c in range(4):
                    hk = g4 * 4 + c
                    ur = up.nxt()
                    bufs.append(ur)
                    k.dma(ur[:], uvb[l][:, :], q="pool", extra_r=[s.EI[:, hk:hk + 1]],
                          fn=lambda e, ur=ur, hk=hk: e.indirect_dma_start(out=ur[:].ap, out_offset=None, in_=uvb[l].base[:, :],
                                                                         in_offset=bass.IndirectOffsetOnAxis(ap=s.EI[:, hk:hk + 1].ap, axis=0),
                                                                         bounds_check=breg, oob_is_err=False))
                for c in range(4):
                    hk = g4 * 4 + c
                    jk = jkp.nxt()
                    k.stt(jk[:], bufs[c][:, 0:D], 1.0, s.h2[:], ALU.mult, ALU.mult, accum=s.dots[:, hk:hk + 1])
                sl = slice(g4 * 4, g4 * 4 + 4)
                k.act(s.wg[:, sl], s.dots[:, sl], AF.Gelu)
                k.tt(s.wg[:, sl], s.wg[:, sl], b3(s.gsm, gflat[:, sl]), ALU.mult)
                for c in range(4):
                    hk = g4 * 4 + c
                    dg = dgp.nxt()
                    k.act(dg[:], ident_bf[:], AF.Copy, scale=s.wg[:, hk:hk + 1])
                    k.mm(pacc[0][:, :], dg[:], bufs[c][:, D:D + 512], start=(hk == 0), stop=(hk == 127))
                    k.mm(pacc[1][:, :], dg[:], bufs[c][:, D + 512:2 * D], start=(hk == 0), stop=(hk == 127))
                if nxt_gen is not None:
                    next(nxt_gen, None)
            if nxt_gen is not None:
                for _ in nxt_gen:
                    pass
            for hf in range(2):
                k.tt(acc[:, hf * 512:(hf + 1) * 512], pacc[hf][:, :], grep[:, 1, s.col, hf * 512:(hf + 1) * 512], ALU.mult)
            k.tt(s.xt[:], s.xt[:], acc[:], ALU.add)
            k.dma(s.rv[0][s.rv[1]], s.xt[:], q="sp")

        tiles = [(b, ci) for b in range(NB) for ci in range(NKC) if (ci >= 2 or need_ctx)]
        g0 = front(sets[0], *tiles[0])
        for _ in g0:
            pass
        for ti in range(len(tiles)):
            nxt = front(sets[(ti + 1) % 2], *tiles[ti + 1]) if ti + 1 < len(tiles) else None
            back(sets[ti % 2], nxt)
        k.pop()

    breg_box = []

    def phase_final():
        k.push()
        fr = k.sb("fnr", [128, D], F32)
        k.dma(fr[:], RO(fnw.base.partition_broadcast(128)))
        xp = Pool(k, "fx", [128, D], F32, 3)
        yp = Pool(k, "fy", [128, D], F32, 3)
        for b in range(NB):
            for i in range(S // 128):
                xt = xp.nxt()
                k.dma(xt[:], xres[b, i * 128:(i + 1) * 128, :])
                ss = ssp.nxt()
                y = yp.nxt()
                k.act(y[:], xt[:], AF.Square, accum=ss[:, 0:1])
                k.act(ss[:, 1:2], ss[:, 0:1], AF.Sqrt, bias=EPS, scale=1.0 / D)
                k.recip(ss[:, 1:2], ss[:, 1:2])
                k.ts(y[:], xt[:], ss[:, 1:2], ALU.mult)
                k.tt(y[:], y[:], fr[:], ALU.mult, e="pool")
                k.dma(out_d.k((b, i))[b, i * 128:(i + 1) * 128, :], y[:], q="pool")
        k.pop()

    Rq32 = k.sb("Rq32", [32, 32], BF16)
    k.dma(Rq32[:], RO(c_Rq.base[0:32, 0:32]))

    outs = []
    for l in layers:
        phase_mod(l)
        load_layer_small(l)
        if stop == "mod":
            break
        phase_proj(l)
        if stop == "proj":
            break
        need_ctx = (l < NL - 1)
        phase_attn(l, need_ctx)
        if stop == "attn":
            break
        phase_na(l, need_ctx)
        if stop == "na":
            break
        phase_ret(l, need_ctx)
        if stop == "ret":
            break
        phase_merge(l, need_ctx)
        if stop == "merge":
            break
        phase_peer(l, need_ctx)
        if stop == "peer":
            break
    if stop is None or stop == "final":
        phase_final()

    k.finish()
    return k


_CONST_CACHE = {}


def host_shared(inp):
    f = lambda a: np.ascontiguousarray(np.asarray(a, dtype=np.float32))
    sh = {}
    if "c" not in _CONST_CACHE:
        c = {}
        c.update(rope_tables())
        c.update(misc_consts())
        _CONST_CACHE["c"] = c
    sh.update(_CONST_CACHE["c"])
    sh["mod_w"] = f(inp["mod_w"])
    sh["mod_b"] = f(inp["mod_b"])
    sh["mod_bT"] = f(np.asarray(inp["mod_b"]).reshape(NL, 48, 128).transpose(0, 2, 1))
    sh["n1wT"] = f(np.asarray(inp["norm1_w"]).reshape(NL, 8, 128).transpose(0, 2, 1))
    sh["n2wT"] = f(np.asarray(inp["norm2_w"]).reshape(NL, 8, 128).transpose(0, 2, 1))
    sh["w_in"] = f(inp["w_in"])
    sh["qnT"] = f(np.asarray(inp["mla_q_norm"]).reshape(NL, 2, 128).transpose(0, 2, 1))
    sh["kvnT"] = f(np.asarray(inp["mla_kv_norm"]).reshape(NL, 128, 1))
    sh["gqnT"] = f(np.tile(np.asarray(inp["gqa_q_norm"]), (1, 2)).reshape(NL, 128, 1))
    sh["gknT"] = f(np.tile(np.asarray(inp["gqa_k_norm"]), (1, 2)).reshape(NL, 128, 1))
    sh["mla_w_uq"] = f(inp["mla_w_uq"])
    sh["mla_w_ukv"] = f(inp["mla_w_ukv"])
    sh["nabm"] = na_biasmask(np.asarray(inp["na_bias"], dtype=np.float32))
    sh["ret_decay_logit"] = f(np.asarray(inp["ret_decay_logit"]).reshape(NL, 8))
    sh["ret_gn_w"] = f(inp["ret_gn_w"])
    sh["w_branch"] = f(inp["w_branch"])
    sh["w_out"] = f(inp["w_out"])
    sh["peer_w_q"] = f(inp["peer_w_q"])
    pk = np.asarray(inp["peer_keys"], dtype=np.float32)
    kb = np.zeros((NL, 8, 128, 256), np.float32)
    kb[:, :, 0:64, 0:128] = pk[:, :, 0].transpose(0, 1, 3, 2)
    kb[:, :, 64:128, 128:256] = pk[:, :, 1].transpose(0, 1, 3, 2)
    sh["keysbd"] = kb
    for i in range(NL):
        sh["peer_u%d" % i] = f(np.asarray(inp["peer_u"])[i])
        sh["peer_v%d" % i] = f(np.asarray(inp["peer_v"])[i])
    sh["final_norm_w"] = f(inp["final_norm_w"])
    return sh


def host_core(inp, b0, NB):
    x = np.asarray(inp["x"], dtype=np.float32)
    d = {}
    d["x"] = np.ascontiguousarray(x[b0:b0 + NB])
    d["ctx"] = np.ascontiguousarray(np.asarray(inp["ctx"], dtype=np.float32)[b0:b0 + NB])
    cc = np.concatenate([np.asarray(inp["c"], dtype=np.float32)[b0:b0 + NB], np.asarray(inp["c_ctx"], dtype=np.float32)[None, :]], 0)
    d["cT3"] = np.ascontiguousarray(cc.reshape(NB + 1, 8, 128).transpose(2, 1, 0))
    return d


_PROG = {}


def kernel(**inputs):
    NB = 2
    if "k" not in _PROG:
        _PROG["k"] = build(NB=NB)
    k = _PROG["k"]
    sh = host_shared(inputs)
    in_maps = []
    for c in range(NCORES):
        m = dict(sh)
        m.update(host_core(inputs, c * NB, NB))
        in_maps.append(m)
    res = run_bass_kernel_spmd(k.nc, in_maps, core_ids=list(range(NCORES)))
    out = np.concatenate([np.asarray(r["out"]) for r in res.results], axis=0)
    return np.ascontiguousarray(out.astype(np.float32))
```

```python
import ml_dtypes
from concourse.bass_utils import run_bass_kernel_spmd
import contextlib
import numpy as np
import concourse.bass as bass
import concourse.mybir as mybir

F32 = mybir.dt.float32
BF16 = mybir.dt.bfloat16
I32 = mybir.dt.int32
U32 = mybir.dt.uint32
ALU = mybir.AluOpType
AF = mybir.ActivationFunctionType
AX = mybir.AxisListType


class Dep:
    __slots__ = ("w", "r")

    def __init__(self):
        self.w = None
        self.r = []


class View:
    __slots__ = ("t", "key", "ap")

    def __init__(self, t, key, ap):
        self.t = t
        self.key = key
        self.ap = ap


class _Sub:
    def __init__(self, t, key):
        self.t = t
        self.key = key

    def __getitem__(self, idx):
        return View(self.t, self.key, self.t.base[idx])


class T:
    def __init__(self, name, base, space):
        self.name = name
        self.base = base
        self.space = space
        self.whole = Dep()
        self.subs = {}

    def __getitem__(self, idx):
        return View(self, None, self.base[idx])

    def k(self, key):
        return _Sub(self, key)

    def v(self, ap, key=None):
        return View(self, key, ap)

    def deps(self, key):
        if key is None:
            return [self.whole] + list(self.subs.values())
        if key not in self.subs:
            self.subs[key] = Dep()
        return [self.whole, self.subs[key]]

    def own(self, key):
        if key is None:
            return self.whole
        if key not in self.subs:
            self.subs[key] = Dep()
        return self.subs[key]


class K:
    ENG = ("pe", "act", "dve", "pool", "sp")

    def __init__(self, n_dma_sems=24):
        self.nc = bass.Bass("TRN2", target_bir_lowering=False)
        self.es = contextlib.ExitStack()
        self.stack = [self.es]
        nc = self.nc
        self.eng = {"pe": nc.tensor, "act": nc.scalar, "dve": nc.vector, "pool": nc.gpsimd, "sp": nc.sync}
        self.sems = []
        self.semval = []
        self.esem = {}
        for e in ("pe", "act", "dve", "pool"):
            self.esem[e] = self._newsem("s_" + e)
        self.dsem = {}
        self.dnext = {}
        for q in ("sp", "pool", "act"):
            n = n_dma_sems if q != "act" else 8
            self.dsem[q] = [self._newsem(f"d_{q}{i}") for i in range(n)]
            self.dnext[q] = 0
        self.known = {e: {} for e in self.ENG}
        self.n_inst = 0
        self.embed = True
        self.n_wait = 0
        self._uid = 0

    def _newsem(self, name):
        h = self.es.enter_context(self.nc.semaphore(name))
        self.sems.append(h)
        self.semval.append(0)
        return len(self.sems) - 1

    def uid(self, p):
        self._uid += 1
        return f"{p}{self._uid}"

    def sb(self, name, shape, dtype):
        h = self.stack[-1].enter_context(self.nc.sbuf_tensor(self.uid("s_" + name + "_"), list(shape), dtype))
        return T(name, h, "sb")

    def ps(self, name, shape, dtype=F32):
        h = self.stack[-1].enter_context(self.nc.psum_tensor(self.uid("p_" + name + "_"), list(shape), dtype))
        return T(name, h, "ps")

    def push(self):
        self.stack.append(contextlib.ExitStack())

    def pop(self):
        self.barrier()
        self.stack.pop().close()

    def barrier(self, engines=("pe", "act", "dve", "pool", "sp")):
        for e in engines:
            for s in range(len(self.sems)):
                if self.semval[s] > 0:
                    self._wait(e, (s, self.semval[s]))

    def dram(self, name, shape, dtype, kind="Internal"):
        h = self.nc.dram_tensor(name, list(shape), dtype, kind=kind)
        return T(name, h.ap(), "dram")

    def _wait(self, e, ev):
        if ev is None:
            return
        s, v = ev
        if self.known[e].get(s, 0) >= v:
            return
        self.eng[e].wait_ge(self.sems[s], v)
        self.known[e][s] = v
        self.n_wait += 1

    def _sync_in(self, e, r, w):
        need = {}

        def add(ev):
            if ev is None:
                return
            s, v = ev
            if e == "pe" and s == self.esem["pe"]:
                return
            if need.get(s, 0) < v:
                need[s] = v

        for v in r:
            for d in v.t.deps(v.key):
                add(d.w)
        for v in w:
            for d in v.t.deps(v.key):
                add(d.w)
                for rv in d.r:
                    add(rv)
        pend = [(s, v) for s, v in need.items() if self.known[e].get(s, 0) < v]
        if self.embed and pend:
            for ev in pend[:-1]:
                self._wait(e, ev)
            return pend[-1]
        for ev in pend:
            self._wait(e, ev)
        return None

    def _sync_out(self, ev, r, w):
        for v in r:
            v.t.own(v.key).r.append(ev)
            if len(v.t.own(v.key).r) > 64:
                mx = {}
                for s, val in v.t.own(v.key).r:
                    mx[s] = max(mx.get(s, 0), val)
                v.t.own(v.key).r = list(mx.items())
        for v in w:
            if v.key is None:
                v.t.subs = {}
            d = v.t.own(v.key)
            d.w = ev
            d.r = []

    def op(self, e, fn, r=(), w=()):
        r = [x for x in r if isinstance(x, View)]
        w = [x for x in w if isinstance(x, View)]
        emb = self._sync_in(e, r, w)
        ins = fn(self.eng[e])
        if emb is not None:
            ins._wait_ge(self.sems[emb[0]], emb[1])
            self.known[e][emb[0]] = emb[1]
        s = self.esem[e]
        self.semval[s] += 1
        ins.then_inc(self.sems[s], 1)
        ev = (s, self.semval[s])
        self._sync_out(ev, r, w)
        self.n_inst += 1
        return ins

    def dma(self, out, in_, q="sp", fn=None, extra_r=()):
        r = [in_] + list(extra_r)
        w = [out]
        pool = self.dsem[q]
        i = self.dnext[q]
        self.dnext[q] = (i + 1) % len(pool)
        s = pool[i]
        self._wait(q, (s, self.semval[s]))
        emb = self._sync_in(q, r, w)
        if fn is None:
            ins = self.eng[q].dma_start(out=out.ap, in_=in_.ap)
        else:
            ins = fn(self.eng[q])
        if emb is not None:
            ins._wait_ge(self.sems[emb[0]], emb[1])
            self.known[q][emb[0]] = emb[1]
        self.semval[s] += 16
        ins.then_inc(self.sems[s], 16)
        ev = (s, self.semval[s])
        self._sync_out(ev, r, w)
        self.n_inst += 1
        return ins

    def wait_all(self, e, views):
        for v in views:
            for d in v.t.deps(v.key):
                self._wait(e, d.w)

    def mm(self, out, lhsT, rhs, start=True, stop=True, **kw):
        return self.op("pe", lambda e: e.matmul(out.ap, lhsT.ap, rhs.ap, start=start, stop=stop, **kw),
                       r=[lhsT, rhs] + ([] if start else [out]), w=[out])

    def tr(self, out, in_, ident):
        return self.op("pe", lambda e: e.transpose(out.ap, in_.ap, ident.ap), r=[in_, ident], w=[out])

    def act(self, out, in_, func, bias=None, scale=None, accum=None, e="act"):
        kw = {}
        r = [in_]
        w = [out]
        if bias is not None:
            kw["bias"] = bias.ap if isinstance(bias, View) else bias
            r.append(bias)
        if scale is not None:
            kw["scale"] = scale.ap if isinstance(scale, View) else scale
            r.append(scale)
        if accum is not None:
            kw["accum_out"] = accum.ap
            w.append(accum)
        return self.op(e, lambda en: en.activation(out=out.ap, in_=in_.ap, func=func, **kw), r=r, w=w)

    def tt(self, out, a, b, op, e="dve"):
        return self.op(e, lambda en: en.tensor_tensor(out.ap, a.ap, b.ap, op), r=[a, b], w=[out])

    def ts(self, out, a, s1, op0, s2=None, op1=None, e="dve", accum=None):
        r = [a, s1, s2]
        w = [out]
        kw = {}
        if accum is not None:
            kw["accum_out"] = accum.ap
            w.append(accum)
        g = lambda s: s.ap if isinstance(s, View) else s
        if op1 is None:
            return self.op(e, lambda en: en.tensor_scalar(out.ap, a.ap, g(s1), None, op0, **kw), r=r, w=w)
        return self.op(e, lambda en: en.tensor_scalar(out.ap, a.ap, g(s1), g(s2), op0, op1, **kw), r=r, w=w)

    def stt(self, out, a, s, b, op0, op1, e="dve", accum=None):
        g = lambda x: x.ap if isinstance(x, View) else x
        w = [out]
        kw = {}
        if accum is not None:
            kw["accum_out"] = accum.ap
            w.append(accum)
        return self.op(e, lambda en: en.scalar_tensor_tensor(out.ap, a.ap, g(s), b.ap, op0, op1, **kw),
                       r=[a, s, b], w=w)

    def copy(self, out, in_, e="dve"):
        if e == "act":
            return self.op(e, lambda en: en.copy(out.ap, in_.ap), r=[in_], w=[out])
        return self.op(e, lambda en: en.tensor_copy(out.ap, in_.ap), r=[in_], w=[out])

    def recip(self, out, in_):
        return self.op("dve", lambda en: en.reciprocal(out.ap, in_.ap), r=[in_], w=[out])

    def memset(self, out, val, e="dve"):
        return self.op(e, lambda en: en.memset(out.ap, val), r=[], w=[out])

    def reduce(self, out, in_, op, axis=AX.X, e="dve"):
        return self.op(e, lambda en: en.tensor_reduce(out.ap, in_.ap, axis, op), r=[in_], w=[out])

    def finish(self):
        self.barrier(engines=("sp",))

D = 1024
S = 4096
LC = 256
TT = S + LC
NL = 2
GW = 64
EPS = 1e-6
INC = 7072
NCORES = 8
BF = ml_dtypes.bfloat16
import os
PEXP = os.environ.get('PEXP', '')


class Pool:
    def __init__(self, k, name, shape, dtype, n, space="sb"):
        mk = k.sb if space == "sb" else k.ps
        self.t = [mk(f"{name}{i}", shape, dtype) for i in range(n)]
        self.i = 0

    def nxt(self):
        t = self.t[self.i]
        self.i = (self.i + 1) % len(self.t)
        return t


def rope_tables():
    t = np.arange(S)
    row = (t // GW).astype(np.float32)
    col = (t % GW).astype(np.float32)

    def tab(hd):
        half = hd // 2
        h = half // 2
        freqs = (np.float32(10000.0) ** (-np.arange(h, dtype=np.float32) / np.float32(h))).astype(np.float32)
        C = np.zeros((hd, S), np.float32)
        Sn = np.zeros((hd, S), np.float32)
        for ax, pos in enumerate((row, col)):
            ang = (pos[:, None] * freqs[None, :]).astype(np.float32)
            c = np.cos(ang).astype(np.float32).T
            s = np.sin(ang).astype(np.float32).T
            o = ax * half
            C[o:o + h] = c
            C[o + h:o + 2 * h] = c
            Sn[o:o + h] = s
            Sn[o + h:o + 2 * h] = s
        return C, Sn

    def rmat(hd):
        half = hd // 2
        h = half // 2
        Rm = np.zeros((hd, hd), np.float32)
        for ax in range(2):
            o = ax * half
            for i in range(h):
                Rm[o + i, o + h + i] = -1.0
                Rm[o + h + i, o + i] = 1.0
        return Rm.T.copy()

    Cg, Sg = tab(64)
    Cm, Sm = tab(32)
    out = {}
    out["ropeCg"] = np.concatenate([Cg, Cg], 0)
    out["ropeSg"] = np.concatenate([Sg, Sg], 0)
    z = np.zeros((32, S), np.float32)
    out["ropeCm"] = np.concatenate([Cm, z, Cm], 0)
    out["ropeSm"] = np.concatenate([Sm, z, Sm], 0)
    Rg = np.zeros((128, 128), np.float32)
    Rg[0:64, 0:64] = rmat(64)
    Rg[64:128, 64:128] = rmat(64)
    Rq = np.zeros((96, 96), np.float32)
    Rq[64:96, 64:96] = rmat(32)
    Rq[0:32, 0:32] = rmat(32)
    out["Rg"] = Rg.astype(BF)
    out["Rq"] = Rq.astype(BF)
    return out


def misc_consts():
    out = {}
    out["ident_bf"] = np.eye(128, dtype=np.float32).astype(BF)
    out["ident_f"] = np.eye(128, dtype=np.float32)
    out["ones_f"] = np.ones((128, 128), np.float32)
    ob = np.zeros((128, 128), np.float32)
    ob[0:64, 0:64] = 1.0
    ob[64:, 64:] = 1.0
    out["onesblk_f"] = ob
    io = np.zeros((128, 8, 16, 16), np.float32)
    io[:] = np.arange(16, dtype=np.float32)[None, None, None, :]
    out["iota4"] = io
    m = np.arange(128)[:, None]
    n = np.arange(128)[None, :]
    Df = np.where(n >= m, (n - m), 100000).astype(np.float32)
    Db = np.where(n <= m, (m - n), 100000).astype(np.float32)
    out["retD"] = np.stack([Df, Db], 0)
    qa = np.zeros((2, 128, 128), np.float32)
    qa[0] = (np.arange(128) + 1)[None, :]
    qa[1] = (128 - np.arange(128))[None, :]
    out["retQA"] = qa
    ka = np.zeros((128, 2), np.float32)
    ka[:, 0] = 127 - np.arange(128)
    ka[:, 1] = np.arange(128)
    out["retKA"] = ka
    return out


def na_biasmask(na_bias):
    L = na_bias.shape[0]
    out = np.full((L, 4, 3, 8, 128, 512), -30000.0, np.float32)
    for pat, qg in enumerate((0, 3, 7)):
        r0 = 8 * qg
        kr0 = int(np.clip(r0 - 4, 0, 48))
        r = r0 + np.arange(8)
        rs = np.clip(r - 4, 0, 56)
        c = np.arange(64)
        cs = np.clip(c - 8, 0, 48)
        for kc in range(8):
            kr = kr0 + 2 * kc + np.arange(2)
            KR = kr[:, None, None, None]
            CP = c[None, :, None, None]
            R = r[None, None, :, None]
            RS = rs[None, None, :, None]
            C = c[None, None, None, :]
            CS = cs[None, None, None, :]
            valid = (KR >= RS) & (KR < RS + 8) & (CP >= CS) & (CP < CS + 16)
            rel_r = np.clip(KR - R + 7, 0, 14)
            rel_c = np.clip(CP - C + 15, 0, 30)
            rel_r, rel_c, valid = np.broadcast_arrays(rel_r, rel_c, valid)
            for l in range(L):
                for h in range(4):
                    vals = na_bias[l, h][rel_r, rel_c]
                    out[l, h, pat, kc] = np.where(valid, vals, np.float32(-30000.0)).reshape(128, 512)
    return out


def build(NB=2, layers=(0, 1), dbg=(), stop=None):
    k = K()
    nc = k.nc
    dbg = set(dbg)
    IN = lambda name, shape, dt=F32: k.dram(name, shape, dt, kind="ExternalInput")

    def SCR(name, shape, dt=BF16):
        return k.dram(name, shape, dt, kind=("ExternalOutput" if name in dbg else "Internal"))

    x_in = IN("x", [NB, S, D])
    ctx_in = IN("ctx", [NB, LC, D])
    cT3 = IN("cT3", [128, 8, NB + 1])
    mod_w = IN("mod_w", [NL, D, 6 * D])
    mod_bT = IN("mod_bT", [NL, 128, 48])
    mod_b = IN("mod_b", [NL, 6 * D])
    n1wT = IN("n1wT", [NL, 128, 8])
    n2wT = IN("n2wT", [NL, 128, 8])
    w_in = IN("w_in", [NL, D, INC])
    qnT = IN("qnT", [NL, 128, 2])
    kvnT = IN("kvnT", [NL, 128, 1])
    gqnT = IN("gqnT", [NL, 128, 1])
    gknT = IN("gknT", [NL, 128, 1])
    wuq = IN("mla_w_uq", [NL, 256, 384])
    wukv = IN("mla_w_ukv", [NL, 128, 512])
    nabm = IN("nabm", [NL, 4, 3, 8, 128, 512])
    rdl = IN("ret_decay_logit", [NL, 8])
    gnw = IN("ret_gn_w", [NL, 256])
    w_branch = IN("w_branch", [NL, 4, 256, D])
    w_out = IN("w_out", [NL, D, D])
    peer_wq = IN("peer_w_q", [NL, D, D])
    keysbd = IN("keysbd", [NL, 8, 128, 256])
    peer_u = [IN("peer_u%d" % i, [16384, D]) for i in range(NL)]
    peer_v = [IN("peer_v%d" % i, [16384, D]) for i in range(NL)]
    fnw = IN("final_norm_w", [D])
    c_ident_bf = IN("ident_bf", [128, 128], BF16)
    c_ident_f = IN("ident_f", [128, 128])
    c_ones_f = IN("ones_f", [128, 128])
    c_onesblk = IN("onesblk_f", [128, 128])
    c_iota4 = IN("iota4", [128, 8, 16, 16])
    c_retD = IN("retD", [2, 128, 128])
    c_retQA = IN("retQA", [2, 128, 128])
    c_retKA = IN("retKA", [128, 2])
    c_Cg = IN("ropeCg", [128, S])
    c_Sg = IN("ropeSg", [128, S])
    c_Cm = IN("ropeCm", [96, S])
    c_Sm = IN("ropeSm", [96, S])
    c_Rg = IN("Rg", [128, 128], BF16)
    c_Rq = IN("Rq", [96, 96], BF16)
    out_d = k.dram("out", [NB, S, D], F32, kind="ExternalOutput")

    xres = SCR("xres", [NB, S, D], F32)
    cres = SCR("cres", [NB, LC, D], F32)
    wb_in = SCR("wb_in", [NL, D, INC])
    wb_uq = SCR("wb_uq", [NL, 256, 384])
    wb_ukv = SCR("wb_ukv", [NL, 128, 512])
    wb_br = SCR("wb_br", [NL, 4, 256, D])
    wb_out = SCR("wb_out", [NL, D, D])
    wb_pq = SCR("wb_pq", [NL, D, D])
    wb_keys = SCR("wb_keys", [NL, 8, 128, 256])
    uvb = [SCR("uvb%d" % i, [16384, 2 * D]) for i in range(NL)]
    nabb = SCR("nabb", [NL, 4, 3, 8, 128, 512])
    mqT = SCR("mqT", [384, NB, TT])
    mknT = SCR("mknT", [256, NB, TT])
    mkpT = SCR("mkpT", [32, NB, TT])
    mv = SCR("mv", [NB, TT, 256])
    gqT = SCR("gqT", [256, NB, TT])
    gkT = SCR("gkT", [128, NB, TT])
    gv = SCR("gv", [NB, TT, 128])
    nqT = SCR("nqT", [256, NB, TT])
    nkT = SCR("nkT", [256, NB, TT])
    nv = SCR("nv", [NB, TT, 256])
    rqT = SCR("rqT", [256, NB, TT])
    rkT = SCR("rkT", [256, NB, TT])
    rkv = SCR("rkv", [NB, TT, 512])
    rg = SCR("rg", [NB, TT, 512])
    gT = SCR("gT", [4096, NB, TT])
    oT = SCR("oT", [1024, NB, TT])

    ident_bf = k.sb("ident_bf", [128, 128], BF16)
    ident_f = k.sb("ident_f", [128, 128], F32)
    ones_f = k.sb("ones_f", [128, 128], F32)
    onesblk = k.sb("onesblk", [128, 128], F32)
    Rg = k.sb("Rg", [128, 128], BF16)
    Rq = k.sb("Rq", [96, 96], BF16)
    for sbt, dr in ((ident_bf, c_ident_bf), (ident_f, c_ident_f), (ones_f, c_ones_f), (onesblk, c_onesblk), (Rg, c_Rg)):
        k.dma(sbt[:], dr[:, :])
    k.dma(Rq[:], c_Rq[:, :])

    psA = Pool(k, "psA", [128, 512], F32, 6, space="ps")
    psB = Pool(k, "psB", [128, 512], F32, 2, space="ps")
    P = psA.nxt

    def bfview(pt, a, b, p0=0, p1=128):
        return pt.v(pt.base.bitcast(BF16)[p0:p1, a:b])

    evn = [0]

    def evac(out, in_, scale=None, func=None):
        evn[0] += 1
        if func is not None:
            return k.act(out, in_, func, scale=scale)
        if evn[0] % 2 == 0:
            return k.act(out, in_, AF.Copy, scale=scale)
        if scale is None:
            return k.copy(out, in_)
        return k.ts(out, in_, scale, ALU.mult)

    CV = {}
    cvn = [0]

    def conv_pools(nbuf, engs):
        CV["f"] = Pool(k, "cvf", [128, 2048], F32, nbuf)
        CV["b"] = Pool(k, "cvb", [128, 2048], BF16, nbuf)
        CV["e"] = engs

    def conv_gen(l):
        def flat(ap, pat, **kw):
            return ap.rearrange(pat, **kw)
        yield from convert(flat(w_in.base[l], "(p a) c -> p (a c)", p=128), wb_in, flat(wb_in.base[l], "(p a) c -> p (a c)", p=128), 8 * INC)
        yield from convert(flat(wuq.base[l], "(p a) c -> p (a c)", p=128), wb_uq, flat(wb_uq.base[l], "(p a) c -> p (a c)", p=128), 2 * 384)
        yield from convert(wukv.base[l], wb_ukv, wb_ukv.base[l], 512)
        for i in range(4):
            yield from convert(flat(w_branch.base[l, i], "(p a) c -> p (a c)", p=128), wb_br, flat(wb_br.base[l, i], "(p a) c -> p (a c)", p=128), 2 * D)
        yield from convert(flat(w_out.base[l], "(p a) c -> p (a c)", p=128), wb_out, flat(wb_out.base[l], "(p a) c -> p (a c)", p=128), 8 * D)
        yield from convert(flat(peer_wq.base[l], "(p a) c -> p (a c)", p=128), wb_pq, flat(wb_pq.base[l], "(p a) c -> p (a c)", p=128), 8 * D)
        for h in range(8):
            yield from convert(keysbd.base[l, h], wb_keys, wb_keys.base[l, h], 256)
        for h in range(4):
            for t_ in range(3):
                for c in range(8):
                    yield from convert(nabm.base[l, h, t_, c], nabb, nabb.base[l, h, t_, c], 512)
        for a in range(128):
            yield from convert(peer_u[l].base.rearrange("(p a) c -> p a c", p=128)[:, a, :], uvb[l], uvb[l].base.rearrange("(p a) c -> p a c", p=128)[:, a, 0:D], D)
            yield from convert(peer_v[l].base.rearrange("(p a) c -> p a c", p=128)[:, a, :], uvb[l], uvb[l].base.rearrange("(p a) c -> p a c", p=128)[:, a, D:2 * D], D)

    ro = T("ro", None, "dram")

    def RO(ap):
        return View(ro, "r", ap)

    def convert(src_ap, dst_t, dst_ap, nper):
        for c0 in range(0, nper, 2048):
            n = min(2048, nper - c0)
            f = CV["f"].nxt()
            b = CV["b"].nxt()
            k.dma(f[:, 0:n], RO(src_ap[:, c0:c0 + n]))
            e = CV["e"][cvn[0] % len(CV["e"])]
            cvn[0] += 1
            k.copy(b[:, 0:n], f[:, 0:n], e=e)
            k.dma(dst_t.v(dst_ap[:, c0:c0 + n], key=("cv", cvn[0])), b[:, 0:n], q="pool")
            yield

    k.push()
    conv_pools(8, ("dve", "act"))
    for _ in conv_gen(layers[0]):
        pass
    k.pop()

    modF = k.sb("modF", [128, 48, NB + 1], F32)
    A1 = k.sb("A1", [128, 8, NB + 1], F32)
    A2 = k.sb("A2", [128, 8, NB + 1], F32)
    grep = k.sb("grep", [128, 2, NB + 1, D], F32)
    cT = k.sb("cT", [128, 8, NB + 1], F32)
    k.dma(cT[:], cT3[:, :, :])
    k.act(cT[:], cT[:], AF.Silu)
    mbT = k.sb("mbT", [128, 48], F32)
    nwT = k.sb("nwT", [128, 2, 8], F32)

    def phase_mod(l):
        k.push()
        mwp = Pool(k, "mw", [128, 8, 512], F32, 2)
        mbrep = k.sb("mbrep", [128, 512], F32)
        cTb = k.sb("cTb", [128, NB + 1, 8, 128], F32)
        for col in range(NB + 1):
            for j in range(8):
                k.copy(cTb[:, col, j, :], cT.v(cT.base[:, j, col:col + 1].to_broadcast([128, 128])))
        k.dma(mbT[:], mod_bT[l])
        k.dma(nwT[:, 0, :], n1wT[l])
        k.dma(nwT[:, 1, :], n2wT[l])
        mwv = mod_w.base[l].rearrange("(j p) c -> p j c", p=128)
        fm_blocks = {0: 0, 1: 4, 2: 8, 3: 12, 6: 24, 7: 28, 8: 32, 9: 36}
        g_blocks = {4: (0, 0), 5: (0, 512), 10: (1, 0), 11: (1, 512)}
        for cb in range(12):
            mw = mwp.nxt()
            k.dma(mw[:], RO(mwv[:, :, cb * 512:(cb + 1) * 512]))
            if cb in fm_blocks:
                for o4 in range(4):
                    oc = cb * 4 + o4
                    p = P()
                    for j in range(8):
                        k.mm(p[:, 0:NB + 1], mw[:, j, o4 * 128:(o4 + 1) * 128], cT[:, j, :], start=(j == 0), stop=(j == 7))
                    k.ts(modF[:, oc, :], p[:, 0:NB + 1], mbT[:, oc:oc + 1], ALU.add)
            else:
                gi, off = g_blocks[cb]
                k.dma(mbrep[:], RO(mod_b.base[l, cb * 512:(cb + 1) * 512].partition_broadcast(128)))
                for col in range(NB + 1):
                    p = P()
                    for j in range(8):
                        k.mm(p[:, :], cTb[:, col, j, :], mw[:, j, :], start=(j == 0), stop=(j == 7))
                    k.tt(grep[:, gi, col, off:off + 512], p[:, :], mbrep[:], ALU.add)
        for j in range(8):
            k.ts(A1[:, j, :], modF[:, 8 + j, :], 1.0, ALU.add)
            k.ts(A1[:, j, :], A1[:, j, :], nwT[:, 0, j:j + 1], ALU.mult)
            k.ts(A2[:, j, :], modF[:, 32 + j, :], 1.0, ALU.add)
            k.ts(A2[:, j, :], A2[:, j, :], nwT[:, 1, j:j + 1], ALU.mult)
        k.pop()

    NP = {}

    def norm_pools():
        NP["junk"] = Pool(k, "junk", [128, D], BF16, 2)
        NP["xn"] = Pool(k, "xn", [128, D], BF16, 2)
    ssp = Pool(k, "ss", [128, 2], F32, 4)

    def norm_T(xt, hxT, i, Am, Bm, col):
        ss = ssp.nxt()
        jk = NP["junk"].nxt()
        k.act(jk[:], xt[:], AF.Square, accum=ss[:, 0:1])
        k.act(ss[:, 1:2], ss[:, 0:1], AF.Sqrt, bias=EPS, scale=1.0 / D)
        k.recip(ss[:, 1:2], ss[:, 1:2])
        xn = NP["xn"].nxt()
        k.ts(xn[:], xt[:], ss[:, 1:2], ALU.mult)
        pt = P()
        for j in range(8):
            k.tr(bfview(pt, j * 128, (j + 1) * 128), xn[:, j * 128:(j + 1) * 128], ident_bf[:])
        for j in range(8):
            o = hxT[:, j, i * 128:(i + 1) * 128]
            src = bfview(pt, j * 128, (j + 1) * 128)
            if j % 2 == 0:
                k.act(o, src, AF.Identity, bias=Bm(j, col), scale=Am[:, j, col:col + 1])
            else:
                k.ts(o, src, Am[:, j, col:col + 1], ALU.mult, Bm(j, col), ALU.add)

    wuq_sb = k.sb("wuq_sb", [128, 2, 384], BF16)
    wkn_sb = k.sb("wkn_sb", [128, 4, 64], BF16)
    wkv_sb = k.sb("wkv_sb", [128, 4, 64], BF16)
    nrmw = k.sb("nrmw", [128, 5], F32)

    def fm_rstd(rsp, sqs, ones_t, nfeat, n):
        p = P()
        for c, sq in enumerate(sqs):
            k.mm(p[:, 0:n], ones_t[:], sq, start=(c == 0), stop=(c == len(sqs) - 1))
        rs = rsp.nxt()
        k.act(rs[:, 0:n], p[:, 0:n], AF.Sqrt, bias=EPS, scale=1.0 / nfeat)
        k.recip(rs[:, 0:n], rs[:, 0:n])
        return rs

    def load_layer_small(l):
        k.dma(wuq_sb[:], wb_uq.v(wb_uq.base[l].rearrange("(c p) m -> p c m", p=128)))
        wv = wb_ukv.base[l].rearrange("p (h t e) -> p h t e", h=4, t=2)
        k.dma(wkn_sb[:], wb_ukv.v(wv[:, :, 0, :]))
        k.dma(wkv_sb[:], wb_ukv.v(wv[:, :, 1, :]))
        k.dma(nrmw[:, 0:2], qnT[l])
        k.dma(nrmw[:, 2:3], kvnT[l])
        k.dma(nrmw[:, 3:4], gqnT[l])
        k.dma(nrmw[:, 4:5], gknT[l])

    def groups_for(l, with_ctx=True):
        gs = []
        for b in range(NB):
            gs.append(dict(b=b, t0=0, n=LC, lat=False, col=NB, s0=0))
            for g in range(S // 512):
                gs.append(dict(b=b, t0=LC + g * 512, n=512, lat=True, col=b, s0=g * 512))
        return gs

    def src_rows(l, g, i):
        b = g["b"]
        if g["lat"]:
            r0 = g["s0"] + i * 128
            return (RO(x_in.base[b, r0:r0 + 128, :]) if l == layers[0] and l == 0 else xres[b, r0:r0 + 128, :])
        r0 = i * 128
        return (RO(ctx_in.base[b, r0:r0 + 128, :]) if l == 0 else cres[b, r0:r0 + 128, :])

    def phase_proj(l):
        k.push()
        norm_pools()
        xpool = Pool(k, "xt", [128, D], F32, 3)
        hxTp = Pool(k, "hxT", [128, 8, 512], BF16, 2)
        wpool = Pool(k, "wblk", [128, 8, 512], BF16, 3)
        sqp = Pool(k, "sq", [128, 2, 512], F32, 2)
        rsp = Pool(k, "rs", [128, 512], F32, 2)
        nrm = Pool(k, "nrm", [128, 2, 512], BF16, 2)
        stg = Pool(k, "stg", [128, 512], BF16, 6)
        tmpf = Pool(k, "tmpf", [128, 512], F32, 4)
        ropeT = Pool(k, "ropeT", [128, 4, 512], F32, 2)
        wv = wb_in.base[l].rearrange("(j p) c -> p j c", p=128)
        blocks = [(0, 416), (416, 512), (928, 512), (1440, 512), (1952, 512), (2464, 512)] + [(2976 + 512 * i, 512) for i in range(8)]
        for g in groups_for(l):
            b, t0, n, lat, col, s0 = g["b"], g["t0"], g["n"], g["lat"], g["col"], g["s0"]
            nt = n // 128
            hxT = hxTp.nxt()
            for i in range(nt):
                xt = xpool.nxt()
                k.dma(xt[:], src_rows(l, g, i))
                norm_T(xt, hxT, i, A1, lambda j, c: modF[:, j, c:c + 1], col)
            if lat:
                rt = ropeT.nxt()
                k.dma(rt[:, 0, 0:n], RO(c_Cg.base[:, s0:s0 + n]))
                k.dma(rt[:, 1, 0:n], RO(c_Sg.base[:, s0:s0 + n]))
                k.dma(rt[0:96, 2, 0:n], RO(c_Cm.base[:, s0:s0 + n]))
                k.dma(rt[0:96, 3, 0:n], RO(c_Sm.base[:, s0:s0 + n]))

            def loadw(bi):
                c0, ncol = blocks[bi]
                wt = wpool.nxt()
                k.dma(wt[:, :, 0:ncol], wb_in.v(wv[:, :, c0:c0 + ncol]))
                return wt

            def fm(wt, c0, ncol):
                p = P()
                for j in range(8):
                    k.mm(p[0:ncol, 0:n], wt[:, j, c0:c0 + ncol], hxT[:, j, 0:n], start=(j == 0), stop=(j == 7))
                return p

            def tm(wt, c0, ncol, i):
                p = P()
                for j in range(8):
                    k.mm(p[:, 0:ncol], hxT[:, j, i * 128:(i + 1) * 128], wt[:, j, c0:c0 + ncol], start=(j == 0), stop=(j == 7))
                return p

            def store_fm(dst, r0, nr, src):
                k.dma(dst.k((b, t0, r0))[r0:r0 + nr, b, t0:t0 + n], src, q="pool")

            def store_tm(dst, i, src):
                k.dma(dst.k((b, t0, i))[b, t0 + i * 128:t0 + (i + 1) * 128, :], src, q="pool")

            def rope(xs, p0, p1, Rt, ci, si):
                R = Rt.base.shape[0]
                p2 = P()
                k.mm(p2[0:R, 0:n], Rt[:, :], xs[0:R, 0:n])
                t1 = tmpf.nxt()
                t2 = tmpf.nxt()
                k.tt(t1[p0:p1, 0:n], p2[p0:p1, 0:n], rt[p0:p1, si, 0:n], ALU.mult)
                k.tt(t2[p0:p1, 0:n], xs[p0:p1, 0:n], rt[p0:p1, ci, 0:n], ALU.mult)
                k.tt(xs[p0:p1, 0:n], t1[p0:p1, 0:n], t2[p0:p1, 0:n], ALU.add)

            w0 = loadw(0)
            w1 = loadw(1)
            pc = [fm(w0, 0, 128), fm(w0, 128, 128)]
            sq = sqp.nxt()
            for c in range(2):
                k.act(sq[:, c, 0:n], pc[c][:, 0:n], AF.Square)
            rs = fm_rstd(rsp, [sq[:, 0, 0:n], sq[:, 1, 0:n]], ones_f, 256, n)
            cqn = nrm.nxt()
            for c in range(2):
                k.stt(cqn[:, c, 0:n], pc[c][:, 0:n], nrmw[:, c:c + 1], rs[:, 0:n], ALU.mult, ALU.mult)
            for h in range(4):
                p = P()
                for c in range(2):
                    k.mm(p[0:96, 0:n], wuq_sb[:, c, h * 96:(h + 1) * 96], cqn[:, c, 0:n], start=(c == 0), stop=(c == 1))
                qa = stg.nxt()
                evac(qa[0:96, 0:n], p[0:96, 0:n])
                if lat:
                    rope(qa, 64, 96, Rq, 2, 3)
                store_fm(mqT, h * 96, 96, qa[0:96, 0:n])
            pk = fm(w0, 256, 128)
            sq = sqp.nxt()
            k.act(sq[:, 0, 0:n], pk[:, 0:n], AF.Square)
            rs = fm_rstd(rsp, [sq[:, 0, 0:n]], ones_f, 128, n)
            ckn = nrm.nxt()
            k.stt(ckn[:, 0, 0:n], pk[:, 0:n], nrmw[:, 2:3], rs[:, 0:n], ALU.mult, ALU.mult)
            for h in range(4):
                p = P()
                k.mm(p[0:64, 0:n], wkn_sb[:, h, :], ckn[:, 0, 0:n])
                kn = stg.nxt()
                evac(kn[0:64, 0:n], p[0:64, 0:n])
                store_fm(mknT, h * 64, 64, kn[0:64, 0:n])
            for i in range(nt):
                p = P()
                k.mm(p[:, 0:256], ckn[:, 0, i * 128:(i + 1) * 128], wkv_sb.v(wkv_sb.base[:, :, :].rearrange("p h e -> p (h e)")))
                vt = stg.nxt()
                evac(vt[:, 0:256], p[:, 0:256])
                store_tm(mv, i, vt[:, 0:256])
            pp = fm(w0, 384, 32)
            kp = stg.nxt()
            evac(kp[0:32, 0:n], pp[0:32, 0:n])
            if lat:
                rope(kp, 0, 32, Rq.v(Rq.base[0:32, 0:32]) if False else Rq32, 2, 3)
            store_fm(mkpT, 0, 32, kp[0:32, 0:n])
            w2 = loadw(2)
            for (c0, dst, r0, nw) in ((0, gqT, 0, 3), (128, gqT, 128, 3), (256, gkT, 0, 4)):
                p = fm(w1, c0, 128)
                sq = sqp.nxt()
                k.act(sq[:, 0, 0:n], p[:, 0:n], AF.Square)
                rs = fm_rstd(rsp, [sq[:, 0, 0:n]], onesblk, 64, n)
                xo = stg.nxt()
                k.stt(xo[:, 0:n], p[:, 0:n], nrmw[:, nw:nw + 1], rs[:, 0:n], ALU.mult, ALU.mult)
                if lat:
                    rope(xo, 0, 128, Rg, 0, 1)
                store_fm(dst, r0, 128, xo[:, 0:n])
            for i in range(nt):
                p = tm(w1, 384, 128, i)
                vt = stg.nxt()
                evac(vt[:, 0:128], p[:, 0:128])
                store_tm(gv, i, vt[:, 0:128])
            w3 = loadw(3)
            for c in range(4):
                p = fm(w2, c * 128, 128)
                xo = stg.nxt()
                evac(xo[:, 0:n], p[:, 0:n], scale=(0.125 if c < 2 else None))
                store_fm(nqT if c < 2 else nkT, (c % 2) * 128, 128, xo[:, 0:n])
            w4 = loadw(4)
            for i in range(nt):
                p = tm(w3, 0, 256, i)
                vt = stg.nxt()
                evac(vt[:, 0:256], p[:, 0:256])
                store_tm(nv, i, vt[:, 0:256])
            for c in range(2):
                p = fm(w3, 256 + c * 128, 128)
                xo = stg.nxt()
                evac(xo[:, 0:n], p[:, 0:n])
                store_fm(rqT, c * 128, 128, xo[:, 0:n])
            w5 = loadw(5)
            for c in range(2):
                p = fm(w4, c * 128, 128)
                xo = stg.nxt()
                evac(xo[:, 0:n], p[:, 0:n], scale=0.125)
                store_fm(rkT, c * 128, 128, xo[:, 0:n])
            for i in range(nt):
                p = tm(w4, 0, 512, i)
                vt = stg.nxt()
                evac(vt[:, 0:256], p[:, 0:256], scale=0.125)
                evac(vt[:, 256:512], p[:, 256:512])
                store_tm(rkv, i, vt[:, 0:512])
            wn = loadw(6)
            for i in range(nt):
                p = tm(w5, 0, 512, i)
                vt = stg.nxt()
                evac(vt[:, 0:512], p[:, 0:512])
                store_tm(rg, i, vt[:, 0:512])
            for gb in range(8):
                wc = wn
                if gb < 7:
                    wn = loadw(7 + gb)
                for c in range(4):
                    p = fm(wc, c * 128, 128)
                    xo = stg.nxt()
                    k.act(xo[:, 0:n], p[:, 0:n], AF.Sigmoid)
                    store_fm(gT, gb * 512 + c * 128, 128, xo[:, 0:n])
        k.pop()

    pend_epi = []

    def attn_flush():
        while pend_epi:
            pend_epi.pop(0)()

    def attn_core(pools, KT, dk, V, kchunks, q_src, nq, scale, o_dst, bias_fn=None):
        qpool, ptp, osp, obp = pools
        qt = q_src
        po = psB.nxt()
        n_ = len(kchunks)
        pss = {}

        def qk(ci):
            kc = kchunks[ci]
            ps = P()
            bm = bias_fn(kc) if bias_fn is not None else None
            k.mm(ps[:, 0:nq], KT[0:dk, kc * 128:(kc + 1) * 128], qt[0:dk, 0:nq], start=True, stop=(bm is None))
            if bm is not None:
                k.mm(ps[:, 0:nq], ident_bf[:], bm, start=False, stop=True)
            pss[ci] = ps

        qk(0)
        if n_ > 1:
            qk(1)
        attn_flush()
        for ci in range(n_):
            pt = ptp.nxt()
            k.act(pt[:, 0:nq], pss.pop(ci)[:, 0:nq], AF.Exp, scale=scale)
            if ci + 2 < n_:
                qk(ci + 2)
            k.mm(po[0:65, 0:nq], V[:, kchunks[ci], 0:65], pt[:, 0:nq], start=(ci == 0), stop=(ci == n_ - 1))

        def epi():
            os_ = osp.nxt()
            k.copy(os_[0:65, 0:nq], po[0:65, 0:nq])
            k.recip(os_[64:65, 0:nq], os_[64:65, 0:nq])
            pb = P()
            k.mm(pb[0:64, 0:nq], ones_f[64:65, 0:64], os_[64:65, 0:nq])
            ob = obp.nxt()
            k.tt(ob[0:64, 0:nq], os_[0:64, 0:nq], pb[0:64, 0:nq], ALU.mult)
            k.dma(o_dst, ob[0:64, 0:nq], q="pool")
        pend_epi.append(epi)

    def attn_pools():
        qpool = Pool(k, "aq", [96, 512], BF16, 2)
        qpoolB = Pool(k, "aqB", [128, 512], BF16, 2)
        for t_ in qpoolB.t:
            k.memset(t_[:], 0.0)
        qpool.padded = qpoolB
        ptp = Pool(k, "apt", [128, 512], BF16, 4)
        osp = Pool(k, "aos", [65, 512], F32, 2)
        obp = Pool(k, "aob", [64, 512], BF16, 2)
        return (qpool, ptp, osp, obp)

    NKC = TT // 128

    def load_V(Vt, src_t, b, c0):
        k.dma(Vt[:, :, 0:64], src_t.v(src_t.base[b, :, c0:c0 + 64].rearrange("(c p) e -> p c e", p=128)))

    def run_attn_jobs(pools, jobs, KTp, Vp, bmp=None, filler=None, nfill=0):
        qpool = pools[0]
        kvbuf = {}

        def ensure_kv(key, loader):
            if key not in kvbuf:
                KT, Vt = (KTp[1] if key[0] == "g" else KTp[0]).nxt() if isinstance(KTp, tuple) else KTp.nxt(), Vp.nxt()
                loader(KT, Vt)
                kvbuf[key] = (KT, Vt)

        def prefetch(j):
            qt = (qpool.padded if j.get("pad") else qpool).nxt()
            k.dma(qt[0:j["dk"], 0:j["nq"]], j["qsrc"])
            j["qt"] = qt
            if j.get("bmsrc") is not None:
                bm = bmp.nxt()
                k.dma(bm[:], j["bmsrc"])
                j["bm"] = bm

        ensure_kv(jobs[0]["kv"], jobs[0]["kvload"])
        prefetch(jobs[0])
        for i, j in enumerate(jobs):
            if i + 1 < len(jobs):
                prefetch(jobs[i + 1])
            if i == 0 or jobs[i - 1]["kv"] != j["kv"]:
                for j2 in jobs[i + 1:]:
                    if j2["kv"] != j["kv"]:
                        ensure_kv(j2["kv"], j2["kvload"])
                        break
                for key in [kk for kk in kvbuf if kk != j["kv"] and all(kk != j2["kv"] for j2 in jobs[i:])]:
                    del kvbuf[key]
            KT, Vt = kvbuf[j["kv"]]
            bfn = (j["bias"](j["bm"]) if j.get("bias") is not None else None)
            attn_core(pools, KT, (128 if j.get("pad") else j["dk"]), Vt, j["chunks"], j["qt"], j["nq"], j["scale"], j["odst"], bias_fn=bfn)
            if filler is not None:
                for _ in range(nfill):
                    next(filler, None)
        attn_flush()
        if filler is not None:
            for _ in filler:
                pass

    def phase_attn(l, need_ctx):
        k.push()
        pools = attn_pools()
        KTp = Pool(k, "aKT", [96, TT], BF16, 2)
        KTg = Pool(k, "aKTg", [128, TT], BF16, 2)
        for t_ in KTg.t:
            k.memset(t_[64:128, :], 0.0)
        Vp = Pool(k, "aV", [128, NKC, 65], BF16, 2)
        for Vt in Vp.t:
            k.memset(Vt[:, :, 64:65], 1.0)
        allk = list(range(NKC))
        jobs = []
        for b in range(NB):
            for h in range(4):
                def kvl(KT, Vt, b=b, h=h):
                    k.dma(KT[0:64, :], mknT[h * 64:(h + 1) * 64, b, :])
                    k.dma(KT[64:96, :], mkpT[:, b, :])
                    load_V(Vt, mv, b, h * 64)
                for qg in range(8):
                    t0 = LC + qg * 512
                    jobs.append(dict(kv=("m", b, h), kvload=kvl, dk=96, chunks=allk, qsrc=mqT[h * 96:(h + 1) * 96, b, t0:t0 + 512], nq=512,
                                     scale=96 ** -0.5, odst=oT.k((b, t0, h))[h * 64:(h + 1) * 64, b, t0:t0 + 512]))
                if need_ctx:
                    jobs.append(dict(kv=("m", b, h), kvload=kvl, dk=96, chunks=[0, 1], qsrc=mqT[h * 96:(h + 1) * 96, b, 0:LC], nq=LC,
                                     scale=96 ** -0.5, odst=oT.k((b, 0, h))[h * 64:(h + 1) * 64, b, 0:LC]))
            for kv in range(2):
                def kvl(KT, Vt, b=b, kv=kv):
                    k.dma(KT[0:64, :], gkT[kv * 64:(kv + 1) * 64, b, :])
                    load_V(Vt, gv, b, kv * 64)
                for gq in range(2):
                    h = kv * 2 + gq
                    for qg in range(8):
                        t0 = LC + qg * 512
                        jobs.append(dict(kv=("g", b, kv), kvload=kvl, pad=True, dk=64, chunks=allk, qsrc=gqT[h * 64:(h + 1) * 64, b, t0:t0 + 512], nq=512,
                                         scale=0.125, odst=oT.k((b, t0, 4 + h))[256 + h * 64:256 + (h + 1) * 64, b, t0:t0 + 512]))
                    if need_ctx:
                        jobs.append(dict(kv=("g", b, kv), kvload=kvl, pad=True, dk=64, chunks=[0, 1], qsrc=gqT[h * 64:(h + 1) * 64, b, 0:LC], nq=LC,
                                         scale=0.125, odst=oT.k((b, 0, 4 + h))[256 + h * 64:256 + (h + 1) * 64, b, 0:LC]))
        filler = None
        nxt_l = [x for x in layers if x > l]
        if nxt_l:
            conv_pools(6, ("dve",))
            filler = conv_gen(nxt_l[0])
        run_attn_jobs(pools, jobs, (KTp, KTg), Vp, filler=filler, nfill=5)
        k.pop()

    def phase_na(l, need_ctx):
        k.push()
        pools = attn_pools()
        KTp = Pool(k, "nKT", [128, TT], BF16, 2)
        for t_ in KTp.t:
            k.memset(t_[64:128, :], 0.0)
        Vp = Pool(k, "nV", [128, NKC, 65], BF16, 2)
        bmp = Pool(k, "nbm", [128, 8, 512], BF16, 3)
        for Vt in Vp.t:
            k.memset(Vt[:, :, 64:65], 1.0)
        jobs = []
        for b in range(NB):
            for h in range(4):
                def kvl(KT, Vt, b=b, h=h):
                    k.dma(KT[0:64, :], nkT[h * 64:(h + 1) * 64, b, :])
                    load_V(Vt, nv, b, h * 64)
                for qg in range(8):
                    t0 = LC + qg * 512
                    pat = 0 if qg == 0 else (2 if qg == 7 else 1)
                    kr0 = min(max(8 * qg - 4, 0), 48)
                    kc0 = 2 + kr0 // 2
                    jobs.append(dict(kv=("n", b, h), kvload=kvl, pad=True, dk=64, chunks=[0, 1] + [kc0 + j for j in range(8)],
                                     qsrc=nqT[h * 64:(h + 1) * 64, b, t0:t0 + 512], nq=512, scale=1.0,
                                     odst=oT.k((b, t0, 8 + h))[512 + h * 64:512 + (h + 1) * 64, b, t0:t0 + 512],
                                     bmsrc=nabb.v(nabb.base[l, h, pat].rearrange("c p q -> p c q")),
                                     bias=lambda bm, kc0=kc0: (lambda kc: (bm[:, kc - kc0, :] if kc >= 2 else None))))
                if need_ctx:
                    jobs.append(dict(kv=("n", b, h), kvload=kvl, pad=True, dk=64, chunks=[0, 1], qsrc=nqT[h * 64:(h + 1) * 64, b, 0:LC], nq=LC, scale=1.0,
                                     odst=oT.k((b, 0, 8 + h))[512 + h * 64:512 + (h + 1) * 64, b, 0:LC]))
        run_attn_jobs(pools, jobs, KTp, Vp, bmp)
        k.pop()

    def phase_ret(l, need_ctx):
        k.push()
        lg = k.sb("lg", [128, 8], F32)
        k.dma(lg[:], RO(rdl.base[l].partition_broadcast(128)))
        k.act(lg[:], lg[:], AF.Exp, scale=-1.0)
        k.ts(lg[:], lg[:], 1.0, ALU.add)
        k.act(lg[:], lg[:], AF.Ln)
        k.ts(lg[:], lg[:], -1.0, ALU.mult)
        Dd = k.sb("Dd", [128, 2, 128], F32)
        QA = k.sb("QA", [128, 2, 128], F32)
        KA = k.sb("KA", [128, 2], F32)
        k.dma(Dd[:], RO(c_retD.base.rearrange("d p n -> p d n")))
        k.dma(QA[:], RO(c_retQA.base.rearrange("d p n -> p d n")))
        k.dma(KA[:], RO(c_retKA.base))
        decT = k.sb("decT", [128, 2, 4, 128], F32)
        qdec = k.sb("qdec", [128, 2, 4, 128], F32)
        kdec = k.sb("kdec", [128, 2, 4], F32)
        cdec = k.sb("cdec", [128, 8], F32)
        gnr = k.sb("gnr", [128, 256], F32)
        k.dma(gnr[:], RO(gnw.base[l].partition_broadcast(128)))
        for d in range(2):
            for h in range(4):
                k.act(decT[:, d, h, :], Dd[:, d, :], AF.Exp, scale=lg[:, d * 4 + h:d * 4 + h + 1])
                k.act(qdec[:, d, h, :], QA[:, d, :], AF.Exp, scale=lg[:, d * 4 + h:d * 4 + h + 1])
            k.act(kdec[:, d, :], lg[:, d * 4:(d + 1) * 4], AF.Exp, scale=KA[:, d:d + 1])
        k.act(cdec[:], lg[:], AF.Exp, scale=128.0)
        QT = k.sb("rQT", [128, 2, TT], BF16)
        KT = k.sb("rKT", [128, 2, TT], BF16)
        KVt = k.sb("rKV", [128, NKC, 512], BF16)
        G = k.sb("rG", [128, NKC, 512], BF16)
        Ys = [k.sb("rY", [128, NKC, 256], F32), k.sb("rYb", [128, NKC, 256], BF16)]
        Sts = [k.sb("rS", [128, 2, 64], F32) for _ in range(2)]
        Sbs = [k.sb("rSb", [128, 2, 64], BF16) for _ in range(2)]
        kdp = Pool(k, "rkd", [128, 256], BF16, 2)
        idp = Pool(k, "rid", [128, 128], BF16, 3)
        qdp = Pool(k, "rqd", [128, 128], BF16, 3)
        osb = Pool(k, "ros", [128, 256], F32, 2)
        sgp = Pool(k, "rsg", [128, 256], F32, 2)
        stp = Pool(k, "rst", [128, 4], F32, 4)
        ybp = Pool(k, "ryb", [128, 256], BF16, 2)
        ysp = Pool(k, "rys", [128, 2, 128], BF16, 2)
        b3 = lambda t_, ap: t_.v(ap)
        for b in range(NB):
            for c in range(2):
                k.dma(QT[:, c, :], rqT[c * 128:(c + 1) * 128, b, :])
                k.dma(KT[:, c, :], rkT[c * 128:(c + 1) * 128, b, :])
            k.dma(KVt[:], rkv.v(rkv.base[b].rearrange("(c p) f -> p c f", p=128)))
            k.dma(G[:], rg.v(rg.base[b].rearrange("(c p) f -> p c f", p=128)))
            for d in range(2):
                k.memset(Sts[d][:], 0.0)
                k.memset(Sbs[d][:], 0.0)
            orders = [list(range(NKC)), [1, 0] + list(range(NKC - 1, 1, -1))]
            for step in range(2 * NKC):
                for _once in (0,):
                    d = step % 2
                    ci = orders[d][step // 2]
                    St, Sb, Y = Sts[d], Sbs[d], Ys[d]
                    tk = slice(ci * 128, (ci + 1) * 128)
                    Kd = kdp.nxt()
                    k.tt(b3(Kd, Kd.base[:, :].rearrange("p (h e) -> p h e", h=4)),
                         b3(KVt, KVt.base[:, ci, 0:256].rearrange("p (h e) -> p h e", h=4)),
                         b3(kdec, kdec.base[:, d, :].unsqueeze(2).to_broadcast([128, 4, 64])), ALU.mult)
                    pso = psB.nxt()
                    for h in range(4):
                        c2, po = h // 2, (h % 2) * 64
                        ps1 = P()
                        k.mm(ps1[:, 0:128], KT[po:po + 64, c2, tk], QT[po:po + 64, c2, tk])
                        idT = idp.nxt()
                        k.tt(idT[:], ps1[:, 0:128], decT[:, d, h, :], ALU.mult)
                        Qd = qdp.nxt()
                        k.tt(Qd[po:po + 64, :], QT[po:po + 64, c2, tk], qdec[po:po + 64, d, h, :], ALU.mult)
                        k.mm(pso[:, h * 64:(h + 1) * 64], idT[:], KVt[:, ci, 256 + h * 64:256 + (h + 1) * 64], start=True, stop=False)
                        k.mm(pso[:, h * 64:(h + 1) * 64], Qd[po:po + 64, :], Sb[po:po + 64, c2, :], start=False, stop=True)
                    for c2 in range(2):
                        ps2 = P()
                        k.mm(ps2[:, 0:128], Kd[:, c2 * 128:(c2 + 1) * 128], KVt[:, ci, 256 + c2 * 128:256 + (c2 + 1) * 128])
                        for hh in range(2):
                            po = hh * 64
                            h = c2 * 2 + hh
                            k.stt(St[po:po + 64, c2, :], St[po:po + 64, c2, :], cdec[po:po + 64, d * 4 + h:d * 4 + h + 1],
                                  ps2[po:po + 64, po:po + 64], ALU.mult, ALU.add)
                    k.copy(Sb[:], St[:])
                    if ci < 2 and not need_ctx:
                        continue
                    o = osb.nxt()
                    o3 = b3(o, o.base[:, :].rearrange("p (h e) -> p h e", h=4))
                    k.copy(o[:], pso[:, 0:256])
                    st = stp.nxt()
                    k.reduce(st[:], o3, ALU.add)
                    k.ts(st[:], st[:], 1.0 / 64, ALU.mult)
                    k.tt(o3, o3, b3(st, st.base[:, :].unsqueeze(2).to_broadcast([128, 4, 64])), ALU.subtract)
                    sq = sgp.nxt()
                    k.tt(sq[:], o[:], o[:], ALU.mult)
                    st2 = stp.nxt()
                    k.reduce(st2[:], b3(sq, sq.base[:, :].rearrange("p (h e) -> p h e", h=4)), ALU.add)
                    k.act(st2[:], st2[:], AF.Sqrt, bias=EPS, scale=1.0 / 64)
                    k.recip(st2[:], st2[:])
                    k.tt(o3, o3, b3(st2, st2.base[:, :].unsqueeze(2).to_broadcast([128, 4, 64])), ALU.mult)
                    k.tt(o[:], o[:], gnr[:], ALU.mult)
                    sg = sgp.nxt()
                    k.act(sg[:], G[:, ci, d * 256:(d + 1) * 256], AF.Silu)
                    k.tt(Y[:, ci, :], o[:], sg[:], ALU.mult)
            for ci in (range(NKC) if need_ctx else range(2, NKC)):
                yb = ybp.nxt()
                k.tt(yb[:], Ys[0][:, ci, :], Ys[1][:, ci, :], ALU.add)
                pt = P()
                for c2 in range(2):
                    k.tr(bfview(pt, c2 * 128, (c2 + 1) * 128), yb[:, c2 * 128:(c2 + 1) * 128], ident_bf[:])
                ys = ysp.nxt()
                k.copy(b3(ys, ys.base[:, :, :].rearrange("p c t -> p (c t)")), bfview(pt, 0, 256))
                for c2 in range(2):
                    k.dma(oT.k((b, ci, 12 + c2))[768 + c2 * 128:768 + (c2 + 1) * 128, b, ci * 128:(ci + 1) * 128], ys[:, c2, :], q="pool")
        k.pop()

    def phase_merge(l, need_ctx):
        k.push()
        wbr = k.sb("wbr", [128, 4, 2, D], BF16)
        wo = k.sb("wo", [128, 8, D], BF16)
        for i in range(4):
            k.dma(wbr[:, i, :, :], wb_br.v(wb_br.base[l, i].rearrange("(kk p) m -> p kk m", p=128)))
        k.dma(wo[:], wb_out.v(wb_out.base[l].rearrange("(j p) m -> p j m", p=128)))
        osp = Pool(k, "mo", [128, 8, 512], BF16, 2)
        mgl = Pool(k, "mgl", [128, 512], BF16, 6)
        accp = Pool(k, "macc", [128, 512], F32, 2)
        tmpp = Pool(k, "mtmp", [128, 512], F32, 2)
        accT = Pool(k, "maT", [128, 8, 512], BF16, 2)
        xp = Pool(k, "mx", [128, D], F32, 3)
        for g in groups_for(l):
            b, t0, n, lat, col = g["b"], g["t0"], g["n"], g["lat"], g["col"]
            if not lat and not need_ctx:
                continue
            nt = n // 128
            o_sb = osp.nxt()
            k.dma(o_sb[:, :, 0:n], oT.v(oT.base[:, b, t0:t0 + n].rearrange("(kk p) t -> p kk t", p=128)))
            aT = accT.nxt()
            for m in range(8):
                acc = accp.nxt()
                for i in range(4):
                    p = P()
                    for kk in range(2):
                        k.mm(p[:, 0:n], wbr[:, i, kk, m * 128:(m + 1) * 128], o_sb[:, 2 * i + kk, 0:n], start=(kk == 0), stop=(kk == 1))
                    gl = mgl.nxt()
                    k.dma(gl[:, 0:n], gT[i * 1024 + m * 128:i * 1024 + (m + 1) * 128, b, t0:t0 + n])
                    if i == 0:
                        k.tt(acc[:, 0:n], p[:, 0:n], gl[:, 0:n], ALU.mult)
                    else:
                        tp_ = tmpp.nxt()
                        k.tt(tp_[:, 0:n], p[:, 0:n], gl[:, 0:n], ALU.mult)
                        k.tt(acc[:, 0:n], acc[:, 0:n], tp_[:, 0:n], ALU.add, e="pool")
                k.copy(aT[:, m, 0:n], acc[:, 0:n], e="act")
            for i in range(nt):
                xt = xp.nxt()
                k.dma(xt[:], src_rows(l, g, i))
                for hf in range(2):
                    p = P()
                    for m in range(8):
                        k.mm(p[:, :], aT[:, m, i * 128:(i + 1) * 128], wo[:, m, hf * 512:(hf + 1) * 512], start=(m == 0), stop=(m == 7))
                    tp_ = tmpp.nxt()
                    k.tt(tp_[:], p[:, :], grep[:, 0, col, hf * 512:(hf + 1) * 512], ALU.mult)
                    k.tt(xt[:, hf * 512:(hf + 1) * 512], xt[:, hf * 512:(hf + 1) * 512], tp_[:], ALU.add, e="pool")
                dst = xres.k((b, g["s0"] + i * 128))[b, g["s0"] + i * 128:g["s0"] + (i + 1) * 128, :] if lat else cres.k((b, i))[b, i * 128:(i + 1) * 128, :]
                k.dma(dst, xt[:], q="pool")
        k.pop()

    def phase_peer(l, need_ctx):
        k.push()
        norm_pools()
        wq = k.sb("pwq", [128, 8, D], BF16)
        kb = k.sb("pkb", [128, 8, 256], BF16)
        iota16 = k.sb("piota", [128, 16], F32)
        k.dma(wq[:], wb_pq.v(wb_pq.base[l].rearrange("(j p) m -> p j m", p=128)))
        k.dma(kb[:], wb_keys.v(wb_keys.base[l].rearrange("h p c -> p h c")))
        k.dma(iota16[:], RO(c_iota4.base[:, 0, 0, :]))
        xp = Pool(k, "px", [128, D], F32, 2)
        hTp = Pool(k, "phT", [128, 8, 128], BF16, 2)
        qTp = Pool(k, "pqT", [128, 128], BF16, 3)
        wkp = Pool(k, "pwk", [128, 256], F32, 2)
        up = Pool(k, "pu", [128, 2 * D], BF16, 16)
        accp = Pool(k, "pacc", [128, D], F32, 1)
        jkp = Pool(k, "pjk", [128, D], BF16, 2)
        dgp = Pool(k, "pdg", [128, 128], BF16, 4)
        b3 = lambda t_, ap: t_.v(ap)
        B2 = lambda j, c: modF[:, 24 + j, c:c + 1]
        if not breg_box:
            breg_box.append(nc.gpsimd.to_reg(16383))
        breg = breg_box[0]

        class St:
            pass
        sets = []
        for i in range(2):
            s = St()
            s.h2 = k.sb("ph2", [128, D], BF16)
            s.s_sb = k.sb("ps_sb", [128, 8, 256], F32)
            s.V1 = k.sb("pV1", [128, 8, 2, 16], F32)
            s.I1 = k.sb("pI1", [128, 8, 2, 16], U32)
            s.I1f = k.sb("pI1f", [128, 8, 2, 16], F32)
            s.cand = k.sb("pcand", [128, 8, 256], F32)
            s.CS = k.sb("pCS", [128, 8, 16], F32)
            s.POS = k.sb("pPOS", [128, 8, 16], U32)
            s.AB = k.sb("pAB", [128, 2, 8, 16], U32)
            s.ABf = k.sb("pABf", [128, 2, 8, 16], F32)
            s.isel = k.sb("pisel", [128, 2, 8, 16], F32)
            s.EI = k.sb("pEI", [128, 128], I32)
            s.gsm = k.sb("pgsm", [128, 8, 16], F32)
            s.zs = k.sb("pzs", [128, 8], F32)
            s.dots = k.sb("pdots", [128, 128], F32)
            s.wg = k.sb("pwg", [128, 128], F32)
            sets.append(s)

        def top16(vals, idx, src):
            wk = wkp.nxt()
            n_ = src.ap.shape[-1]
            v0, v1 = vals
            i0_, i1_ = idx
            k.op("dve", lambda e: e.max(out=v0.ap, in_=src.ap), r=[src], w=[v0])
            k.op("dve", lambda e: e.max_index(out=i0_.ap, in_max=v0.ap, in_values=src.ap), r=[v0, src], w=[i0_])
            k.op("dve", lambda e: e.match_replace(out=wk[:, 0:n_].ap, in_to_replace=v0.ap, in_values=src.ap, imm_value=-1e30), r=[v0, src], w=[wk[:, 0:n_]])
            k.op("dve", lambda e: e.max(out=v1.ap, in_=wk[:, 0:n_].ap), r=[wk[:, 0:n_]], w=[v1])
            k.op("dve", lambda e: e.max_index(out=i1_.ap, in_max=v1.ap, in_values=wk[:, 0:n_].ap), r=[v1, wk[:, 0:n_]], w=[i1_])

        def front(s, b, ci):
            lat = ci >= 2
            s.col = b if lat else NB
            s.rv = (xres.k((b, (ci - 2) * 128)), (b, slice((ci - 2) * 128, (ci - 1) * 128), slice(None))) if lat else (cres.k((b, ci)), (b, slice(ci * 128, (ci + 1) * 128), slice(None)))
            s.xt = xp.nxt()
            k.dma(s.xt[:], s.rv[0][s.rv[1]])
            hT = hTp.nxt()
            norm_T(s.xt, hT, 0, A2, B2, s.col)
            yield
            pt = P()
            for j in range(8):
                k.tr(bfview(pt, j * 128, (j + 1) * 128), hT[:, j, :], ident_bf[:])
            k.copy(s.h2[:], bfview(pt, 0, 1024), e="act")
            yield
            for h in range(8):
                p = P()
                for j in range(8):
                    k.mm(p[:, 0:128], wq[:, j, h * 128:(h + 1) * 128], hT[:, j, :], start=(j == 0), stop=(j == 7))
                qT = qTp.nxt()
                k.copy(qT[:], p[:, 0:128], e="act")
                p2 = P()
                k.mm(p2[:, 0:256], qT[:], kb[:, h, :])
                k.copy(s.s_sb[:, h, :], p2[:, 0:256], e="act")
                yield
            for h in range(8):
                for p_ in range(2):
                    top16((s.V1[:, h, p_, 0:8], s.V1[:, h, p_, 8:16]), (s.I1[:, h, p_, 0:8], s.I1[:, h, p_, 8:16]), s.s_sb[:, h, p_ * 128:(p_ + 1) * 128])
                yield
            k.tt(b3(s.cand, s.cand.base[:, :, :].rearrange("p h (a c) -> p h a c", a=16)),
                 b3(s.V1, s.V1.base[:, :, 0, :].unsqueeze(3).to_broadcast([128, 8, 16, 16])),
                 b3(s.V1, s.V1.base[:, :, 1, :].unsqueeze(2).to_broadcast([128, 8, 16, 16])), ALU.add)
            yield
            for h in range(8):
                top16((s.CS[:, h, 0:8], s.CS[:, h, 8:16]), (s.POS[:, h, 0:8], s.POS[:, h, 8:16]), s.cand[:, h, :])
                yield
            k.op("dve", lambda e: e.tensor_single_scalar(s.AB[:, 0, :, :].ap, s.POS[:].ap, 4, ALU.logical_shift_right), r=[s.POS[:]], w=[s.AB[:]])
            k.op("dve", lambda e: e.tensor_single_scalar(s.AB[:, 1, :, :].ap, s.POS[:].ap, 15, ALU.bitwise_and), r=[s.POS[:]], w=[s.AB[:]])
            k.copy(s.ABf[:], s.AB[:])
            k.copy(s.I1f[:], s.I1[:])
            yield
            E4 = b3(s.cand, s.cand.base[:, :, :].rearrange("p h (a c) -> p h a c", a=16))
            io = b3(iota16, iota16.base[:, :].unsqueeze(1).unsqueeze(1).to_broadcast([128, 8, 16, 16]))
            for s_ in range(2):
                k.tt(E4, b3(s.ABf, s.ABf.base[:, s_, :, :].unsqueeze(3).to_broadcast([128, 8, 16, 16])), io, ALU.is_equal)
                k.tt(E4, E4, b3(s.I1f, s.I1f.base[:, :, s_, :].unsqueeze(2).to_broadcast([128, 8, 16, 16])), ALU.mult)
                k.reduce(s.isel[:, s_, :, :], E4, ALU.add)
                yield
            k.stt(s.isel[:, 0, :, :], s.isel[:, 0, :, :], 128.0, s.isel[:, 1, :, :], ALU.mult, ALU.add)
            k.copy(b3(s.EI, s.EI.base[:, :].rearrange("p (h c) -> p h c", h=8)), s.isel[:, 0, :, :])
            k.tt(s.gsm[:], s.CS[:], b3(s.CS, s.CS.base[:, :, 0:1].to_broadcast([128, 8, 16])), ALU.subtract)
            k.act(s.gsm[:], s.gsm[:], AF.Exp)
            k.reduce(s.zs[:], s.gsm[:], ALU.add)
            k.recip(s.zs[:], s.zs[:])
            k.tt(s.gsm[:], s.gsm[:], b3(s.zs, s.zs.base[:, :].unsqueeze(2).to_broadcast([128, 8, 16])), ALU.mult)
            yield

        def back(s, nxt_gen):
            acc = accp.nxt()
            pacc = (psB.nxt(), psB.nxt())
            gflat = s.gsm.base[:, :, :].rearrange("p h c -> p (h c)")
            for g4 in range(32):
                bufs = []
                for c in range(4):
                    hk = g4 * 4 + c
                    ur = up.nxt()
                    bufs.append(ur)
                    k.dma(ur[:], uvb[l][:, :], q="pool", extra_r=[s.EI[:, hk:hk + 1]],
                          fn=lambda e, ur=ur, hk=hk: e.indirect_dma_start(out=ur[:].ap, out_offset=None, in_=uvb[l].base[:, :],
                                                                         in_offset=bass.IndirectOffsetOnAxis(ap=s.EI[:, hk:hk + 1].ap, axis=0),
                                                                         bounds_check=breg, oob_is_err=False))
                for c in range(4):
                    hk = g4 * 4 + c
                    jk = jkp.nxt()
                    k.stt(jk[:], bufs[c][:, 0:D], 1.0, s.h2[:], ALU.mult, ALU.mult, accum=s.dots[:, hk:hk + 1])
                sl = slice(g4 * 4, g4 * 4 + 4)
                k.act(s.wg[:, sl], s.dots[:, sl], AF.Gelu)
                k.tt(s.wg[:, sl], s.wg[:, sl], b3(s.gsm, gflat[:, sl]), ALU.mult)
                for c in range(4):
                    hk = g4 * 4 + c
                    dg = dgp.nxt()
                    k.act(dg[:], ident_bf[:], AF.Copy, scale=s.wg[:, hk:hk + 1])
                    k.mm(pacc[0][:, :], dg[:], bufs[c][:, D:D + 512], start=(hk == 0), stop=(hk == 127))
                    k.mm(pacc[1][:, :], dg[:], bufs[c][:, D + 512:2 * D], start=(hk == 0), stop=(hk == 127))
                if nxt_gen is not None:
                    next(nxt_gen, None)
            if nxt_gen is not None:
                for _ in nxt_gen:
                    pass
            for hf in range(2):
                k.tt(acc[:, hf * 512:(hf + 1) * 512], pacc[hf][:, :], grep[:, 1, s.col, hf * 512:(hf + 1) * 512], ALU.mult)
            k.tt(s.xt[:], s.xt[:], acc[:], ALU.add)
            k.dma(s.rv[0][s.rv[1]], s.xt[:], q="sp")

        tiles = [(b, ci) for b in range(NB) for ci in range(NKC) if (ci >= 2 or need_ctx)]
        g0 = front(sets[0], *tiles[0])
        for _ in g0:
            pass
        for ti in range(len(tiles)):
            nxt = front(sets[(ti + 1) % 2], *tiles[ti + 1]) if ti + 1 < len(tiles) else None
            back(sets[ti % 2], nxt)
        k.pop()

    breg_box = []

    def phase_final():
        k.push()
        fr = k.sb("fnr", [128, D], F32)
        k.dma(fr[:], RO(fnw.base.partition_broadcast(128)))
        xp = Pool(k, "fx", [128, D], F32, 3)
        yp = Pool(k, "fy", [128, D], F32, 3)
        for b in range(NB):
            for i in range(S // 128):
                xt = xp.nxt()
                k.dma(xt[:], xres[b, i * 128:(i + 1) * 128, :])
                ss = ssp.nxt()
                y = yp.nxt()
                k.act(y[:], xt[:], AF.Square, accum=ss[:, 0:1])
                k.act(ss[:, 1:2], ss[:, 0:1], AF.Sqrt, bias=EPS, scale=1.0 / D)
                k.recip(ss[:, 1:2], ss[:, 1:2])
                k.ts(y[:], xt[:], ss[:, 1:2], ALU.mult)
                k.tt(y[:], y[:], fr[:], ALU.mult, e="pool")
                k.dma(out_d.k((b, i))[b, i * 128:(i + 1) * 128, :], y[:], q="pool")
        k.pop()

    Rq32 = k.sb("Rq32", [32, 32], BF16)
    k.dma(Rq32[:], RO(c_Rq.base[0:32, 0:32]))

    outs = []
    for l in layers:
        phase_mod(l)
        load_layer_small(l)
        if stop == "mod":
            break
        phase_proj(l)
        if stop == "proj":
            break
        need_ctx = (l < NL - 1)
        phase_attn(l, need_ctx)
        if stop == "attn":
            break
        phase_na(l, need_ctx)
        if stop == "na":
            break
        phase_ret(l, need_ctx)
        if stop == "ret":
            break
        phase_merge(l, need_ctx)
        if stop == "merge":
            break
        phase_peer(l, need_ctx)
        if stop == "peer":
            break
    if stop is None or stop == "final":
        phase_final()

    k.finish()
    return k


_CONST_CACHE = {}


def host_shared(inp):
    f = lambda a: np.ascontiguousarray(np.asarray(a, dtype=np.float32))
    sh = {}
    if "c" not in _CONST_CACHE:
        c = {}
        c.update(rope_tables())
        c.update(misc_consts())
        _CONST_CACHE["c"] = c
    sh.update(_CONST_CACHE["c"])
    sh["mod_w"] = f(inp["mod_w"])
    sh["mod_b"] = f(inp["mod_b"])
    sh["mod_bT"] = f(np.asarray(inp["mod_b"]).reshape(NL, 48, 128).transpose(0, 2, 1))
    sh["n1wT"] = f(np.asarray(inp["norm1_w"]).reshape(NL, 8, 128).transpose(0, 2, 1))
    sh["n2wT"] = f(np.asarray(inp["norm2_w"]).reshape(NL, 8, 128).transpose(0, 2, 1))
    sh["w_in"] = f(inp["w_in"])
    sh["qnT"] = f(np.asarray(inp["mla_q_norm"]).reshape(NL, 2, 128).transpose(0, 2, 1))
    sh["kvnT"] = f(np.asarray(inp["mla_kv_norm"]).reshape(NL, 128, 1))
    sh["gqnT"] = f(np.tile(np.asarray(inp["gqa_q_norm"]), (1, 2)).reshape(NL, 128, 1))
    sh["gknT"] = f(np.tile(np.asarray(inp["gqa_k_norm"]), (1, 2)).reshape(NL, 128, 1))
    sh["mla_w_uq"] = f(inp["mla_w_uq"])
    sh["mla_w_ukv"] = f(inp["mla_w_ukv"])
    sh["nabm"] = na_biasmask(np.asarray(inp["na_bias"], dtype=np.float32))
    sh["ret_decay_logit"] = f(np.asarray(inp["ret_decay_logit"]).reshape(NL, 8))
    sh["ret_gn_w"] = f(inp["ret_gn_w"])
    sh["w_branch"] = f(inp["w_branch"])
    sh["w_out"] = f(inp["w_out"])
    sh["peer_w_q"] = f(inp["peer_w_q"])
    pk = np.asarray(inp["peer_keys"], dtype=np.float32)
    kb = np.zeros((NL, 8, 128, 256), np.float32)
    kb[:, :, 0:64, 0:128] = pk[:, :, 0].transpose(0, 1, 3, 2)
    kb[:, :, 64:128, 128:256] = pk[:, :, 1].transpose(0, 1, 3, 2)
    sh["keysbd"] = kb
    for i in range(NL):
        sh["peer_u%d" % i] = f(np.asarray(inp["peer_u"])[i])
        sh["peer_v%d" % i] = f(np.asarray(inp["peer_v"])[i])
    sh["final_norm_w"] = f(inp["final_norm_w"])
    return sh


def host_core(inp, b0, NB):
    x = np.asarray(inp["x"], dtype=np.float32)
    d = {}
    d["x"] = np.ascontiguousarray(x[b0:b0 + NB])
    d["ctx"] = np.ascontiguousarray(np.asarray(inp["ctx"], dtype=np.float32)[b0:b0 + NB])
    cc = np.concatenate([np.asarray(inp["c"], dtype=np.float32)[b0:b0 + NB], np.asarray(inp["c_ctx"], dtype=np.float32)[None, :]], 0)
    d["cT3"] = np.ascontiguousarray(cc.reshape(NB + 1, 8, 128).transpose(2, 1, 0))
    return d


_PROG = {}


def kernel(**inputs):
    NB = 2
    if "k" not in _PROG:
        _PROG["k"] = build(NB=NB)
    k = _PROG["k"]
    sh = host_shared(inputs)
    in_maps = []
    for c in range(NCORES):
        m = dict(sh)
        m.update(host_core(inputs, c * NB, NB))
        in_maps.append(m)
    res = run_bass_kernel_spmd(k.nc, in_maps, core_ids=list(range(NCORES)))
    out = np.concatenate([np.asarray(r["out"]) for r in res.results], axis=0)
    return np.ascontiguousarray(out.astype(np.float32))
```

```python
import ml_dtypes
from concourse.bass_utils import run_bass_kernel_spmd
import contextlib
import numpy as np
import concourse.bass as bass
import concourse.mybir as mybir

F32 = mybir.dt.float32
BF16 = mybir.dt.bfloat16
I32 = mybir.dt.int32
U32 = mybir.dt.uint32
ALU = mybir.AluOpType
AF = mybir.ActivationFunctionType
AX = mybir.AxisListType


class Dep:
    __slots__ = ("w", "r")

    def __init__(self):
        self.w = None
        self.r = []


class View:
    __slots__ = ("t", "key", "ap")

    def __init__(self, t, key, ap):
        self.t = t
        self.key = key
        self.ap = ap


class _Sub:
    def __init__(self, t, key):
        self.t = t
        self.key = key

    def __getitem__(self, idx):
        return View(self.t, self.key, self.t.base[idx])


class T:
    def __init__(self, name, base, space):
        self.name = name
        self.base = base
        self.space = space
        self.whole = Dep()
        self.subs = {}

    def __getitem__(self, idx):
        return View(self, None, self.base[idx])

    def k(self, key):
        return _Sub(self, key)

    def v(self, ap, key=None):
        return View(self, key, ap)

    def deps(self, key):
        if key is None:
            return [self.whole] + list(self.subs.values())
        if key not in self.subs:
            self.subs[key] = Dep()
        return [self.whole, self.subs[key]]

    def own(self, key):
        if key is None:
            return self.whole
        if key not in self.subs:
            self.subs[key] = Dep()
        return self.subs[key]


class K:
    ENG = ("pe", "act", "dve", "pool", "sp")

    def __init__(self, n_dma_sems=24):
        self.nc = bass.Bass("TRN2", target_bir_lowering=False)
        self.es = contextlib.ExitStack()
        self.stack = [self.es]
        nc = self.nc
        self.eng = {"pe": nc.tensor, "act": nc.scalar, "dve": nc.vector, "pool": nc.gpsimd, "sp": nc.sync}
        self.sems = []
        self.semval = []
        self.esem = {}
        for e in ("pe", "act", "dve", "pool"):
            self.esem[e] = self._newsem("s_" + e)
        self.dsem = {}
        self.dnext = {}
        for q in ("sp", "pool", "act"):
            n = n_dma_sems if q != "act" else 8
            self.dsem[q] = [self._newsem(f"d_{q}{i}") for i in range(n)]
            self.dnext[q] = 0
        self.known = {e: {} for e in self.ENG}
        self.n_inst = 0
        self.embed = True
        self.n_wait = 0
        self._uid = 0

    def _newsem(self, name):
        h = self.es.enter_context(self.nc.semaphore(name))
        self.sems.append(h)
        self.semval.append(0)
        return len(self.sems) - 1

    def uid(self, p):
        self._uid += 1
        return f"{p}{self._uid}"

    def sb(self, name, shape, dtype):
        h = self.stack[-1].enter_context(self.nc.sbuf_tensor(self.uid("s_" + name + "_"), list(shape), dtype))
        return T(name, h, "sb")

    def ps(self, name, shape, dtype=F32):
        h = self.stack[-1].enter_context(self.nc.psum_tensor(self.uid("p_" + name + "_"), list(shape), dtype))
        return T(name, h, "ps")

    def push(self):
        self.stack.append(contextlib.ExitStack())

    def pop(self):
        self.barrier()
        self.stack.pop().close()

    def barrier(self, engines=("pe", "act", "dve", "pool", "sp")):
        for e in engines:
            for s in range(len(self.sems)):
                if self.semval[s] > 0:
                    self._wait(e, (s, self.semval[s]))

    def dram(self, name, shape, dtype, kind="Internal"):
        h = self.nc.dram_tensor(name, list(shape), dtype, kind=kind)
        return T(name, h.ap(), "dram")

    def _wait(self, e, ev):
        if ev is None:
            return
        s, v = ev
        if self.known[e].get(s, 0) >= v:
            return
        self.eng[e].wait_ge(self.sems[s], v)
        self.known[e][s] = v
        self.n_wait += 1

    def _sync_in(self, e, r, w):
        need = {}

        def add(ev):
            if ev is None:
                return
            s, v = ev
            if e == "pe" and s == self.esem["pe"]:
                return
            if need.get(s, 0) < v:
                need[s] = v

        for v in r:
            for d in v.t.deps(v.key):
                add(d.w)
        for v in w:
            for d in v.t.deps(v.key):
                add(d.w)
                for rv in d.r:
                    add(rv)
        pend = [(s, v) for s, v in need.items() if self.known[e].get(s, 0) < v]
        if self.embed and pend:
            for ev in pend[:-1]:
                self._wait(e, ev)
            return pend[-1]
        for ev in pend:
            self._wait(e, ev)
        return None

    def _sync_out(self, ev, r, w):
        for v in r:
            v.t.own(v.key).r.append(ev)
            if len(v.t.own(v.key).r) > 64:
                mx = {}
                for s, val in v.t.own(v.key).r:
                    mx[s] = max(mx.get(s, 0), val)
                v.t.own(v.key).r = list(mx.items())
        for v in w:
            if v.key is None:
                v.t.subs = {}
            d = v.t.own(v.key)
            d.w = ev
            d.r = []

    def op(self, e, fn, r=(), w=()):
        r = [x for x in r if isinstance(x, View)]
        w = [x for x in w if isinstance(x, View)]
        emb = self._sync_in(e, r, w)
        ins = fn(self.eng[e])
        if emb is not None:
            ins._wait_ge(self.sems[emb[0]], emb[1])
            self.known[e][emb[0]] = emb[1]
        s = self.esem[e]
        self.semval[s] += 1
        ins.then_inc(self.sems[s], 1)
        ev = (s, self.semval[s])
        self._sync_out(ev, r, w)
        self.n_inst += 1
        return ins

    def dma(self, out, in_, q="sp", fn=None, extra_r=()):
        r = [in_] + list(extra_r)
        w = [out]
        pool = self.dsem[q]
        i = self.dnext[q]
        self.dnext[q] = (i + 1) % len(pool)
        s = pool[i]
        self._wait(q, (s, self.semval[s]))
        emb = self._sync_in(q, r, w)
        if fn is None:
            ins = self.eng[q].dma_start(out=out.ap, in_=in_.ap)
        else:
            ins = fn(self.eng[q])
        if emb is not None:
            ins._wait_ge(self.sems[emb[0]], emb[1])
            self.known[q][emb[0]] = emb[1]
        self.semval[s] += 16
        ins.then_inc(self.sems[s], 16)
        ev = (s, self.semval[s])
        self._sync_out(ev, r, w)
        self.n_inst += 1
        return ins

    def wait_all(self, e, views):
        for v in views:
            for d in v.t.deps(v.key):
                self._wait(e, d.w)

    def mm(self, out, lhsT, rhs, start=True, stop=True, **kw):
        return self.op("pe", lambda e: e.matmul(out.ap, lhsT.ap, rhs.ap, start=start, stop=stop, **kw),
                       r=[lhsT, rhs] + ([] if start else [out]), w=[out])

    def tr(self, out, in_, ident):
        return self.op("pe", lambda e: e.transpose(out.ap, in_.ap, ident.ap), r=[in_, ident], w=[out])

    def act(self, out, in_, func, bias=None, scale=None, accum=None, e="act"):
        kw = {}
        r = [in_]
        w = [out]
        if bias is not None:
            kw["bias"] = bias.ap if isinstance(bias, View) else bias
            r.append(bias)
        if scale is not None:
            kw["scale"] = scale.ap if isinstance(scale, View) else scale
            r.append(scale)
        if accum is not None:
            kw["accum_out"] = accum.ap
            w.append(accum)
        return self.op(e, lambda en: en.activation(out=out.ap, in_=in_.ap, func=func, **kw), r=r, w=w)

    def tt(self, out, a, b, op, e="dve"):
        return self.op(e, lambda en: en.tensor_tensor(out.ap, a.ap, b.ap, op), r=[a, b], w=[out])

    def ts(self, out, a, s1, op0, s2=None, op1=None, e="dve", accum=None):
        r = [a, s1, s2]
        w = [out]
        kw = {}
        if accum is not None:
            kw["accum_out"] = accum.ap
            w.append(accum)
        g = lambda s: s.ap if isinstance(s, View) else s
        if op1 is None:
            return self.op(e, lambda en: en.tensor_scalar(out.ap, a.ap, g(s1), None, op0, **kw), r=r, w=w)
        return self.op(e, lambda en: en.tensor_scalar(out.ap, a.ap, g(s1), g(s2), op0, op1, **kw), r=r, w=w)

    def stt(self, out, a, s, b, op0, op1, e="dve", accum=None):
        g = lambda x: x.ap if isinstance(x, View) else x
        w = [out]
        kw = {}
        if accum is not None:
            kw["accum_out"] = accum.ap
            w.append(accum)
        return self.op(e, lambda en: en.scalar_tensor_tensor(out.ap, a.ap, g(s), b.ap, op0, op1, **kw),
                       r=[a, s, b], w=w)

    def copy(self, out, in_, e="dve"):
        if e == "act":
            return self.op(e, lambda en: en.copy(out.ap, in_.ap), r=[in_], w=[out])
        return self.op(e, lambda en: en.tensor_copy(out.ap, in_.ap), r=[in_], w=[out])

    def recip(self, out, in_):
        return self.op("dve", lambda en: en.reciprocal(out.ap, in_.ap), r=[in_], w=[out])

    def memset(self, out, val, e="dve"):
        return self.op(e, lambda en: en.memset(out.ap, val), r=[], w=[out])

    def reduce(self, out, in_, op, axis=AX.X, e="dve"):
        return self.op(e, lambda en: en.tensor_reduce(out.ap, in_.ap, axis, op), r=[in_], w=[out])

    def finish(self):
        self.barrier(engines=("sp",))

D = 1024
S = 4096
LC = 256
TT = S + LC
NL = 2
GW = 64
EPS = 1e-6
INC = 7072
NCORES = 8
BF = ml_dtypes.bfloat16
import os
PEXP = os.environ.get('PEXP', '')


class Pool:
    def __init__(self, k, name, shape, dtype, n, space="sb"):
        mk = k.sb if space == "sb" else k.ps
        self.t = [mk(f"{name}{i}", shape, dtype) for i in range(n)]
        self.i = 0

    def nxt(self):
        t = self.t[self.i]
        self.i = (self.i + 1) % len(self.t)
        return t


def rope_tables():
    t = np.arange(S)
    row = (t // GW).astype(np.float32)
    col = (t % GW).astype(np.float32)

    def tab(hd):
        half = hd // 2
        h = half // 2
        freqs = (np.float32(10000.0) ** (-np.arange(h, dtype=np.float32) / np.float32(h))).astype(np.float32)
        C = np.zeros((hd, S), np.float32)
        Sn = np.zeros((hd, S), np.float32)
        for ax, pos in enumerate((row, col)):
            ang = (pos[:, None] * freqs[None, :]).astype(np.float32)
            c = np.cos(ang).astype(np.float32).T
            s = np.sin(ang).astype(np.float32).T
            o = ax * half
            C[o:o + h] = c
            C[o + h:o + 2 * h] = c
            Sn[o:o + h] = s
            Sn[o + h:o + 2 * h] = s
        return C, Sn

    def rmat(hd):
        half = hd // 2
        h = half // 2
        Rm = np.zeros((hd, hd), np.float32)
        for ax in range(2):
            o = ax * half
            for i in range(h):
                Rm[o + i, o + h + i] = -1.0
                Rm[o + h + i, o + i] = 1.0
        return Rm.T.copy()

    Cg, Sg = tab(64)
    Cm, Sm = tab(32)
    out = {}
    out["ropeCg"] = np.concatenate([Cg, Cg], 0)
    out["ropeSg"] = np.concatenate([Sg, Sg], 0)
    z = np.zeros((32, S), np.float32)
    out["ropeCm"] = np.concatenate([Cm, z, Cm], 0)
    out["ropeSm"] = np.concatenate([Sm, z, Sm], 0)
    Rg = np.zeros((128, 128), np.float32)
    Rg[0:64, 0:64] = rmat(64)
    Rg[64:128, 64:128] = rmat(64)
    Rq = np.zeros((96, 96), np.float32)
    Rq[64:96, 64:96] = rmat(32)
    Rq[0:32, 0:32] = rmat(32)
    out["Rg"] = Rg.astype(BF)
    out["Rq"] = Rq.astype(BF)
    return out


def misc_consts():
    out = {}
    out["ident_bf"] = np.eye(128, dtype=np.float32).astype(BF)
    out["ident_f"] = np.eye(128, dtype=np.float32)
    out["ones_f"] = np.ones((128, 128), np.float32)
    ob = np.zeros((128, 128), np.float32)
    ob[0:64, 0:64] = 1.0
    ob[64:, 64:] = 1.0
    out["onesblk_f"] = ob
    io = np.zeros((128, 8, 16, 16), np.float32)
    io[:] = np.arange(16, dtype=np.float32)[None, None, None, :]
    out["iota4"] = io
    m = np.arange(128)[:, None]
    n = np.arange(128)[None, :]
    Df = np.where(n >= m, (n - m), 100000).astype(np.float32)
    Db = np.where(n <= m, (m - n), 100000).astype(np.float32)
    out["retD"] = np.stack([Df, Db], 0)
    qa = np.zeros((2, 128, 128), np.float32)
    qa[0] = (np.arange(128) + 1)[None, :]
    qa[1] = (128 - np.arange(128))[None, :]
    out["retQA"] = qa
    ka = np.zeros((128, 2), np.float32)
    ka[:, 0] = 127 - np.arange(128)
    ka[:, 1] = np.arange(128)
    out["retKA"] = ka
    return out


def na_biasmask(na_bias):
    L = na_bias.shape[0]
    out = np.full((L, 4, 3, 8, 128, 512), -30000.0, np.float32)
    for pat, qg in enumerate((0, 3, 7)):
        r0 = 8 * qg
        kr0 = int(np.clip(r0 - 4, 0, 48))
        r = r0 + np.arange(8)
        rs = np.clip(r - 4, 0, 56)
        c = np.arange(64)
        cs = np.clip(c - 8, 0, 48)
        for kc in range(8):
            kr = kr0 + 2 * kc + np.arange(2)
            KR = kr[:, None, None, None]
            CP = c[None, :, None, None]
            R = r[None, None, :, None]
            RS = rs[None, None, :, None]
            C = c[None, None, None, :]
            CS = cs[None, None, None, :]
            valid = (KR >= RS) & (KR < RS + 8) & (CP >= CS) & (CP < CS + 16)
            rel_r = np.clip(KR - R + 7, 0, 14)
            rel_c = np.clip(CP - C + 15, 0, 30)
            rel_r, rel_c, valid = np.broadcast_arrays(rel_r, rel_c, valid)
            for l in range(L):
                for h in range(4):
                    vals = na_bias[l, h][rel_r, rel_c]
                    out[l, h, pat, kc] = np.where(valid, vals, np.float32(-30000.0)).reshape(128, 512)
    return out


def build(NB=2, layers=(0, 1), dbg=(), stop=None):
    k = K()
    nc = k.nc
    dbg = set(dbg)
    IN = lambda name, shape, dt=F32: k.dram(name, shape, dt, kind="ExternalInput")

    def SCR(name, shape, dt=BF16):
        return k.dram(name, shape, dt, kind=("ExternalOutput" if name in dbg else "Internal"))

    x_in = IN("x", [NB, S, D])
    ctx_in = IN("ctx", [NB, LC, D])
    cT3 = IN("cT3", [128, 8, NB + 1])
    mod_w = IN("mod_w", [NL, D, 6 * D])
    mod_bT = IN("mod_bT", [NL, 128, 48])
    mod_b = IN("mod_b", [NL, 6 * D])
    n1wT = IN("n1wT", [NL, 128, 8])
    n2wT = IN("n2wT", [NL, 128, 8])
    w_in = IN("w_in", [NL, D, INC])
    qnT = IN("qnT", [NL, 128, 2])
    kvnT = IN("kvnT", [NL, 128, 1])
    gqnT = IN("gqnT", [NL, 128, 1])
    gknT = IN("gknT", [NL, 128, 1])
    wuq = IN("mla_w_uq", [NL, 256, 384])
    wukv = IN("mla_w_ukv", [NL, 128, 512])
    nabm = IN("nabm", [NL, 4, 3, 8, 128, 512])
    rdl = IN("ret_decay_logit", [NL, 8])
    gnw = IN("ret_gn_w", [NL, 256])
    w_branch = IN("w_branch", [NL, 4, 256, D])
    w_out = IN("w_out", [NL, D, D])
    peer_wq = IN("peer_w_q", [NL, D, D])
    keysbd = IN("keysbd", [NL, 8, 128, 256])
    peer_u = [IN("peer_u%d" % i, [16384, D]) for i in range(NL)]
    peer_v = [IN("peer_v%d" % i, [16384, D]) for i in range(NL)]
    fnw = IN("final_norm_w", [D])
    c_ident_bf = IN("ident_bf", [128, 128], BF16)
    c_ident_f = IN("ident_f", [128, 128])
    c_ones_f = IN("ones_f", [128, 128])
    c_onesblk = IN("onesblk_f", [128, 128])
    c_iota4 = IN("iota4", [128, 8, 16, 16])
    c_retD = IN("retD", [2, 128, 128])
    c_retQA = IN("retQA", [2, 128, 128])
    c_retKA = IN("retKA", [128, 2])
    c_Cg = IN("ropeCg", [128, S])
    c_Sg = IN("ropeSg", [128, S])
    c_Cm = IN("ropeCm", [96, S])
    c_Sm = IN("ropeSm", [96, S])
    c_Rg = IN("Rg", [128, 128], BF16)
    c_Rq = IN("Rq", [96, 96], BF16)
    out_d = k.dram("out", [NB, S, D], F32, kind="ExternalOutput")

    xres = SCR("xres", [NB, S, D], F32)
    cres = SCR("cres", [NB, LC, D], F32)
    wb_in = SCR("wb_in", [NL, D, INC])
    wb_uq = SCR("wb_uq", [NL, 256, 384])
    wb_ukv = SCR("wb_ukv", [NL, 128, 512])
    wb_br = SCR("wb_br", [NL, 4, 256, D])
    wb_out = SCR("wb_out", [NL, D, D])
    wb_pq = SCR("wb_pq", [NL, D, D])
    wb_keys = SCR("wb_keys", [NL, 8, 128, 256])
    uvb = [SCR("uvb%d" % i, [16384, 2 * D]) for i in range(NL)]
    nabb = SCR("nabb", [NL, 4, 3, 8, 128, 512])
    mqT = SCR("mqT", [384, NB, TT])
    mknT = SCR("mknT", [256, NB, TT])
    mkpT = SCR("mkpT", [32, NB, TT])
    mv = SCR("mv", [NB, TT, 256])
    gqT = SCR("gqT", [256, NB, TT])
    gkT = SCR("gkT", [128, NB, TT])
    gv = SCR("gv", [NB, TT, 128])
    nqT = SCR("nqT", [256, NB, TT])
    nkT = SCR("nkT", [256, NB, TT])
    nv = SCR("nv", [NB, TT, 256])
    rqT = SCR("rqT", [256, NB, TT])
    rkT = SCR("rkT", [256, NB, TT])
    rkv = SCR("rkv", [NB, TT, 512])
    rg = SCR("rg", [NB, TT, 512])
    gT = SCR("gT", [4096, NB, TT])
    oT = SCR("oT", [1024, NB, TT])

    ident_bf = k.sb("ident_bf", [128, 128], BF16)
    ident_f = k.sb("ident_f", [128, 128], F32)
    ones_f = k.sb("ones_f", [128, 128], F32)
    onesblk = k.sb("onesblk", [128, 128], F32)
    Rg = k.sb("Rg", [128, 128], BF16)
    Rq = k.sb("Rq", [96, 96], BF16)
    for sbt, dr in ((ident_bf, c_ident_bf), (ident_f, c_ident_f), (ones_f, c_ones_f), (onesblk, c_onesblk), (Rg, c_Rg)):
        k.dma(sbt[:], dr[:, :])
    k.dma(Rq[:], c_Rq[:, :])

    psA = Pool(k, "psA", [128, 512], F32, 6, space="ps")
    psB = Pool(k, "psB", [128, 512], F32, 2, space="ps")
    P = psA.nxt

    def bfview(pt, a, b, p0=0, p1=128):
        return pt.v(pt.base.bitcast(BF16)[p0:p1, a:b])

    evn = [0]

    def evac(out, in_, scale=None, func=None):
        evn[0] += 1
        if func is not None:
            return k.act(out, in_, func, scale=scale)
        if evn[0] % 2 == 0:
            return k.act(out, in_, AF.Copy, scale=scale)
        if scale is None:
            return k.copy(out, in_)
        return k.ts(out, in_, scale, ALU.mult)

    CV = {}
    cvn = [0]

    def conv_pools(nbuf, engs):
        CV["f"] = Pool(k, "cvf", [128, 2048], F32, nbuf)
        CV["b"] = Pool(k, "cvb", [128, 2048], BF16, nbuf)
        CV["e"] = engs

    def conv_gen(l):
        def flat(ap, pat, **kw):
            return ap.rearrange(pat, **kw)
        yield from convert(flat(w_in.base[l], "(p a) c -> p (a c)", p=128), wb_in, flat(wb_in.base[l], "(p a) c -> p (a c)", p=128), 8 * INC)
        yield from convert(flat(wuq.base[l], "(p a) c -> p (a c)", p=128), wb_uq, flat(wb_uq.base[l], "(p a) c -> p (a c)", p=128), 2 * 384)
        yield from convert(wukv.base[l], wb_ukv, wb_ukv.base[l], 512)
        for i in range(4):
            yield from convert(flat(w_branch.base[l, i], "(p a) c -> p (a c)", p=128), wb_br, flat(wb_br.base[l, i], "(p a) c -> p (a c)", p=128), 2 * D)
        yield from convert(flat(w_out.base[l], "(p a) c -> p (a c)", p=128), wb_out, flat(wb_out.base[l], "(p a) c -> p (a c)", p=128), 8 * D)
        yield from convert(flat(peer_wq.base[l], "(p a) c -> p (a c)", p=128), wb_pq, flat(wb_pq.base[l], "(p a) c -> p (a c)", p=128), 8 * D)
        for h in range(8):
            yield from convert(keysbd.base[l, h], wb_keys, wb_keys.base[l, h], 256)
        for h in range(4):
            for t_ in range(3):
                for c in range(8):
                    yield from convert(nabm.base[l, h, t_, c], nabb, nabb.base[l, h, t_, c], 512)
        for a in range(128):
            yield from convert(peer_u[l].base.rearrange("(p a) c -> p a c", p=128)[:, a, :], uvb[l], uvb[l].base.rearrange("(p a) c -> p a c", p=128)[:, a, 0:D], D)
            yield from convert(peer_v[l].base.rearrange("(p a) c -> p a c", p=128)[:, a, :], uvb[l], uvb[l].base.rearrange("(p a) c -> p a c", p=128)[:, a, D:2 * D], D)

    ro = T("ro", None, "dram")

    def RO(ap):
        return View(ro, "r", ap)

    def convert(src_ap, dst_t, dst_ap, nper):
        for c0 in range(0, nper, 2048):
            n = min(2048, nper - c0)
            f = CV["f"].nxt()
            b = CV["b"].nxt()
            k.dma(f[:, 0:n], RO(src_ap[:, c0:c0 + n]))
            e = CV["e"][cvn[0] % len(CV["e"])]
            cvn[0] += 1
            k.copy(b[:, 0:n], f[:, 0:n], e=e)
            k.dma(dst_t.v(dst_ap[:, c0:c0 + n], key=("cv", cvn[0])), b[:, 0:n], q="pool")
            yield

    k.push()
    conv_pools(8, ("dve", "act"))
    for _ in conv_gen(layers[0]):
        pass
    k.pop()

    modF = k.sb("modF", [128, 48, NB + 1], F32)
    A1 = k.sb("A1", [128, 8, NB + 1], F32)
    A2 = k.sb("A2", [128, 8, NB + 1], F32)
    grep = k.sb("grep", [128, 2, NB + 1, D], F32)
    cT = k.sb("cT", [128, 8, NB + 1], F32)
    k.dma(cT[:], cT3[:, :, :])
    k.act(cT[:], cT[:], AF.Silu)
    mbT = k.sb("mbT", [128, 48], F32)
    nwT = k.sb("nwT", [128, 2, 8], F32)

    def phase_mod(l):
        k.push()
        mwp = Pool(k, "mw", [128, 8, 512], F32, 2)
        mbrep = k.sb("mbrep", [128, 512], F32)
        cTb = k.sb("cTb", [128, NB + 1, 8, 128], F32)
        for col in range(NB + 1):
            for j in range(8):
                k.copy(cTb[:, col, j, :], cT.v(cT.base[:, j, col:col + 1].to_broadcast([128, 128])))
        k.dma(mbT[:], mod_bT[l])
        k.dma(nwT[:, 0, :], n1wT[l])
        k.dma(nwT[:, 1, :], n2wT[l])
        mwv = mod_w.base[l].rearrange("(j p) c -> p j c", p=128)
        fm_blocks = {0: 0, 1: 4, 2: 8, 3: 12, 6: 24, 7: 28, 8: 32, 9: 36}
        g_blocks = {4: (0, 0), 5: (0, 512), 10: (1, 0), 11: (1, 512)}
        for cb in range(12):
            mw = mwp.nxt()
            k.dma(mw[:], RO(mwv[:, :, cb * 512:(cb + 1) * 512]))
            if cb in fm_blocks:
                for o4 in range(4):
                    oc = cb * 4 + o4
                    p = P()
                    for j in range(8):
                        k.mm(p[:, 0:NB + 1], mw[:, j, o4 * 128:(o4 + 1) * 128], cT[:, j, :], start=(j == 0), stop=(j == 7))
                    k.ts(modF[:, oc, :], p[:, 0:NB + 1], mbT[:, oc:oc + 1], ALU.add)
            else:
                gi, off = g_blocks[cb]
                k.dma(mbrep[:], RO(mod_b.base[l, cb * 512:(cb + 1) * 512].partition_broadcast(128)))
                for col in range(NB + 1):
                    p = P()
                    for j in range(8):
                        k.mm(p[:, :], cTb[:, col, j, :], mw[:, j, :], start=(j == 0), stop=(j == 7))
                    k.tt(grep[:, gi, col, off:off + 512], p[:, :], mbrep[:], ALU.add)
        for j in range(8):
            k.ts(A1[:, j, :], modF[:, 8 + j, :], 1.0, ALU.add)
            k.ts(A1[:, j, :], A1[:, j, :], nwT[:, 0, j:j + 1], ALU.mult)
            k.ts(A2[:, j, :], modF[:, 32 + j, :], 1.0, ALU.add)
            k.ts(A2[:, j, :], A2[:, j, :], nwT[:, 1, j:j + 1], ALU.mult)
        k.pop()

    NP = {}

    def norm_pools():
        NP["junk"] = Pool(k, "junk", [128, D], BF16, 2)
        NP["xn"] = Pool(k, "xn", [128, D], BF16, 2)
    ssp = Pool(k, "ss", [128, 2], F32, 4)

    def norm_T(xt, hxT, i, Am, Bm, col):
        ss = ssp.nxt()
        jk = NP["junk"].nxt()
        k.act(jk[:], xt[:], AF.Square, accum=ss[:, 0:1])
        k.act(ss[:, 1:2], ss[:, 0:1], AF.Sqrt, bias=EPS, scale=1.0 / D)
        k.recip(ss[:, 1:2], ss[:, 1:2])
        xn = NP["xn"].nxt()
        k.ts(xn[:], xt[:], ss[:, 1:2], ALU.mult)
        pt = P()
        for j in range(8):
            k.tr(bfview(pt, j * 128, (j + 1) * 128), xn[:, j * 128:(j + 1) * 128], ident_bf[:])
        for j in range(8):
            o = hxT[:, j, i * 128:(i + 1) * 128]
            src = bfview(pt, j * 128, (j + 1) * 128)
            if j % 2 == 0:
                k.act(o, src, AF.Identity, bias=Bm(j, col), scale=Am[:, j, col:col + 1])
            else:
                k.ts(o, src, Am[:, j, col:col + 1], ALU.mult, Bm(j, col), ALU.add)

    wuq_sb = k.sb("wuq_sb", [128, 2, 384], BF16)
    wkn_sb = k.sb("wkn_sb", [128, 4, 64], BF16)
    wkv_sb = k.sb("wkv_sb", [128, 4, 64], BF16)
    nrmw = k.sb("nrmw", [128, 5], F32)

    def fm_rstd(rsp, sqs, ones_t, nfeat, n):
        p = P()
        for c, sq in enumerate(sqs):
            k.mm(p[:, 0:n], ones_t[:], sq, start=(c == 0), stop=(c == len(sqs) - 1))
        rs = rsp.nxt()
        k.act(rs[:, 0:n], p[:, 0:n], AF.Sqrt, bias=EPS, scale=1.0 / nfeat)
        k.recip(rs[:, 0:n], rs[:, 0:n])
        return rs

    def load_layer_small(l):
        k.dma(wuq_sb[:], wb_uq.v(wb_uq.base[l].rearrange("(c p) m -> p c m", p=128)))
        wv = wb_ukv.base[l].rearrange("p (h t e) -> p h t e", h=4, t=2)
        k.dma(wkn_sb[:], wb_ukv.v(wv[:, :, 0, :]))
        k.dma(wkv_sb[:], wb_ukv.v(wv[:, :, 1, :]))
        k.dma(nrmw[:, 0:2], qnT[l])
        k.dma(nrmw[:, 2:3], kvnT[l])
        k.dma(nrmw[:, 3:4], gqnT[l])
        k.dma(nrmw[:, 4:5], gknT[l])

    def groups_for(l, with_ctx=True):
        gs = []
        for b in range(NB):
            gs.append(dict(b=b, t0=0, n=LC, lat=False, col=NB, s0=0))
            for g in range(S // 512):
                gs.append(dict(b=b, t0=LC + g * 512, n=512, lat=True, col=b, s0=g * 512))
        return gs

    def src_rows(l, g, i):
        b = g["b"]
        if g["lat"]:
            r0 = g["s0"] + i * 128
            return (RO(x_in.base[b, r0:r0 + 128, :]) if l == layers[0] and l == 0 else xres[b, r0:r0 + 128, :])
        r0 = i * 128
        return (RO(ctx_in.base[b, r0:r0 + 128, :]) if l == 0 else cres[b, r0:r0 + 128, :])

    def phase_proj(l):
        k.push()
        norm_pools()
        xpool = Pool(k, "xt", [128, D], F32, 3)
        hxTp = Pool(k, "hxT", [128, 8, 512], BF16, 2)
        wpool = Pool(k, "wblk", [128, 8, 512], BF16, 3)
        sqp = Pool(k, "sq", [128, 2, 512], F32, 2)
        rsp = Pool(k, "rs", [128, 512], F32, 2)
        nrm = Pool(k, "nrm", [128, 2, 512], BF16, 2)
        stg = Pool(k, "stg", [128, 512], BF16, 6)
        tmpf = Pool(k, "tmpf", [128, 512], F32, 4)
        ropeT = Pool(k, "ropeT", [128, 4, 512], F32, 2)
        wv = wb_in.base[l].rearrange("(j p) c -> p j c", p=128)
        blocks = [(0, 416), (416, 512), (928, 512), (1440, 512), (1952, 512), (2464, 512)] + [(2976 + 512 * i, 512) for i in range(8)]
        def norm_gen(g, box):
            n_, s0_ = g["n"], g["s0"]
            hx = hxTp.nxt()
            box["hxT"] = hx
            if g["lat"]:
                rt_ = ropeT.nxt()
                k.dma(rt_[:, 0, 0:n_], RO(c_Cg.base[:, s0_:s0_ + n_]))
                k.dma(rt_[:, 1, 0:n_], RO(c_Sg.base[:, s0_:s0_ + n_]))
                k.dma(rt_[0:96, 2, 0:n_], RO(c_Cm.base[:, s0_:s0_ + n_]))
                k.dma(rt_[0:96, 3, 0:n_], RO(c_Sm.base[:, s0_:s0_ + n_]))
                box["rt"] = rt_
            for i in range(n_ // 128):
                xt = xpool.nxt()
                k.dma(xt[:], src_rows(l, g, i))
                norm_T(xt, hx, i, A1, lambda j, c: modF[:, j, c:c + 1], g["col"])
                yield

        gs_all = groups_for(l)
        nbox = {}
        for _ in norm_gen(gs_all[0], nbox):
            pass
        for gi, g in enumerate(gs_all):
            b, t0, n, lat, col, s0 = g["b"], g["t0"], g["n"], g["lat"], g["col"], g["s0"]
            nt = n // 128
            hxT = nbox["hxT"]
            rt = nbox.get("rt")
            nbox = {}
            ngen = norm_gen(gs_all[gi + 1], nbox) if gi + 1 < len(gs_all) else iter(())

            def adv():
                next(ngen, None)

            def loadw(bi):
                c0, ncol = blocks[bi]
                wt = wpool.nxt()
                k.dma(wt[:, :, 0:ncol], wb_in.v(wv[:, :, c0:c0 + ncol]))
                return wt

            def fm(wt, c0, ncol):
                p = P()
                for j in range(8):
                    k.mm(p[0:ncol, 0:n], wt[:, j, c0:c0 + ncol], hxT[:, j, 0:n], start=(j == 0), stop=(j == 7))
                return p

            def tm(wt, c0, ncol, i):
                p = P()
                for j in range(8):
                    k.mm(p[:, 0:ncol], hxT[:, j, i * 128:(i + 1) * 128], wt[:, j, c0:c0 + ncol], start=(j == 0), stop=(j == 7))
                return p

            def store_fm(dst, r0, nr, src):
                k.dma(dst.k((b, t0, r0))[r0:r0 + nr, b, t0:t0 + n], src, q="pool")

            def store_tm(dst, i, src):
                k.dma(dst.k((b, t0, i))[b, t0 + i * 128:t0 + (i + 1) * 128, :], src, q="pool")

            def rope(xs, p0, p1, Rt, ci, si):
                R = Rt.base.shape[0]
                p2 = P()
                k.mm(p2[0:R, 0:n], Rt[:, :], xs[0:R, 0:n])
                t1 = tmpf.nxt()
                t2 = tmpf.nxt()
                k.tt(t1[p0:p1, 0:n], p2[p0:p1, 0:n], rt[p0:p1, si, 0:n], ALU.mult)
                k.tt(t2[p0:p1, 0:n], xs[p0:p1, 0:n], rt[p0:p1, ci, 0:n], ALU.mult)
                k.tt(xs[p0:p1, 0:n], t1[p0:p1, 0:n], t2[p0:p1, 0:n], ALU.add)

            w0 = loadw(0)
            w1 = loadw(1)
            pc = [fm(w0, 0, 128), fm(w0, 128, 128)]
            sq = sqp.nxt()
            for c in range(2):
                k.act(sq[:, c, 0:n], pc[c][:, 0:n], AF.Square)
            rs = fm_rstd(rsp, [sq[:, 0, 0:n], sq[:, 1, 0:n]], ones_f, 256, n)
            cqn = nrm.nxt()
            for c in range(2):
                k.stt(cqn[:, c, 0:n], pc[c][:, 0:n], nrmw[:, c:c + 1], rs[:, 0:n], ALU.mult, ALU.mult)
            for h in range(4):
                p = P()
                for c in range(2):
                    k.mm(p[0:96, 0:n], wuq_sb[:, c, h * 96:(h + 1) * 96], cqn[:, c, 0:n], start=(c == 0), stop=(c == 1))
                qa = stg.nxt()
                evac(qa[0:96, 0:n], p[0:96, 0:n])
                if lat:
                    rope(qa, 64, 96, Rq, 2, 3)
                store_fm(mqT, h * 96, 96, qa[0:96, 0:n])
            pk = fm(w0, 256, 128)
            sq = sqp.nxt()
            k.act(sq[:, 0, 0:n], pk[:, 0:n], AF.Square)
            rs = fm_rstd(rsp, [sq[:, 0, 0:n]], ones_f, 128, n)
            ckn = nrm.nxt()
            k.stt(ckn[:, 0, 0:n], pk[:, 0:n], nrmw[:, 2:3], rs[:, 0:n], ALU.mult, ALU.mult)
            for h in range(4):
                p = P()
                k.mm(p[0:64, 0:n], wkn_sb[:, h, :], ckn[:, 0, 0:n])
                kn = stg.nxt()
                evac(kn[0:64, 0:n], p[0:64, 0:n])
                store_fm(mknT, h * 64, 64, kn[0:64, 0:n])
            for i in range(nt):
                p = P()
                k.mm(p[:, 0:256], ckn[:, 0, i * 128:(i + 1) * 128], wkv_sb.v(wkv_sb.base[:, :, :].rearrange("p h e -> p (h e)")))
                vt = stg.nxt()
                evac(vt[:, 0:256], p[:, 0:256])
                store_tm(mv, i, vt[:, 0:256])
            pp = fm(w0, 384, 32)
            kp = stg.nxt()
            evac(kp[0:32, 0:n], pp[0:32, 0:n])
            if lat:
                rope(kp, 0, 32, Rq.v(Rq.base[0:32, 0:32]) if False else Rq32, 2, 3)
            store_fm(mkpT, 0, 32, kp[0:32, 0:n])
            w2 = loadw(2)
            for (c0, dst, r0, nw) in ((0, gqT, 0, 3), (128, gqT, 128, 3), (256, gkT, 0, 4)):
                p = fm(w1, c0, 128)
                sq = sqp.nxt()
                k.act(sq[:, 0, 0:n], p[:, 0:n], AF.Square)
                rs = fm_rstd(rsp, [sq[:, 0, 0:n]], onesblk, 64, n)
                xo = stg.nxt()
                k.stt(xo[:, 0:n], p[:, 0:n], nrmw[:, nw:nw + 1], rs[:, 0:n], ALU.mult, ALU.mult)
                if lat:
                    rope(xo, 0, 128, Rg, 0, 1)
                store_fm(dst, r0, 128, xo[:, 0:n])
            for i in range(nt):
                p = tm(w1, 384, 128, i)
                vt = stg.nxt()
                evac(vt[:, 0:128], p[:, 0:128])
                store_tm(gv, i, vt[:, 0:128])
            adv()
            w3 = loadw(3)
            for c in range(4):
                p = fm(w2, c * 128, 128)
                xo = stg.nxt()
                evac(xo[:, 0:n], p[:, 0:n], scale=(0.125 if c < 2 else None))
                store_fm(nqT if c < 2 else nkT, (c % 2) * 128, 128, xo[:, 0:n])
            adv()
            w4 = loadw(4)
            for i in range(nt):
                p = tm(w3, 0, 256, i)
                vt = stg.nxt()
                evac(vt[:, 0:256], p[:, 0:256])
                store_tm(nv, i, vt[:, 0:256])
            for c in range(2):
                p = fm(w3, 256 + c * 128, 128)
                xo = stg.nxt()
                evac(xo[:, 0:n], p[:, 0:n])
                store_fm(rqT, c * 128, 128, xo[:, 0:n])
            adv()
            w5 = loadw(5)
            for c in range(2):
                p = fm(w4, c * 128, 128)
                xo = stg.nxt()
                evac(xo[:, 0:n], p[:, 0:n], scale=0.125)
                store_fm(rkT, c * 128, 128, xo[:, 0:n])
            for i in range(nt):
                p = tm(w4, 0, 512, i)
                vt = stg.nxt()
                evac(vt[:, 0:256], p[:, 0:256], scale=0.125)
                evac(vt[:, 256:512], p[:, 256:512])
                store_tm(rkv, i, vt[:, 0:512])
            adv()
            wn = loadw(6)
            for i in range(nt):
                p = tm(w5, 0, 512, i)
                vt = stg.nxt()
                evac(vt[:, 0:512], p[:, 0:512])
                store_tm(rg, i, vt[:, 0:512])
            for gb in range(8):
                wc = wn
                if gb < 7:
                    wn = loadw(7 + gb)
                for c in range(4):
                    p = fm(wc, c * 128, 128)
                    xo = stg.nxt()
                    k.act(xo[:, 0:n], p[:, 0:n], AF.Sigmoid)
                    store_fm(gT, gb * 512 + c * 128, 128, xo[:, 0:n])
            for _ in ngen:
                pass
        k.pop()

    pend_epi = []

    def attn_flush():
        while pend_epi:
            pend_epi.pop(0)()

    def attn_core(pools, KT, dk, V, kchunks, q_src, nq, scale, o_dst, bias_fn=None):
        qpool, ptp, osp, obp = pools
        qt = q_src
        po = psB.nxt()
        n_ = len(kchunks)
        pss = {}

        def qk(ci):
            kc = kchunks[ci]
            ps = P()
            bm = bias_fn(kc) if bias_fn is not None else None
            k.mm(ps[:, 0:nq], KT[0:dk, kc * 128:(kc + 1) * 128], qt[0:dk, 0:nq], start=True, stop=(bm is None))
            if bm is not None:
                k.mm(ps[:, 0:nq], ident_bf[:], bm, start=False, stop=True)
            pss[ci] = ps

        qk(0)
        if n_ > 1:
            qk(1)
        attn_flush()
        for ci in range(n_):
            pt = ptp.nxt()
            k.act(pt[:, 0:nq], pss.pop(ci)[:, 0:nq], AF.Exp, scale=scale)
            if ci + 2 < n_:
                qk(ci + 2)
            k.mm(po[0:65, 0:nq], V[:, kchunks[ci], 0:65], pt[:, 0:nq], start=(ci == 0), stop=(ci == n_ - 1))

        def epi():
            os_ = osp.nxt()
            k.copy(os_[0:65, 0:nq], po[0:65, 0:nq])
            k.recip(os_[64:65, 0:nq], os_[64:65, 0:nq])
            pb = P()
            k.mm(pb[0:64, 0:nq], ones_f[64:65, 0:64], os_[64:65, 0:nq])
            ob = obp.nxt()
            k.tt(ob[0:64, 0:nq], os_[0:64, 0:nq], pb[0:64, 0:nq], ALU.mult)
            k.dma(o_dst, ob[0:64, 0:nq], q="pool")
        pend_epi.append(epi)

    def attn_pools():
        qpool = Pool(k, "aq", [96, 512], BF16, 2)
        qpoolB = Pool(k, "aqB", [128, 512], BF16, 2)
        for t_ in qpoolB.t:
            k.memset(t_[:], 0.0)
        qpool.padded = qpoolB
        ptp = Pool(k, "apt", [128, 512], BF16, 4)
        osp = Pool(k, "aos", [65, 512], F32, 2)
        obp = Pool(k, "aob", [64, 512], BF16, 2)
        return (qpool, ptp, osp, obp)

    NKC = TT // 128

    def load_V(Vt, src_t, b, c0):
        k.dma(Vt[:, :, 0:64], src_t.v(src_t.base[b, :, c0:c0 + 64].rearrange("(c p) e -> p c e", p=128)))

    def run_attn_jobs(pools, jobs, KTp, Vp, bmp=None, filler=None, nfill=0):
        qpool = pools[0]
        kvbuf = {}

        def ensure_kv(key, loader):
            if key not in kvbuf:
                KT, Vt = (KTp[1] if key[0] == "g" else KTp[0]).nxt() if isinstance(KTp, tuple) else KTp.nxt(), Vp.nxt()
                loader(KT, Vt)
                kvbuf[key] = (KT, Vt)

        def prefetch(j):
            qt = (qpool.padded if j.get("pad") else qpool).nxt()
            k.dma(qt[0:j["dk"], 0:j["nq"]], j["qsrc"])
            j["qt"] = qt
            if j.get("bmsrc") is not None:
                bm = bmp.nxt()
                k.dma(bm[:], j["bmsrc"])
                j["bm"] = bm

        ensure_kv(jobs[0]["kv"], jobs[0]["kvload"])
        prefetch(jobs[0])
        for i, j in enumerate(jobs):
            if i + 1 < len(jobs):
                prefetch(jobs[i + 1])
            if i == 0 or jobs[i - 1]["kv"] != j["kv"]:
                for j2 in jobs[i + 1:]:
                    if j2["kv"] != j["kv"]:
                        ensure_kv(j2["kv"], j2["kvload"])
                        break
                for key in [kk for kk in kvbuf if kk != j["kv"] and all(kk != j2["kv"] for j2 in jobs[i:])]:
                    del kvbuf[key]
            KT, Vt = kvbuf[j["kv"]]
            bfn = (j["bias"](j["bm"]) if j.get("bias") is not None else None)
            attn_core(pools, KT, (128 if j.get("pad") else j["dk"]), Vt, j["chunks"], j["qt"], j["nq"], j["scale"], j["odst"], bias_fn=bfn)
            if filler is not None:
                for _ in range(nfill):
                    next(filler, None)
        attn_flush()
        if filler is not None:
            for _ in filler:
                pass

    def phase_attn(l, need_ctx):
        k.push()
        pools = attn_pools()
        KTp = Pool(k, "aKT", [96, TT], BF16, 2)
        KTg = Pool(k, "aKTg", [128, TT], BF16, 2)
        for t_ in KTg.t:
            k.memset(t_[64:128, :], 0.0)
        Vp = Pool(k, "aV", [128, NKC, 65], BF16, 2)
        for Vt in Vp.t:
            k.memset(Vt[:, :, 64:65], 1.0)
        allk = list(range(NKC))
        jobs = []
        for b in range(NB):
            for h in range(4):
                def kvl(KT, Vt, b=b, h=h):
                    k.dma(KT[0:64, :], mknT[h * 64:(h + 1) * 64, b, :])
                    k.dma(KT[64:96, :], mkpT[:, b, :])
                    load_V(Vt, mv, b, h * 64)
                for qg in range(8):
                    t0 = LC + qg * 512
                    jobs.append(dict(kv=("m", b, h), kvload=kvl, dk=96, chunks=allk, qsrc=mqT[h * 96:(h + 1) * 96, b, t0:t0 + 512], nq=512,
                                     scale=96 ** -0.5, odst=oT.k((b, t0, h))[h * 64:(h + 1) * 64, b, t0:t0 + 512]))
                if need_ctx:
                    jobs.append(dict(kv=("m", b, h), kvload=kvl, dk=96, chunks=[0, 1], qsrc=mqT[h * 96:(h + 1) * 96, b, 0:LC], nq=LC,
                                     scale=96 ** -0.5, odst=oT.k((b, 0, h))[h * 64:(h + 1) * 64, b, 0:LC]))
            for kv in range(2):
                def kvl(KT, Vt, b=b, kv=kv):
                    k.dma(KT[0:64, :], gkT[kv * 64:(kv + 1) * 64, b, :])
                    load_V(Vt, gv, b, kv * 64)
                for gq in range(2):
                    h = kv * 2 + gq
                    for qg in range(8):
                        t0 = LC + qg * 512
                        jobs.append(dict(kv=("g", b, kv), kvload=kvl, pad=True, dk=64, chunks=allk, qsrc=gqT[h * 64:(h + 1) * 64, b, t0:t0 + 512], nq=512,
                                         scale=0.125, odst=oT.k((b, t0, 4 + h))[256 + h * 64:256 + (h + 1) * 64, b, t0:t0 + 512]))
                    if need_ctx:
                        jobs.append(dict(kv=("g", b, kv), kvload=kvl, pad=True, dk=64, chunks=[0, 1], qsrc=gqT[h * 64:(h + 1) * 64, b, 0:LC], nq=LC,
                                         scale=0.125, odst=oT.k((b, 0, 4 + h))[256 + h * 64:256 + (h + 1) * 64, b, 0:LC]))
        filler = None
        nxt_l = [x for x in layers if x > l]
        if nxt_l:
            conv_pools(6, ("dve",))
            filler = conv_gen(nxt_l[0])
        run_attn_jobs(pools, jobs, (KTp, KTg), Vp, filler=filler, nfill=5)
        k.pop()

    def phase_na(l, need_ctx):
        k.push()
        pools = attn_pools()
        KTp = Pool(k, "nKT", [128, TT], BF16, 2)
        for t_ in KTp.t:
            k.memset(t_[64:128, :], 0.0)
        Vp = Pool(k, "nV", [128, NKC, 65], BF16, 2)
        bmp = Pool(k, "nbm", [128, 8, 512], BF16, 3)
        for Vt in Vp.t:
            k.memset(Vt[:, :, 64:65], 1.0)
        jobs = []
        for b in range(NB):
            for h in range(4):
                def kvl(KT, Vt, b=b, h=h):
                    k.dma(KT[0:64, :], nkT[h * 64:(h + 1) * 64, b, :])
                    load_V(Vt, nv, b, h * 64)
                for qg in range(8):
                    t0 = LC + qg * 512
                    pat = 0 if qg == 0 else (2 if qg == 7 else 1)
                    kr0 = min(max(8 * qg - 4, 0), 48)
                    kc0 = 2 + kr0 // 2
                    jobs.append(dict(kv=("n", b, h), kvload=kvl, pad=True, dk=64, chunks=[0, 1] + [kc0 + j for j in range(8)],
                                     qsrc=nqT[h * 64:(h + 1) * 64, b, t0:t0 + 512], nq=512, scale=1.0,
                                     odst=oT.k((b, t0, 8 + h))[512 + h * 64:512 + (h + 1) * 64, b, t0:t0 + 512],
                                     bmsrc=nabb.v(nabb.base[l, h, pat].rearrange("c p q -> p c q")),
                                     bias=lambda bm, kc0=kc0: (lambda kc: (bm[:, kc - kc0, :] if kc >= 2 else None))))
                if need_ctx:
                    jobs.append(dict(kv=("n", b, h), kvload=kvl, pad=True, dk=64, chunks=[0, 1], qsrc=nqT[h * 64:(h + 1) * 64, b, 0:LC], nq=LC, scale=1.0,
                                     odst=oT.k((b, 0, 8 + h))[512 + h * 64:512 + (h + 1) * 64, b, 0:LC]))
        run_attn_jobs(pools, jobs, KTp, Vp, bmp)
        k.pop()

    def phase_ret(l, need_ctx):
        k.push()
        lg = k.sb("lg", [128, 8], F32)
        k.dma(lg[:], RO(rdl.base[l].partition_broadcast(128)))
        k.act(lg[:], lg[:], AF.Exp, scale=-1.0)
        k.ts(lg[:], lg[:], 1.0, ALU.add)
        k.act(lg[:], lg[:], AF.Ln)
        k.ts(lg[:], lg[:], -1.0, ALU.mult)
        Dd = k.sb("Dd", [128, 2, 128], F32)
        QA = k.sb("QA", [128, 2, 128], F32)
        KA = k.sb("KA", [128, 2], F32)
        k.dma(Dd[:], RO(c_retD.base.rearrange("d p n -> p d n")))
        k.dma(QA[:], RO(c_retQA.base.rearrange("d p n -> p d n")))
        k.dma(KA[:], RO(c_retKA.base))
        decT = k.sb("decT", [128, 2, 4, 128], F32)
        qdec = k.sb("qdec", [128, 2, 4, 128], F32)
        kdec = k.sb("kdec", [128, 2, 4], F32)
        cdec = k.sb("cdec", [128, 8], F32)
        gnr = k.sb("gnr", [128, 256], F32)
        k.dma(gnr[:], RO(gnw.base[l].partition_broadcast(128)))
        for d in range(2):
            for h in range(4):
                k.act(decT[:, d, h, :], Dd[:, d, :], AF.Exp, scale=lg[:, d * 4 + h:d * 4 + h + 1])
                k.act(qdec[:, d, h, :], QA[:, d, :], AF.Exp, scale=lg[:, d * 4 + h:d * 4 + h + 1])
            k.act(kdec[:, d, :], lg[:, d * 4:(d + 1) * 4], AF.Exp, scale=KA[:, d:d + 1])
        k.act(cdec[:], lg[:], AF.Exp, scale=128.0)
        QT = k.sb("rQT", [128, 2, TT], BF16)
        KT = k.sb("rKT", [128, 2, TT], BF16)
        KVt = k.sb("rKV", [128, NKC, 512], BF16)
        G = k.sb("rG", [128, NKC, 512], BF16)
        Ys = [k.sb("rY", [128, NKC, 256], F32), k.sb("rYb", [128, NKC, 256], BF16)]
        Sts = [k.sb("rS", [128, 2, 64], F32) for _ in range(2)]
        Sbs = [k.sb("rSb", [128, 2, 64], BF16) for _ in range(2)]
        kdp = Pool(k, "rkd", [128, 256], BF16, 2)
        idp = Pool(k, "rid", [128, 128], BF16, 3)
        qdp = Pool(k, "rqd", [128, 128], BF16, 3)
        osb = Pool(k, "ros", [128, 256], F32, 2)
        sgp = Pool(k, "rsg", [128, 256], F32, 2)
        stp = Pool(k, "rst", [128, 4], F32, 4)
        ybp = Pool(k, "ryb", [128, 256], BF16, 2)
        ysp = Pool(k, "rys", [128, 2, 128], BF16, 2)
        b3 = lambda t_, ap: t_.v(ap)
        for b in range(NB):
            for c in range(2):
                k.dma(QT[:, c, :], rqT[c * 128:(c + 1) * 128, b, :])
                k.dma(KT[:, c, :], rkT[c * 128:(c + 1) * 128, b, :])
            k.dma(KVt[:], rkv.v(rkv.base[b].rearrange("(c p) f -> p c f", p=128)))
            k.dma(G[:], rg.v(rg.base[b].rearrange("(c p) f -> p c f", p=128)))
            for d in range(2):
                k.memset(Sts[d][:], 0.0)
                k.memset(Sbs[d][:], 0.0)
            orders = [list(range(NKC)), [1, 0] + list(range(NKC - 1, 1, -1))]
            for step in range(2 * NKC):
                for _once in (0,):
                    d = step % 2
                    ci = orders[d][step // 2]
                    St, Sb, Y = Sts[d], Sbs[d], Ys[d]
                    tk = slice(ci * 128, (ci + 1) * 128)
                    Kd = kdp.nxt()
                    k.tt(b3(Kd, Kd.base[:, :].rearrange("p (h e) -> p h e", h=4)),
                         b3(KVt, KVt.base[:, ci, 0:256].rearrange("p (h e) -> p h e", h=4)),
                         b3(kdec, kdec.base[:, d, :].unsqueeze(2).to_broadcast([128, 4, 64])), ALU.mult)
                    pso = psB.nxt()
                    for h in range(4):
                        c2, po = h // 2, (h % 2) * 64
                        ps1 = P()
                        k.mm(ps1[:, 0:128], KT[po:po + 64, c2, tk], QT[po:po + 64, c2, tk])
                        idT = idp.nxt()
                        k.tt(idT[:], ps1[:, 0:128], decT[:, d, h, :], ALU.mult)
                        Qd = qdp.nxt()
                        k.tt(Qd[po:po + 64, :], QT[po:po + 64, c2, tk], qdec[po:po + 64, d, h, :], ALU.mult)
                        k.mm(pso[:, h * 64:(h + 1) * 64], idT[:], KVt[:, ci, 256 + h * 64:256 + (h + 1) * 64], start=True, stop=False)
                        k.mm(pso[:, h * 64:(h + 1) * 64], Qd[po:po + 64, :], Sb[po:po + 64, c2, :], start=False, stop=True)
                    for c2 in range(2):
                        ps2 = P()
                        k.mm(ps2[:, 0:128], Kd[:, c2 * 128:(c2 + 1) * 128], KVt[:, ci, 256 + c2 * 128:256 + (c2 + 1) * 128])
                        for hh in range(2):
                            po = hh * 64
                            h = c2 * 2 + hh
                            k.stt(St[po:po + 64, c2, :], St[po:po + 64, c2, :], cdec[po:po + 64, d * 4 + h:d * 4 + h + 1],
                                  ps2[po:po + 64, po:po + 64], ALU.mult, ALU.add)
                    k.copy(Sb[:], St[:])
                    if ci < 2 and not need_ctx:
                        continue
                    o = osb.nxt()
                    o3 = b3(o, o.base[:, :].rearrange("p (h e) -> p h e", h=4))
                    k.copy(o[:], pso[:, 0:256])
                    st = stp.nxt()
                    k.reduce(st[:], o3, ALU.add)
                    k.ts(st[:], st[:], 1.0 / 64, ALU.mult)
                    k.tt(o3, o3, b3(st, st.base[:, :].unsqueeze(2).to_broadcast([128, 4, 64])), ALU.subtract)
                    sq = sgp.nxt()
                    k.tt(sq[:], o[:], o[:], ALU.mult)
                    st2 = stp.nxt()
                    k.reduce(st2[:], b3(sq, sq.base[:, :].rearrange("p (h e) -> p h e", h=4)), ALU.add)
                    k.act(st2[:], st2[:], AF.Sqrt, bias=EPS, scale=1.0 / 64)
                    k.recip(st2[:], st2[:])
                    k.tt(o3, o3, b3(st2, st2.base[:, :].unsqueeze(2).to_broadcast([128, 4, 64])), ALU.mult)
                    k.tt(o[:], o[:], gnr[:], ALU.mult)
                    sg = sgp.nxt()
                    k.act(sg[:], G[:, ci, d * 256:(d + 1) * 256], AF.Silu)
                    k.tt(Y[:, ci, :], o[:], sg[:], ALU.mult)
            for ci in (range(NKC) if need_ctx else range(2, NKC)):
                yb = ybp.nxt()
                k.tt(yb[:], Ys[0][:, ci, :], Ys[1][:, ci, :], ALU.add)
                pt = P()
                for c2 in range(2):
                    k.tr(bfview(pt, c2 * 128, (c2 + 1) * 128), yb[:, c2 * 128:(c2 + 1) * 128], ident_bf[:])
                ys = ysp.nxt()
                k.copy(b3(ys, ys.base[:, :, :].rearrange("p c t -> p (c t)")), bfview(pt, 0, 256))
                for c2 in range(2):
                    k.dma(oT.k((b, ci, 12 + c2))[768 + c2 * 128:768 + (c2 + 1) * 128, b, ci * 128:(ci + 1) * 128], ys[:, c2, :], q="pool")
        k.pop()

    def phase_merge(l, need_ctx):
        k.push()
        wbr = k.sb("wbr", [128, 4, 2, D], BF16)
        wo = k.sb("wo", [128, 8, D], BF16)
        for i in range(4):
            k.dma(wbr[:, i, :, :], wb_br.v(wb_br.base[l, i].rearrange("(kk p) m -> p kk m", p=128)))
        k.dma(wo[:], wb_out.v(wb_out.base[l].rearrange("(j p) m -> p j m", p=128)))
        osp = Pool(k, "mo", [128, 8, 512], BF16, 2)
        mgl = Pool(k, "mgl", [128, 512], BF16, 6)
        accp = Pool(k, "macc", [128, 512], F32, 2)
        tmpp = Pool(k, "mtmp", [128, 512], F32, 2)
        accT = Pool(k, "maT", [128, 8, 512], BF16, 2)
        xp = Pool(k, "mx", [128, D], F32, 3)
        for g in groups_for(l):
            b, t0, n, lat, col = g["b"], g["t0"], g["n"], g["lat"], g["col"]
            if not lat and not need_ctx:
                continue
            nt = n // 128
            o_sb = osp.nxt()
            k.dma(o_sb[:, :, 0:n], oT.v(oT.base[:, b, t0:t0 + n].rearrange("(kk p) t -> p kk t", p=128)))
            aT = accT.nxt()
            for m in range(8):
                acc = accp.nxt()
                for i in range(4):
                    p = P()
                    for kk in range(2):
                        k.mm(p[:, 0:n], wbr[:, i, kk, m * 128:(m + 1) * 128], o_sb[:, 2 * i + kk, 0:n], start=(kk == 0), stop=(kk == 1))
                    gl = mgl.nxt()
                    k.dma(gl[:, 0:n], gT[i * 1024 + m * 128:i * 1024 + (m + 1) * 128, b, t0:t0 + n])
                    if i == 0:
                        k.tt(acc[:, 0:n], p[:, 0:n], gl[:, 0:n], ALU.mult)
                    else:
                        tp_ = tmpp.nxt()
                        k.tt(tp_[:, 0:n], p[:, 0:n], gl[:, 0:n], ALU.mult)
                        k.tt(acc[:, 0:n], acc[:, 0:n], tp_[:, 0:n], ALU.add, e="pool")
                k.copy(aT[:, m, 0:n], acc[:, 0:n], e="act")
            for i in range(nt):
                xt = xp.nxt()
                k.dma(xt[:], src_rows(l, g, i))
                for hf in range(2):
                    p = P()
                    for m in range(8):
                        k.mm(p[:, :], aT[:, m, i * 128:(i + 1) * 128], wo[:, m, hf * 512:(hf + 1) * 512], start=(m == 0), stop=(m == 7))
                    tp_ = tmpp.nxt()
                    k.tt(tp_[:], p[:, :], grep[:, 0, col, hf * 512:(hf + 1) * 512], ALU.mult)
                    k.tt(xt[:, hf * 512:(hf + 1) * 512], xt[:, hf * 512:(hf + 1) * 512], tp_[:], ALU.add, e="pool")
                dst = xres.k((b, g["s0"] + i * 128))[b, g["s0"] + i * 128:g["s0"] + (i + 1) * 128, :] if lat else cres.k((b, i))[b, i * 128:(i + 1) * 128, :]
                k.dma(dst, xt[:], q="pool")
        k.pop()

    def phase_peer(l, need_ctx):
        k.push()
        norm_pools()
        wq = k.sb("pwq", [128, 8, D], BF16)
        kb = k.sb("pkb", [128, 8, 256], BF16)
        iota16 = k.sb("piota", [128, 16], F32)
        k.dma(wq[:], wb_pq.v(wb_pq.base[l].rearrange("(j p) m -> p j m", p=128)))
        k.dma(kb[:], wb_keys.v(wb_keys.base[l].rearrange("h p c -> p h c")))
        k.dma(iota16[:], RO(c_iota4.base[:, 0, 0, :]))
        xp = Pool(k, "px", [128, D], F32, 2)
        hTp = Pool(k, "phT", [128, 8, 128], BF16, 2)
        qTp = Pool(k, "pqT", [128, 128], BF16, 3)
        wkp = Pool(k, "pwk", [128, 256], F32, 2)
        up = Pool(k, "pu", [128, 2 * D], BF16, 16)
        accp = Pool(k, "pacc", [128, D], F32, 1)
        jkp = Pool(k, "pjk", [128, D], BF16, 2)
        dgp = Pool(k, "pdg", [128, 128], BF16, 4)
        b3 = lambda t_, ap: t_.v(ap)
        B2 = lambda j, c: modF[:, 24 + j, c:c + 1]
        if not breg_box:
            breg_box.append(nc.gpsimd.to_reg(16383))
        breg = breg_box[0]

        class St:
            pass
        sets = []
        for i in range(2):
            s = St()
            s.h2 = k.sb("ph2", [128, D], BF16)
            s.s_sb = k.sb("ps_sb", [128, 8, 256], F32)
            s.V1 = k.sb("pV1", [128, 8, 2, 16], F32)
            s.I1 = k.sb("pI1", [128, 8, 2, 16], U32)
            s.I1f = k.sb("pI1f", [128, 8, 2, 16], F32)
            s.cand = k.sb("pcand", [128, 8, 256], F32)
            s.CS = k.sb("pCS", [128, 8, 16], F32)
            s.POS = k.sb("pPOS", [128, 8, 16], U32)
            s.AB = k.sb("pAB", [128, 2, 8, 16], U32)
            s.ABf = k.sb("pABf", [128, 2, 8, 16], F32)
            s.isel = k.sb("pisel", [128, 2, 8, 16], F32)
            s.EI = k.sb("pEI", [128, 128], I32)
            s.gsm = k.sb("pgsm", [128, 8, 16], F32)
            s.zs = k.sb("pzs", [128, 8], F32)
            s.dots = k.sb("pdots", [128, 128], F32)
            s.wg = k.sb("pwg", [128, 128], F32)
            sets.append(s)

        def top16(vals, idx, src):
            wk = wkp.nxt()
            n_ = src.ap.shape[-1]
            v0, v1 = vals
            i0_, i1_ = idx
            k.op("dve", lambda e: e.max(out=v0.ap, in_=src.ap), r=[src], w=[v0])
            k.op("dve", lambda e: e.max_index(out=i0_.ap, in_max=v0.ap, in_values=src.ap), r=[v0, src], w=[i0_])
            k.op("dve", lambda e: e.match_replace(out=wk[:, 0:n_].ap, in_to_replace=v0.ap, in_values=src.ap, imm_value=-1e30), r=[v0, src], w=[wk[:, 0:n_]])
            k.op("dve", lambda e: e.max(out=v1.ap, in_=wk[:, 0:n_].ap), r=[wk[:, 0:n_]], w=[v1])
            k.op("dve", lambda e: e.max_index(out=i1_.ap, in_max=v1.ap, in_values=wk[:, 0:n_].ap), r=[v1, wk[:, 0:n_]], w=[i1_])

        def front(s, b, ci):
            lat = ci >= 2
            s.col = b if lat else NB
            s.rv = (xres.k((b, (ci - 2) * 128)), (b, slice((ci - 2) * 128, (ci - 1) * 128), slice(None))) if lat else (cres.k((b, ci)), (b, slice(ci * 128, (ci + 1) * 128), slice(None)))
            s.xt = xp.nxt()
            k.dma(s.xt[:], s.rv[0][s.rv[1]])
            hT = hTp.nxt()
            norm_T(s.xt, hT, 0, A2, B2, s.col)
            yield
            pt = P()
            for j in range(8):
                k.tr(bfview(pt, j * 128, (j + 1) * 128), hT[:, j, :], ident_bf[:])
            k.copy(s.h2[:], bfview(pt, 0, 1024), e="act")
            yield
            for h in range(8):
                p = P()
                for j in range(8):
                    k.mm(p[:, 0:128], wq[:, j, h * 128:(h + 1) * 128], hT[:, j, :], start=(j == 0), stop=(j == 7))
                qT = qTp.nxt()
                k.copy(qT[:], p[:, 0:128], e="act")
                p2 = P()
                k.mm(p2[:, 0:256], qT[:], kb[:, h, :])
                k.copy(s.s_sb[:, h, :], p2[:, 0:256], e="act")
                yield
            for h in range(8):
                for p_ in range(2):
                    top16((s.V1[:, h, p_, 0:8], s.V1[:, h, p_, 8:16]), (s.I1[:, h, p_, 0:8], s.I1[:, h, p_, 8:16]), s.s_sb[:, h, p_ * 128:(p_ + 1) * 128])
                yield
            k.tt(b3(s.cand, s.cand.base[:, :, :].rearrange("p h (a c) -> p h a c", a=16)),
                 b3(s.V1, s.V1.base[:, :, 0, :].unsqueeze(3).to_broadcast([128, 8, 16, 16])),
                 b3(s.V1, s.V1.base[:, :, 1, :].unsqueeze(2).to_broadcast([128, 8, 16, 16])), ALU.add)
            yield
            for h in range(8):
                top16((s.CS[:, h, 0:8], s.CS[:, h, 8:16]), (s.POS[:, h, 0:8], s.POS[:, h, 8:16]), s.cand[:, h, :])
                yield
            k.op("dve", lambda e: e.tensor_single_scalar(s.AB[:, 0, :, :].ap, s.POS[:].ap, 4, ALU.logical_shift_right), r=[s.POS[:]], w=[s.AB[:]])
            k.op("dve", lambda e: e.tensor_single_scalar(s.AB[:, 1, :, :].ap, s.POS[:].ap, 15, ALU.bitwise_and), r=[s.POS[:]], w=[s.AB[:]])
            k.copy(s.ABf[:], s.AB[:])
            k.copy(s.I1f[:], s.I1[:])
            yield
            E4 = b3(s.cand, s.cand.base[:, :, :].rearrange("p h (a c) -> p h a c", a=16))
            io = b3(iota16, iota16.base[:, :].unsqueeze(1).unsqueeze(1).to_broadcast([128, 8, 16, 16]))
            for s_ in range(2):
                k.tt(E4, b3(s.ABf, s.ABf.base[:, s_, :, :].unsqueeze(3).to_broadcast([128, 8, 16, 16])), io, ALU.is_equal)
                k.tt(E4, E4, b3(s.I1f, s.I1f.base[:, :, s_, :].unsqueeze(2).to_broadcast([128, 8, 16, 16])), ALU.mult)
                k.reduce(s.isel[:, s_, :, :], E4, ALU.add)
                yield
            k.stt(s.isel[:, 0, :, :], s.isel[:, 0, :, :], 128.0, s.isel[:, 1, :, :], ALU.mult, ALU.add)
            k.copy(b3(s.EI, s.EI.base[:, :].rearrange("p (h c) -> p h c", h=8)), s.isel[:, 0, :, :])
            k.tt(s.gsm[:], s.CS[:], b3(s.CS, s.CS.base[:, :, 0:1].to_broadcast([128, 8, 16])), ALU.subtract)
            k.act(s.gsm[:], s.gsm[:], AF.Exp)
            k.reduce(s.zs[:], s.gsm[:], ALU.add)
            k.recip(s.zs[:], s.zs[:])
            k.tt(s.gsm[:], s.gsm[:], b3(s.zs, s.zs.base[:, :].unsqueeze(2).to_broadcast([128, 8, 16])), ALU.mult)
            yield

        def back(s, nxt_gen):
            acc = accp.nxt()
            pacc = (psB.nxt(), psB.nxt())
            gflat = s.gsm.base[:, :, :].rearrange("p h c -> p (h c)")
            for g4 in range(32):
                bufs = []
                for c in range(4):
                    hk = g4 * 4 + c
                    ur = up.nxt()
                    bufs.append(ur)
                    k.dma(ur[:], uvb[l][:, :], q="pool", extra_r=[s.EI[:, hk:hk + 1]],
                          fn=lambda e, ur=ur, hk=hk: e.indirect_dma_start(out=ur[:].ap, out_offset=None, in_=uvb[l].base[:, :],
                                                                         in_offset=bass.IndirectOffsetOnAxis(ap=s.EI[:, hk:hk + 1].ap, axis=0),
                                                                         bounds_check=breg, oob_is_err=False))
                for c in range(4):
                    hk = g4 * 4 + c
                    jk = jkp.nxt()
                    k.stt(jk[:], bufs[c][:, 0:D], 1.0, s.h2[:], ALU.mult, ALU.mult, accum=s.dots[:, hk:hk + 1])
                sl = slice(g4 * 4, g4 * 4 + 4)
                k.act(s.wg[:, sl], s.dots[:, sl], AF.Gelu)
                k.tt(s.wg[:, sl], s.wg[:, sl], b3(s.gsm, gflat[:, sl]), ALU.mult)
                for c in range(4):
                    hk = g4 * 4 + c
                    dg = dgp.nxt()
                    k.act(dg[:], ident_bf[:], AF.Copy, scale=s.wg[:, hk:hk + 1])
                    k.mm(pacc[0][:, :], dg[:], bufs[c][:, D:D + 512], start=(hk == 0), stop=(hk == 127))
                    k.mm(pacc[1][:, :], dg[:], bufs[c][:, D + 512:2 * D], start=(hk == 0), stop=(hk == 127))
                if nxt_gen is not None:
                    next(nxt_gen, None)
            if nxt_gen is not None:
                for _ in nxt_gen:
                    pass
            for hf in range(2):
                k.tt(acc[:, hf * 512:(hf + 1) * 512], pacc[hf][:, :], grep[:, 1, s.col, hf * 512:(hf + 1) * 512], ALU.mult)
            k.tt(s.xt[:], s.xt[:], acc[:], ALU.add)
            k.dma(s.rv[0][s.rv[1]], s.xt[:], q="sp")

        tiles = [(b, ci) for b in range(NB) for ci in range(NKC) if (ci >= 2 or need_ctx)]
        g0 = front(sets[0], *tiles[0])
        for _ in g0:
            pass
        for ti in range(len(tiles)):
            nxt = front(sets[(ti + 1) % 2], *tiles[ti + 1]) if ti + 1 < len(tiles) else None
            back(sets[ti % 2], nxt)
        k.pop()

    breg_box = []

    def phase_final():
        k.push()
        fr = k.sb("fnr", [128, D], F32)
        k.dma(fr[:], RO(fnw.base.partition_broadcast(128)))
        xp = Pool(k, "fx", [128, D], F32, 3)
        yp = Pool(k, "fy", [128, D], F32, 3)
        for b in range(NB):
            for i in range(S // 128):
                xt = xp.nxt()
                k.dma(xt[:], xres[b, i * 128:(i + 1) * 128, :])
                ss = ssp.nxt()
                y = yp.nxt()
                k.act(y[:], xt[:], AF.Square, accum=ss[:, 0:1])
                k.act(ss[:, 1:2], ss[:, 0:1], AF.Sqrt, bias=EPS, scale=1.0 / D)
                k.recip(ss[:, 1:2], ss[:, 1:2])
                k.ts(y[:], xt[:], ss[:, 1:2], ALU.mult)
                k.tt(y[:], y[:], fr[:], ALU.mult, e="pool")
                k.dma(out_d.k((b, i))[b, i * 128:(i + 1) * 128, :], y[:], q="pool")
        k.pop()

    Rq32 = k.sb("Rq32", [32, 32], BF16)
    k.dma(Rq32[:], RO(c_Rq.base[0:32, 0:32]))

    outs = []
    for l in layers:
        phase_mod(l)
        load_layer_small(l)
        if stop == "mod":
            break
        phase_proj(l)
        if stop == "proj":
            break
        need_ctx = (l < NL - 1)
        phase_attn(l, need_ctx)
        if stop == "attn":
            break
        phase_na(l, need_ctx)
        if stop == "na":
            break
        phase_ret(l, need_ctx)
        if stop == "ret":
            break
        phase_merge(l, need_ctx)
        if stop == "merge":
            break
        phase_peer(l, need_ctx)
        if stop == "peer":
            break
    if stop is None or stop == "final":
        phase_final()

    k.finish()
    return k


_CONST_CACHE = {}


def host_shared(inp):
    f = lambda a: np.ascontiguousarray(np.asarray(a, dtype=np.float32))
    sh = {}
    if "c" not in _CONST_CACHE:
        c = {}
        c.update(rope_tables())
        c.update(misc_consts())
        _CONST_CACHE["c"] = c
    sh.update(_CONST_CACHE["c"])
    sh["mod_w"] = f(inp["mod_w"])
    sh["mod_b"] = f(inp["mod_b"])
    sh["mod_bT"] = f(np.asarray(inp["mod_b"]).reshape(NL, 48, 128).transpose(0, 2, 1))
    sh["n1wT"] = f(np.asarray(inp["norm1_w"]).reshape(NL, 8, 128).transpose(0, 2, 1))
    sh["n2wT"] = f(np.asarray(inp["norm2_w"]).reshape(NL, 8, 128).transpose(0, 2, 1))
    sh["w_in"] = f(inp["w_in"])
    sh["qnT"] = f(np.asarray(inp["mla_q_norm"]).reshape(NL, 2, 128).transpose(0, 2, 1))
    sh["kvnT"] = f(np.asarray(inp["mla_kv_norm"]).reshape(NL, 128, 1))
    sh["gqnT"] = f(np.tile(np.asarray(inp["gqa_q_norm"]), (1, 2)).reshape(NL, 128, 1))
    sh["gknT"] = f(np.tile(np.asarray(inp["gqa_k_norm"]), (1, 2)).reshape(NL, 128, 1))
    sh["mla_w_uq"] = f(inp["mla_w_uq"])
    sh["mla_w_ukv"] = f(inp["mla_w_ukv"])
    sh["nabm"] = na_biasmask(np.asarray(inp["na_bias"], dtype=np.float32))
    sh["ret_decay_logit"] = f(np.asarray(inp["ret_decay_logit"]).reshape(NL, 8))
    sh["ret_gn_w"] = f(inp["ret_gn_w"])
    sh["w_branch"] = f(inp["w_branch"])
    sh["w_out"] = f(inp["w_out"])
    sh["peer_w_q"] = f(inp["peer_w_q"])
    pk = np.asarray(inp["peer_keys"], dtype=np.float32)
    kb = np.zeros((NL, 8, 128, 256), np.float32)
    kb[:, :, 0:64, 0:128] = pk[:, :, 0].transpose(0, 1, 3, 2)
    kb[:, :, 64:128, 128:256] = pk[:, :, 1].transpose(0, 1, 3, 2)
    sh["keysbd"] = kb
    for i in range(NL):
        sh["peer_u%d" % i] = f(np.asarray(inp["peer_u"])[i])
        sh["peer_v%d" % i] = f(np.asarray(inp["peer_v"])[i])
    sh["final_norm_w"] = f(inp["final_norm_w"])
    return sh


def host_core(inp, b0, NB):
    x = np.asarray(inp["x"], dtype=np.float32)
    d = {}
    d["x"] = np.ascontiguousarray(x[b0:b0 + NB])
    d["ctx"] = np.ascontiguousarray(np.asarray(inp["ctx"], dtype=np.float32)[b0:b0 + NB])
    cc = np.concatenate([np.asarray(inp["c"], dtype=np.float32)[b0:b0 + NB], np.asarray(inp["c_ctx"], dtype=np.float32)[None, :]], 0)
    d["cT3"] = np.ascontiguousarray(cc.reshape(NB + 1, 8, 128).transpose(2, 1, 0))
    return d


_PROG = {}


def kernel(**inputs):
    NB = 2
    if "k" not in _PROG:
        _PROG["k"] = build(NB=NB)
    k = _PROG["k"]
    sh = host_shared(inputs)
    in_maps = []
    for c in range(NCORES):
        m = dict(sh)
        m.update(host_core(inputs, c * NB, NB))
        in_maps.append(m)
    res = run_bass_kernel_spmd(k.nc, in_maps, core_ids=list(range(NCORES)))
    out = np.concatenate([np.asarray(r["out"]) for r in res.results], axis=0)
    return np.ascontiguousarray(out.astype(np.float32))
```

```python
import ml_dtypes
from concourse.bass_utils import run_bass_kernel_spmd
import contextlib
import numpy as np
import concourse.bass as bass
import concourse.mybir as mybir

F32 = mybir.dt.float32
BF16 = mybir.dt.bfloat16
I32 = mybir.dt.int32
U32 = mybir.dt.uint32
ALU = mybir.AluOpType
AF = mybir.ActivationFunctionType
AX = mybir.AxisListType


class Dep:
    __slots__ = ("w", "r")

    def __init__(self):
        self.w = None
        self.r = []


class View:
    __slots__ = ("t", "key", "ap")

    def __init__(self, t, key, ap):
        self.t = t
        self.key = key
        self.ap = ap


class _Sub:
    def __init__(self, t, key):
        self.t = t
        self.key = key

    def __getitem__(self, idx):
        return View(self.t, self.key, self.t.base[idx])


class T:
    def __init__(self, name, base, space):
        self.name = name
        self.base = base
        self.space = space
        self.whole = Dep()
        self.subs = {}

    def __getitem__(self, idx):
        return View(self, None, self.base[idx])

    def k(self, key):
        return _Sub(self, key)

    def v(self, ap, key=None):
        return View(self, key, ap)

    def deps(self, key):
        if key is None:
            return [self.whole] + list(self.subs.values())
        if key not in self.subs:
            self.subs[key] = Dep()
        return [self.whole, self.subs[key]]

    def own(self, key):
        if key is None:
            return self.whole
        if key not in self.subs:
            self.subs[key] = Dep()
        return self.subs[key]


class K:
    ENG = ("pe", "act", "dve", "pool", "sp")

    def __init__(self, n_dma_sems=24):
        self.nc = bass.Bass("TRN2", target_bir_lowering=False)
        self.es = contextlib.ExitStack()
        self.stack = [self.es]
        nc = self.nc
        self.eng = {"pe": nc.tensor, "act": nc.scalar, "dve": nc.vector, "pool": nc.gpsimd, "sp": nc.sync}
        self.sems = []
        self.semval = []
        self.esem = {}
        for e in ("pe", "act", "dve", "pool"):
            self.esem[e] = self._newsem("s_" + e)
        self.dsem = {}
        self.dnext = {}
        for q in ("sp", "pool", "act"):
            n = n_dma_sems if q != "act" else 8
            self.dsem[q] = [self._newsem(f"d_{q}{i}") for i in range(n)]
            self.dnext[q] = 0
        self.known = {e: {} for e in self.ENG}
        self.n_inst = 0
        self.embed = True
        self.n_wait = 0
        self._uid = 0

    def _newsem(self, name):
        h = self.es.enter_context(self.nc.semaphore(name))
        self.sems.append(h)
        self.semval.append(0)
        return len(self.sems) - 1

    def uid(self, p):
        self._uid += 1
        return f"{p}{self._uid}"

    def sb(self, name, shape, dtype):
        h = self.stack[-1].enter_context(self.nc.sbuf_tensor(self.uid("s_" + name + "_"), list(shape), dtype))
        return T(name, h, "sb")

    def ps(self, name, shape, dtype=F32):
        h = self.stack[-1].enter_context(self.nc.psum_tensor(self.uid("p_" + name + "_"), list(shape), dtype))
        return T(name, h, "ps")

    def push(self):
        self.stack.append(contextlib.ExitStack())

    def pop(self):
        self.barrier()
        self.stack.pop().close()

    def barrier(self, engines=("pe", "act", "dve", "pool", "sp")):
        for e in engines:
            for s in range(len(self.sems)):
                if self.semval[s] > 0:
                    self._wait(e, (s, self.semval[s]))

    def dram(self, name, shape, dtype, kind="Internal"):
        h = self.nc.dram_tensor(name, list(shape), dtype, kind=kind)
        return T(name, h.ap(), "dram")

    def _wait(self, e, ev):
        if ev is None:
            return
        s, v = ev
        if self.known[e].get(s, 0) >= v:
            return
        self.eng[e].wait_ge(self.sems[s], v)
        self.known[e][s] = v
        self.n_wait += 1

    def _sync_in(self, e, r, w):
        need = {}

        def add(ev):
            if ev is None:
                return
            s, v = ev
            if e == "pe" and s == self.esem["pe"]:
                return
            if need.get(s, 0) < v:
                need[s] = v

        for v in r:
            for d in v.t.deps(v.key):
                add(d.w)
        for v in w:
            for d in v.t.deps(v.key):
                add(d.w)
                for rv in d.r:
                    add(rv)
        pend = [(s, v) for s, v in need.items() if self.known[e].get(s, 0) < v]
        if self.embed and pend:
            for ev in pend[:-1]:
                self._wait(e, ev)
            return pend[-1]
        for ev in pend:
            self._wait(e, ev)
        return None

    def _sync_out(self, ev, r, w):
        for v in r:
            v.t.own(v.key).r.append(ev)
            if len(v.t.own(v.key).r) > 64:
                mx = {}
                for s, val in v.t.own(v.key).r:
                    mx[s] = max(mx.get(s, 0), val)
                v.t.own(v.key).r = list(mx.items())
        for v in w:
            if v.key is None:
                v.t.subs = {}
            d = v.t.own(v.key)
            d.w = ev
            d.r = []

    def op(self, e, fn, r=(), w=()):
        r = [x for x in r if isinstance(x, View)]
        w = [x for x in w if isinstance(x, View)]
        emb = self._sync_in(e, r, w)
        ins = fn(self.eng[e])
        if emb is not None:
            ins._wait_ge(self.sems[emb[0]], emb[1])
            self.known[e][emb[0]] = emb[1]
        s = self.esem[e]
        self.semval[s] += 1
        ins.then_inc(self.sems[s], 1)
        ev = (s, self.semval[s])
        self._sync_out(ev, r, w)
        self.n_inst += 1
        return ins

    def dma(self, out, in_, q="sp", fn=None, extra_r=()):
        r = [in_] + list(extra_r)
        w = [out]
        pool = self.dsem[q]
        i = self.dnext[q]
        self.dnext[q] = (i + 1) % len(pool)
        s = pool[i]
        self._wait(q, (s, self.semval[s]))
        emb = self._sync_in(q, r, w)
        if fn is None:
            ins = self.eng[q].dma_start(out=out.ap, in_=in_.ap)
        else:
            ins = fn(self.eng[q])
        if emb is not None:
            ins._wait_ge(self.sems[emb[0]], emb[1])
            self.known[q][emb[0]] = emb[1]
        self.semval[s] += 16
        ins.then_inc(self.sems[s], 16)
        ev = (s, self.semval[s])
        self._sync_out(ev, r, w)
        self.n_inst += 1
        return ins

    def wait_all(self, e, views):
        for v in views:
            for d in v.t.deps(v.key):
                self._wait(e, d.w)

    def mm(self, out, lhsT, rhs, start=True, stop=True, **kw):
        return self.op("pe", lambda e: e.matmul(out.ap, lhsT.ap, rhs.ap, start=start, stop=stop, **kw),
                       r=[lhsT, rhs] + ([] if start else [out]), w=[out])

    def tr(self, out, in_, ident):
        return self.op("pe", lambda e: e.transpose(out.ap, in_.ap, ident.ap), r=[in_, ident], w=[out])

    def act(self, out, in_, func, bias=None, scale=None, accum=None, e="act"):
        kw = {}
        r = [in_]
        w = [out]
        if bias is not None:
            kw["bias"] = bias.ap if isinstance(bias, View) else bias
            r.append(bias)
        if scale is not None:
            kw["scale"] = scale.ap if isinstance(scale, View) else scale
            r.append(scale)
        if accum is not None:
            kw["accum_out"] = accum.ap
            w.append(accum)
        return self.op(e, lambda en: en.activation(out=out.ap, in_=in_.ap, func=func, **kw), r=r, w=w)

    def tt(self, out, a, b, op, e="dve"):
        return self.op(e, lambda en: en.tensor_tensor(out.ap, a.ap, b.ap, op), r=[a, b], w=[out])

    def ts(self, out, a, s1, op0, s2=None, op1=None, e="dve", accum=None):
        r = [a, s1, s2]
        w = [out]
        kw = {}
        if accum is not None:
            kw["accum_out"] = accum.ap
            w.append(accum)
        g = lambda s: s.ap if isinstance(s, View) else s
        if op1 is None:
            return self.op(e, lambda en: en.tensor_scalar(out.ap, a.ap, g(s1), None, op0, **kw), r=r, w=w)
        return self.op(e, lambda en: en.tensor_scalar(out.ap, a.ap, g(s1), g(s2), op0, op1, **kw), r=r, w=w)

    def stt(self, out, a, s, b, op0, op1, e="dve", accum=None):
        g = lambda x: x.ap if isinstance(x, View) else x
        w = [out]
        kw = {}
        if accum is not None:
            kw["accum_out"] = accum.ap
            w.append(accum)
        return self.op(e, lambda en: en.scalar_tensor_tensor(out.ap, a.ap, g(s), b.ap, op0, op1, **kw),
                       r=[a, s, b], w=w)

    def copy(self, out, in_, e="dve"):
        if e == "act":
            return self.op(e, lambda en: en.copy(out.ap, in_.ap), r=[in_], w=[out])
        return self.op(e, lambda en: en.tensor_copy(out.ap, in_.ap), r=[in_], w=[out])

    def recip(self, out, in_):
        return self.op("dve", lambda en: en.reciprocal(out.ap, in_.ap), r=[in_], w=[out])

    def memset(self, out, val, e="dve"):
        return self.op(e, lambda en: en.memset(out.ap, val), r=[], w=[out])

    def reduce(self, out, in_, op, axis=AX.X, e="dve"):
        return self.op(e, lambda en: en.tensor_reduce(out.ap, in_.ap, axis, op), r=[in_], w=[out])

    def finish(self):
        self.barrier(engines=("sp",))

D = 1024
S = 4096
LC = 256
TT = S + LC
NL = 2
GW = 64
EPS = 1e-6
INC = 7072
NCORES = 8
BF = ml_dtypes.bfloat16
import os
PEXP = os.environ.get('PEXP', '')


class Pool:
    def __init__(self, k, name, shape, dtype, n, space="sb"):
        mk = k.sb if space == "sb" else k.ps
        self.t = [mk(f"{name}{i}", shape, dtype) for i in range(n)]
        self.i = 0

    def nxt(self):
        t = self.t[self.i]
        self.i = (self.i + 1) % len(self.t)
        return t


def rope_tables():
    t = np.arange(S)
    row = (t // GW).astype(np.float32)
    col = (t % GW).astype(np.float32)

    def tab(hd):
        half = hd // 2
        h = half // 2
        freqs = (np.float32(10000.0) ** (-np.arange(h, dtype=np.float32) / np.float32(h))).astype(np.float32)
        C = np.zeros((hd, S), np.float32)
        Sn = np.zeros((hd, S), np.float32)
        for ax, pos in enumerate((row, col)):
            ang = (pos[:, None] * freqs[None, :]).astype(np.float32)
            c = np.cos(ang).astype(np.float32).T
            s = np.sin(ang).astype(np.float32).T
            o = ax * half
            C[o:o + h] = c
            C[o + h:o + 2 * h] = c
            Sn[o:o + h] = s
            Sn[o + h:o + 2 * h] = s
        return C, Sn

    def rmat(hd):
        half = hd // 2
        h = half // 2
        Rm = np.zeros((hd, hd), np.float32)
        for ax in range(2):
            o = ax * half
            for i in range(h):
                Rm[o + i, o + h + i] = -1.0
                Rm[o + h + i, o + i] = 1.0
        return Rm.T.copy()

    Cg, Sg = tab(64)
    Cm, Sm = tab(32)
    out = {}
    out["ropeCg"] = np.concatenate([Cg, Cg], 0)
    out["ropeSg"] = np.concatenate([Sg, Sg], 0)
    z = np.zeros((32, S), np.float32)
    out["ropeCm"] = np.concatenate([Cm, z, Cm], 0)
    out["ropeSm"] = np.concatenate([Sm, z, Sm], 0)
    Rg = np.zeros((128, 128), np.float32)
    Rg[0:64, 0:64] = rmat(64)
    Rg[64:128, 64:128] = rmat(64)
    Rq = np.zeros((96, 96), np.float32)
    Rq[64:96, 64:96] = rmat(32)
    Rq[0:32, 0:32] = rmat(32)
    out["Rg"] = Rg.astype(BF)
    out["Rq"] = Rq.astype(BF)
    return out


def misc_consts():
    out = {}
    out["ident_bf"] = np.eye(128, dtype=np.float32).astype(BF)
    out["ident_f"] = np.eye(128, dtype=np.float32)
    out["ones_f"] = np.ones((128, 128), np.float32)
    ob = np.zeros((128, 128), np.float32)
    ob[0:64, 0:64] = 1.0
    ob[64:, 64:] = 1.0
    out["onesblk_f"] = ob
    io = np.zeros((128, 8, 16, 16), np.float32)
    io[:] = np.arange(16, dtype=np.float32)[None, None, None, :]
    out["iota4"] = io
    m = np.arange(128)[:, None]
    n = np.arange(128)[None, :]
    Df = np.where(n >= m, (n - m), 100000).astype(np.float32)
    Db = np.where(n <= m, (m - n), 100000).astype(np.float32)
    out["retD"] = np.stack([Df, Db], 0)
    qa = np.zeros((2, 128, 128), np.float32)
    qa[0] = (np.arange(128) + 1)[None, :]
    qa[1] = (128 - np.arange(128))[None, :]
    out["retQA"] = qa
    ka = np.zeros((128, 2), np.float32)
    ka[:, 0] = 127 - np.arange(128)
    ka[:, 1] = np.arange(128)
    out["retKA"] = ka
    return out


def na_biasmask(na_bias):
    L = na_bias.shape[0]
    out = np.full((L, 4, 3, 8, 128, 512), -30000.0, np.float32)
    for pat, qg in enumerate((0, 3, 7)):
        r0 = 8 * qg
        kr0 = int(np.clip(r0 - 4, 0, 48))
        r = r0 + np.arange(8)
        rs = np.clip(r - 4, 0, 56)
        c = np.arange(64)
        cs = np.clip(c - 8, 0, 48)
        for kc in range(8):
            kr = kr0 + 2 * kc + np.arange(2)
            KR = kr[:, None, None, None]
            CP = c[None, :, None, None]
            R = r[None, None, :, None]
            RS = rs[None, None, :, None]
            C = c[None, None, None, :]
            CS = cs[None, None, None, :]
            valid = (KR >= RS) & (KR < RS + 8) & (CP >= CS) & (CP < CS + 16)
            rel_r = np.clip(KR - R + 7, 0, 14)
            rel_c = np.clip(CP - C + 15, 0, 30)
            rel_r, rel_c, valid = np.broadcast_arrays(rel_r, rel_c, valid)
            for l in range(L):
                for h in range(4):
                    vals = na_bias[l, h][rel_r, rel_c]
                    out[l, h, pat, kc] = np.where(valid, vals, np.float32(-30000.0)).reshape(128, 512)
    return out


def build(NB=2, layers=(0, 1), dbg=(), stop=None):
    k = K()
    nc = k.nc
    dbg = set(dbg)
    IN = lambda name, shape, dt=F32: k.dram(name, shape, dt, kind="ExternalInput")

    def SCR(name, shape, dt=BF16):
        return k.dram(name, shape, dt, kind=("ExternalOutput" if name in dbg else "Internal"))

    x_in = IN("x", [NB, S, D])
    ctx_in = IN("ctx", [NB, LC, D])
    cT3 = IN("cT3", [128, 8, NB + 1])
    mod_w = IN("mod_w", [NL, D, 6 * D])
    mod_bT = IN("mod_bT", [NL, 128, 48])
    mod_b = IN("mod_b", [NL, 6 * D])
    n1wT = IN("n1wT", [NL, 128, 8])
    n2wT = IN("n2wT", [NL, 128, 8])
    w_in = IN("w_in", [NL, D, INC])
    qnT = IN("qnT", [NL, 128, 2])
    kvnT = IN("kvnT", [NL, 128, 1])
    gqnT = IN("gqnT", [NL, 128, 1])
    gknT = IN("gknT", [NL, 128, 1])
    wuq = IN("mla_w_uq", [NL, 256, 384])
    wukv = IN("mla_w_ukv", [NL, 128, 512])
    nabm = IN("nabm", [NL, 4, 3, 8, 128, 512])
    rdl = IN("ret_decay_logit", [NL, 8])
    gnw = IN("ret_gn_w", [NL, 256])
    w_branch = IN("w_branch", [NL, 4, 256, D])
    w_out = IN("w_out", [NL, D, D])
    peer_wq = IN("peer_w_q", [NL, D, D])
    keysbd = IN("keysbd", [NL, 8, 128, 256])
    peer_u = [IN("peer_u%d" % i, [16384, D]) for i in range(NL)]
    peer_v = [IN("peer_v%d" % i, [16384, D]) for i in range(NL)]
    fnw = IN("final_norm_w", [D])
    c_ident_bf = IN("ident_bf", [128, 128], BF16)
    c_ident_f = IN("ident_f", [128, 128])
    c_ones_f = IN("ones_f", [128, 128])
    c_onesblk = IN("onesblk_f", [128, 128])
    c_iota4 = IN("iota4", [128, 8, 16, 16])
    c_retD = IN("retD", [2, 128, 128])
    c_retQA = IN("retQA", [2, 128, 128])
    c_retKA = IN("retKA", [128, 2])
    c_Cg = IN("ropeCg", [128, S])
    c_Sg = IN("ropeSg", [128, S])
    c_Cm = IN("ropeCm", [96, S])
    c_Sm = IN("ropeSm", [96, S])
    c_Rg = IN("Rg", [128, 128], BF16)
    c_Rq = IN("Rq", [96, 96], BF16)
    out_d = k.dram("out", [NB, S, D], F32, kind="ExternalOutput")

    xres = SCR("xres", [NB, S, D], F32)
    cres = SCR("cres", [NB, LC, D], F32)
    wb_in = SCR("wb_in", [NL, D, INC])
    wb_uq = SCR("wb_uq", [NL, 256, 384])
    wb_ukv = SCR("wb_ukv", [NL, 128, 512])
    wb_br = SCR("wb_br", [NL, 4, 256, D])
    wb_out = SCR("wb_out", [NL, D, D])
    wb_pq = SCR("wb_pq", [NL, D, D])
    wb_keys = SCR("wb_keys", [NL, 8, 128, 256])
    uvb = [SCR("uvb%d" % i, [16384, 2 * D]) for i in range(NL)]
    nabb = SCR("nabb", [NL, 4, 3, 8, 128, 512])
    mqT = SCR("mqT", [384, NB, TT])
    mknT = SCR("mknT", [256, NB, TT])
    mkpT = SCR("mkpT", [32, NB, TT])
    mv = SCR("mv", [NB, TT, 256])
    gqT = SCR("gqT", [256, NB, TT])
    gkT = SCR("gkT", [128, NB, TT])
    gv = SCR("gv", [NB, TT, 128])
    nqT = SCR("nqT", [256, NB, TT])
    nkT = SCR("nkT", [256, NB, TT])
    nv = SCR("nv", [NB, TT, 256])
    rqT = SCR("rqT", [256, NB, TT])
    rkT = SCR("rkT", [256, NB, TT])
    rkv = SCR("rkv", [NB, TT, 512])
    rg = SCR("rg", [NB, TT, 512])
    gT = SCR("gT", [4096, NB, TT])
    oT = SCR("oT", [1024, NB, TT])

    ident_bf = k.sb("ident_bf", [128, 128], BF16)
    ident_f = k.sb("ident_f", [128, 128], F32)
    ones_f = k.sb("ones_f", [128, 128], F32)
    onesblk = k.sb("onesblk", [128, 128], F32)
    Rg = k.sb("Rg", [128, 128], BF16)
    Rq = k.sb("Rq", [96, 96], BF16)
    for sbt, dr in ((ident_bf, c_ident_bf), (ident_f, c_ident_f), (ones_f, c_ones_f), (onesblk, c_onesblk), (Rg, c_Rg)):
        k.dma(sbt[:], dr[:, :])
    k.dma(Rq[:], c_Rq[:, :])

    psA = Pool(k, "psA", [128, 512], F32, 6, space="ps")
    psB = Pool(k, "psB", [128, 512], F32, 2, space="ps")
    P = psA.nxt

    def bfview(pt, a, b, p0=0, p1=128):
        return pt.v(pt.base.bitcast(BF16)[p0:p1, a:b])

    evn = [0]

    def evac(out, in_, scale=None, func=None):
        evn[0] += 1
        if func is not None:
            return k.act(out, in_, func, scale=scale)
        if evn[0] % 2 == 0:
            return k.act(out, in_, AF.Copy, scale=scale)
        if scale is None:
            return k.copy(out, in_)
        return k.ts(out, in_, scale, ALU.mult)

    CV = {}
    cvn = [0]

    def conv_pools(nbuf, engs):
        CV["f"] = Pool(k, "cvf", [128, 2048], F32, nbuf)
        CV["b"] = Pool(k, "cvb", [128, 2048], BF16, nbuf)
        CV["e"] = engs

    def conv_gen(l):
        def flat(ap, pat, **kw):
            return ap.rearrange(pat, **kw)
        yield from convert(flat(w_in.base[l], "(p a) c -> p (a c)", p=128), wb_in, flat(wb_in.base[l], "(p a) c -> p (a c)", p=128), 8 * INC)
        yield from convert(flat(wuq.base[l], "(p a) c -> p (a c)", p=128), wb_uq, flat(wb_uq.base[l], "(p a) c -> p (a c)", p=128), 2 * 384)
        yield from convert(wukv.base[l], wb_ukv, wb_ukv.base[l], 512)
        for i in range(4):
            yield from convert(flat(w_branch.base[l, i], "(p a) c -> p (a c)", p=128), wb_br, flat(wb_br.base[l, i], "(p a) c -> p (a c)", p=128), 2 * D)
        yield from convert(flat(w_out.base[l], "(p a) c -> p (a c)", p=128), wb_out, flat(wb_out.base[l], "(p a) c -> p (a c)", p=128), 8 * D)
        yield from convert(flat(peer_wq.base[l], "(p a) c -> p (a c)", p=128), wb_pq, flat(wb_pq.base[l], "(p a) c -> p (a c)", p=128), 8 * D)
        for h in range(8):
            yield from convert(keysbd.base[l, h], wb_keys, wb_keys.base[l, h], 256)
        for h in range(4):
            for t_ in range(3):
                for c in range(8):
                    yield from convert(nabm.base[l, h, t_, c], nabb, nabb.base[l, h, t_, c], 512)
        for a in range(128):
            yield from convert(peer_u[l].base.rearrange("(p a) c -> p a c", p=128)[:, a, :], uvb[l], uvb[l].base.rearrange("(p a) c -> p a c", p=128)[:, a, 0:D], D)
            yield from convert(peer_v[l].base.rearrange("(p a) c -> p a c", p=128)[:, a, :], uvb[l], uvb[l].base.rearrange("(p a) c -> p a c", p=128)[:, a, D:2 * D], D)

    ro = T("ro", None, "dram")

    def RO(ap):
        return View(ro, "r", ap)

    def convert(src_ap, dst_t, dst_ap, nper):
        for c0 in range(0, nper, 2048):
            n = min(2048, nper - c0)
            f = CV["f"].nxt()
            b = CV["b"].nxt()
            k.dma(f[:, 0:n], RO(src_ap[:, c0:c0 + n]))
            e = CV["e"][cvn[0] % len(CV["e"])]
            cvn[0] += 1
            k.copy(b[:, 0:n], f[:, 0:n], e=e)
            k.dma(dst_t.v(dst_ap[:, c0:c0 + n], key=("cv", cvn[0])), b[:, 0:n], q="pool")
            yield

    k.push()
    conv_pools(8, ("dve", "act"))
    for _ in conv_gen(layers[0]):
        pass
    k.pop()

    modF = k.sb("modF", [128, 48, NB + 1], F32)
    A1 = k.sb("A1", [128, 8, NB + 1], F32)
    A2 = k.sb("A2", [128, 8, NB + 1], F32)
    grep = k.sb("grep", [128, 2, NB + 1, D], F32)
    cT = k.sb("cT", [128, 8, NB + 1], F32)
    k.dma(cT[:], cT3[:, :, :])
    k.act(cT[:], cT[:], AF.Silu)
    mbT = k.sb("mbT", [128, 48], F32)
    nwT = k.sb("nwT", [128, 2, 8], F32)

    def phase_mod(l):
        k.push()
        mwp = Pool(k, "mw", [128, 8, 512], F32, 2)
        mbrep = k.sb("mbrep", [128, 512], F32)
        cTb = k.sb("cTb", [128, NB + 1, 8, 128], F32)
        for col in range(NB + 1):
            for j in range(8):
                k.copy(cTb[:, col, j, :], cT.v(cT.base[:, j, col:col + 1].to_broadcast([128, 128])))
        k.dma(mbT[:], mod_bT[l])
        k.dma(nwT[:, 0, :], n1wT[l])
        k.dma(nwT[:, 1, :], n2wT[l])
        mwv = mod_w.base[l].rearrange("(j p) c -> p j c", p=128)
        fm_blocks = {0: 0, 1: 4, 2: 8, 3: 12, 6: 24, 7: 28, 8: 32, 9: 36}
        g_blocks = {4: (0, 0), 5: (0, 512), 10: (1, 0), 11: (1, 512)}
        for cb in range(12):
            mw = mwp.nxt()
            k.dma(mw[:], RO(mwv[:, :, cb * 512:(cb + 1) * 512]))
            if cb in fm_blocks:
                for o4 in range(4):
                    oc = cb * 4 + o4
                    p = P()
                    for j in range(8):
                        k.mm(p[:, 0:NB + 1], mw[:, j, o4 * 128:(o4 + 1) * 128], cT[:, j, :], start=(j == 0), stop=(j == 7))
                    k.ts(modF[:, oc, :], p[:, 0:NB + 1], mbT[:, oc:oc + 1], ALU.add)
            else:
                gi, off = g_blocks[cb]
                k.dma(mbrep[:], RO(mod_b.base[l, cb * 512:(cb + 1) * 512].partition_broadcast(128)))
                for col in range(NB + 1):
                    p = P()
                    for j in range(8):
                        k.mm(p[:, :], cTb[:, col, j, :], mw[:, j, :], start=(j == 0), stop=(j == 7))
                    k.tt(grep[:, gi, col, off:off + 512], p[:, :], mbrep[:], ALU.add)
        for j in range(8):
            k.ts(A1[:, j, :], modF[:, 8 + j, :], 1.0, ALU.add)
            k.ts(A1[:, j, :], A1[:, j, :], nwT[:, 0, j:j + 1], ALU.mult)
            k.ts(A2[:, j, :], modF[:, 32 + j, :], 1.0, ALU.add)
            k.ts(A2[:, j, :], A2[:, j, :], nwT[:, 1, j:j + 1], ALU.mult)
        k.pop()

    NP = {}

    def norm_pools():
        NP["junk"] = Pool(k, "junk", [128, D], BF16, 2)
        NP["xn"] = Pool(k, "xn", [128, D], BF16, 2)
    ssp = Pool(k, "ss", [128, 2], F32, 4)

    def norm_T(xt, hxT, i, Am, Bm, col):
        ss = ssp.nxt()
        jk = NP["junk"].nxt()
        k.act(jk[:], xt[:], AF.Square, accum=ss[:, 0:1])
        k.act(ss[:, 1:2], ss[:, 0:1], AF.Sqrt, bias=EPS, scale=1.0 / D)
        k.recip(ss[:, 1:2], ss[:, 1:2])
        xn = NP["xn"].nxt()
        k.ts(xn[:], xt[:], ss[:, 1:2], ALU.mult)
        pt = P()
        for j in range(8):
            k.tr(bfview(pt, j * 128, (j + 1) * 128), xn[:, j * 128:(j + 1) * 128], ident_bf[:])
        for j in range(8):
            o = hxT[:, j, i * 128:(i + 1) * 128]
            src = bfview(pt, j * 128, (j + 1) * 128)
            if j % 2 == 0:
                k.act(o, src, AF.Identity, bias=Bm(j, col), scale=Am[:, j, col:col + 1])
            else:
                k.ts(o, src, Am[:, j, col:col + 1], ALU.mult, Bm(j, col), ALU.add)

    wuq_sb = k.sb("wuq_sb", [128, 2, 384], BF16)
    wkn_sb = k.sb("wkn_sb", [128, 4, 64], BF16)
    wkv_sb = k.sb("wkv_sb", [128, 4, 64], BF16)
    nrmw = k.sb("nrmw", [128, 5], F32)

    def fm_rstd(rsp, sqs, ones_t, nfeat, n):
        p = P()
        for c, sq in enumerate(sqs):
            k.mm(p[:, 0:n], ones_t[:], sq, start=(c == 0), stop=(c == len(sqs) - 1))
        rs = rsp.nxt()
        k.act(rs[:, 0:n], p[:, 0:n], AF.Sqrt, bias=EPS, scale=1.0 / nfeat)
        k.recip(rs[:, 0:n], rs[:, 0:n])
        return rs

    def load_layer_small(l):
        k.dma(wuq_sb[:], wb_uq.v(wb_uq.base[l].rearrange("(c p) m -> p c m", p=128)))
        wv = wb_ukv.base[l].rearrange("p (h t e) -> p h t e", h=4, t=2)
        k.dma(wkn_sb[:], wb_ukv.v(wv[:, :, 0, :]))
        k.dma(wkv_sb[:], wb_ukv.v(wv[:, :, 1, :]))
        k.dma(nrmw[:, 0:2], qnT[l])
        k.dma(nrmw[:, 2:3], kvnT[l])
        k.dma(nrmw[:, 3:4], gqnT[l])
        k.dma(nrmw[:, 4:5], gknT[l])

    def groups_for(l, with_ctx=True):
        gs = []
        for b in range(NB):
            gs.append(dict(b=b, t0=0, n=LC, lat=False, col=NB, s0=0))
            for g in range(S // 512):
                gs.append(dict(b=b, t0=LC + g * 512, n=512, lat=True, col=b, s0=g * 512))
        return gs

    def src_rows(l, g, i):
        b = g["b"]
        if g["lat"]:
            r0 = g["s0"] + i * 128
            return (RO(x_in.base[b, r0:r0 + 128, :]) if l == layers[0] and l == 0 else xres[b, r0:r0 + 128, :])
        r0 = i * 128
        return (RO(ctx_in.base[b, r0:r0 + 128, :]) if l == 0 else cres[b, r0:r0 + 128, :])

    def phase_proj(l):
        k.push()
        norm_pools()
        xpool = Pool(k, "xt", [128, D], F32, 3)
        hxTp = Pool(k, "hxT", [128, 8, 512], BF16, 2)
        wpool = Pool(k, "wblk", [128, 8, 512], BF16, 3)
        sqp = Pool(k, "sq", [128, 2, 512], F32, 2)
        rsp = Pool(k, "rs", [128, 512], F32, 2)
        nrm = Pool(k, "nrm", [128, 2, 512], BF16, 2)
        stg = Pool(k, "stg", [128, 512], BF16, 6)
        tmpf = Pool(k, "tmpf", [128, 512], F32, 4)
        ropeT = Pool(k, "ropeT", [128, 4, 512], F32, 2)
        wv = wb_in.base[l].rearrange("(j p) c -> p j c", p=128)
        blocks = [(0, 416), (416, 512), (928, 512), (1440, 512), (1952, 512), (2464, 512)] + [(2976 + 512 * i, 512) for i in range(8)]
        def norm_gen(g, box):
            n_, s0_ = g["n"], g["s0"]
            hx = hxTp.nxt()
            box["hxT"] = hx
            if g["lat"]:
                rt_ = ropeT.nxt()
                k.dma(rt_[:, 0, 0:n_], RO(c_Cg.base[:, s0_:s0_ + n_]))
                k.dma(rt_[:, 1, 0:n_], RO(c_Sg.base[:, s0_:s0_ + n_]))
                k.dma(rt_[0:96, 2, 0:n_], RO(c_Cm.base[:, s0_:s0_ + n_]))
                k.dma(rt_[0:96, 3, 0:n_], RO(c_Sm.base[:, s0_:s0_ + n_]))
                box["rt"] = rt_
            for i in range(n_ // 128):
                xt = xpool.nxt()
                k.dma(xt[:], src_rows(l, g, i))
                norm_T(xt, hx, i, A1, lambda j, c: modF[:, j, c:c + 1], g["col"])
                yield

        gs_all = groups_for(l)
        nbox = {}
        for _ in norm_gen(gs_all[0], nbox):
            pass
        for gi, g in enumerate(gs_all):
            b, t0, n, lat, col, s0 = g["b"], g["t0"], g["n"], g["lat"], g["col"], g["s0"]
            nt = n // 128
            hxT = nbox["hxT"]
            rt = nbox.get("rt")
            nbox = {}
            ngen = norm_gen(gs_all[gi + 1], nbox) if gi + 1 < len(gs_all) else iter(())

            def adv():
                next(ngen, None)

            def loadw(bi):
                c0, ncol = blocks[bi]
                wt = wpool.nxt()
                k.dma(wt[:, :, 0:ncol], wb_in.v(wv[:, :, c0:c0 + ncol]))
                return wt

            def fm(wt, c0, ncol):
                p = P()
                for j in range(8):
                    k.mm(p[0:ncol, 0:n], wt[:, j, c0:c0 + ncol], hxT[:, j, 0:n], start=(j == 0), stop=(j == 7))
                return p

            def tm(wt, c0, ncol, i):
                p = P()
                for j in range(8):
                    k.mm(p[:, 0:ncol], hxT[:, j, i * 128:(i + 1) * 128], wt[:, j, c0:c0 + ncol], start=(j == 0), stop=(j == 7))
                return p

            def store_fm(dst, r0, nr, src):
                k.dma(dst.k((b, t0, r0))[r0:r0 + nr, b, t0:t0 + n], src, q="pool")

            def store_tm(dst, i, src):
                k.dma(dst.k((b, t0, i))[b, t0 + i * 128:t0 + (i + 1) * 128, :], src, q="pool")

            def rope(xs, p0, p1, Rt, ci, si):
                R = Rt.base.shape[0]
                p2 = P()
                k.mm(p2[0:R, 0:n], Rt[:, :], xs[0:R, 0:n])
                t1 = tmpf.nxt()
                t2 = tmpf.nxt()
                k.tt(t1[p0:p1, 0:n], p2[p0:p1, 0:n], rt[p0:p1, si, 0:n], ALU.mult)
                k.tt(t2[p0:p1, 0:n], xs[p0:p1, 0:n], rt[p0:p1, ci, 0:n], ALU.mult)
                k.tt(xs[p0:p1, 0:n], t1[p0:p1, 0:n], t2[p0:p1, 0:n], ALU.add)

            w0 = loadw(0)
            w1 = loadw(1)
            pc = [fm(w0, 0, 128), fm(w0, 128, 128)]
            sq = sqp.nxt()
            for c in range(2):
                k.act(sq[:, c, 0:n], pc[c][:, 0:n], AF.Square)
            rs = fm_rstd(rsp, [sq[:, 0, 0:n], sq[:, 1, 0:n]], ones_f, 256, n)
            cqn = nrm.nxt()
            for c in range(2):
                k.stt(cqn[:, c, 0:n], pc[c][:, 0:n], nrmw[:, c:c + 1], rs[:, 0:n], ALU.mult, ALU.mult)
            for h in range(4):
                p = P()
                for c in range(2):
                    k.mm(p[0:96, 0:n], wuq_sb[:, c, h * 96:(h + 1) * 96], cqn[:, c, 0:n], start=(c == 0), stop=(c == 1))
                qa = stg.nxt()
                evac(qa[0:96, 0:n], p[0:96, 0:n])
                if lat:
                    rope(qa, 64, 96, Rq, 2, 3)
                store_fm(mqT, h * 96, 96, qa[0:96, 0:n])
            pk = fm(w0, 256, 128)
            sq = sqp.nxt()
            k.act(sq[:, 0, 0:n], pk[:, 0:n], AF.Square)
            rs = fm_rstd(rsp, [sq[:, 0, 0:n]], ones_f, 128, n)
            ckn = nrm.nxt()
            k.stt(ckn[:, 0, 0:n], pk[:, 0:n], nrmw[:, 2:3], rs[:, 0:n], ALU.mult, ALU.mult)
            for h in range(4):
                p = P()
                k.mm(p[0:64, 0:n], wkn_sb[:, h, :], ckn[:, 0, 0:n])
                kn = stg.nxt()
                evac(kn[0:64, 0:n], p[0:64, 0:n])
                store_fm(mknT, h * 64, 64, kn[0:64, 0:n])
            for i in range(nt):
                p = P()
                k.mm(p[:, 0:256], ckn[:, 0, i * 128:(i + 1) * 128], wkv_sb.v(wkv_sb.base[:, :, :].rearrange("p h e -> p (h e)")))
                vt = stg.nxt()
                evac(vt[:, 0:256], p[:, 0:256])
                store_tm(mv, i, vt[:, 0:256])
            pp = fm(w0, 384, 32)
            kp = stg.nxt()
            evac(kp[0:32, 0:n], pp[0:32, 0:n])
            if lat:
                rope(kp, 0, 32, Rq.v(Rq.base[0:32, 0:32]) if False else Rq32, 2, 3)
            store_fm(mkpT, 0, 32, kp[0:32, 0:n])
            w2 = loadw(2)
            for (c0, dst, r0, nw) in ((0, gqT, 0, 3), (128, gqT, 128, 3), (256, gkT, 0, 4)):
                p = fm(w1, c0, 128)
                sq = sqp.nxt()
                k.act(sq[:, 0, 0:n], p[:, 0:n], AF.Square)
                rs = fm_rstd(rsp, [sq[:, 0, 0:n]], onesblk, 64, n)
                xo = stg.nxt()
                k.stt(xo[:, 0:n], p[:, 0:n], nrmw[:, nw:nw + 1], rs[:, 0:n], ALU.mult, ALU.mult)
                if lat:
                    rope(xo, 0, 128, Rg, 0, 1)
                store_fm(dst, r0, 128, xo[:, 0:n])
            for i in range(nt):
                p = tm(w1, 384, 128, i)
                vt = stg.nxt()
                evac(vt[:, 0:128], p[:, 0:128])
                store_tm(gv, i, vt[:, 0:128])
            adv()
            w3 = loadw(3)
            for c in range(4):
                p = fm(w2, c * 128, 128)
                xo = stg.nxt()
                evac(xo[:, 0:n], p[:, 0:n], scale=(0.125 if c < 2 else None))
                store_fm(nqT if c < 2 else nkT, (c % 2) * 128, 128, xo[:, 0:n])
            adv()
            w4 = loadw(4)
            for i in range(nt):
                p = tm(w3, 0, 256, i)
                vt = stg.nxt()
                evac(vt[:, 0:256], p[:, 0:256])
                store_tm(nv, i, vt[:, 0:256])
            for c in range(2):
                p = fm(w3, 256 + c * 128, 128)
                xo = stg.nxt()
                evac(xo[:, 0:n], p[:, 0:n])
                store_fm(rqT, c * 128, 128, xo[:, 0:n])
            adv()
            w5 = loadw(5)
            for c in range(2):
                p = fm(w4, c * 128, 128)
                xo = stg.nxt()
                evac(xo[:, 0:n], p[:, 0:n], scale=0.125)
                store_fm(rkT, c * 128, 128, xo[:, 0:n])
            for i in range(nt):
                p = tm(w4, 0, 512, i)
                vt = stg.nxt()
                evac(vt[:, 0:256], p[:, 0:256], scale=0.125)
                evac(vt[:, 256:512], p[:, 256:512])
                store_tm(rkv, i, vt[:, 0:512])
            adv()
            wn = loadw(6)
            for i in range(nt):
                p = tm(w5, 0, 512, i)
                vt = stg.nxt()
                evac(vt[:, 0:512], p[:, 0:512])
                store_tm(rg, i, vt[:, 0:512])
            for gb in range(8):
                wc = wn
                if gb < 7:
                    wn = loadw(7 + gb)
                for c in range(4):
                    p = fm(wc, c * 128, 128)
                    xo = stg.nxt()
                    k.act(xo[:, 0:n], p[:, 0:n], AF.Sigmoid)
                    store_fm(gT, gb * 512 + c * 128, 128, xo[:, 0:n])
            for _ in ngen:
                pass
        k.pop()

    pend_epi = []

    def attn_flush():
        while pend_epi:
            pend_epi.pop(0)()

    def attn_core(pools, KT, dk, V, kchunks, q_src, nq, scale, o_dst, bias_fn=None):
        qpool, ptp, osp, obp = pools
        qt = q_src
        po = psB.nxt()
        n_ = len(kchunks)
        pss = {}

        def qk(ci):
            kc = kchunks[ci]
            ps = P()
            bm = bias_fn(kc) if bias_fn is not None else None
            k.mm(ps[:, 0:nq], KT[0:dk, kc * 128:(kc + 1) * 128], qt[0:dk, 0:nq], start=True, stop=(bm is None))
            if bm is not None:
                k.mm(ps[:, 0:nq], ident_bf[:], bm, start=False, stop=True)
            pss[ci] = ps

        qk(0)
        if n_ > 1:
            qk(1)
        attn_flush()
        for ci in range(n_):
            pt = ptp.nxt()
            k.act(pt[:, 0:nq], pss.pop(ci)[:, 0:nq], AF.Exp, scale=scale)
            if ci + 2 < n_:
                qk(ci + 2)
            k.mm(po[0:65, 0:nq], V[:, kchunks[ci], 0:65], pt[:, 0:nq], start=(ci == 0), stop=(ci == n_ - 1))

        def epi():
            os_ = osp.nxt()
            k.copy(os_[0:65, 0:nq], po[0:65, 0:nq])
            k.recip(os_[64:65, 0:nq], os_[64:65, 0:nq])
            pb = P()
            k.mm(pb[0:64, 0:nq], ones_f[64:65, 0:64], os_[64:65, 0:nq])
            ob = obp.nxt()
            k.tt(ob[0:64, 0:nq], os_[0:64, 0:nq], pb[0:64, 0:nq], ALU.mult)
            k.dma(o_dst, ob[0:64, 0:nq], q="pool")
        pend_epi.append(epi)

    def attn_pools():
        qpool = Pool(k, "aq", [96, 512], BF16, 2)
        qpoolB = Pool(k, "aqB", [128, 512], BF16, 2)
        for t_ in qpoolB.t:
            k.memset(t_[:], 0.0)
        qpool.padded = qpoolB
        ptp = Pool(k, "apt", [128, 512], BF16, 4)
        osp = Pool(k, "aos", [65, 512], F32, 2)
        obp = Pool(k, "aob", [64, 512], BF16, 2)
        return (qpool, ptp, osp, obp)

    NKC = TT // 128

    def load_V(Vt, src_t, b, c0):
        k.dma(Vt[:, :, 0:64], src_t.v(src_t.base[b, :, c0:c0 + 64].rearrange("(c p) e -> p c e", p=128)))

    def run_attn_jobs(pools, jobs, KTp, Vp, bmp=None, filler=None, nfill=0):
        qpool = pools[0]
        kvbuf = {}

        def ensure_kv(key, loader):
            if key not in kvbuf:
                KT, Vt = (KTp[1] if key[0] == "g" else KTp[0]).nxt() if isinstance(KTp, tuple) else KTp.nxt(), Vp.nxt()
                loader(KT, Vt)
                kvbuf[key] = (KT, Vt)

        def prefetch(j):
            qt = (qpool.padded if j.get("pad") else qpool).nxt()
            k.dma(qt[0:j["dk"], 0:j["nq"]], j["qsrc"])
            j["qt"] = qt
            if j.get("bmsrc") is not None:
                bm = bmp.nxt()
                k.dma(bm[:], j["bmsrc"])
                j["bm"] = bm

        ensure_kv(jobs[0]["kv"], jobs[0]["kvload"])
        prefetch(jobs[0])
        for i, j in enumerate(jobs):
            if i + 1 < len(jobs):
                prefetch(jobs[i + 1])
            if i == 0 or jobs[i - 1]["kv"] != j["kv"]:
                for j2 in jobs[i + 1:]:
                    if j2["kv"] != j["kv"]:
                        ensure_kv(j2["kv"], j2["kvload"])
                        break
                for key in [kk for kk in kvbuf if kk != j["kv"] and all(kk != j2["kv"] for j2 in jobs[i:])]:
                    del kvbuf[key]
            KT, Vt = kvbuf[j["kv"]]
            bfn = (j["bias"](j["bm"]) if j.get("bias") is not None else None)
            attn_core(pools, KT, (128 if j.get("pad") else j["dk"]), Vt, j["chunks"], j["qt"], j["nq"], j["scale"], j["odst"], bias_fn=bfn)
            if filler is not None:
                for _ in range(nfill):
                    next(filler, None)
        attn_flush()
        if filler is not None:
            for _ in filler:
                pass

    def phase_attn(l, need_ctx):
        k.push()
        pools = attn_pools()
        KTp = Pool(k, "aKT", [96, TT], BF16, 2)
        KTg = Pool(k, "aKTg", [128, TT], BF16, 2)
        for t_ in KTg.t:
            k.memset(t_[64:128, :], 0.0)
        Vp = Pool(k, "aV", [128, NKC, 65], BF16, 2)
        for Vt in Vp.t:
            k.memset(Vt[:, :, 64:65], 1.0)
        allk = list(range(NKC))
        jobs = []
        for b in range(NB):
            for h in range(4):
                def kvl(KT, Vt, b=b, h=h):
                    k.dma(KT[0:64, :], mknT[h * 64:(h + 1) * 64, b, :])
                    k.dma(KT[64:96, :], mkpT[:, b, :])
                    load_V(Vt, mv, b, h * 64)
                for qg in range(8):
                    t0 = LC + qg * 512
                    jobs.append(dict(kv=("m", b, h), kvload=kvl, dk=96, chunks=allk, qsrc=mqT[h * 96:(h + 1) * 96, b, t0:t0 + 512], nq=512,
                                     scale=96 ** -0.5, odst=oT.k((b, t0, h))[h * 64:(h + 1) * 64, b, t0:t0 + 512]))
                if need_ctx:
                    jobs.append(dict(kv=("m", b, h), kvload=kvl, dk=96, chunks=[0, 1], qsrc=mqT[h * 96:(h + 1) * 96, b, 0:LC], nq=LC,
                                     scale=96 ** -0.5, odst=oT.k((b, 0, h))[h * 64:(h + 1) * 64, b, 0:LC]))
            for kv in range(2):
                def kvl(KT, Vt, b=b, kv=kv):
                    k.dma(KT[0:64, :], gkT[kv * 64:(kv + 1) * 64, b, :])
                    load_V(Vt, gv, b, kv * 64)
                for gq in range(2):
                    h = kv * 2 + gq
                    for qg in range(8):
                        t0 = LC + qg * 512
                        jobs.append(dict(kv=("g", b, kv), kvload=kvl, pad=True, dk=64, chunks=allk, qsrc=gqT[h * 64:(h + 1) * 64, b, t0:t0 + 512], nq=512,
                                         scale=0.125, odst=oT.k((b, t0, 4 + h))[256 + h * 64:256 + (h + 1) * 64, b, t0:t0 + 512]))
                    if need_ctx:
                        jobs.append(dict(kv=("g", b, kv), kvload=kvl, pad=True, dk=64, chunks=[0, 1], qsrc=gqT[h * 64:(h + 1) * 64, b, 0:LC], nq=LC,
                                         scale=0.125, odst=oT.k((b, 0, 4 + h))[256 + h * 64:256 + (h + 1) * 64, b, 0:LC]))
        filler = None
        nxt_l = [x for x in layers if x > l]
        if nxt_l:
            conv_pools(6, ("dve",))
            filler = conv_gen(nxt_l[0])
        run_attn_jobs(pools, jobs, (KTp, KTg), Vp, filler=filler, nfill=5)
        k.pop()

    def phase_na(l, need_ctx):
        k.push()
        pools = attn_pools()
        KTp = Pool(k, "nKT", [128, TT], BF16, 2)
        for t_ in KTp.t:
            k.memset(t_[64:128, :], 0.0)
        Vp = Pool(k, "nV", [128, NKC, 65], BF16, 2)
        bmp = Pool(k, "nbm", [128, 8, 512], BF16, 3)
        for Vt in Vp.t:
            k.memset(Vt[:, :, 64:65], 1.0)
        jobs = []
        for b in range(NB):
            for h in range(4):
                def kvl(KT, Vt, b=b, h=h):
                    k.dma(KT[0:64, :], nkT[h * 64:(h + 1) * 64, b, :])
                    load_V(Vt, nv, b, h * 64)
                for qg in range(8):
                    t0 = LC + qg * 512
                    pat = 0 if qg == 0 else (2 if qg == 7 else 1)
                    kr0 = min(max(8 * qg - 4, 0), 48)
                    kc0 = 2 + kr0 // 2
                    jobs.append(dict(kv=("n", b, h), kvload=kvl, pad=True, dk=64, chunks=[0, 1] + [kc0 + j for j in range(8)],
                                     qsrc=nqT[h * 64:(h + 1) * 64, b, t0:t0 + 512], nq=512, scale=1.0,
                                     odst=oT.k((b, t0, 8 + h))[512 + h * 64:512 + (h + 1) * 64, b, t0:t0 + 512],
                                     bmsrc=nabb.v(nabb.base[l, h, pat].rearrange("c p q -> p c q")),
                                     bias=lambda bm, kc0=kc0: (lambda kc: (bm[:, kc - kc0, :] if kc >= 2 else None))))
                if need_ctx:
                    jobs.append(dict(kv=("n", b, h), kvload=kvl, pad=True, dk=64, chunks=[0, 1], qsrc=nqT[h * 64:(h + 1) * 64, b, 0:LC], nq=LC, scale=1.0,
                                     odst=oT.k((b, 0, 8 + h))[512 + h * 64:512 + (h + 1) * 64, b, 0:LC]))
        run_attn_jobs(pools, jobs, KTp, Vp, bmp)
        k.pop()

    def phase_ret(l, need_ctx):
        k.push()
        lg = k.sb("lg", [128, 8], F32)
        k.dma(lg[:], RO(rdl.base[l].partition_broadcast(128)))
        k.act(lg[:], lg[:], AF.Exp, scale=-1.0)
        k.ts(lg[:], lg[:], 1.0, ALU.add)
        k.act(lg[:], lg[:], AF.Ln)
        k.ts(lg[:], lg[:], -1.0, ALU.mult)
        Dd = k.sb("Dd", [128, 2, 128], F32)
        QA = k.sb("QA", [128, 2, 128], F32)
        KA = k.sb("KA", [128, 2], F32)
        k.dma(Dd[:], RO(c_retD.base.rearrange("d p n -> p d n")))
        k.dma(QA[:], RO(c_retQA.base.rearrange("d p n -> p d n")))
        k.dma(KA[:], RO(c_retKA.base))
        decT = k.sb("decT", [128, 2, 4, 128], F32)
        qdec = k.sb("qdec", [128, 2, 4, 128], F32)
        kdec = k.sb("kdec", [128, 2, 4], F32)
        cdec = k.sb("cdec", [128, 8], F32)
        gnr = k.sb("gnr", [128, 256], F32)
        k.dma(gnr[:], RO(gnw.base[l].partition_broadcast(128)))
        for d in range(2):
            for h in range(4):
                k.act(decT[:, d, h, :], Dd[:, d, :], AF.Exp, scale=lg[:, d * 4 + h:d * 4 + h + 1])
                k.act(qdec[:, d, h, :], QA[:, d, :], AF.Exp, scale=lg[:, d * 4 + h:d * 4 + h + 1])
            k.act(kdec[:, d, :], lg[:, d * 4:(d + 1) * 4], AF.Exp, scale=KA[:, d:d + 1])
        k.act(cdec[:], lg[:], AF.Exp, scale=128.0)
        QT = k.sb("rQT", [128, 2, TT], BF16)
        KT = k.sb("rKT", [128, 2, TT], BF16)
        KVt = k.sb("rKV", [128, NKC, 512], BF16)
        G = k.sb("rG", [128, NKC, 512], BF16)
        Ys = [k.sb("rY", [128, NKC, 256], F32), k.sb("rYb", [128, NKC, 256], BF16)]
        Sts = [k.sb("rS", [128, 2, 64], F32) for _ in range(2)]
        Sbs = [k.sb("rSb", [128, 2, 64], BF16) for _ in range(2)]
        kdp = Pool(k, "rkd", [128, 256], BF16, 2)
        idp = Pool(k, "rid", [128, 128], BF16, 3)
        qdp = Pool(k, "rqd", [128, 128], BF16, 3)
        osb = Pool(k, "ros", [128, 256], F32, 2)
        sgp = Pool(k, "rsg", [128, 256], F32, 2)
        stp = Pool(k, "rst", [128, 4], F32, 4)
        ybp = Pool(k, "ryb", [128, 256], BF16, 2)
        ysp = Pool(k, "rys", [128, 2, 128], BF16, 2)
        b3 = lambda t_, ap: t_.v(ap)
        for b in range(NB):
            for c in range(2):
                k.dma(QT[:, c, :], rqT[c * 128:(c + 1) * 128, b, :])
                k.dma(KT[:, c, :], rkT[c * 128:(c + 1) * 128, b, :])
            k.dma(KVt[:], rkv.v(rkv.base[b].rearrange("(c p) f -> p c f", p=128)))
            k.dma(G[:], rg.v(rg.base[b].rearrange("(c p) f -> p c f", p=128)))
            for d in range(2):
                k.memset(Sts[d][:], 0.0)
                k.memset(Sbs[d][:], 0.0)
            orders = [list(range(NKC)), [1, 0] + list(range(NKC - 1, 1, -1))]
            for step in range(2 * NKC):
                for _once in (0,):
                    d = step % 2
                    ci = orders[d][step // 2]
                    St, Sb, Y = Sts[d], Sbs[d], Ys[d]
                    tk = slice(ci * 128, (ci + 1) * 128)
                    Kd = kdp.nxt()
                    k.tt(b3(Kd, Kd.base[:, :].rearrange("p (h e) -> p h e", h=4)),
                         b3(KVt, KVt.base[:, ci, 0:256].rearrange("p (h e) -> p h e", h=4)),
                         b3(kdec, kdec.base[:, d, :].unsqueeze(2).to_broadcast([128, 4, 64])), ALU.mult)
                    pso = psB.nxt()
                    for h in range(4):
                        c2, po = h // 2, (h % 2) * 64
                        ps1 = P()
                        k.mm(ps1[:, 0:128], KT[po:po + 64, c2, tk], QT[po:po + 64, c2, tk])
                        idT = idp.nxt()
                        k.tt(idT[:], ps1[:, 0:128], decT[:, d, h, :], ALU.mult)
                        Qd = qdp.nxt()
                        k.tt(Qd[po:po + 64, :], QT[po:po + 64, c2, tk], qdec[po:po + 64, d, h, :], ALU.mult)
                        k.mm(pso[:, h * 64:(h + 1) * 64], idT[:], KVt[:, ci, 256 + h * 64:256 + (h + 1) * 64], start=True, stop=False)
                        k.mm(pso[:, h * 64:(h + 1) * 64], Qd[po:po + 64, :], Sb[po:po + 64, c2, :], start=False, stop=True)
                    for c2 in range(2):
                        ps2 = P()
                        k.mm(ps2[:, 0:128], Kd[:, c2 * 128:(c2 + 1) * 128], KVt[:, ci, 256 + c2 * 128:256 + (c2 + 1) * 128])
                        for hh in range(2):
                            po = hh * 64
                            h = c2 * 2 + hh
                            k.stt(St[po:po + 64, c2, :], St[po:po + 64, c2, :], cdec[po:po + 64, d * 4 + h:d * 4 + h + 1],
                                  ps2[po:po + 64, po:po + 64], ALU.mult, ALU.add)
                    k.copy(Sb[:], St[:])
                    if ci < 2 and not need_ctx:
                        continue
                    o = osb.nxt()
                    o3 = b3(o, o.base[:, :].rearrange("p (h e) -> p h e", h=4))
                    k.copy(o[:], pso[:, 0:256])
                    st = stp.nxt()
                    k.reduce(st[:], o3, ALU.add)
                    k.ts(st[:], st[:], 1.0 / 64, ALU.mult)
                    k.tt(o3, o3, b3(st, st.base[:, :].unsqueeze(2).to_broadcast([128, 4, 64])), ALU.subtract)
                    sq = sgp.nxt()
                    k.tt(sq[:], o[:], o[:], ALU.mult)
                    st2 = stp.nxt()
                    k.reduce(st2[:], b3(sq, sq.base[:, :].rearrange("p (h e) -> p h e", h=4)), ALU.add)
                    k.act(st2[:], st2[:], AF.Sqrt, bias=EPS, scale=1.0 / 64)
                    k.recip(st2[:], st2[:])
                    k.tt(o3, o3, b3(st2, st2.base[:, :].unsqueeze(2).to_broadcast([128, 4, 64])), ALU.mult)
                    k.tt(o[:], o[:], gnr[:], ALU.mult)
                    sg = sgp.nxt()
                    k.act(sg[:], G[:, ci, d * 256:(d + 1) * 256], AF.Silu)
                    k.tt(Y[:, ci, :], o[:], sg[:], ALU.mult)
            for ci in (range(NKC) if need_ctx else range(2, NKC)):
                yb = ybp.nxt()
                k.tt(yb[:], Ys[0][:, ci, :], Ys[1][:, ci, :], ALU.add)
                pt = P()
                for c2 in range(2):
                    k.tr(bfview(pt, c2 * 128, (c2 + 1) * 128), yb[:, c2 * 128:(c2 + 1) * 128], ident_bf[:])
                ys = ysp.nxt()
                k.copy(b3(ys, ys.base[:, :, :].rearrange("p c t -> p (c t)")), bfview(pt, 0, 256))
                for c2 in range(2):
                    k.dma(oT.k((b, ci, 12 + c2))[768 + c2 * 128:768 + (c2 + 1) * 128, b, ci * 128:(ci + 1) * 128], ys[:, c2, :], q="pool")
        k.pop()

    def phase_merge(l, need_ctx):
        k.push()
        wbr = k.sb("wbr", [128, 4, 2, D], BF16)
        wo = k.sb("wo", [128, 8, D], BF16)
        for i in range(4):
            k.dma(wbr[:, i, :, :], wb_br.v(wb_br.base[l, i].rearrange("(kk p) m -> p kk m", p=128)))
        k.dma(wo[:], wb_out.v(wb_out.base[l].rearrange("(j p) m -> p j m", p=128)))
        osp = Pool(k, "mo", [128, 8, 512], BF16, 2)
        mgl = Pool(k, "mgl", [128, 512], BF16, 6)
        gbp = Pool(k, "mgb", [128, 512], BF16, 4)
        accp = Pool(k, "macc", [128, 512], F32, 2)
        tmpp = Pool(k, "mtmp", [128, 512], F32, 2)
        accT = Pool(k, "maT", [128, 8, 512], BF16, 2)
        xp = Pool(k, "mx", [128, D], F32, 3)
        for g in groups_for(l):
            b, t0, n, lat, col = g["b"], g["t0"], g["n"], g["lat"], g["col"]
            if not lat and not need_ctx:
                continue
            nt = n // 128
            o_sb = osp.nxt()
            k.dma(o_sb[:, :, 0:n], oT.v(oT.base[:, b, t0:t0 + n].rearrange("(kk p) t -> p kk t", p=128)))
            aT = accT.nxt()
            pend = None
            pas = {}

            def emit_acc(pd):
                m_, i_, gb_ = pd
                k.mm(pas[m_][:, 0:n], ident_bf[:], gb_[:, 0:n], start=(i_ == 0), stop=(i_ == 3))
                if i_ == 3:
                    k.copy(aT[:, m_, 0:n], pas[m_][:, 0:n], e="act")

            for m in range(8):
                pas[m] = psB.nxt()
                for i in range(4):
                    p = P()
                    for kk in range(2):
                        k.mm(p[:, 0:n], wbr[:, i, kk, m * 128:(m + 1) * 128], o_sb[:, 2 * i + kk, 0:n], start=(kk == 0), stop=(kk == 1))
                    gl = mgl.nxt()
                    k.dma(gl[:, 0:n], gT[i * 1024 + m * 128:i * 1024 + (m + 1) * 128, b, t0:t0 + n])
                    gb_ = gbp.nxt()
                    k.tt(gb_[:, 0:n], p[:, 0:n], gl[:, 0:n], ALU.mult)
                    if pend is not None:
                        emit_acc(pend)
                    pend = (m, i, gb_)
            emit_acc(pend)
            for i in range(nt):
                xt = xp.nxt()
                k.dma(xt[:], src_rows(l, g, i))
                for hf in range(2):
                    p = P()
                    for m in range(8):
                        k.mm(p[:, :], aT[:, m, i * 128:(i + 1) * 128], wo[:, m, hf * 512:(hf + 1) * 512], start=(m == 0), stop=(m == 7))
                    tp_ = tmpp.nxt()
                    k.tt(tp_[:], p[:, :], grep[:, 0, col, hf * 512:(hf + 1) * 512], ALU.mult)
                    k.tt(xt[:, hf * 512:(hf + 1) * 512], xt[:, hf * 512:(hf + 1) * 512], tp_[:], ALU.add, e="pool")
                dst = xres.k((b, g["s0"] + i * 128))[b, g["s0"] + i * 128:g["s0"] + (i + 1) * 128, :] if lat else cres.k((b, i))[b, i * 128:(i + 1) * 128, :]
                k.dma(dst, xt[:], q="pool")
        k.pop()

    def phase_peer(l, need_ctx):
        k.push()
        norm_pools()
        wq = k.sb("pwq", [128, 8, D], BF16)
        kb = k.sb("pkb", [128, 8, 256], BF16)
        iota16 = k.sb("piota", [128, 16], F32)
        k.dma(wq[:], wb_pq.v(wb_pq.base[l].rearrange("(j p) m -> p j m", p=128)))
        k.dma(kb[:], wb_keys.v(wb_keys.base[l].rearrange("h p c -> p h c")))
        k.dma(iota16[:], RO(c_iota4.base[:, 0, 0, :]))
        xp = Pool(k, "px", [128, D], F32, 2)
        hTp = Pool(k, "phT", [128, 8, 128], BF16, 2)
        qTp = Pool(k, "pqT", [128, 128], BF16, 3)
        wkp = Pool(k, "pwk", [128, 256], F32, 2)
        up = Pool(k, "pu", [128, 2 * D], BF16, 16)
        accp = Pool(k, "pacc", [128, D], F32, 1)
        jkp = Pool(k, "pjk", [128, D], BF16, 2)
        dgp = Pool(k, "pdg", [128, 128], BF16, 4)
        b3 = lambda t_, ap: t_.v(ap)
        B2 = lambda j, c: modF[:, 24 + j, c:c + 1]
        if not breg_box:
            breg_box.append(nc.gpsimd.to_reg(16383))
        breg = breg_box[0]

        class St:
            pass
        sets = []
        for i in range(2):
            s = St()
            s.h2 = k.sb("ph2", [128, D], BF16)
            s.s_sb = k.sb("ps_sb", [128, 8, 256], F32)
            s.V1 = k.sb("pV1", [128, 8, 2, 16], F32)
            s.I1 = k.sb("pI1", [128, 8, 2, 16], U32)
            s.I1f = k.sb("pI1f", [128, 8, 2, 16], F32)
            s.cand = k.sb("pcand", [128, 8, 256], F32)
            s.CS = k.sb("pCS", [128, 8, 16], F32)
            s.POS = k.sb("pPOS", [128, 8, 16], U32)
            s.AB = k.sb("pAB", [128, 2, 8, 16], U32)
            s.ABf = k.sb("pABf", [128, 2, 8, 16], F32)
            s.isel = k.sb("pisel", [128, 2, 8, 16], F32)
            s.EI = k.sb("pEI", [128, 128], I32)
            s.gsm = k.sb("pgsm", [128, 8, 16], F32)
            s.zs = k.sb("pzs", [128, 8], F32)
            s.dots = k.sb("pdots", [128, 128], F32)
            s.wg = k.sb("pwg", [128, 128], F32)
            sets.append(s)

        def top16(vals, idx, src):
            wk = wkp.nxt()
            n_ = src.ap.shape[-1]
            v0, v1 = vals
            i0_, i1_ = idx
            k.op("dve", lambda e: e.max(out=v0.ap, in_=src.ap), r=[src], w=[v0])
            k.op("dve", lambda e: e.max_index(out=i0_.ap, in_max=v0.ap, in_values=src.ap), r=[v0, src], w=[i0_])
            k.op("dve", lambda e: e.match_replace(out=wk[:, 0:n_].ap, in_to_replace=v0.ap, in_values=src.ap, imm_value=-1e30), r=[v0, src], w=[wk[:, 0:n_]])
            k.op("dve", lambda e: e.max(out=v1.ap, in_=wk[:, 0:n_].ap), r=[wk[:, 0:n_]], w=[v1])
            k.op("dve", lambda e: e.max_index(out=i1_.ap, in_max=v1.ap, in_values=wk[:, 0:n_].ap), r=[v1, wk[:, 0:n_]], w=[i1_])

        def front(s, b, ci):
            lat = ci >= 2
            s.col = b if lat else NB
            s.rv = (xres.k((b, (ci - 2) * 128)), (b, slice((ci - 2) * 128, (ci - 1) * 128), slice(None))) if lat else (cres.k((b, ci)), (b, slice(ci * 128, (ci + 1) * 128), slice(None)))
            s.xt = xp.nxt()
            k.dma(s.xt[:], s.rv[0][s.rv[1]])
            hT = hTp.nxt()
            norm_T(s.xt, hT, 0, A2, B2, s.col)
            yield
            pt = P()
            for j in range(8):
                k.tr(bfview(pt, j * 128, (j + 1) * 128), hT[:, j, :], ident_bf[:])
            k.copy(s.h2[:], bfview(pt, 0, 1024), e="act")
            yield
            for h in range(8):
                p = P()
                for j in range(8):
                    k.mm(p[:, 0:128], wq[:, j, h * 128:(h + 1) * 128], hT[:, j, :], start=(j == 0), stop=(j == 7))
                qT = qTp.nxt()
                k.copy(qT[:], p[:, 0:128], e="act")
                p2 = P()
                k.mm(p2[:, 0:256], qT[:], kb[:, h, :])
                k.copy(s.s_sb[:, h, :], p2[:, 0:256], e="act")
                yield
            for h in range(8):
                for p_ in range(2):
                    top16((s.V1[:, h, p_, 0:8], s.V1[:, h, p_, 8:16]), (s.I1[:, h, p_, 0:8], s.I1[:, h, p_, 8:16]), s.s_sb[:, h, p_ * 128:(p_ + 1) * 128])
                yield
            k.tt(b3(s.cand, s.cand.base[:, :, :].rearrange("p h (a c) -> p h a c", a=16)),
                 b3(s.V1, s.V1.base[:, :, 0, :].unsqueeze(3).to_broadcast([128, 8, 16, 16])),
                 b3(s.V1, s.V1.base[:, :, 1, :].unsqueeze(2).to_broadcast([128, 8, 16, 16])), ALU.add)
            yield
            for h in range(8):
                top16((s.CS[:, h, 0:8], s.CS[:, h, 8:16]), (s.POS[:, h, 0:8], s.POS[:, h, 8:16]), s.cand[:, h, :])
                yield
            k.op("dve", lambda e: e.tensor_single_scalar(s.AB[:, 0, :, :].ap, s.POS[:].ap, 4, ALU.logical_shift_right), r=[s.POS[:]], w=[s.AB[:]])
            k.op("dve", lambda e: e.tensor_single_scalar(s.AB[:, 1, :, :].ap, s.POS[:].ap, 15, ALU.bitwise_and), r=[s.POS[:]], w=[s.AB[:]])
            k.copy(s.ABf[:], s.AB[:])
            k.copy(s.I1f[:], s.I1[:])
            yield
            E4 = b3(s.cand, s.cand.base[:, :, :].rearrange("p h (a c) -> p h a c", a=16))
            io = b3(iota16, iota16.base[:, :].unsqueeze(1).unsqueeze(1).to_broadcast([128, 8, 16, 16]))
            for s_ in range(2):
                k.tt(E4, b3(s.ABf, s.ABf.base[:, s_, :, :].unsqueeze(3).to_broadcast([128, 8, 16, 16])), io, ALU.is_equal)
                k.tt(E4, E4, b3(s.I1f, s.I1f.base[:, :, s_, :].unsqueeze(2).to_broadcast([128, 8, 16, 16])), ALU.mult)
                k.reduce(s.isel[:, s_, :, :], E4, ALU.add)
                yield
            k.stt(s.isel[:, 0, :, :], s.isel[:, 0, :, :], 128.0, s.isel[:, 1, :, :], ALU.mult, ALU.add)
            k.copy(b3(s.EI, s.EI.base[:, :].rearrange("p (h c) -> p h c", h=8)), s.isel[:, 0, :, :])
            k.tt(s.gsm[:], s.CS[:], b3(s.CS, s.CS.base[:, :, 0:1].to_broadcast([128, 8, 16])), ALU.subtract)
            k.act(s.gsm[:], s.gsm[:], AF.Exp)
            k.reduce(s.zs[:], s.gsm[:], ALU.add)
            k.recip(s.zs[:], s.zs[:])
            k.tt(s.gsm[:], s.gsm[:], b3(s.zs, s.zs.base[:, :].unsqueeze(2).to_broadcast([128, 8, 16])), ALU.mult)
            yield

        def back(s, nxt_gen):
            acc = accp.nxt()
            pacc = (psB.nxt(), psB.nxt())
            gflat = s.gsm.base[:, :, :].rearrange("p h c -> p (h c)")
            for g4 in range(32):
                bufs = []
                for c in range(4):
                    hk = g4 * 4 + c
                    ur = up.nxt()
                    bufs.append(ur)
                    k.dma(ur[:], uvb[l][:, :], q="pool", extra_r=[s.EI[:, hk:hk + 1]],
                          fn=lambda e, ur=ur, hk=hk: e.indirect_dma_start(out=ur[:].ap, out_offset=None, in_=uvb[l].base[:, :],
                                                                         in_offset=bass.IndirectOffsetOnAxis(ap=s.EI[:, hk:hk + 1].ap, axis=0),
                                                                         bounds_check=breg, oob_is_err=False))
                for c in range(4):
                    hk = g4 * 4 + c
                    jk = jkp.nxt()
                    k.stt(jk[:], bufs[c][:, 0:D], 1.0, s.h2[:], ALU.mult, ALU.mult, accum=s.dots[:, hk:hk + 1])
                sl = slice(g4 * 4, g4 * 4 + 4)
                k.act(s.wg[:, sl], s.dots[:, sl], AF.Gelu)
                k.tt(s.wg[:, sl], s.wg[:, sl], b3(s.gsm, gflat[:, sl]), ALU.mult)
                for c in range(4):
                    hk = g4 * 4 + c
                    dg = dgp.nxt()
                    k.act(dg[:], ident_bf[:], AF.Copy, scale=s.wg[:, hk:hk + 1])
                    k.mm(pacc[0][:, :], dg[:], bufs[c][:, D:D + 512], start=(hk == 0), stop=(hk == 127))
                    k.mm(pacc[1][:, :], dg[:], bufs[c][:, D + 512:2 * D], start=(hk == 0), stop=(hk == 127))
                if nxt_gen is not None:
                    next(nxt_gen, None)
            if nxt_gen is not None:
                for _ in nxt_gen:
                    pass
            for hf in range(2):
                k.tt(acc[:, hf * 512:(hf + 1) * 512], pacc[hf][:, :], grep[:, 1, s.col, hf * 512:(hf + 1) * 512], ALU.mult)
            k.tt(s.xt[:], s.xt[:], acc[:], ALU.add)
            k.dma(s.rv[0][s.rv[1]], s.xt[:], q="sp")

        tiles = [(b, ci) for b in range(NB) for ci in range(NKC) if (ci >= 2 or need_ctx)]
        g0 = front(sets[0], *tiles[0])
        for _ in g0:
            pass
        for ti in range(len(tiles)):
            nxt = front(sets[(ti + 1) % 2], *tiles[ti + 1]) if ti + 1 < len(tiles) else None
            back(sets[ti % 2], nxt)
        k.pop()

    breg_box = []

    def phase_final():
        k.push()
        fr = k.sb("fnr", [128, D], F32)
        k.dma(fr[:], RO(fnw.base.partition_broadcast(128)))
        xp = Pool(k, "fx", [128, D], F32, 3)
        yp = Pool(k, "fy", [128, D], F32, 3)
        for b in range(NB):
            for i in range(S // 128):
                xt = xp.nxt()
                k.dma(xt[:], xres[b, i * 128:(i + 1) * 128, :])
                ss = ssp.nxt()
                y = yp.nxt()
                k.act(y[:], xt[:], AF.Square, accum=ss[:, 0:1])
                k.act(ss[:, 1:2], ss[:, 0:1], AF.Sqrt, bias=EPS, scale=1.0 / D)
                k.recip(ss[:, 1:2], ss[:, 1:2])
                k.ts(y[:], xt[:], ss[:, 1:2], ALU.mult)
                k.tt(y[:], y[:], fr[:], ALU.mult, e="pool")
                k.dma(out_d.k((b, i))[b, i * 128:(i + 1) * 128, :], y[:], q="pool")
        k.pop()

    Rq32 = k.sb("Rq32", [32, 32], BF16)
    k.dma(Rq32[:], RO(c_Rq.base[0:32, 0:32]))

    outs = []
    for l in layers:
        phase_mod(l)
        load_layer_small(l)
        if stop == "mod":
            break
        phase_proj(l)
        if stop == "proj":
            break
        need_ctx = (l < NL - 1)
        phase_attn(l, need_ctx)
        if stop == "attn":
            break
        phase_na(l, need_ctx)
        if stop == "na":
            break
        phase_ret(l, need_ctx)
        if stop == "ret":
            break
        phase_merge(l, need_ctx)
        if stop == "merge":
            break
        phase_peer(l, need_ctx)
        if stop == "peer":
            break
    if stop is None or stop == "final":
        phase_final()

    k.finish()
    return k


_CONST_CACHE = {}


def host_shared(inp):
    f = lambda a: np.ascontiguousarray(np.asarray(a, dtype=np.float32))
    sh = {}
    if "c" not in _CONST_CACHE:
        c = {}
        c.update(rope_tables())
        c.update(misc_consts())
        _CONST_CACHE["c"] = c
    sh.update(_CONST_CACHE["c"])
    sh["mod_w"] = f(inp["mod_w"])
    sh["mod_b"] = f(inp["mod_b"])
    sh["mod_bT"] = f(np.asarray(inp["mod_b"]).reshape(NL, 48, 128).transpose(0, 2, 1))
    sh["n1wT"] = f(np.asarray(inp["norm1_w"]).reshape(NL, 8, 128).transpose(0, 2, 1))
    sh["n2wT"] = f(np.asarray(inp["norm2_w"]).reshape(NL, 8, 128).transpose(0, 2, 1))
    sh["w_in"] = f(inp["w_in"])
    sh["qnT"] = f(np.asarray(inp["mla_q_norm"]).reshape(NL, 2, 128).transpose(0, 2, 1))
    sh["kvnT"] = f(np.asarray(inp["mla_kv_norm"]).reshape(NL, 128, 1))
    sh["gqnT"] = f(np.tile(np.asarray(inp["gqa_q_norm"]), (1, 2)).reshape(NL, 128, 1))
    sh["gknT"] = f(np.tile(np.asarray(inp["gqa_k_norm"]), (1, 2)).reshape(NL, 128, 1))
    sh["mla_w_uq"] = f(inp["mla_w_uq"])
    sh["mla_w_ukv"] = f(inp["mla_w_ukv"])
    sh["nabm"] = na_biasmask(np.asarray(inp["na_bias"], dtype=np.float32))
    sh["ret_decay_logit"] = f(np.asarray(inp["ret_decay_logit"]).reshape(NL, 8))
    sh["ret_gn_w"] = f(inp["ret_gn_w"])
    sh["w_branch"] = f(inp["w_branch"])
    sh["w_out"] = f(inp["w_out"])
    sh["peer_w_q"] = f(inp["peer_w_q"])
    pk = np.asarray(inp["peer_keys"], dtype=np.float32)
    kb = np.zeros((NL, 8, 128, 256), np.float32)
    kb[:, :, 0:64, 0:128] = pk[:, :, 0].transpose(0, 1, 3, 2)
    kb[:, :, 64:128, 128:256] = pk[:, :, 1].transpose(0, 1, 3, 2)
    sh["keysbd"] = kb
    for i in range(NL):
        sh["peer_u%d" % i] = f(np.asarray(inp["peer_u"])[i])
        sh["peer_v%d" % i] = f(np.asarray(inp["peer_v"])[i])
    sh["final_norm_w"] = f(inp["final_norm_w"])
    return sh


def host_core(inp, b0, NB):
    x = np.asarray(inp["x"], dtype=np.float32)
    d = {}
    d["x"] = np.ascontiguousarray(x[b0:b0 + NB])
    d["ctx"] = np.ascontiguousarray(np.asarray(inp["ctx"], dtype=np.float32)[b0:b0 + NB])
    cc = np.concatenate([np.asarray(inp["c"], dtype=np.float32)[b0:b0 + NB], np.asarray(inp["c_ctx"], dtype=np.float32)[None, :]], 0)
    d["cT3"] = np.ascontiguousarray(cc.reshape(NB + 1, 8, 128).transpose(2, 1, 0))
    return d


_PROG = {}


def kernel(**inputs):
    NB = 2
    if "k" not in _PROG:
        _PROG["k"] = build(NB=NB)
    k = _PROG["k"]
    sh = host_shared(inputs)
    in_maps = []
    for c in range(NCORES):
        m = dict(sh)
        m.update(host_core(inputs, c * NB, NB))
        in_maps.append(m)
    res = run_bass_kernel_spmd(k.nc, in_maps, core_ids=list(range(NCORES)))
    out = np.concatenate([np.asarray(r["out"]) for r in res.results], axis=0)
    return np.ascontiguousarray(out.astype(np.float32))
```

```python
import ml_dtypes
from concourse.bass_utils import run_bass_kernel_spmd
import contextlib
import numpy as np
import concourse.bass as bass
import concourse.mybir as mybir

F32 = mybir.dt.float32
BF16 = mybir.dt.bfloat16
I32 = mybir.dt.int32
U32 = mybir.dt.uint32
ALU = mybir.AluOpType
AF = mybir.ActivationFunctionType
AX = mybir.AxisListType


class Dep:
    __slots__ = ("w", "r")

    def __init__(self):
        self.w = None
        self.r = []


class View:
    __slots__ = ("t", "key", "ap")

    def __init__(self, t, key, ap):
        self.t = t
        self.key = key
        self.ap = ap


class _Sub:
    def __init__(self, t, key):
        self.t = t
        self.key = key

    def __getitem__(self, idx):
        return View(self.t, self.key, self.t.base[idx])


class T:
    def __init__(self, name, base, space):
        self.name = name
        self.base = base
        self.space = space
        self.whole = Dep()
        self.subs = {}

    def __getitem__(self, idx):
        return View(self, None, self.base[idx])

    def k(self, key):
        return _Sub(self, key)

    def v(self, ap, key=None):
        return View(self, key, ap)

    def deps(self, key):
        if key is None:
            return [self.whole] + list(self.subs.values())
        if key not in self.subs:
            self.subs[key] = Dep()
        return [self.whole, self.subs[key]]

    def own(self, key):
        if key is None:
            return self.whole
        if key not in self.subs:
            self.subs[key] = Dep()
        return self.subs[key]


class K:
    ENG = ("pe", "act", "dve", "pool", "sp")

    def __init__(self, n_dma_sems=24):
        self.nc = bass.Bass("TRN2", target_bir_lowering=False)
        self.es = contextlib.ExitStack()
        self.stack = [self.es]
        nc = self.nc
        self.eng = {"pe": nc.tensor, "act": nc.scalar, "dve": nc.vector, "pool": nc.gpsimd, "sp": nc.sync}
        self.sems = []
        self.semval = []
        self.esem = {}
        for e in ("pe", "act", "dve", "pool"):
            self.esem[e] = self._newsem("s_" + e)
        self.dsem = {}
        self.dnext = {}
        for q in ("sp", "pool", "act"):
            n = n_dma_sems if q != "act" else 8
            self.dsem[q] = [self._newsem(f"d_{q}{i}") for i in range(n)]
            self.dnext[q] = 0
        self.known = {e: {} for e in self.ENG}
        self.n_inst = 0
        self.embed = True
        self.n_wait = 0
        self._uid = 0

    def _newsem(self, name):
        h = self.es.enter_context(self.nc.semaphore(name))
        self.sems.append(h)
        self.semval.append(0)
        return len(self.sems) - 1

    def uid(self, p):
        self._uid += 1
        return f"{p}{self._uid}"

    def sb(self, name, shape, dtype):
        h = self.stack[-1].enter_context(self.nc.sbuf_tensor(self.uid("s_" + name + "_"), list(shape), dtype))
        return T(name, h, "sb")

    def ps(self, name, shape, dtype=F32):
        h = self.stack[-1].enter_context(self.nc.psum_tensor(self.uid("p_" + name + "_"), list(shape), dtype))
        return T(name, h, "ps")

    def push(self):
        self.stack.append(contextlib.ExitStack())

    def pop(self):
        self.barrier()
        self.stack.pop().close()

    def barrier(self, engines=("pe", "act", "dve", "pool", "sp")):
        for e in engines:
            for s in range(len(self.sems)):
                if self.semval[s] > 0:
                    self._wait(e, (s, self.semval[s]))

    def dram(self, name, shape, dtype, kind="Internal"):
        h = self.nc.dram_tensor(name, list(shape), dtype, kind=kind)
        return T(name, h.ap(), "dram")

    def _wait(self, e, ev):
        if ev is None:
            return
        s, v = ev
        if self.known[e].get(s, 0) >= v:
            return
        self.eng[e].wait_ge(self.sems[s], v)
        self.known[e][s] = v
        self.n_wait += 1

    def _sync_in(self, e, r, w):
        need = {}

        def add(ev):
            if ev is None:
                return
            s, v = ev
            if e == "pe" and s == self.esem["pe"]:
                return
            if need.get(s, 0) < v:
                need[s] = v

        for v in r:
            for d in v.t.deps(v.key):
                add(d.w)
        for v in w:
            for d in v.t.deps(v.key):
                add(d.w)
                for rv in d.r:
                    add(rv)
        pend = [(s, v) for s, v in need.items() if self.known[e].get(s, 0) < v]
        if self.embed and pend:
            for ev in pend[:-1]:
                self._wait(e, ev)
            return pend[-1]
        for ev in pend:
            self._wait(e, ev)
        return None

    def _sync_out(self, ev, r, w):
        for v in r:
            v.t.own(v.key).r.append(ev)
            if len(v.t.own(v.key).r) > 64:
                mx = {}
                for s, val in v.t.own(v.key).r:
                    mx[s] = max(mx.get(s, 0), val)
                v.t.own(v.key).r = list(mx.items())
        for v in w:
            if v.key is None:
                v.t.subs = {}
            d = v.t.own(v.key)
            d.w = ev
            d.r = []

    def op(self, e, fn, r=(), w=()):
        r = [x for x in r if isinstance(x, View)]
        w = [x for x in w if isinstance(x, View)]
        emb = self._sync_in(e, r, w)
        ins = fn(self.eng[e])
        if emb is not None:
            ins._wait_ge(self.sems[emb[0]], emb[1])
            self.known[e][emb[0]] = emb[1]
        s = self.esem[e]
        self.semval[s] += 1
        ins.then_inc(self.sems[s], 1)
        ev = (s, self.semval[s])
        self._sync_out(ev, r, w)
        self.n_inst += 1
        return ins

    def dma(self, out, in_, q="sp", fn=None, extra_r=()):
        r = [in_] + list(extra_r)
        w = [out]
        pool = self.dsem[q]
        i = self.dnext[q]
        self.dnext[q] = (i + 1) % len(pool)
        s = pool[i]
        self._wait(q, (s, self.semval[s]))
        emb = self._sync_in(q, r, w)
        if fn is None:
            ins = self.eng[q].dma_start(out=out.ap, in_=in_.ap)
        else:
            ins = fn(self.eng[q])
        if emb is not None:
            ins._wait_ge(self.sems[emb[0]], emb[1])
            self.known[q][emb[0]] = emb[1]
        self.semval[s] += 16
        ins.then_inc(self.sems[s], 16)
        ev = (s, self.semval[s])
        self._sync_out(ev, r, w)
        self.n_inst += 1
        return ins

    def wait_all(self, e, views):
        for v in views:
            for d in v.t.deps(v.key):
                self._wait(e, d.w)

    def mm(self, out, lhsT, rhs, start=True, stop=True, **kw):
        return self.op("pe", lambda e: e.matmul(out.ap, lhsT.ap, rhs.ap, start=start, stop=stop, **kw),
                       r=[lhsT, rhs] + ([] if start else [out]), w=[out])

    def tr(self, out, in_, ident):
        return self.op("pe", lambda e: e.transpose(out.ap, in_.ap, ident.ap), r=[in_, ident], w=[out])

    def act(self, out, in_, func, bias=None, scale=None, accum=None, e="act"):
        kw = {}
        r = [in_]
        w = [out]
        if bias is not None:
            kw["bias"] = bias.ap if isinstance(bias, View) else bias
            r.append(bias)
        if scale is not None:
            kw["scale"] = scale.ap if isinstance(scale, View) else scale
            r.append(scale)
        if accum is not None:
            kw["accum_out"] = accum.ap
            w.append(accum)
        return self.op(e, lambda en: en.activation(out=out.ap, in_=in_.ap, func=func, **kw), r=r, w=w)

    def tt(self, out, a, b, op, e="dve"):
        return self.op(e, lambda en: en.tensor_tensor(out.ap, a.ap, b.ap, op), r=[a, b], w=[out])

    def ts(self, out, a, s1, op0, s2=None, op1=None, e="dve", accum=None):
        r = [a, s1, s2]
        w = [out]
        kw = {}
        if accum is not None:
            kw["accum_out"] = accum.ap
            w.append(accum)
        g = lambda s: s.ap if isinstance(s, View) else s
        if op1 is None:
            return self.op(e, lambda en: en.tensor_scalar(out.ap, a.ap, g(s1), None, op0, **kw), r=r, w=w)
        return self.op(e, lambda en: en.tensor_scalar(out.ap, a.ap, g(s1), g(s2), op0, op1, **kw), r=r, w=w)

    def stt(self, out, a, s, b, op0, op1, e="dve", accum=None):
        g = lambda x: x.ap if isinstance(x, View) else x
        w = [out]
        kw = {}
        if accum is not None:
            kw["accum_out"] = accum.ap
            w.append(accum)
        return self.op(e, lambda en: en.scalar_tensor_tensor(out.ap, a.ap, g(s), b.ap, op0, op1, **kw),
                       r=[a, s, b], w=w)

    def copy(self, out, in_, e="dve"):
        if e == "act":
            return self.op(e, lambda en: en.copy(out.ap, in_.ap), r=[in_], w=[out])
        return self.op(e, lambda en: en.tensor_copy(out.ap, in_.ap), r=[in_], w=[out])

    def recip(self, out, in_):
        return self.op("dve", lambda en: en.reciprocal(out.ap, in_.ap), r=[in_], w=[out])

    def memset(self, out, val, e="dve"):
        return self.op(e, lambda en: en.memset(out.ap, val), r=[], w=[out])

    def reduce(self, out, in_, op, axis=AX.X, e="dve"):
        return self.op(e, lambda en: en.tensor_reduce(out.ap, in_.ap, axis, op), r=[in_], w=[out])

    def finish(self):
        self.barrier(engines=("sp",))

D = 1024
S = 4096
LC = 256
TT = S + LC
NL = 2
GW = 64
EPS = 1e-6
INC = 7072
NCORES = 8
BF = ml_dtypes.bfloat16
import os
PEXP = os.environ.get('PEXP', '')


class Pool:
    def __init__(self, k, name, shape, dtype, n, space="sb"):
        mk = k.sb if space == "sb" else k.ps
        self.t = [mk(f"{name}{i}", shape, dtype) for i in range(n)]
        self.i = 0

    def nxt(self):
        t = self.t[self.i]
        self.i = (self.i + 1) % len(self.t)
        return t


def rope_tables():
    t = np.arange(S)
    row = (t // GW).astype(np.float32)
    col = (t % GW).astype(np.float32)

    def tab(hd):
        half = hd // 2
        h = half // 2
        freqs = (np.float32(10000.0) ** (-np.arange(h, dtype=np.float32) / np.float32(h))).astype(np.float32)
        C = np.zeros((hd, S), np.float32)
        Sn = np.zeros((hd, S), np.float32)
        for ax, pos in enumerate((row, col)):
            ang = (pos[:, None] * freqs[None, :]).astype(np.float32)
            c = np.cos(ang).astype(np.float32).T
            s = np.sin(ang).astype(np.float32).T
            o = ax * half
            C[o:o + h] = c
            C[o + h:o + 2 * h] = c
            Sn[o:o + h] = s
            Sn[o + h:o + 2 * h] = s
        return C, Sn

    def rmat(hd):
        half = hd // 2
        h = half // 2
        Rm = np.zeros((hd, hd), np.float32)
        for ax in range(2):
            o = ax * half
            for i in range(h):
                Rm[o + i, o + h + i] = -1.0
                Rm[o + h + i, o + i] = 1.0
        return Rm.T.copy()

    Cg, Sg = tab(64)
    Cm, Sm = tab(32)
    out = {}
    out["ropeCg"] = np.concatenate([Cg, Cg], 0)
    out["ropeSg"] = np.concatenate([Sg, Sg], 0)
    z = np.zeros((32, S), np.float32)
    out["ropeCm"] = np.concatenate([Cm, z, Cm], 0)
    out["ropeSm"] = np.concatenate([Sm, z, Sm], 0)
    Rg = np.zeros((128, 128), np.float32)
    Rg[0:64, 0:64] = rmat(64)
    Rg[64:128, 64:128] = rmat(64)
    Rq = np.zeros((96, 96), np.float32)
    Rq[64:96, 64:96] = rmat(32)
    Rq[0:32, 0:32] = rmat(32)
    out["Rg"] = Rg.astype(BF)
    out["Rq"] = Rq.astype(BF)
    return out


def misc_consts():
    out = {}
    out["ident_bf"] = np.eye(128, dtype=np.float32).astype(BF)
    out["ident_f"] = np.eye(128, dtype=np.float32)
    out["ones_f"] = np.ones((128, 128), np.float32)
    ob = np.zeros((128, 128), np.float32)
    ob[0:64, 0:64] = 1.0
    ob[64:, 64:] = 1.0
    out["onesblk_f"] = ob
    io = np.zeros((128, 8, 16, 16), np.float32)
    io[:] = np.arange(16, dtype=np.float32)[None, None, None, :]
    out["iota4"] = io
    m = np.arange(128)[:, None]
    n = np.arange(128)[None, :]
    Df = np.where(n >= m, (n - m), 100000).astype(np.float32)
    Db = np.where(n <= m, (m - n), 100000).astype(np.float32)
    out["retD"] = np.stack([Df, Db], 0)
    qa = np.zeros((2, 128, 128), np.float32)
    qa[0] = (np.arange(128) + 1)[None, :]
    qa[1] = (128 - np.arange(128))[None, :]
    out["retQA"] = qa
    ka = np.zeros((128, 2), np.float32)
    ka[:, 0] = 127 - np.arange(128)
    ka[:, 1] = np.arange(128)
    out["retKA"] = ka
    return out


def na_biasmask(na_bias):
    L = na_bias.shape[0]
    out = np.full((L, 4, 3, 8, 128, 512), -30000.0, np.float32)
    for pat, qg in enumerate((0, 3, 7)):
        r0 = 8 * qg
        kr0 = int(np.clip(r0 - 4, 0, 48))
        r = r0 + np.arange(8)
        rs = np.clip(r - 4, 0, 56)
        c = np.arange(64)
        cs = np.clip(c - 8, 0, 48)
        for kc in range(8):
            kr = kr0 + 2 * kc + np.arange(2)
            KR = kr[:, None, None, None]
            CP = c[None, :, None, None]
            R = r[None, None, :, None]
            RS = rs[None, None, :, None]
            C = c[None, None, None, :]
            CS = cs[None, None, None, :]
            valid = (KR >= RS) & (KR < RS + 8) & (CP >= CS) & (CP < CS + 16)
            rel_r = np.clip(KR - R + 7, 0, 14)
            rel_c = np.clip(CP - C + 15, 0, 30)
            rel_r, rel_c, valid = np.broadcast_arrays(rel_r, rel_c, valid)
            for l in range(L):
                for h in range(4):
                    vals = na_bias[l, h][rel_r, rel_c]
                    out[l, h, pat, kc] = np.where(valid, vals, np.float32(-30000.0)).reshape(128, 512)
    return out


def build(NB=2, layers=(0, 1), dbg=(), stop=None):
    k = K()
    nc = k.nc
    dbg = set(dbg)
    IN = lambda name, shape, dt=F32: k.dram(name, shape, dt, kind="ExternalInput")

    def SCR(name, shape, dt=BF16):
        return k.dram(name, shape, dt, kind=("ExternalOutput" if name in dbg else "Internal"))

    x_in = IN("x", [NB, S, D])
    ctx_in = IN("ctx", [NB, LC, D])
    cT3 = IN("cT3", [128, 8, NB + 1])
    mod_w = IN("mod_w", [NL, D, 6 * D])
    mod_bT = IN("mod_bT", [NL, 128, 48])
    mod_b = IN("mod_b", [NL, 6 * D])
    n1wT = IN("n1wT", [NL, 128, 8])
    n2wT = IN("n2wT", [NL, 128, 8])
    w_in = IN("w_in", [NL, D, INC])
    qnT = IN("qnT", [NL, 128, 2])
    kvnT = IN("kvnT", [NL, 128, 1])
    gqnT = IN("gqnT", [NL, 128, 1])
    gknT = IN("gknT", [NL, 128, 1])
    wuq = IN("mla_w_uq", [NL, 256, 384])
    wukv = IN("mla_w_ukv", [NL, 128, 512])
    nabm = IN("nabm", [NL, 4, 3, 8, 128, 512])
    rdl = IN("ret_decay_logit", [NL, 8])
    gnw = IN("ret_gn_w", [NL, 256])
    w_branch = IN("w_branch", [NL, 4, 256, D])
    w_out = IN("w_out", [NL, D, D])
    peer_wq = IN("peer_w_q", [NL, D, D])
    keysbd = IN("keysbd", [NL, 8, 128, 256])
    peer_u = [IN("peer_u%d" % i, [16384, D]) for i in range(NL)]
    peer_v = [IN("peer_v%d" % i, [16384, D]) for i in range(NL)]
    fnw = IN("final_norm_w", [D])
    c_ident_bf = IN("ident_bf", [128, 128], BF16)
    c_ident_f = IN("ident_f", [128, 128])
    c_ones_f = IN("ones_f", [128, 128])
    c_onesblk = IN("onesblk_f", [128, 128])
    c_iota4 = IN("iota4", [128, 8, 16, 16])
    c_retD = IN("retD", [2, 128, 128])
    c_retQA = IN("retQA", [2, 128, 128])
    c_retKA = IN("retKA", [128, 2])
    c_Cg = IN("ropeCg", [128, S])
    c_Sg = IN("ropeSg", [128, S])
    c_Cm = IN("ropeCm", [96, S])
    c_Sm = IN("ropeSm", [96, S])
    c_Rg = IN("Rg", [128, 128], BF16)
    c_Rq = IN("Rq", [96, 96], BF16)
    out_d = k.dram("out", [NB, S, D], F32, kind="ExternalOutput")

    xres = SCR("xres", [NB, S, D], F32)
    cres = SCR("cres", [NB, LC, D], F32)
    wb_in = SCR("wb_in", [NL, D, INC])
    wb_uq = SCR("wb_uq", [NL, 256, 384])
    wb_ukv = SCR("wb_ukv", [NL, 128, 512])
    wb_br = SCR("wb_br", [NL, 4, 256, D])
    wb_out = SCR("wb_out", [NL, D, D])
    wb_pq = SCR("wb_pq", [NL, D, D])
    wb_keys = SCR("wb_keys", [NL, 8, 128, 256])
    uvb = [SCR("uvb%d" % i, [16384, 2 * D]) for i in range(NL)]
    nabb = SCR("nabb", [NL, 4, 3, 8, 128, 512])
    mqT = SCR("mqT", [384, NB, TT])
    mknT = SCR("mknT", [256, NB, TT])
    mkpT = SCR("mkpT", [32, NB, TT])
    mv = SCR("mv", [NB, TT, 256])
    gqT = SCR("gqT", [256, NB, TT])
    gkT = SCR("gkT", [128, NB, TT])
    gv = SCR("gv", [NB, TT, 128])
    nqT = SCR("nqT", [256, NB, TT])
    nkT = SCR("nkT", [256, NB, TT])
    nv = SCR("nv", [NB, TT, 256])
    rqT = SCR("rqT", [256, NB, TT])
    rkT = SCR("rkT", [256, NB, TT])
    rkv = SCR("rkv", [NB, TT, 512])
    rg = SCR("rg", [NB, TT, 512])
    gT = SCR("gT", [4096, NB, TT])
    oT = SCR("oT", [1024, NB, TT])

    ident_bf = k.sb("ident_bf", [128, 128], BF16)
    ident_f = k.sb("ident_f", [128, 128], F32)
    ones_f = k.sb("ones_f", [128, 128], F32)
    onesblk = k.sb("onesblk", [128, 128], F32)
    Rg = k.sb("Rg", [128, 128], BF16)
    Rq = k.sb("Rq", [96, 96], BF16)
    for sbt, dr in ((ident_bf, c_ident_bf), (ident_f, c_ident_f), (ones_f, c_ones_f), (onesblk, c_onesblk), (Rg, c_Rg)):
        k.dma(sbt[:], dr[:, :])
    k.dma(Rq[:], c_Rq[:, :])

    psA = Pool(k, "psA", [128, 512], F32, 6, space="ps")
    psB = Pool(k, "psB", [128, 512], F32, 2, space="ps")
    P = psA.nxt

    def bfview(pt, a, b, p0=0, p1=128):
        return pt.v(pt.base.bitcast(BF16)[p0:p1, a:b])

    evn = [0]

    def evac(out, in_, scale=None, func=None):
        evn[0] += 1
        if func is not None:
            return k.act(out, in_, func, scale=scale)
        if evn[0] % 2 == 0:
            return k.act(out, in_, AF.Copy, scale=scale)
        if scale is None:
            return k.copy(out, in_)
        return k.ts(out, in_, scale, ALU.mult)

    CV = {}
    cvn = [0]

    def conv_pools(nbuf, engs):
        CV["f"] = Pool(k, "cvf", [128, 2048], F32, nbuf)
        CV["b"] = Pool(k, "cvb", [128, 2048], BF16, nbuf)
        CV["e"] = engs

    def conv_gen(l):
        def flat(ap, pat, **kw):
            return ap.rearrange(pat, **kw)
        yield from convert(flat(w_in.base[l], "(p a) c -> p (a c)", p=128), wb_in, flat(wb_in.base[l], "(p a) c -> p (a c)", p=128), 8 * INC)
        yield from convert(flat(wuq.base[l], "(p a) c -> p (a c)", p=128), wb_uq, flat(wb_uq.base[l], "(p a) c -> p (a c)", p=128), 2 * 384)
        yield from convert(wukv.base[l], wb_ukv, wb_ukv.base[l], 512)
        for i in range(4):
            yield from convert(flat(w_branch.base[l, i], "(p a) c -> p (a c)", p=128), wb_br, flat(wb_br.base[l, i], "(p a) c -> p (a c)", p=128), 2 * D)
        yield from convert(flat(w_out.base[l], "(p a) c -> p (a c)", p=128), wb_out, flat(wb_out.base[l], "(p a) c -> p (a c)", p=128), 8 * D)
        yield from convert(flat(peer_wq.base[l], "(p a) c -> p (a c)", p=128), wb_pq, flat(wb_pq.base[l], "(p a) c -> p (a c)", p=128), 8 * D)
        for h in range(8):
            yield from convert(keysbd.base[l, h], wb_keys, wb_keys.base[l, h], 256)
        for h in range(4):
            for t_ in range(3):
                for c in range(8):
                    yield from convert(nabm.base[l, h, t_, c], nabb, nabb.base[l, h, t_, c], 512)
        for a in range(128):
            yield from convert(peer_u[l].base.rearrange("(p a) c -> p a c", p=128)[:, a, :], uvb[l], uvb[l].base.rearrange("(p a) c -> p a c", p=128)[:, a, 0:D], D)
            yield from convert(peer_v[l].base.rearrange("(p a) c -> p a c", p=128)[:, a, :], uvb[l], uvb[l].base.rearrange("(p a) c -> p a c", p=128)[:, a, D:2 * D], D)

    ro = T("ro", None, "dram")

    def RO(ap):
        return View(ro, "r", ap)

    def convert(src_ap, dst_t, dst_ap, nper):
        for c0 in range(0, nper, 2048):
            n = min(2048, nper - c0)
            f = CV["f"].nxt()
            b = CV["b"].nxt()
            k.dma(f[:, 0:n], RO(src_ap[:, c0:c0 + n]))
            e = CV["e"][cvn[0] % len(CV["e"])]
            cvn[0] += 1
            k.copy(b[:, 0:n], f[:, 0:n], e=e)
            k.dma(dst_t.v(dst_ap[:, c0:c0 + n], key=("cv", cvn[0])), b[:, 0:n], q="pool")
            yield

    k.push()
    conv_pools(8, ("dve", "act"))
    for _ in conv_gen(layers[0]):
        pass
    k.pop()

    modF = k.sb("modF", [128, 48, NB + 1], F32)
    A1 = k.sb("A1", [128, 8, NB + 1], F32)
    A2 = k.sb("A2", [128, 8, NB + 1], F32)
    grep = k.sb("grep", [128, 2, NB + 1, D], F32)
    cT = k.sb("cT", [128, 8, NB + 1], F32)
    k.dma(cT[:], cT3[:, :, :])
    k.act(cT[:], cT[:], AF.Silu)
    mbT = k.sb("mbT", [128, 48], F32)
    nwT = k.sb("nwT", [128, 2, 8], F32)

    def phase_mod(l):
        k.push()
        mwp = Pool(k, "mw", [128, 8, 512], F32, 2)
        mbrep = k.sb("mbrep", [128, 512], F32)
        cTb = k.sb("cTb", [128, NB + 1, 8, 128], F32)
        for col in range(NB + 1):
            for j in range(8):
                k.copy(cTb[:, col, j, :], cT.v(cT.base[:, j, col:col + 1].to_broadcast([128, 128])))
        k.dma(mbT[:], mod_bT[l])
        k.dma(nwT[:, 0, :], n1wT[l])
        k.dma(nwT[:, 1, :], n2wT[l])
        mwv = mod_w.base[l].rearrange("(j p) c -> p j c", p=128)
        fm_blocks = {0: 0, 1: 4, 2: 8, 3: 12, 6: 24, 7: 28, 8: 32, 9: 36}
        g_blocks = {4: (0, 0), 5: (0, 512), 10: (1, 0), 11: (1, 512)}
        for cb in range(12):
            mw = mwp.nxt()
            k.dma(mw[:], RO(mwv[:, :, cb * 512:(cb + 1) * 512]))
            if cb in fm_blocks:
                for o4 in range(4):
                    oc = cb * 4 + o4
                    p = P()
                    for j in range(8):
                        k.mm(p[:, 0:NB + 1], mw[:, j, o4 * 128:(o4 + 1) * 128], cT[:, j, :], start=(j == 0), stop=(j == 7))
                    k.ts(modF[:, oc, :], p[:, 0:NB + 1], mbT[:, oc:oc + 1], ALU.add)
            else:
                gi, off = g_blocks[cb]
                k.dma(mbrep[:], RO(mod_b.base[l, cb * 512:(cb + 1) * 512].partition_broadcast(128)))
                for col in range(NB + 1):
                    p = P()
                    for j in range(8):
                        k.mm(p[:, :], cTb[:, col, j, :], mw[:, j, :], start=(j == 0), stop=(j == 7))
                    k.tt(grep[:, gi, col, off:off + 512], p[:, :], mbrep[:], ALU.add)
        for j in range(8):
            k.ts(A1[:, j, :], modF[:, 8 + j, :], 1.0, ALU.add)
            k.ts(A1[:, j, :], A1[:, j, :], nwT[:, 0, j:j + 1], ALU.mult)
            k.ts(A2[:, j, :], modF[:, 32 + j, :], 1.0, ALU.add)
            k.ts(A2[:, j, :], A2[:, j, :], nwT[:, 1, j:j + 1], ALU.mult)
        k.pop()

    NP = {}

    def norm_pools():
        NP["junk"] = Pool(k, "junk", [128, D], BF16, 2)
        NP["xn"] = Pool(k, "xn", [128, D], BF16, 2)
    ssp = Pool(k, "ss", [128, 2], F32, 4)

    def norm_T(xt, hxT, i, Am, Bm, col):
        ss = ssp.nxt()
        jk = NP["junk"].nxt()
        k.act(jk[:], xt[:], AF.Square, accum=ss[:, 0:1])
        k.act(ss[:, 1:2], ss[:, 0:1], AF.Sqrt, bias=EPS, scale=1.0 / D)
        k.recip(ss[:, 1:2], ss[:, 1:2])
        xn = NP["xn"].nxt()
        k.ts(xn[:], xt[:], ss[:, 1:2], ALU.mult)
        pt = P()
        for j in range(8):
            k.tr(bfview(pt, j * 128, (j + 1) * 128), xn[:, j * 128:(j + 1) * 128], ident_bf[:])
        for j in range(8):
            o = hxT[:, j, i * 128:(i + 1) * 128]
            src = bfview(pt, j * 128, (j + 1) * 128)
            if j % 2 == 0:
                k.act(o, src, AF.Identity, bias=Bm(j, col), scale=Am[:, j, col:col + 1])
            else:
                k.ts(o, src, Am[:, j, col:col + 1], ALU.mult, Bm(j, col), ALU.add)

    wuq_sb = k.sb("wuq_sb", [128, 2, 384], BF16)
    wkn_sb = k.sb("wkn_sb", [128, 4, 64], BF16)
    wkv_sb = k.sb("wkv_sb", [128, 4, 64], BF16)
    nrmw = k.sb("nrmw", [128, 5], F32)

    def fm_rstd(rsp, sqs, ones_t, nfeat, n):
        p = P()
        for c, sq in enumerate(sqs):
            k.mm(p[:, 0:n], ones_t[:], sq, start=(c == 0), stop=(c == len(sqs) - 1))
        rs = rsp.nxt()
        k.act(rs[:, 0:n], p[:, 0:n], AF.Sqrt, bias=EPS, scale=1.0 / nfeat)
        k.recip(rs[:, 0:n], rs[:, 0:n])
        return rs

    def load_layer_small(l):
        k.dma(wuq_sb[:], wb_uq.v(wb_uq.base[l].rearrange("(c p) m -> p c m", p=128)))
        wv = wb_ukv.base[l].rearrange("p (h t e) -> p h t e", h=4, t=2)
        k.dma(wkn_sb[:], wb_ukv.v(wv[:, :, 0, :]))
        k.dma(wkv_sb[:], wb_ukv.v(wv[:, :, 1, :]))
        k.dma(nrmw[:, 0:2], qnT[l])
        k.dma(nrmw[:, 2:3], kvnT[l])
        k.dma(nrmw[:, 3:4], gqnT[l])
        k.dma(nrmw[:, 4:5], gknT[l])

    def groups_for(l, with_ctx=True):
        gs = []
        for b in range(NB):
            gs.append(dict(b=b, t0=0, n=LC, lat=False, col=NB, s0=0))
            for g in range(S // 512):
                gs.append(dict(b=b, t0=LC + g * 512, n=512, lat=True, col=b, s0=g * 512))
        return gs

    def src_rows(l, g, i):
        b = g["b"]
        if g["lat"]:
            r0 = g["s0"] + i * 128
            return (RO(x_in.base[b, r0:r0 + 128, :]) if l == layers[0] and l == 0 else xres[b, r0:r0 + 128, :])
        r0 = i * 128
        return (RO(ctx_in.base[b, r0:r0 + 128, :]) if l == 0 else cres[b, r0:r0 + 128, :])

    def phase_proj(l):
        k.push()
        norm_pools()
        xpool = Pool(k, "xt", [128, D], F32, 3)
        hxTp = Pool(k, "hxT", [128, 8, 512], BF16, 2)
        wpool = Pool(k, "wblk", [128, 8, 512], BF16, 3)
        sqp = Pool(k, "sq", [128, 2, 512], F32, 2)
        rsp = Pool(k, "rs", [128, 512], F32, 2)
        nrm = Pool(k, "nrm", [128, 2, 512], BF16, 2)
        stg = Pool(k, "stg", [128, 512], BF16, 6)
        tmpf = Pool(k, "tmpf", [128, 512], F32, 4)
        ropeT = Pool(k, "ropeT", [128, 4, 512], F32, 2)
        wv = wb_in.base[l].rearrange("(j p) c -> p j c", p=128)
        blocks = [(0, 416), (416, 512), (928, 512), (1440, 512), (1952, 512), (2464, 512)] + [(2976 + 512 * i, 512) for i in range(8)]
        def norm_gen(g, box):
            n_, s0_ = g["n"], g["s0"]
            hx = hxTp.nxt()
            box["hxT"] = hx
            if g["lat"]:
                rt_ = ropeT.nxt()
                k.dma(rt_[:, 0, 0:n_], RO(c_Cg.base[:, s0_:s0_ + n_]))
                k.dma(rt_[:, 1, 0:n_], RO(c_Sg.base[:, s0_:s0_ + n_]))
                k.dma(rt_[0:96, 2, 0:n_], RO(c_Cm.base[:, s0_:s0_ + n_]))
                k.dma(rt_[0:96, 3, 0:n_], RO(c_Sm.base[:, s0_:s0_ + n_]))
                box["rt"] = rt_
            for i in range(n_ // 128):
                xt = xpool.nxt()
                k.dma(xt[:], src_rows(l, g, i))
                norm_T(xt, hx, i, A1, lambda j, c: modF[:, j, c:c + 1], g["col"])
                yield

        gs_all = groups_for(l)
        nbox = {}
        for _ in norm_gen(gs_all[0], nbox):
            pass
        for gi, g in enumerate(gs_all):
            b, t0, n, lat, col, s0 = g["b"], g["t0"], g["n"], g["lat"], g["col"], g["s0"]
            nt = n // 128
            hxT = nbox["hxT"]
            rt = nbox.get("rt")
            nbox = {}
            ngen = norm_gen(gs_all[gi + 1], nbox) if gi + 1 < len(gs_all) else iter(())

            def adv():
                next(ngen, None)

            def loadw(bi):
                c0, ncol = blocks[bi]
                wt = wpool.nxt()
                k.dma(wt[:, :, 0:ncol], wb_in.v(wv[:, :, c0:c0 + ncol]))
                return wt

            def fm(wt, c0, ncol):
                p = P()
                for j in range(8):
                    k.mm(p[0:ncol, 0:n], wt[:, j, c0:c0 + ncol], hxT[:, j, 0:n], start=(j == 0), stop=(j == 7))
                return p

            def tm(wt, c0, ncol, i):
                p = P()
                for j in range(8):
                    k.mm(p[:, 0:ncol], hxT[:, j, i * 128:(i + 1) * 128], wt[:, j, c0:c0 + ncol], start=(j == 0), stop=(j == 7))
                return p

            def store_fm(dst, r0, nr, src):
                k.dma(dst.k((b, t0, r0))[r0:r0 + nr, b, t0:t0 + n], src, q="pool")

            def store_tm(dst, i, src):
                k.dma(dst.k((b, t0, i))[b, t0 + i * 128:t0 + (i + 1) * 128, :], src, q="pool")

            def rope(xs, p0, p1, Rt, ci, si):
                R = Rt.base.shape[0]
                p2 = P()
                k.mm(p2[0:R, 0:n], Rt[:, :], xs[0:R, 0:n])
                t1 = tmpf.nxt()
                t2 = tmpf.nxt()
                k.tt(t1[p0:p1, 0:n], p2[p0:p1, 0:n], rt[p0:p1, si, 0:n], ALU.mult)
                k.tt(t2[p0:p1, 0:n], xs[p0:p1, 0:n], rt[p0:p1, ci, 0:n], ALU.mult)
                k.tt(xs[p0:p1, 0:n], t1[p0:p1, 0:n], t2[p0:p1, 0:n], ALU.add)

            w0 = loadw(0)
            w1 = loadw(1)
            pc = [fm(w0, 0, 128), fm(w0, 128, 128)]
            sq = sqp.nxt()
            for c in range(2):
                k.act(sq[:, c, 0:n], pc[c][:, 0:n], AF.Square)
            rs = fm_rstd(rsp, [sq[:, 0, 0:n], sq[:, 1, 0:n]], ones_f, 256, n)
            cqn = nrm.nxt()
            for c in range(2):
                k.stt(cqn[:, c, 0:n], pc[c][:, 0:n], nrmw[:, c:c + 1], rs[:, 0:n], ALU.mult, ALU.mult)
            for h in range(4):
                p = P()
                for c in range(2):
                    k.mm(p[0:96, 0:n], wuq_sb[:, c, h * 96:(h + 1) * 96], cqn[:, c, 0:n], start=(c == 0), stop=(c == 1))
                qa = stg.nxt()
                evac(qa[0:96, 0:n], p[0:96, 0:n])
                if lat:
                    rope(qa, 64, 96, Rq, 2, 3)
                store_fm(mqT, h * 96, 96, qa[0:96, 0:n])
            pk = fm(w0, 256, 128)
            sq = sqp.nxt()
            k.act(sq[:, 0, 0:n], pk[:, 0:n], AF.Square)
            rs = fm_rstd(rsp, [sq[:, 0, 0:n]], ones_f, 128, n)
            ckn = nrm.nxt()
            k.stt(ckn[:, 0, 0:n], pk[:, 0:n], nrmw[:, 2:3], rs[:, 0:n], ALU.mult, ALU.mult)
            for h in range(4):
                p = P()
                k.mm(p[0:64, 0:n], wkn_sb[:, h, :], ckn[:, 0, 0:n])
                kn = stg.nxt()
                evac(kn[0:64, 0:n], p[0:64, 0:n])
                store_fm(mknT, h * 64, 64, kn[0:64, 0:n])
            for i in range(nt):
                p = P()
                k.mm(p[:, 0:256], ckn[:, 0, i * 128:(i + 1) * 128], wkv_sb.v(wkv_sb.base[:, :, :].rearrange("p h e -> p (h e)")))
                vt = stg.nxt()
                evac(vt[:, 0:256], p[:, 0:256])
                store_tm(mv, i, vt[:, 0:256])
            pp = fm(w0, 384, 32)
            kp = stg.nxt()
            evac(kp[0:32, 0:n], pp[0:32, 0:n])
            if lat:
                rope(kp, 0, 32, Rq.v(Rq.base[0:32, 0:32]) if False else Rq32, 2, 3)
            store_fm(mkpT, 0, 32, kp[0:32, 0:n])
            w2 = loadw(2)
            for (c0, dst, r0, nw) in ((0, gqT, 0, 3), (128, gqT, 128, 3), (256, gkT, 0, 4)):
                p = fm(w1, c0, 128)
                sq = sqp.nxt()
                k.act(sq[:, 0, 0:n], p[:, 0:n], AF.Square)
                rs = fm_rstd(rsp, [sq[:, 0, 0:n]], onesblk, 64, n)
                xo = stg.nxt()
                k.stt(xo[:, 0:n], p[:, 0:n], nrmw[:, nw:nw + 1], rs[:, 0:n], ALU.mult, ALU.mult)
                if lat:
                    rope(xo, 0, 128, Rg, 0, 1)
                store_fm(dst, r0, 128, xo[:, 0:n])
            for i in range(nt):
                p = tm(w1, 384, 128, i)
                vt = stg.nxt()
                evac(vt[:, 0:128], p[:, 0:128])
                store_tm(gv, i, vt[:, 0:128])
            adv()
            w3 = loadw(3)
            for c in range(4):
                p = fm(w2, c * 128, 128)
                xo = stg.nxt()
                evac(xo[:, 0:n], p[:, 0:n], scale=(0.125 if c < 2 else None))
                store_fm(nqT if c < 2 else nkT, (c % 2) * 128, 128, xo[:, 0:n])
            adv()
            w4 = loadw(4)
            for i in range(nt):
                p = tm(w3, 0, 256, i)
                vt = stg.nxt()
                evac(vt[:, 0:256], p[:, 0:256])
                store_tm(nv, i, vt[:, 0:256])
            for c in range(2):
                p = fm(w3, 256 + c * 128, 128)
                xo = stg.nxt()
                evac(xo[:, 0:n], p[:, 0:n])
                store_fm(rqT, c * 128, 128, xo[:, 0:n])
            adv()
            w5 = loadw(5)
            for c in range(2):
                p = fm(w4, c * 128, 128)
                xo = stg.nxt()
                evac(xo[:, 0:n], p[:, 0:n], scale=0.125)
                store_fm(rkT, c * 128, 128, xo[:, 0:n])
            for i in range(nt):
                p = tm(w4, 0, 512, i)
                vt = stg.nxt()
                evac(vt[:, 0:256], p[:, 0:256], scale=0.125)
                evac(vt[:, 256:512], p[:, 256:512])
                store_tm(rkv, i, vt[:, 0:512])
            adv()
            wn = loadw(6)
            for i in range(nt):
                p = tm(w5, 0, 512, i)
                vt = stg.nxt()
                evac(vt[:, 0:512], p[:, 0:512])
                store_tm(rg, i, vt[:, 0:512])
            for gb in range(8):
                wc = wn
                if gb < 7:
                    wn = loadw(7 + gb)
                for c in range(4):
                    p = fm(wc, c * 128, 128)
                    xo = stg.nxt()
                    k.act(xo[:, 0:n], p[:, 0:n], AF.Sigmoid)
                    store_fm(gT, gb * 512 + c * 128, 128, xo[:, 0:n])
            for _ in ngen:
                pass
        k.pop()

    pend_epi = []

    def attn_flush():
        while pend_epi:
            pend_epi.pop(0)()

    def attn_core(pools, KT, dk, V, kchunks, q_src, nq, scale, o_dst, bias_fn=None):
        qpool, ptp, osp, obp = pools
        qt = q_src
        po = psB.nxt()
        n_ = len(kchunks)
        pss = {}

        def qk(ci):
            kc = kchunks[ci]
            ps = P()
            bm = bias_fn(kc) if bias_fn is not None else None
            k.mm(ps[:, 0:nq], KT[0:dk, kc * 128:(kc + 1) * 128], qt[0:dk, 0:nq], start=True, stop=(bm is None))
            if bm is not None:
                k.mm(ps[:, 0:nq], ident_bf[:], bm, start=False, stop=True)
            pss[ci] = ps

        qk(0)
        if n_ > 1:
            qk(1)
        attn_flush()
        for ci in range(n_):
            pt = ptp.nxt()
            k.act(pt[:, 0:nq], pss.pop(ci)[:, 0:nq], AF.Exp, scale=scale)
            if ci + 2 < n_:
                qk(ci + 2)
            k.mm(po[0:65, 0:nq], V[:, kchunks[ci], 0:65], pt[:, 0:nq], start=(ci == 0), stop=(ci == n_ - 1))

        def epi():
            os_ = osp.nxt()
            k.copy(os_[0:65, 0:nq], po[0:65, 0:nq])
            k.recip(os_[64:65, 0:nq], os_[64:65, 0:nq])
            pb = P()
            k.mm(pb[0:64, 0:nq], ones_f[64:65, 0:64], os_[64:65, 0:nq])
            ob = obp.nxt()
            k.tt(ob[0:64, 0:nq], os_[0:64, 0:nq], pb[0:64, 0:nq], ALU.mult)
            k.dma(o_dst, ob[0:64, 0:nq], q="pool")
        pend_epi.append(epi)

    def attn_pools():
        qpool = Pool(k, "aq", [96, 512], BF16, 2)
        qpoolB = Pool(k, "aqB", [128, 512], BF16, 2)
        for t_ in qpoolB.t:
            k.memset(t_[:], 0.0)
        qpool.padded = qpoolB
        ptp = Pool(k, "apt", [128, 512], BF16, 4)
        osp = Pool(k, "aos", [65, 512], F32, 2)
        obp = Pool(k, "aob", [64, 512], BF16, 2)
        return (qpool, ptp, osp, obp)

    NKC = TT // 128

    def load_V(Vt, src_t, b, c0):
        k.dma(Vt[:, :, 0:64], src_t.v(src_t.base[b, :, c0:c0 + 64].rearrange("(c p) e -> p c e", p=128)))

    def run_attn_jobs(pools, jobs, KTp, Vp, bmp=None, filler=None, nfill=0):
        qpool = pools[0]
        kvbuf = {}

        def ensure_kv(key, loader):
            if key not in kvbuf:
                KT, Vt = (KTp[1] if key[0] == "g" else KTp[0]).nxt() if isinstance(KTp, tuple) else KTp.nxt(), Vp.nxt()
                loader(KT, Vt)
                kvbuf[key] = (KT, Vt)

        def prefetch(j):
            qt = (qpool.padded if j.get("pad") else qpool).nxt()
            k.dma(qt[0:j["dk"], 0:j["nq"]], j["qsrc"])
            j["qt"] = qt
            if j.get("bmsrc") is not None:
                bm = bmp.nxt()
                k.dma(bm[:], j["bmsrc"])
                j["bm"] = bm

        ensure_kv(jobs[0]["kv"], jobs[0]["kvload"])
        prefetch(jobs[0])
        for i, j in enumerate(jobs):
            if i + 1 < len(jobs):
                prefetch(jobs[i + 1])
            if i == 0 or jobs[i - 1]["kv"] != j["kv"]:
                for j2 in jobs[i + 1:]:
                    if j2["kv"] != j["kv"]:
                        ensure_kv(j2["kv"], j2["kvload"])
                        break
                for key in [kk for kk in kvbuf if kk != j["kv"] and all(kk != j2["kv"] for j2 in jobs[i:])]:
                    del kvbuf[key]
            KT, Vt = kvbuf[j["kv"]]
            bfn = (j["bias"](j["bm"]) if j.get("bias") is not None else None)
            attn_core(pools, KT, (128 if j.get("pad") else j["dk"]), Vt, j["chunks"], j["qt"], j["nq"], j["scale"], j["odst"], bias_fn=bfn)
            if filler is not None:
                for _ in range(nfill):
                    next(filler, None)
        attn_flush()
        if filler is not None:
            for _ in filler:
                pass

    def phase_attn(l, need_ctx):
        k.push()
        pools = attn_pools()
        KTp = Pool(k, "aKT", [96, TT], BF16, 2)
        KTg = Pool(k, "aKTg", [128, TT], BF16, 2)
        for t_ in KTg.t:
            k.memset(t_[64:128, :], 0.0)
        Vp = Pool(k, "aV", [128, NKC, 65], BF16, 2)
        for Vt in Vp.t:
            k.memset(Vt[:, :, 64:65], 1.0)
        allk = list(range(NKC))
        jobs = []
        for b in range(NB):
            for h in range(4):
                def kvl(KT, Vt, b=b, h=h):
                    k.dma(KT[0:64, :], mknT[h * 64:(h + 1) * 64, b, :])
                    k.dma(KT[64:96, :], mkpT[:, b, :])
                    load_V(Vt, mv, b, h * 64)
                for qg in range(8):
                    t0 = LC + qg * 512
                    jobs.append(dict(kv=("m", b, h), kvload=kvl, dk=96, chunks=allk, qsrc=mqT[h * 96:(h + 1) * 96, b, t0:t0 + 512], nq=512,
                                     scale=96 ** -0.5, odst=oT.k((b, t0, h))[h * 64:(h + 1) * 64, b, t0:t0 + 512]))
                if need_ctx:
                    jobs.append(dict(kv=("m", b, h), kvload=kvl, dk=96, chunks=[0, 1], qsrc=mqT[h * 96:(h + 1) * 96, b, 0:LC], nq=LC,
                                     scale=96 ** -0.5, odst=oT.k((b, 0, h))[h * 64:(h + 1) * 64, b, 0:LC]))
            for kv in range(2):
                def kvl(KT, Vt, b=b, kv=kv):
                    k.dma(KT[0:64, :], gkT[kv * 64:(kv + 1) * 64, b, :])
                    load_V(Vt, gv, b, kv * 64)
                for gq in range(2):
                    h = kv * 2 + gq
                    for qg in range(8):
                        t0 = LC + qg * 512
                        jobs.append(dict(kv=("g", b, kv), kvload=kvl, pad=True, dk=64, chunks=allk, qsrc=gqT[h * 64:(h + 1) * 64, b, t0:t0 + 512], nq=512,
                                         scale=0.125, odst=oT.k((b, t0, 4 + h))[256 + h * 64:256 + (h + 1) * 64, b, t0:t0 + 512]))
                    if need_ctx:
                        jobs.append(dict(kv=("g", b, kv), kvload=kvl, pad=True, dk=64, chunks=[0, 1], qsrc=gqT[h * 64:(h + 1) * 64, b, 0:LC], nq=LC,
                                         scale=0.125, odst=oT.k((b, 0, 4 + h))[256 + h * 64:256 + (h + 1) * 64, b, 0:LC]))
        filler = None
        nxt_l = [x for x in layers if x > l]
        if nxt_l:
            conv_pools(6, ("dve",))
            filler = conv_gen(nxt_l[0])
        run_attn_jobs(pools, jobs, (KTp, KTg), Vp, filler=filler, nfill=5)
        k.pop()

    def phase_na(l, need_ctx):
        k.push()
        pools = attn_pools()
        KTp = Pool(k, "nKT", [128, TT], BF16, 2)
        for t_ in KTp.t:
            k.memset(t_[64:128, :], 0.0)
        Vp = Pool(k, "nV", [128, NKC, 65], BF16, 2)
        bmp = Pool(k, "nbm", [128, 8, 512], BF16, 3)
        for Vt in Vp.t:
            k.memset(Vt[:, :, 64:65], 1.0)
        jobs = []
        for b in range(NB):
            for h in range(4):
                def kvl(KT, Vt, b=b, h=h):
                    k.dma(KT[0:64, :], nkT[h * 64:(h + 1) * 64, b, :])
                    load_V(Vt, nv, b, h * 64)
                for qg in range(8):
                    t0 = LC + qg * 512
                    pat = 0 if qg == 0 else (2 if qg == 7 else 1)
                    kr0 = min(max(8 * qg - 4, 0), 48)
                    kc0 = 2 + kr0 // 2
                    jobs.append(dict(kv=("n", b, h), kvload=kvl, pad=True, dk=64, chunks=[0, 1] + [kc0 + j for j in range(8)],
                                     qsrc=nqT[h * 64:(h + 1) * 64, b, t0:t0 + 512], nq=512, scale=1.0,
                                     odst=oT.k((b, t0, 8 + h))[512 + h * 64:512 + (h + 1) * 64, b, t0:t0 + 512],
                                     bmsrc=nabb.v(nabb.base[l, h, pat].rearrange("c p q -> p c q")),
                                     bias=lambda bm, kc0=kc0: (lambda kc: (bm[:, kc - kc0, :] if kc >= 2 else None))))
                if need_ctx:
                    jobs.append(dict(kv=("n", b, h), kvload=kvl, pad=True, dk=64, chunks=[0, 1], qsrc=nqT[h * 64:(h + 1) * 64, b, 0:LC], nq=LC, scale=1.0,
                                     odst=oT.k((b, 0, 8 + h))[512 + h * 64:512 + (h + 1) * 64, b, 0:LC]))
        run_attn_jobs(pools, jobs, KTp, Vp, bmp)
        k.pop()

    def phase_ret(l, need_ctx):
        k.push()
        lg = k.sb("lg", [128, 8], F32)
        k.dma(lg[:], RO(rdl.base[l].partition_broadcast(128)))
        k.act(lg[:], lg[:], AF.Exp, scale=-1.0)
        k.ts(lg[:], lg[:], 1.0, ALU.add)
        k.act(lg[:], lg[:], AF.Ln)
        k.ts(lg[:], lg[:], -1.0, ALU.mult)
        Dd = k.sb("Dd", [128, 2, 128], F32)
        QA = k.sb("QA", [128, 2, 128], F32)
        KA = k.sb("KA", [128, 2], F32)
        k.dma(Dd[:], RO(c_retD.base.rearrange("d p n -> p d n")))
        k.dma(QA[:], RO(c_retQA.base.rearrange("d p n -> p d n")))
        k.dma(KA[:], RO(c_retKA.base))
        decT = k.sb("decT", [128, 2, 4, 128], F32)
        qdec = k.sb("qdec", [128, 2, 4, 128], F32)
        kdec = k.sb("kdec", [128, 2, 4], F32)
        cdec = k.sb("cdec", [128, 8], F32)
        gnr = k.sb("gnr", [128, 256], F32)
        k.dma(gnr[:], RO(gnw.base[l].partition_broadcast(128)))
        for d in range(2):
            for h in range(4):
                k.act(decT[:, d, h, :], Dd[:, d, :], AF.Exp, scale=lg[:, d * 4 + h:d * 4 + h + 1])
                k.act(qdec[:, d, h, :], QA[:, d, :], AF.Exp, scale=lg[:, d * 4 + h:d * 4 + h + 1])
            k.act(kdec[:, d, :], lg[:, d * 4:(d + 1) * 4], AF.Exp, scale=KA[:, d:d + 1])
        k.act(cdec[:], lg[:], AF.Exp, scale=128.0)
        QT = k.sb("rQT", [128, 2, TT], BF16)
        KT = k.sb("rKT", [128, 2, TT], BF16)
        KVt = k.sb("rKV", [128, NKC, 512], BF16)
        G = k.sb("rG", [128, NKC, 512], BF16)
        Ys = [k.sb("rY", [128, NKC, 256], F32), k.sb("rYb", [128, NKC, 256], BF16)]
        Sts = [k.sb("rS", [128, 2, 64], F32) for _ in range(2)]
        Sbs = [k.sb("rSb", [128, 2, 64], BF16) for _ in range(2)]
        kdp = Pool(k, "rkd", [128, 256], BF16, 2)
        idp = Pool(k, "rid", [128, 128], BF16, 3)
        qdp = Pool(k, "rqd", [128, 128], BF16, 3)
        osb = Pool(k, "ros", [128, 256], F32, 2)
        sgp = Pool(k, "rsg", [128, 256], F32, 2)
        stp = Pool(k, "rst", [128, 4], F32, 4)
        ybp = Pool(k, "ryb", [128, 256], BF16, 2)
        ysp = Pool(k, "rys", [128, 2, 128], BF16, 2)
        b3 = lambda t_, ap: t_.v(ap)
        for b in range(NB):
            for c in range(2):
                k.dma(QT[:, c, :], rqT[c * 128:(c + 1) * 128, b, :])
                k.dma(KT[:, c, :], rkT[c * 128:(c + 1) * 128, b, :])
            k.dma(KVt[:], rkv.v(rkv.base[b].rearrange("(c p) f -> p c f", p=128)))
            k.dma(G[:], rg.v(rg.base[b].rearrange("(c p) f -> p c f", p=128)))
            for d in range(2):
                k.memset(Sts[d][:], 0.0)
                k.memset(Sbs[d][:], 0.0)
            orders = [list(range(NKC)), [1, 0] + list(range(NKC - 1, 1, -1))]
            for step in range(2 * NKC):
                for _once in (0,):
                    d = step % 2
                    ci = orders[d][step // 2]
                    St, Sb, Y = Sts[d], Sbs[d], Ys[d]
                    tk = slice(ci * 128, (ci + 1) * 128)
                    Kd = kdp.nxt()
                    k.tt(b3(Kd, Kd.base[:, :].rearrange("p (h e) -> p h e", h=4)),
                         b3(KVt, KVt.base[:, ci, 0:256].rearrange("p (h e) -> p h e", h=4)),
                         b3(kdec, kdec.base[:, d, :].unsqueeze(2).to_broadcast([128, 4, 64])), ALU.mult)
                    pso = psB.nxt()
                    for h in range(4):
                        c2, po = h // 2, (h % 2) * 64
                        ps1 = P()
                        k.mm(ps1[:, 0:128], KT[po:po + 64, c2, tk], QT[po:po + 64, c2, tk])
                        idT = idp.nxt()
                        k.tt(idT[:], ps1[:, 0:128], decT[:, d, h, :], ALU.mult)
                        Qd = qdp.nxt()
                        k.tt(Qd[po:po + 64, :], QT[po:po + 64, c2, tk], qdec[po:po + 64, d, h, :], ALU.mult)
                        k.mm(pso[:, h * 64:(h + 1) * 64], idT[:], KVt[:, ci, 256 + h * 64:256 + (h + 1) * 64], start=True, stop=False)
                        k.mm(pso[:, h * 64:(h + 1) * 64], Qd[po:po + 64, :], Sb[po:po + 64, c2, :], start=False, stop=True)
                    for c2 in range(2):
                        ps2 = P()
                        k.mm(ps2[:, 0:128], Kd[:, c2 * 128:(c2 + 1) * 128], KVt[:, ci, 256 + c2 * 128:256 + (c2 + 1) * 128])
                        for hh in range(2):
                            po = hh * 64
                            h = c2 * 2 + hh
                            k.stt(St[po:po + 64, c2, :], St[po:po + 64, c2, :], cdec[po:po + 64, d * 4 + h:d * 4 + h + 1],
                                  ps2[po:po + 64, po:po + 64], ALU.mult, ALU.add)
                    k.copy(Sb[:], St[:])
                    if ci < 2 and not need_ctx:
                        continue
                    o = osb.nxt()
                    o3 = b3(o, o.base[:, :].rearrange("p (h e) -> p h e", h=4))
                    k.copy(o[:], pso[:, 0:256])
                    st = stp.nxt()
                    k.reduce(st[:], o3, ALU.add)
                    k.ts(st[:], st[:], 1.0 / 64, ALU.mult)
                    k.tt(o3, o3, b3(st, st.base[:, :].unsqueeze(2).to_broadcast([128, 4, 64])), ALU.subtract)
                    sq = sgp.nxt()
                    k.tt(sq[:], o[:], o[:], ALU.mult)
                    st2 = stp.nxt()
                    k.reduce(st2[:], b3(sq, sq.base[:, :].rearrange("p (h e) -> p h e", h=4)), ALU.add)
                    k.act(st2[:], st2[:], AF.Sqrt, bias=EPS, scale=1.0 / 64)
                    k.recip(st2[:], st2[:])
                    k.tt(o3, o3, b3(st2, st2.base[:, :].unsqueeze(2).to_broadcast([128, 4, 64])), ALU.mult)
                    k.tt(o[:], o[:], gnr[:], ALU.mult)
                    sg = sgp.nxt()
                    k.act(sg[:], G[:, ci, d * 256:(d + 1) * 256], AF.Silu)
                    k.tt(Y[:, ci, :], o[:], sg[:], ALU.mult)
            for ci in (range(NKC) if need_ctx else range(2, NKC)):
                yb = ybp.nxt()
                k.tt(yb[:], Ys[0][:, ci, :], Ys[1][:, ci, :], ALU.add)
                pt = P()
                for c2 in range(2):
                    k.tr(bfview(pt, c2 * 128, (c2 + 1) * 128), yb[:, c2 * 128:(c2 + 1) * 128], ident_bf[:])
                ys = ysp.nxt()
                k.copy(b3(ys, ys.base[:, :, :].rearrange("p c t -> p (c t)")), bfview(pt, 0, 256))
                for c2 in range(2):
                    k.dma(oT.k((b, ci, 12 + c2))[768 + c2 * 128:768 + (c2 + 1) * 128, b, ci * 128:(ci + 1) * 128], ys[:, c2, :], q="pool")
        k.pop()

    def phase_merge(l, need_ctx):
        k.push()
        wbr = k.sb("wbr", [128, 4, 2, D], BF16)
        wo = k.sb("wo", [128, 8, D], BF16)
        for i in range(4):
            k.dma(wbr[:, i, :, :], wb_br.v(wb_br.base[l, i].rearrange("(kk p) m -> p kk m", p=128)))
        k.dma(wo[:], wb_out.v(wb_out.base[l].rearrange("(j p) m -> p j m", p=128)))
        osp = Pool(k, "mo", [128, 8, 512], BF16, 2)
        mgl = Pool(k, "mgl", [128, 512], BF16, 6)
        accp = Pool(k, "macc", [128, 512], F32, 2)
        tmpp = Pool(k, "mtmp", [128, 512], F32, 2)
        accT = Pool(k, "maT", [128, 8, 512], BF16, 2)
        xp = Pool(k, "mx", [128, D], F32, 3)
        for g in groups_for(l):
            b, t0, n, lat, col = g["b"], g["t0"], g["n"], g["lat"], g["col"]
            if not lat and not need_ctx:
                continue
            nt = n // 128
            o_sb = osp.nxt()
            k.dma(o_sb[:, :, 0:n], oT.v(oT.base[:, b, t0:t0 + n].rearrange("(kk p) t -> p kk t", p=128)))
            aT = accT.nxt()
            for m in range(8):
                acc = accp.nxt()
                for i in range(4):
                    p = P()
                    for kk in range(2):
                        k.mm(p[:, 0:n], wbr[:, i, kk, m * 128:(m + 1) * 128], o_sb[:, 2 * i + kk, 0:n], start=(kk == 0), stop=(kk == 1))
                    gl = mgl.nxt()
                    k.dma(gl[:, 0:n], gT[i * 1024 + m * 128:i * 1024 + (m + 1) * 128, b, t0:t0 + n])
                    if i == 0:
                        k.tt(acc[:, 0:n], p[:, 0:n], gl[:, 0:n], ALU.mult)
                    else:
                        tp_ = tmpp.nxt()
                        k.tt(tp_[:, 0:n], p[:, 0:n], gl[:, 0:n], ALU.mult)
                        k.tt(acc[:, 0:n], acc[:, 0:n], tp_[:, 0:n], ALU.add, e="pool")
                k.copy(aT[:, m, 0:n], acc[:, 0:n], e="act")
            for i in range(nt):
                xt = xp.nxt()
                k.dma(xt[:], src_rows(l, g, i))
                for hf in range(2):
                    p = P()
                    for m in range(8):
                        k.mm(p[:, :], aT[:, m, i * 128:(i + 1) * 128], wo[:, m, hf * 512:(hf + 1) * 512], start=(m == 0), stop=(m == 7))
                    tp_ = tmpp.nxt()
                    k.tt(tp_[:], p[:, :], grep[:, 0, col, hf * 512:(hf + 1) * 512], ALU.mult)
                    k.tt(xt[:, hf * 512:(hf + 1) * 512], xt[:, hf * 512:(hf + 1) * 512], tp_[:], ALU.add, e="pool")
                dst = xres.k((b, g["s0"] + i * 128))[b, g["s0"] + i * 128:g["s0"] + (i + 1) * 128, :] if lat else cres.k((b, i))[b, i * 128:(i + 1) * 128, :]
                k.dma(dst, xt[:], q="pool")
        k.pop()

    def phase_peer(l, need_ctx):
        k.push()
        norm_pools()
        wq = k.sb("pwq", [128, 8, D], BF16)
        kb = k.sb("pkb", [128, 8, 256], BF16)
        iota16 = k.sb("piota", [128, 16], F32)
        k.dma(wq[:], wb_pq.v(wb_pq.base[l].rearrange("(j p) m -> p j m", p=128)))
        k.dma(kb[:], wb_keys.v(wb_keys.base[l].rearrange("h p c -> p h c")))
        k.dma(iota16[:], RO(c_iota4.base[:, 0, 0, :]))
        xp = Pool(k, "px", [128, D], F32, 2)
        hTp = Pool(k, "phT", [128, 8, 128], BF16, 2)
        qTp = Pool(k, "pqT", [128, 128], BF16, 3)
        wkp = Pool(k, "pwk", [128, 256], F32, 2)
        up = Pool(k, "pu", [128, 2 * D], BF16, 16)
        accp = Pool(k, "pacc", [128, D], F32, 1)
        jkp = Pool(k, "pjk", [128, D], BF16, 2)
        dgp = Pool(k, "pdg", [128, 128], BF16, 4)
        b3 = lambda t_, ap: t_.v(ap)
        B2 = lambda j, c: modF[:, 24 + j, c:c + 1]
        if not breg_box:
            breg_box.append(nc.gpsimd.to_reg(16383))
        breg = breg_box[0]

        class St:
            pass
        sets = []
        for i in range(2):
            s = St()
            s.h2 = k.sb("ph2", [128, D], BF16)
            s.s_sb = k.sb("ps_sb", [128, 8, 256], F32)
            s.V1 = k.sb("pV1", [128, 8, 2, 16], F32)
            s.I1 = k.sb("pI1", [128, 8, 2, 16], U32)
            s.I1f = k.sb("pI1f", [128, 8, 2, 16], F32)
            s.cand = k.sb("pcand", [128, 8, 256], F32)
            s.CS = k.sb("pCS", [128, 8, 16], F32)
            s.POS = k.sb("pPOS", [128, 8, 16], U32)
            s.AB = k.sb("pAB", [128, 2, 8, 16], U32)
            s.ABf = k.sb("pABf", [128, 2, 8, 16], F32)
            s.isel = k.sb("pisel", [128, 2, 8, 16], F32)
            s.EI = k.sb("pEI", [128, 128], I32)
            s.gsm = k.sb("pgsm", [128, 8, 16], F32)
            s.zs = k.sb("pzs", [128, 8], F32)
            s.dots = k.sb("pdots", [128, 128], F32)
            s.wg = k.sb("pwg", [128, 128], F32)
            sets.append(s)

        def top16(vals, idx, src):
            wk = wkp.nxt()
            n_ = src.ap.shape[-1]
            v0, v1 = vals
            i0_, i1_ = idx
            k.op("dve", lambda e: e.max(out=v0.ap, in_=src.ap), r=[src], w=[v0])
            k.op("dve", lambda e: e.max_index(out=i0_.ap, in_max=v0.ap, in_values=src.ap), r=[v0, src], w=[i0_])
            k.op("dve", lambda e: e.match_replace(out=wk[:, 0:n_].ap, in_to_replace=v0.ap, in_values=src.ap, imm_value=-1e30), r=[v0, src], w=[wk[:, 0:n_]])
            k.op("dve", lambda e: e.max(out=v1.ap, in_=wk[:, 0:n_].ap), r=[wk[:, 0:n_]], w=[v1])
            k.op("dve", lambda e: e.max_index(out=i1_.ap, in_max=v1.ap, in_values=wk[:, 0:n_].ap), r=[v1, wk[:, 0:n_]], w=[i1_])

        def front(s, b, ci):
            lat = ci >= 2
            s.col = b if lat else NB
            s.rv = (xres.k((b, (ci - 2) * 128)), (b, slice((ci - 2) * 128, (ci - 1) * 128), slice(None))) if lat else (cres.k((b, ci)), (b, slice(ci * 128, (ci + 1) * 128), slice(None)))
            s.xt = xp.nxt()
            k.dma(s.xt[:], s.rv[0][s.rv[1]])
            hT = hTp.nxt()
            norm_T(s.xt, hT, 0, A2, B2, s.col)
            yield
            pt = P()
            for j in range(8):
                k.tr(bfview(pt, j * 128, (j + 1) * 128), hT[:, j, :], ident_bf[:])
            k.copy(s.h2[:], bfview(pt, 0, 1024), e="act")
            yield
            for h in range(8):
                p = P()
                for j in range(8):
                    k.mm(p[:, 0:128], wq[:, j, h * 128:(h + 1) * 128], hT[:, j, :], start=(j == 0), stop=(j == 7))
                qT = qTp.nxt()
                k.copy(qT[:], p[:, 0:128], e="act")
                p2 = P()
                k.mm(p2[:, 0:256], qT[:], kb[:, h, :])
                k.copy(s.s_sb[:, h, :], p2[:, 0:256], e="act")
                yield
            for h in range(8):
                for p_ in range(2):
                    top16((s.V1[:, h, p_, 0:8], s.V1[:, h, p_, 8:16]), (s.I1[:, h, p_, 0:8], s.I1[:, h, p_, 8:16]), s.s_sb[:, h, p_ * 128:(p_ + 1) * 128])
                yield
            k.tt(b3(s.cand, s.cand.base[:, :, :].rearrange("p h (a c) -> p h a c", a=16)),
                 b3(s.V1, s.V1.base[:, :, 0, :].unsqueeze(3).to_broadcast([128, 8, 16, 16])),
                 b3(s.V1, s.V1.base[:, :, 1, :].unsqueeze(2).to_broadcast([128, 8, 16, 16])), ALU.add)
            yield
            for h in range(8):
                top16((s.CS[:, h, 0:8], s.CS[:, h, 8:16]), (s.POS[:, h, 0:8], s.POS[:, h, 8:16]), s.cand[:, h, :])
                yield
            k.op("dve", lambda e: e.tensor_single_scalar(s.AB[:, 0, :, :].ap, s.POS[:].ap, 4, ALU.logical_shift_right), r=[s.POS[:]], w=[s.AB[:]])
            k.op("dve", lambda e: e.tensor_single_scalar(s.AB[:, 1, :, :].ap, s.POS[:].ap, 15, ALU.bitwise_and), r=[s.POS[:]], w=[s.AB[:]])
            k.copy(s.ABf[:], s.AB[:])
            k.copy(s.I1f[:], s.I1[:])
            yield
            E4 = b3(s.cand, s.cand.base[:, :, :].rearrange("p h (a c) -> p h a c", a=16))
            io = b3(iota16, iota16.base[:, :].unsqueeze(1).unsqueeze(1).to_broadcast([128, 8, 16, 16]))
            for s_ in range(2):
                k.tt(E4, b3(s.ABf, s.ABf.base[:, s_, :, :].unsqueeze(3).to_broadcast([128, 8, 16, 16])), io, ALU.is_equal)
                k.tt(E4, E4, b3(s.I1f, s.I1f.base[:, :, s_, :].unsqueeze(2).to_broadcast([128, 8, 16, 16])), ALU.mult)
                k.reduce(s.isel[:, s_, :, :], E4, ALU.add)
                yield
            k.stt(s.isel[:, 0, :, :], s.isel[:, 0, :, :], 128.0, s.isel[:, 1, :, :], ALU.mult, ALU.add)
            k.copy(b3(s.EI, s.EI.base[:, :].rearrange("p (h c) -> p h c", h=8)), s.isel[:, 0, :, :])
            k.tt(s.gsm[:], s.CS[:], b3(s.CS, s.CS.base[:, :, 0:1].to_broadcast([128, 8, 16])), ALU.subtract)
            k.act(s.gsm[:], s.gsm[:], AF.Exp)
            k.reduce(s.zs[:], s.gsm[:], ALU.add)
            k.recip(s.zs[:], s.zs[:])
            k.tt(s.gsm[:], s.gsm[:], b3(s.zs, s.zs.base[:, :].unsqueeze(2).to_broadcast([128, 8, 16])), ALU.mult)
            yield

        def back(s, nxt_gen):
            acc = accp.nxt()
            pacc = (psB.nxt(), psB.nxt())
            gflat = s.gsm.base[:, :, :].rearrange("p h c -> p (h c)")
            for g4 in range(32):
                bufs = []
                for c in range(4):
                    hk = g4 * 4 + c
                    ur = up.nxt()
                    bufs.append(ur)
                    k.dma(ur[:], uvb[l][:, :], q="pool", extra_r=[s.EI[:, hk:hk + 1]],
                          fn=lambda e, ur=ur, hk=hk: e.indirect_dma_start(out=ur[:].ap, out_offset=None, in_=uvb[l].base[:, :],
                                                                         in_offset=bass.IndirectOffsetOnAxis(ap=s.EI[:, hk:hk + 1].ap, axis=0),
                                                                         bounds_check=breg, oob_is_err=False))
                for c in range(4):
                    hk = g4 * 4 + c
                    jk = jkp.nxt()
                    k.stt(jk[:], bufs[c][:, 0:D], 1.0, s.h2[:], ALU.mult, ALU.mult, accum=s.dots[:, hk:hk + 1])
                sl = slice(g4 * 4, g4 * 4 + 4)
                k.act(s.wg[:, sl], s.dots[:, sl], AF.Gelu)
                if nxt_gen is not None:
                    next(nxt_gen, None)
                k.tt(s.wg[:, sl], s.wg[:, sl], b3(s.gsm, gflat[:, sl]), ALU.mult)
                for c in range(4):
                    hk = g4 * 4 + c
                    dg = dgp.nxt()
                    k.act(dg[:], ident_bf[:], AF.Copy, scale=s.wg[:, hk:hk + 1])
                    k.mm(pacc[0][:, :], dg[:], bufs[c][:, D:D + 512], start=(hk == 0), stop=(hk == 127))
                    k.mm(pacc[1][:, :], dg[:], bufs[c][:, D + 512:2 * D], start=(hk == 0), stop=(hk == 127))
            if nxt_gen is not None:
                for _ in nxt_gen:
                    pass
            for hf in range(2):
                k.tt(acc[:, hf * 512:(hf + 1) * 512], pacc[hf][:, :], grep[:, 1, s.col, hf * 512:(hf + 1) * 512], ALU.mult)
            k.tt(s.xt[:], s.xt[:], acc[:], ALU.add)
            k.dma(s.rv[0][s.rv[1]], s.xt[:], q="sp")

        tiles = [(b, ci) for b in range(NB) for ci in range(NKC) if (ci >= 2 or need_ctx)]
        g0 = front(sets[0], *tiles[0])
        for _ in g0:
            pass
        for ti in range(len(tiles)):
            nxt = front(sets[(ti + 1) % 2], *tiles[ti + 1]) if ti + 1 < len(tiles) else None
            back(sets[ti % 2], nxt)
        k.pop()

    breg_box = []

    def phase_final():
        k.push()
        fr = k.sb("fnr", [128, D], F32)
        k.dma(fr[:], RO(fnw.base.partition_broadcast(128)))
        xp = Pool(k, "fx", [128, D], F32, 3)
        yp = Pool(k, "fy", [128, D], F32, 3)
        for b in range(NB):
            for i in range(S // 128):
                xt = xp.nxt()
                k.dma(xt[:], xres[b, i * 128:(i + 1) * 128, :])
                ss = ssp.nxt()
                y = yp.nxt()
                k.act(y[:], xt[:], AF.Square, accum=ss[:, 0:1])
                k.act(ss[:, 1:2], ss[:, 0:1], AF.Sqrt, bias=EPS, scale=1.0 / D)
                k.recip(ss[:, 1:2], ss[:, 1:2])
                k.ts(y[:], xt[:], ss[:, 1:2], ALU.mult)
                k.tt(y[:], y[:], fr[:], ALU.mult, e="pool")
                k.dma(out_d.k((b, i))[b, i * 128:(i + 1) * 128, :], y[:], q="pool")
        k.pop()

    Rq32 = k.sb("Rq32", [32, 32], BF16)
    k.dma(Rq32[:], RO(c_Rq.base[0:32, 0:32]))

    outs = []
    for l in layers:
        phase_mod(l)
        load_layer_small(l)
        if stop == "mod":
            break
        phase_proj(l)
        if stop == "proj":
            break
        need_ctx = (l < NL - 1)
        phase_attn(l, need_ctx)
        if stop == "attn":
            break
        phase_na(l, need_ctx)
        if stop == "na":
            break
        phase_ret(l, need_ctx)
        if stop == "ret":
            break
        phase_merge(l, need_ctx)
        if stop == "merge":
            break
        phase_peer(l, need_ctx)
        if stop == "peer":
            break
    if stop is None or stop == "final":
        phase_final()

    k.finish()
    return k


_CONST_CACHE = {}


def host_shared(inp):
    f = lambda a: np.ascontiguousarray(np.asarray(a, dtype=np.float32))
    sh = {}
    if "c" not in _CONST_CACHE:
        c = {}
        c.update(rope_tables())
        c.update(misc_consts())
        _CONST_CACHE["c"] = c
    sh.update(_CONST_CACHE["c"])
    sh["mod_w"] = f(inp["mod_w"])
    sh["mod_b"] = f(inp["mod_b"])
    sh["mod_bT"] = f(np.asarray(inp["mod_b"]).reshape(NL, 48, 128).transpose(0, 2, 1))
    sh["n1wT"] = f(np.asarray(inp["norm1_w"]).reshape(NL, 8, 128).transpose(0, 2, 1))
    sh["n2wT"] = f(np.asarray(inp["norm2_w"]).reshape(NL, 8, 128).transpose(0, 2, 1))
    sh["w_in"] = f(inp["w_in"])
    sh["qnT"] = f(np.asarray(inp["mla_q_norm"]).reshape(NL, 2, 128).transpose(0, 2, 1))
    sh["kvnT"] = f(np.asarray(inp["mla_kv_norm"]).reshape(NL, 128, 1))
    sh["gqnT"] = f(np.tile(np.asarray(inp["gqa_q_norm"]), (1, 2)).reshape(NL, 128, 1))
    sh["gknT"] = f(np.tile(np.asarray(inp["gqa_k_norm"]), (1, 2)).reshape(NL, 128, 1))
    sh["mla_w_uq"] = f(inp["mla_w_uq"])
    sh["mla_w_ukv"] = f(inp["mla_w_ukv"])
    sh["nabm"] = na_biasmask(np.asarray(inp["na_bias"], dtype=np.float32))
    sh["ret_decay_logit"] = f(np.asarray(inp["ret_decay_logit"]).reshape(NL, 8))
    sh["ret_gn_w"] = f(inp["ret_gn_w"])
    sh["w_branch"] = f(inp["w_branch"])
    sh["w_out"] = f(inp["w_out"])
    sh["peer_w_q"] = f(inp["peer_w_q"])
    pk = np.asarray(inp["peer_keys"], dtype=np.float32)
    kb = np.zeros((NL, 8, 128, 256), np.float32)
    kb[:, :, 0:64, 0:128] = pk[:, :, 0].transpose(0, 1, 3, 2)
    kb[:, :, 64:128, 128:256] = pk[:, :, 1].transpose(0, 1, 3, 2)
    sh["keysbd"] = kb
    for i in range(NL):
        sh["peer_u%d" % i] = f(np.asarray(inp["peer_u"])[i])
        sh["peer_v%d" % i] = f(np.asarray(inp["peer_v"])[i])
    sh["final_norm_w"] = f(inp["final_norm_w"])
    return sh


def host_core(inp, b0, NB):
    x = np.asarray(inp["x"], dtype=np.float32)
    d = {}
    d["x"] = np.ascontiguousarray(x[b0:b0 + NB])
    d["ctx"] = np.ascontiguousarray(np.asarray(inp["ctx"], dtype=np.float32)[b0:b0 + NB])
    cc = np.concatenate([np.asarray(inp["c"], dtype=np.float32)[b0:b0 + NB], np.asarray(inp["c_ctx"], dtype=np.float32)[None, :]], 0)
    d["cT3"] = np.ascontiguousarray(cc.reshape(NB + 1, 8, 128).transpose(2, 1, 0))
    return d


_PROG = {}


def kernel(**inputs):
    NB = 2
    if "k" not in _PROG:
        _PROG["k"] = build(NB=NB)
    k = _PROG["k"]
    sh = host_shared(inputs)
    in_maps = []
    for c in range(NCORES):
        m = dict(sh)
        m.update(host_core(inputs, c * NB, NB))
        in_maps.append(m)
    res = run_bass_kernel_spmd(k.nc, in_maps, core_ids=list(range(NCORES)))
    out = np.concatenate([np.asarray(r["out"]) for r in res.results], axis=0)
    return np.ascontiguousarray(out.astype(np.float32))
```

```python
import ml_dtypes
from concourse.bass_utils import run_bass_kernel_spmd
import contextlib
import numpy as np
import concourse.bass as bass
import concourse.mybir as mybir

F32 = mybir.dt.float32
BF16 = mybir.dt.bfloat16
I32 = mybir.dt.int32
U32 = mybir.dt.uint32
ALU = mybir.AluOpType
AF = mybir.ActivationFunctionType
AX = mybir.AxisListType


class Dep:
    __slots__ = ("w", "r")

    def __init__(self):
        self.w = None
        self.r = []


class View:
    __slots__ = ("t", "key", "ap")

    def __init__(self, t, key, ap):
        self.t = t
        self.key = key
        self.ap = ap


class _Sub:
    def __init__(self, t, key):
        self.t = t
        self.key = key

    def __getitem__(self, idx):
        return View(self.t, self.key, self.t.base[idx])


class T:
    def __init__(self, name, base, space):
        self.name = name
        self.base = base
        self.space = space
        self.whole = Dep()
        self.subs = {}

    def __getitem__(self, idx):
        return View(self, None, self.base[idx])

    def k(self, key):
        return _Sub(self, key)

    def v(self, ap, key=None):
        return View(self, key, ap)

    def deps(self, key):
        if key is None:
            return [self.whole] + list(self.subs.values())
        if key not in self.subs:
            self.subs[key] = Dep()
        return [self.whole, self.subs[key]]

    def own(self, key):
        if key is None:
            return self.whole
        if key not in self.subs:
            self.subs[key] = Dep()
        return self.subs[key]


class K:
    ENG = ("pe", "act", "dve", "pool", "sp")

    def __init__(self, n_dma_sems=24):
        self.nc = bass.Bass("TRN2", target_bir_lowering=False)
        self.es = contextlib.ExitStack()
        self.stack = [self.es]
        nc = self.nc
        self.eng = {"pe": nc.tensor, "act": nc.scalar, "dve": nc.vector, "pool": nc.gpsimd, "sp": nc.sync}
        self.sems = []
        self.semval = []
        self.esem = {}
        for e in ("pe", "act", "dve", "pool"):
            self.esem[e] = self._newsem("s_" + e)
        self.dsem = {}
        self.dnext = {}
        for q in ("sp", "pool", "act"):
            n = n_dma_sems if q != "act" else 8
            self.dsem[q] = [self._newsem(f"d_{q}{i}") for i in range(n)]
            self.dnext[q] = 0
        self.known = {e: {} for e in self.ENG}
        self.n_inst = 0
        self.embed = True
        self.n_wait = 0
        self._uid = 0

    def _newsem(self, name):
        h = self.es.enter_context(self.nc.semaphore(name))
        self.sems.append(h)
        self.semval.append(0)
        return len(self.sems) - 1

    def uid(self, p):
        self._uid += 1
        return f"{p}{self._uid}"

    def sb(self, name, shape, dtype):
        h = self.stack[-1].enter_context(self.nc.sbuf_tensor(self.uid("s_" + name + "_"), list(shape), dtype))
        return T(name, h, "sb")

    def ps(self, name, shape, dtype=F32):
        h = self.stack[-1].enter_context(self.nc.psum_tensor(self.uid("p_" + name + "_"), list(shape), dtype))
        return T(name, h, "ps")

    def push(self):
        self.stack.append(contextlib.ExitStack())

    def pop(self):
        self.barrier()
        self.stack.pop().close()

    def barrier(self, engines=("pe", "act", "dve", "pool", "sp")):
        for e in engines:
            for s in range(len(self.sems)):
                if self.semval[s] > 0:
                    self._wait(e, (s, self.semval[s]))

    def dram(self, name, shape, dtype, kind="Internal"):
        h = self.nc.dram_tensor(name, list(shape), dtype, kind=kind)
        return T(name, h.ap(), "dram")

    def _wait(self, e, ev):
        if ev is None:
            return
        s, v = ev
        if self.known[e].get(s, 0) >= v:
            return
        self.eng[e].wait_ge(self.sems[s], v)
        self.known[e][s] = v
        self.n_wait += 1

    def _sync_in(self, e, r, w):
        need = {}

        def add(ev):
            if ev is None:
                return
            s, v = ev
            if e == "pe" and s == self.esem["pe"]:
                return
            if need.get(s, 0) < v:
                need[s] = v

        for v in r:
            for d in v.t.deps(v.key):
                add(d.w)
        for v in w:
            for d in v.t.deps(v.key):
                add(d.w)
                for rv in d.r:
                    add(rv)
        pend = [(s, v) for s, v in need.items() if self.known[e].get(s, 0) < v]
        if self.embed and pend:
            for ev in pend[:-1]:
                self._wait(e, ev)
            return pend[-1]
        for ev in pend:
            self._wait(e, ev)
        return None

    def _sync_out(self, ev, r, w):
        for v in r:
            v.t.own(v.key).r.append(ev)
            if len(v.t.own(v.key).r) > 64:
                mx = {}
                for s, val in v.t.own(v.key).r:
                    mx[s] = max(mx.get(s, 0), val)
                v.t.own(v.key).r = list(mx.items())
        for v in w:
            if v.key is None:
                v.t.subs = {}
            d = v.t.own(v.key)
            d.w = ev
            d.r = []

    def op(self, e, fn, r=(), w=()):
        r = [x for x in r if isinstance(x, View)]
        w = [x for x in w if isinstance(x, View)]
        emb = self._sync_in(e, r, w)
        ins = fn(self.eng[e])
        if emb is not None:
            ins._wait_ge(self.sems[emb[0]], emb[1])
            self.known[e][emb[0]] = emb[1]
        s = self.esem[e]
        self.semval[s] += 1
        ins.then_inc(self.sems[s], 1)
        ev = (s, self.semval[s])
        self._sync_out(ev, r, w)
        self.n_inst += 1
        return ins

    def dma(self, out, in_, q="sp", fn=None, extra_r=()):
        r = [in_] + list(extra_r)
        w = [out]
        pool = self.dsem[q]
        i = self.dnext[q]
        self.dnext[q] = (i + 1) % len(pool)
        s = pool[i]
        self._wait(q, (s, self.semval[s]))
        emb = self._sync_in(q, r, w)
        if fn is None:
            ins = self.eng[q].dma_start(out=out.ap, in_=in_.ap)
        else:
            ins = fn(self.eng[q])
        if emb is not None:
            ins._wait_ge(self.sems[emb[0]], emb[1])
            self.known[q][emb[0]] = emb[1]
        self.semval[s] += 16
        ins.then_inc(self.sems[s], 16)
        ev = (s, self.semval[s])
        self._sync_out(ev, r, w)
        self.n_inst += 1
        return ins

    def wait_all(self, e, views):
        for v in views:
            for d in v.t.deps(v.key):
                self._wait(e, d.w)

    def mm(self, out, lhsT, rhs, start=True, stop=True, **kw):
        return self.op("pe", lambda e: e.matmul(out.ap, lhsT.ap, rhs.ap, start=start, stop=stop, **kw),
                       r=[lhsT, rhs] + ([] if start else [out]), w=[out])

    def tr(self, out, in_, ident):
        return self.op("pe", lambda e: e.transpose(out.ap, in_.ap, ident.ap), r=[in_, ident], w=[out])

    def act(self, out, in_, func, bias=None, scale=None, accum=None, e="act"):
        kw = {}
        r = [in_]
        w = [out]
        if bias is not None:
            kw["bias"] = bias.ap if isinstance(bias, View) else bias
            r.append(bias)
        if scale is not None:
            kw["scale"] = scale.ap if isinstance(scale, View) else scale
            r.append(scale)
        if accum is not None:
            kw["accum_out"] = accum.ap
            w.append(accum)
        return self.op(e, lambda en: en.activation(out=out.ap, in_=in_.ap, func=func, **kw), r=r, w=w)

    def tt(self, out, a, b, op, e="dve"):
        return self.op(e, lambda en: en.tensor_tensor(out.ap, a.ap, b.ap, op), r=[a, b], w=[out])

    def ts(self, out, a, s1, op0, s2=None, op1=None, e="dve", accum=None):
        r = [a, s1, s2]
        w = [out]
        kw = {}
        if accum is not None:
            kw["accum_out"] = accum.ap
            w.append(accum)
        g = lambda s: s.ap if isinstance(s, View) else s
        if op1 is None:
            return self.op(e, lambda en: en.tensor_scalar(out.ap, a.ap, g(s1), None, op0, **kw), r=r, w=w)
        return self.op(e, lambda en: en.tensor_scalar(out.ap, a.ap, g(s1), g(s2), op0, op1, **kw), r=r, w=w)

    def stt(self, out, a, s, b, op0, op1, e="dve", accum=None):
        g = lambda x: x.ap if isinstance(x, View) else x
        w = [out]
        kw = {}
        if accum is not None:
            kw["accum_out"] = accum.ap
            w.append(accum)
        return self.op(e, lambda en: en.scalar_tensor_tensor(out.ap, a.ap, g(s), b.ap, op0, op1, **kw),
                       r=[a, s, b], w=w)

    def copy(self, out, in_, e="dve"):
        if e == "act":
            return self.op(e, lambda en: en.copy(out.ap, in_.ap), r=[in_], w=[out])
        return self.op(e, lambda en: en.tensor_copy(out.ap, in_.ap), r=[in_], w=[out])

    def recip(self, out, in_):
        return self.op("dve", lambda en: en.reciprocal(out.ap, in_.ap), r=[in_], w=[out])

    def memset(self, out, val, e="dve"):
        return self.op(e, lambda en: en.memset(out.ap, val), r=[], w=[out])

    def reduce(self, out, in_, op, axis=AX.X, e="dve"):
        return self.op(e, lambda en: en.tensor_reduce(out.ap, in_.ap, axis, op), r=[in_], w=[out])

    def finish(self):
        self.barrier(engines=("sp",))

D = 1024
S = 4096
LC = 256
TT = S + LC
NL = 2
GW = 64
EPS = 1e-6
INC = 7072
NCORES = 8
BF = ml_dtypes.bfloat16
import os
PEXP = os.environ.get('PEXP', '')


class Pool:
    def __init__(self, k, name, shape, dtype, n, space="sb"):
        mk = k.sb if space == "sb" else k.ps
        self.t = [mk(f"{name}{i}", shape, dtype) for i in range(n)]
        self.i = 0

    def nxt(self):
        t = self.t[self.i]
        self.i = (self.i + 1) % len(self.t)
        return t


def rope_tables():
    t = np.arange(S)
    row = (t // GW).astype(np.float32)
    col = (t % GW).astype(np.float32)

    def tab(hd):
        half = hd // 2
        h = half // 2
        freqs = (np.float32(10000.0) ** (-np.arange(h, dtype=np.float32) / np.float32(h))).astype(np.float32)
        C = np.zeros((hd, S), np.float32)
        Sn = np.zeros((hd, S), np.float32)
        for ax, pos in enumerate((row, col)):
            ang = (pos[:, None] * freqs[None, :]).astype(np.float32)
            c = np.cos(ang).astype(np.float32).T
            s = np.sin(ang).astype(np.float32).T
            o = ax * half
            C[o:o + h] = c
            C[o + h:o + 2 * h] = c
            Sn[o:o + h] = s
            Sn[o + h:o + 2 * h] = s
        return C, Sn

    def rmat(hd):
        half = hd // 2
        h = half // 2
        Rm = np.zeros((hd, hd), np.float32)
        for ax in range(2):
            o = ax * half
            for i in range(h):
                Rm[o + i, o + h + i] = -1.0
                Rm[o + h + i, o + i] = 1.0
        return Rm.T.copy()

    Cg, Sg = tab(64)
    Cm, Sm = tab(32)
    out = {}
    out["ropeCg"] = np.concatenate([Cg, Cg], 0)
    out["ropeSg"] = np.concatenate([Sg, Sg], 0)
    z = np.zeros((32, S), np.float32)
    out["ropeCm"] = np.concatenate([Cm, z, Cm], 0)
    out["ropeSm"] = np.concatenate([Sm, z, Sm], 0)
    Rg = np.zeros((128, 128), np.float32)
    Rg[0:64, 0:64] = rmat(64)
    Rg[64:128, 64:128] = rmat(64)
    Rq = np.zeros((96, 96), np.float32)
    Rq[64:96, 64:96] = rmat(32)
    Rq[0:32, 0:32] = rmat(32)
    out["Rg"] = Rg.astype(BF)
    out["Rq"] = Rq.astype(BF)
    return out


def misc_consts():
    out = {}
    out["ident_bf"] = np.eye(128, dtype=np.float32).astype(BF)
    out["ident_f"] = np.eye(128, dtype=np.float32)
    out["ones_f"] = np.ones((128, 128), np.float32)
    ob = np.zeros((128, 128), np.float32)
    ob[0:64, 0:64] = 1.0
    ob[64:, 64:] = 1.0
    out["onesblk_f"] = ob
    io = np.zeros((128, 8, 16, 16), np.float32)
    io[:] = np.arange(16, dtype=np.float32)[None, None, None, :]
    out["iota4"] = io
    m = np.arange(128)[:, None]
    n = np.arange(128)[None, :]
    Df = np.where(n >= m, (n - m), 100000).astype(np.float32)
    Db = np.where(n <= m, (m - n), 100000).astype(np.float32)
    out["retD"] = np.stack([Df, Db], 0)
    qa = np.zeros((2, 128, 128), np.float32)
    qa[0] = (np.arange(128) + 1)[None, :]
    qa[1] = (128 - np.arange(128))[None, :]
    out["retQA"] = qa
    ka = np.zeros((128, 2), np.float32)
    ka[:, 0] = 127 - np.arange(128)
    ka[:, 1] = np.arange(128)
    out["retKA"] = ka
    return out


def na_biasmask(na_bias):
    L = na_bias.shape[0]
    out = np.full((L, 4, 3, 8, 128, 512), -30000.0, np.float32)
    for pat, qg in enumerate((0, 3, 7)):
        r0 = 8 * qg
        kr0 = int(np.clip(r0 - 4, 0, 48))
        r = r0 + np.arange(8)
        rs = np.clip(r - 4, 0, 56)
        c = np.arange(64)
        cs = np.clip(c - 8, 0, 48)
        for kc in range(8):
            kr = kr0 + 2 * kc + np.arange(2)
            KR = kr[:, None, None, None]
            CP = c[None, :, None, None]
            R = r[None, None, :, None]
            RS = rs[None, None, :, None]
            C = c[None, None, None, :]
            CS = cs[None, None, None, :]
            valid = (KR >= RS) & (KR < RS + 8) & (CP >= CS) & (CP < CS + 16)
            rel_r = np.clip(KR - R + 7, 0, 14)
            rel_c = np.clip(CP - C + 15, 0, 30)
            rel_r, rel_c, valid = np.broadcast_arrays(rel_r, rel_c, valid)
            for l in range(L):
                for h in range(4):
                    vals = na_bias[l, h][rel_r, rel_c]
                    out[l, h, pat, kc] = np.where(valid, vals, np.float32(-30000.0)).reshape(128, 512)
    return out


def build(NB=2, layers=(0, 1), dbg=(), stop=None):
    k = K()
    nc = k.nc
    dbg = set(dbg)
    IN = lambda name, shape, dt=F32: k.dram(name, shape, dt, kind="ExternalInput")

    def SCR(name, shape, dt=BF16):
        return k.dram(name, shape, dt, kind=("ExternalOutput" if name in dbg else "Internal"))

    x_in = IN("x", [NB, S, D])
    ctx_in = IN("ctx", [NB, LC, D])
    cT3 = IN("cT3", [128, 8, NB + 1])
    mod_w = IN("mod_w", [NL, D, 6 * D])
    mod_bT = IN("mod_bT", [NL, 128, 48])
    mod_b = IN("mod_b", [NL, 6 * D])
    n1wT = IN("n1wT", [NL, 128, 8])
    n2wT = IN("n2wT", [NL, 128, 8])
    w_in = IN("w_in", [NL, D, INC])
    qnT = IN("qnT", [NL, 128, 2])
    kvnT = IN("kvnT", [NL, 128, 1])
    gqnT = IN("gqnT", [NL, 128, 1])
    gknT = IN("gknT", [NL, 128, 1])
    wuq = IN("mla_w_uq", [NL, 256, 384])
    wukv = IN("mla_w_ukv", [NL, 128, 512])
    nabm = IN("nabm", [NL, 4, 3, 8, 128, 512])
    rdl = IN("ret_decay_logit", [NL, 8])
    gnw = IN("ret_gn_w", [NL, 256])
    w_branch = IN("w_branch", [NL, 4, 256, D])
    w_out = IN("w_out", [NL, D, D])
    peer_wq = IN("peer_w_q", [NL, D, D])
    keysbd = IN("keysbd", [NL, 8, 128, 256])
    peer_u = [IN("peer_u%d" % i, [16384, D]) for i in range(NL)]
    peer_v = [IN("peer_v%d" % i, [16384, D]) for i in range(NL)]
    fnw = IN("final_norm_w", [D])
    c_ident_bf = IN("ident_bf", [128, 128], BF16)
    c_ident_f = IN("ident_f", [128, 128])
    c_ones_f = IN("ones_f", [128, 128])
    c_onesblk = IN("onesblk_f", [128, 128])
    c_iota4 = IN("iota4", [128, 8, 16, 16])
    c_retD = IN("retD", [2, 128, 128])
    c_retQA = IN("retQA", [2, 128, 128])
    c_retKA = IN("retKA", [128, 2])
    c_Cg = IN("ropeCg", [128, S])
    c_Sg = IN("ropeSg", [128, S])
    c_Cm = IN("ropeCm", [96, S])
    c_Sm = IN("ropeSm", [96, S])
    c_Rg = IN("Rg", [128, 128], BF16)
    c_Rq = IN("Rq", [96, 96], BF16)
    out_d = k.dram("out", [NB, S, D], F32, kind="ExternalOutput")

    xres = SCR("xres", [NB, S, D], F32)
    cres = SCR("cres", [NB, LC, D], F32)
    wb_in = SCR("wb_in", [NL, D, INC])
    wb_uq = SCR("wb_uq", [NL, 256, 384])
    wb_ukv = SCR("wb_ukv", [NL, 128, 512])
    wb_br = SCR("wb_br", [NL, 4, 256, D])
    wb_out = SCR("wb_out", [NL, D, D])
    wb_pq = SCR("wb_pq", [NL, D, D])
    wb_keys = SCR("wb_keys", [NL, 8, 128, 256])
    uvb = [SCR("uvb%d" % i, [16384, 2 * D]) for i in range(NL)]
    nabb = SCR("nabb", [NL, 4, 3, 8, 128, 512])
    mqT = SCR("mqT", [384, NB, TT])
    mknT = SCR("mknT", [256, NB, TT])
    mkpT = SCR("mkpT", [32, NB, TT])
    mv = SCR("mv", [NB, TT, 256])
    gqT = SCR("gqT", [256, NB, TT])
    gkT = SCR("gkT", [128, NB, TT])
    gv = SCR("gv", [NB, TT, 128])
    nqT = SCR("nqT", [256, NB, TT])
    nkT = SCR("nkT", [256, NB, TT])
    nv = SCR("nv", [NB, TT, 256])
    rqT = SCR("rqT", [256, NB, TT])
    rkT = SCR("rkT", [256, NB, TT])
    rkv = SCR("rkv", [NB, TT, 512])
    rg = SCR("rg", [NB, TT, 512])
    gT = SCR("gT", [4096, NB, TT])
    oT = SCR("oT", [1024, NB, TT])

    ident_bf = k.sb("ident_bf", [128, 128], BF16)
    ident_f = k.sb("ident_f", [128, 128], F32)
    ones_f = k.sb("ones_f", [128, 128], F32)
    onesblk = k.sb("onesblk", [128, 128], F32)
    Rg = k.sb("Rg", [128, 128], BF16)
    Rq = k.sb("Rq", [96, 96], BF16)
    for sbt, dr in ((ident_bf, c_ident_bf), (ident_f, c_ident_f), (ones_f, c_ones_f), (onesblk, c_onesblk), (Rg, c_Rg)):
        k.dma(sbt[:], dr[:, :])
    k.dma(Rq[:], c_Rq[:, :])

    psA = Pool(k, "psA", [128, 512], F32, 6, space="ps")
    psB = Pool(k, "psB", [128, 512], F32, 2, space="ps")
    P = psA.nxt

    def bfview(pt, a, b, p0=0, p1=128):
        return pt.v(pt.base.bitcast(BF16)[p0:p1, a:b])

    evn = [0]

    def evac(out, in_, scale=None, func=None):
        evn[0] += 1
        if func is not None:
            return k.act(out, in_, func, scale=scale)
        if evn[0] % 2 == 0:
            return k.act(out, in_, AF.Copy, scale=scale)
        if scale is None:
            return k.copy(out, in_)
        return k.ts(out, in_, scale, ALU.mult)

    CV = {}
    cvn = [0]

    def conv_pools(nbuf, engs):
        CV["f"] = Pool(k, "cvf", [128, 2048], F32, nbuf)
        CV["b"] = Pool(k, "cvb", [128, 2048], BF16, nbuf)
        CV["e"] = engs

    def conv_gen(l):
        def flat(ap, pat, **kw):
            return ap.rearrange(pat, **kw)
        yield from convert(flat(w_in.base[l], "(p a) c -> p (a c)", p=128), wb_in, flat(wb_in.base[l], "(p a) c -> p (a c)", p=128), 8 * INC)
        yield from convert(flat(wuq.base[l], "(p a) c -> p (a c)", p=128), wb_uq, flat(wb_uq.base[l], "(p a) c -> p (a c)", p=128), 2 * 384)
        yield from convert(wukv.base[l], wb_ukv, wb_ukv.base[l], 512)
        for i in range(4):
            yield from convert(flat(w_branch.base[l, i], "(p a) c -> p (a c)", p=128), wb_br, flat(wb_br.base[l, i], "(p a) c -> p (a c)", p=128), 2 * D)
        yield from convert(flat(w_out.base[l], "(p a) c -> p (a c)", p=128), wb_out, flat(wb_out.base[l], "(p a) c -> p (a c)", p=128), 8 * D)
        yield from convert(flat(peer_wq.base[l], "(p a) c -> p (a c)", p=128), wb_pq, flat(wb_pq.base[l], "(p a) c -> p (a c)", p=128), 8 * D)
        for h in range(8):
            yield from convert(keysbd.base[l, h], wb_keys, wb_keys.base[l, h], 256)
        for h in range(4):
            for t_ in range(3):
                for c in range(8):
                    yield from convert(nabm.base[l, h, t_, c], nabb, nabb.base[l, h, t_, c], 512)
        for a in range(128):
            yield from convert(peer_u[l].base.rearrange("(p a) c -> p a c", p=128)[:, a, :], uvb[l], uvb[l].base.rearrange("(p a) c -> p a c", p=128)[:, a, 0:D], D)
            yield from convert(peer_v[l].base.rearrange("(p a) c -> p a c", p=128)[:, a, :], uvb[l], uvb[l].base.rearrange("(p a) c -> p a c", p=128)[:, a, D:2 * D], D)

    ro = T("ro", None, "dram")

    def RO(ap):
        return View(ro, "r", ap)

    def convert(src_ap, dst_t, dst_ap, nper):
        for c0 in range(0, nper, 2048):
            n = min(2048, nper - c0)
            f = CV["f"].nxt()
            b = CV["b"].nxt()
            k.dma(f[:, 0:n], RO(src_ap[:, c0:c0 + n]))
            e = CV["e"][cvn[0] % len(CV["e"])]
            cvn[0] += 1
            k.copy(b[:, 0:n], f[:, 0:n], e=e)
            k.dma(dst_t.v(dst_ap[:, c0:c0 + n], key=("cv", cvn[0])), b[:, 0:n], q="pool")
            yield

    k.push()
    conv_pools(8, ("dve", "act"))
    for _ in conv_gen(layers[0]):
        pass
    k.pop()

    modF = k.sb("modF", [128, 48, NB + 1], F32)
    A1 = k.sb("A1", [128, 8, NB + 1], F32)
    A2 = k.sb("A2", [128, 8, NB + 1], F32)
    grep = k.sb("grep", [128, 2, NB + 1, D], F32)
    cT = k.sb("cT", [128, 8, NB + 1], F32)
    k.dma(cT[:], cT3[:, :, :])
    k.act(cT[:], cT[:], AF.Silu)
    mbT = k.sb("mbT", [128, 48], F32)
    nwT = k.sb("nwT", [128, 2, 8], F32)

    def phase_mod(l):
        k.push()
        mwp = Pool(k, "mw", [128, 8, 512], F32, 2)
        mbrep = k.sb("mbrep", [128, 512], F32)
        cTb = k.sb("cTb", [128, NB + 1, 8, 128], F32)
        for col in range(NB + 1):
            for j in range(8):
                k.copy(cTb[:, col, j, :], cT.v(cT.base[:, j, col:col + 1].to_broadcast([128, 128])))
        k.dma(mbT[:], mod_bT[l])
        k.dma(nwT[:, 0, :], n1wT[l])
        k.dma(nwT[:, 1, :], n2wT[l])
        mwv = mod_w.base[l].rearrange("(j p) c -> p j c", p=128)
        fm_blocks = {0: 0, 1: 4, 2: 8, 3: 12, 6: 24, 7: 28, 8: 32, 9: 36}
        g_blocks = {4: (0, 0), 5: (0, 512), 10: (1, 0), 11: (1, 512)}
        for cb in range(12):
            mw = mwp.nxt()
            k.dma(mw[:], RO(mwv[:, :, cb * 512:(cb + 1) * 512]))
            if cb in fm_blocks:
                for o4 in range(4):
                    oc = cb * 4 + o4
                    p = P()
                    for j in range(8):
                        k.mm(p[:, 0:NB + 1], mw[:, j, o4 * 128:(o4 + 1) * 128], cT[:, j, :], start=(j == 0), stop=(j == 7))
                    k.ts(modF[:, oc, :], p[:, 0:NB + 1], mbT[:, oc:oc + 1], ALU.add)
            else:
                gi, off = g_blocks[cb]
                k.dma(mbrep[:], RO(mod_b.base[l, cb * 512:(cb + 1) * 512].partition_broadcast(128)))
                for col in range(NB + 1):
                    p = P()
                    for j in range(8):
                        k.mm(p[:, :], cTb[:, col, j, :], mw[:, j, :], start=(j == 0), stop=(j == 7))
                    k.tt(grep[:, gi, col, off:off + 512], p[:, :], mbrep[:], ALU.add)
        for j in range(8):
            k.ts(A1[:, j, :], modF[:, 8 + j, :], 1.0, ALU.add)
            k.ts(A1[:, j, :], A1[:, j, :], nwT[:, 0, j:j + 1], ALU.mult)
            k.ts(A2[:, j, :], modF[:, 32 + j, :], 1.0, ALU.add)
            k.ts(A2[:, j, :], A2[:, j, :], nwT[:, 1, j:j + 1], ALU.mult)
        k.pop()

    NP = {}

    def norm_pools():
        NP["junk"] = Pool(k, "junk", [128, D], BF16, 2)
        NP["xn"] = Pool(k, "xn", [128, D], BF16, 2)
    ssp = Pool(k, "ss", [128, 2], F32, 4)

    def norm_T(xt, hxT, i, Am, Bm, col):
        ss = ssp.nxt()
        jk = NP["junk"].nxt()
        k.act(jk[:], xt[:], AF.Square, accum=ss[:, 0:1])
        k.act(ss[:, 1:2], ss[:, 0:1], AF.Sqrt, bias=EPS, scale=1.0 / D)
        k.recip(ss[:, 1:2], ss[:, 1:2])
        xn = NP["xn"].nxt()
        k.ts(xn[:], xt[:], ss[:, 1:2], ALU.mult)
        pt = P()
        for j in range(8):
            k.tr(bfview(pt, j * 128, (j + 1) * 128), xn[:, j * 128:(j + 1) * 128], ident_bf[:])
        for j in range(8):
            o = hxT[:, j, i * 128:(i + 1) * 128]
            src = bfview(pt, j * 128, (j + 1) * 128)
            if j % 2 == 0:
                k.act(o, src, AF.Identity, bias=Bm(j, col), scale=Am[:, j, col:col + 1])
            else:
                k.ts(o, src, Am[:, j, col:col + 1], ALU.mult, Bm(j, col), ALU.add)

    wuq_sb = k.sb("wuq_sb", [128, 2, 384], BF16)
    wkn_sb = k.sb("wkn_sb", [128, 4, 64], BF16)
    wkv_sb = k.sb("wkv_sb", [128, 4, 64], BF16)
    nrmw = k.sb("nrmw", [128, 5], F32)

    def fm_rstd(rsp, sqs, ones_t, nfeat, n):
        p = P()
        for c, sq in enumerate(sqs):
            k.mm(p[:, 0:n], ones_t[:], sq, start=(c == 0), stop=(c == len(sqs) - 1))
        rs = rsp.nxt()
        k.act(rs[:, 0:n], p[:, 0:n], AF.Sqrt, bias=EPS, scale=1.0 / nfeat)
        k.recip(rs[:, 0:n], rs[:, 0:n])
        return rs

    def load_layer_small(l):
        k.dma(wuq_sb[:], wb_uq.v(wb_uq.base[l].rearrange("(c p) m -> p c m", p=128)))
        wv = wb_ukv.base[l].rearrange("p (h t e) -> p h t e", h=4, t=2)
        k.dma(wkn_sb[:], wb_ukv.v(wv[:, :, 0, :]))
        k.dma(wkv_sb[:], wb_ukv.v(wv[:, :, 1, :]))
        k.dma(nrmw[:, 0:2], qnT[l])
        k.dma(nrmw[:, 2:3], kvnT[l])
        k.dma(nrmw[:, 3:4], gqnT[l])
        k.dma(nrmw[:, 4:5], gknT[l])

    def groups_for(l, with_ctx=True):
        gs = []
        for b in range(NB):
            gs.append(dict(b=b, t0=0, n=LC, lat=False, col=NB, s0=0))
            for g in range(S // 512):
                gs.append(dict(b=b, t0=LC + g * 512, n=512, lat=True, col=b, s0=g * 512))
        return gs

    def src_rows(l, g, i):
        b = g["b"]
        if g["lat"]:
            r0 = g["s0"] + i * 128
            return (RO(x_in.base[b, r0:r0 + 128, :]) if l == layers[0] and l == 0 else xres[b, r0:r0 + 128, :])
        r0 = i * 128
        return (RO(ctx_in.base[b, r0:r0 + 128, :]) if l == 0 else cres[b, r0:r0 + 128, :])

    def phase_proj(l):
        k.push()
        norm_pools()
        xpool = Pool(k, "xt", [128, D], F32, 3)
        hxTp = Pool(k, "hxT", [128, 8, 512], BF16, 2)
        wpool = Pool(k, "wblk", [128, 8, 512], BF16, 3)
        sqp = Pool(k, "sq", [128, 2, 512], F32, 2)
        rsp = Pool(k, "rs", [128, 512], F32, 2)
        nrm = Pool(k, "nrm", [128, 2, 512], BF16, 2)
        stg = Pool(k, "stg", [128, 512], BF16, 6)
        tmpf = Pool(k, "tmpf", [128, 512], F32, 4)
        ropeT = Pool(k, "ropeT", [128, 4, 512], F32, 2)
        wv = wb_in.base[l].rearrange("(j p) c -> p j c", p=128)
        blocks = [(0, 416), (416, 512), (928, 512), (1440, 512), (1952, 512), (2464, 512)] + [(2976 + 512 * i, 512) for i in range(8)]
        def norm_gen(g, box):
            n_, s0_ = g["n"], g["s0"]
            hx = hxTp.nxt()
            box["hxT"] = hx
            if g["lat"]:
                rt_ = ropeT.nxt()
                k.dma(rt_[:, 0, 0:n_], RO(c_Cg.base[:, s0_:s0_ + n_]))
                k.dma(rt_[:, 1, 0:n_], RO(c_Sg.base[:, s0_:s0_ + n_]))
                k.dma(rt_[0:96, 2, 0:n_], RO(c_Cm.base[:, s0_:s0_ + n_]))
                k.dma(rt_[0:96, 3, 0:n_], RO(c_Sm.base[:, s0_:s0_ + n_]))
                box["rt"] = rt_
            for i in range(n_ // 128):
                xt = xpool.nxt()
                k.dma(xt[:], src_rows(l, g, i))
                norm_T(xt, hx, i, A1, lambda j, c: modF[:, j, c:c + 1], g["col"])
                yield

        gs_all = groups_for(l)
        nbox = {}
        for _ in norm_gen(gs_all[0], nbox):
            pass
        for gi, g in enumerate(gs_all):
            b, t0, n, lat, col, s0 = g["b"], g["t0"], g["n"], g["lat"], g["col"], g["s0"]
            nt = n // 128
            hxT = nbox["hxT"]
            rt = nbox.get("rt")
            nbox = {}
            ngen = norm_gen(gs_all[gi + 1], nbox) if gi + 1 < len(gs_all) else iter(())

            def adv():
                next(ngen, None)

            def loadw(bi):
                c0, ncol = blocks[bi]
                wt = wpool.nxt()
                k.dma(wt[:, :, 0:ncol], wb_in.v(wv[:, :, c0:c0 + ncol]))
                return wt

            def fm(wt, c0, ncol):
                p = P()
                for j in range(8):
                    k.mm(p[0:ncol, 0:n], wt[:, j, c0:c0 + ncol], hxT[:, j, 0:n], start=(j == 0), stop=(j == 7))
                return p

            def tm(wt, c0, ncol, i):
                p = P()
                for j in range(8):
                    k.mm(p[:, 0:ncol], hxT[:, j, i * 128:(i + 1) * 128], wt[:, j, c0:c0 + ncol], start=(j == 0), stop=(j == 7))
                return p

            def store_fm(dst, r0, nr, src):
                k.dma(dst.k((b, t0, r0))[r0:r0 + nr, b, t0:t0 + n], src, q="pool")

            def store_tm(dst, i, src):
                k.dma(dst.k((b, t0, i))[b, t0 + i * 128:t0 + (i + 1) * 128, :], src, q="pool")

            def rope(xs, p0, p1, Rt, ci, si):
                R = Rt.base.shape[0]
                p2 = P()
                k.mm(p2[0:R, 0:n], Rt[:, :], xs[0:R, 0:n])
                t1 = tmpf.nxt()
                t2 = tmpf.nxt()
                k.tt(t1[p0:p1, 0:n], p2[p0:p1, 0:n], rt[p0:p1, si, 0:n], ALU.mult)
                k.tt(t2[p0:p1, 0:n], xs[p0:p1, 0:n], rt[p0:p1, ci, 0:n], ALU.mult)
                k.tt(xs[p0:p1, 0:n], t1[p0:p1, 0:n], t2[p0:p1, 0:n], ALU.add)

            w0 = loadw(0)
            w1 = loadw(1)
            pc = [fm(w0, 0, 128), fm(w0, 128, 128)]
            sq = sqp.nxt()
            for c in range(2):
                k.act(sq[:, c, 0:n], pc[c][:, 0:n], AF.Square)
            rs = fm_rstd(rsp, [sq[:, 0, 0:n], sq[:, 1, 0:n]], ones_f, 256, n)
            cqn = nrm.nxt()
            for c in range(2):
                k.stt(cqn[:, c, 0:n], pc[c][:, 0:n], nrmw[:, c:c + 1], rs[:, 0:n], ALU.mult, ALU.mult)
            for h in range(4):
                p = P()
                for c in range(2):
                    k.mm(p[0:96, 0:n], wuq_sb[:, c, h * 96:(h + 1) * 96], cqn[:, c, 0:n], start=(c == 0), stop=(c == 1))
                qa = stg.nxt()
                evac(qa[0:96, 0:n], p[0:96, 0:n])
                if lat:
                    rope(qa, 64, 96, Rq, 2, 3)
                store_fm(mqT, h * 96, 96, qa[0:96, 0:n])
            pk = fm(w0, 256, 128)
            sq = sqp.nxt()
            k.act(sq[:, 0, 0:n], pk[:, 0:n], AF.Square)
            rs = fm_rstd(rsp, [sq[:, 0, 0:n]], ones_f, 128, n)
            ckn = nrm.nxt()
            k.stt(ckn[:, 0, 0:n], pk[:, 0:n], nrmw[:, 2:3], rs[:, 0:n], ALU.mult, ALU.mult)
            for h in range(4):
                p = P()
                k.mm(p[0:64, 0:n], wkn_sb[:, h, :], ckn[:, 0, 0:n])
                kn = stg.nxt()
                evac(kn[0:64, 0:n], p[0:64, 0:n])
                store_fm(mknT, h * 64, 64, kn[0:64, 0:n])
            for i in range(nt):
                p = P()
                k.mm(p[:, 0:256], ckn[:, 0, i * 128:(i + 1) * 128], wkv_sb.v(wkv_sb.base[:, :, :].rearrange("p h e -> p (h e)")))
                vt = stg.nxt()
                evac(vt[:, 0:256], p[:, 0:256])
                store_tm(mv, i, vt[:, 0:256])
            pp = fm(w0, 384, 32)
            kp = stg.nxt()
            evac(kp[0:32, 0:n], pp[0:32, 0:n])
            if lat:
                rope(kp, 0, 32, Rq.v(Rq.base[0:32, 0:32]) if False else Rq32, 2, 3)
            store_fm(mkpT, 0, 32, kp[0:32, 0:n])
            w2 = loadw(2)
            for (c0, dst, r0, nw) in ((0, gqT, 0, 3), (128, gqT, 128, 3), (256, gkT, 0, 4)):
                p = fm(w1, c0, 128)
                sq = sqp.nxt()
                k.act(sq[:, 0, 0:n], p[:, 0:n], AF.Square)
                rs = fm_rstd(rsp, [sq[:, 0, 0:n]], onesblk, 64, n)
                xo = stg.nxt()
                k.stt(xo[:, 0:n], p[:, 0:n], nrmw[:, nw:nw + 1], rs[:, 0:n], ALU.mult, ALU.mult)
                if lat:
                    rope(xo, 0, 128, Rg, 0, 1)
                store_fm(dst, r0, 128, xo[:, 0:n])
            for i in range(nt):
                p = tm(w1, 384, 128, i)
                vt = stg.nxt()
                evac(vt[:, 0:128], p[:, 0:128])
                store_tm(gv, i, vt[:, 0:128])
            adv()
            w3 = loadw(3)
            for c in range(4):
                p = fm(w2, c * 128, 128)
                xo = stg.nxt()
                evac(xo[:, 0:n], p[:, 0:n], scale=(0.125 if c < 2 else None))
                store_fm(nqT if c < 2 else nkT, (c % 2) * 128, 128, xo[:, 0:n])
            adv()
            w4 = loadw(4)
            for i in range(nt):
                p = tm(w3, 0, 256, i)
                vt = stg.nxt()
                evac(vt[:, 0:256], p[:, 0:256])
                store_tm(nv, i, vt[:, 0:256])
            for c in range(2):
                p = fm(w3, 256 + c * 128, 128)
                xo = stg.nxt()
                evac(xo[:, 0:n], p[:, 0:n])
                store_fm(rqT, c * 128, 128, xo[:, 0:n])
            adv()
            w5 = loadw(5)
            for c in range(2):
                p = fm(w4, c * 128, 128)
                xo = stg.nxt()
                evac(xo[:, 0:n], p[:, 0:n], scale=0.125)
                store_fm(rkT, c * 128, 128, xo[:, 0:n])
            for i in range(nt):
                p = tm(w4, 0, 512, i)
                vt = stg.nxt()
                evac(vt[:, 0:256], p[:, 0:256], scale=0.125)
                evac(vt[:, 256:512], p[:, 256:512])
                store_tm(rkv, i, vt[:, 0:512])
            adv()
            wn = loadw(6)
            for i in range(nt):
                p = tm(w5, 0, 512, i)
                vt = stg.nxt()
                evac(vt[:, 0:512], p[:, 0:512])
                store_tm(rg, i, vt[:, 0:512])
            for gb in range(8):
                wc = wn
                if gb < 7:
                    wn = loadw(7 + gb)
                for c in range(4):
                    p = fm(wc, c * 128, 128)
                    xo = stg.nxt()
                    k.act(xo[:, 0:n], p[:, 0:n], AF.Sigmoid)
                    store_fm(gT, gb * 512 + c * 128, 128, xo[:, 0:n])
            for _ in ngen:
                pass
        k.pop()

    pend_epi = []

    def attn_flush():
        while pend_epi:
            pend_epi.pop(0)()

    def attn_core(pools, KT, dk, V, kchunks, q_src, nq, scale, o_dst, bias_fn=None):
        qpool, ptp, osp, obp = pools
        qt = q_src
        po = psB.nxt()
        n_ = len(kchunks)
        pss = {}

        def qk(ci):
            kc = kchunks[ci]
            ps = P()
            bm = bias_fn(kc) if bias_fn is not None else None
            k.mm(ps[:, 0:nq], KT[0:dk, kc * 128:(kc + 1) * 128], qt[0:dk, 0:nq], start=True, stop=(bm is None))
            if bm is not None:
                k.mm(ps[:, 0:nq], ident_bf[:], bm, start=False, stop=True)
            pss[ci] = ps

        qk(0)
        if n_ > 1:
            qk(1)
        attn_flush()
        for ci in range(n_):
            pt = ptp.nxt()
            k.act(pt[:, 0:nq], pss.pop(ci)[:, 0:nq], AF.Exp, scale=scale)
            if ci + 2 < n_:
                qk(ci + 2)
            k.mm(po[0:65, 0:nq], V[:, kchunks[ci], 0:65], pt[:, 0:nq], start=(ci == 0), stop=(ci == n_ - 1))

        def epi():
            os_ = osp.nxt()
            k.copy(os_[0:65, 0:nq], po[0:65, 0:nq])
            k.recip(os_[64:65, 0:nq], os_[64:65, 0:nq])
            pb = P()
            k.mm(pb[0:64, 0:nq], ones_f[64:65, 0:64], os_[64:65, 0:nq])
            ob = obp.nxt()
            k.tt(ob[0:64, 0:nq], os_[0:64, 0:nq], pb[0:64, 0:nq], ALU.mult)
            k.dma(o_dst, ob[0:64, 0:nq], q="pool")
        pend_epi.append(epi)

    def attn_pools():
        qpool = Pool(k, "aq", [96, 512], BF16, 2)
        qpoolB = Pool(k, "aqB", [128, 512], BF16, 2)
        for t_ in qpoolB.t:
            k.memset(t_[:], 0.0)
        qpool.padded = qpoolB
        ptp = Pool(k, "apt", [128, 512], BF16, 4)
        osp = Pool(k, "aos", [65, 512], F32, 2)
        obp = Pool(k, "aob", [64, 512], BF16, 2)
        return (qpool, ptp, osp, obp)

    NKC = TT // 128

    def load_V(Vt, src_t, b, c0):
        k.dma(Vt[:, :, 0:64], src_t.v(src_t.base[b, :, c0:c0 + 64].rearrange("(c p) e -> p c e", p=128)))

    def run_attn_jobs(pools, jobs, KTp, Vp, bmp=None, filler=None, nfill=0):
        qpool = pools[0]
        kvbuf = {}

        def ensure_kv(key, loader):
            if key not in kvbuf:
                KT, Vt = (KTp[1] if key[0] == "g" else KTp[0]).nxt() if isinstance(KTp, tuple) else KTp.nxt(), Vp.nxt()
                loader(KT, Vt)
                kvbuf[key] = (KT, Vt)

        def prefetch(j):
            qt = (qpool.padded if j.get("pad") else qpool).nxt()
            k.dma(qt[0:j["dk"], 0:j["nq"]], j["qsrc"])
            j["qt"] = qt
            if j.get("bmsrc") is not None:
                bm = bmp.nxt()
                k.dma(bm[:], j["bmsrc"])
                j["bm"] = bm

        ensure_kv(jobs[0]["kv"], jobs[0]["kvload"])
        prefetch(jobs[0])
        for i, j in enumerate(jobs):
            if i + 1 < len(jobs):
                prefetch(jobs[i + 1])
            if i == 0 or jobs[i - 1]["kv"] != j["kv"]:
                for j2 in jobs[i + 1:]:
                    if j2["kv"] != j["kv"]:
                        ensure_kv(j2["kv"], j2["kvload"])
                        break
                for key in [kk for kk in kvbuf if kk != j["kv"] and all(kk != j2["kv"] for j2 in jobs[i:])]:
                    del kvbuf[key]
            KT, Vt = kvbuf[j["kv"]]
            bfn = (j["bias"](j["bm"]) if j.get("bias") is not None else None)
            attn_core(pools, KT, (128 if j.get("pad") else j["dk"]), Vt, j["chunks"], j["qt"], j["nq"], j["scale"], j["odst"], bias_fn=bfn)
            if filler is not None:
                for _ in range(nfill):
                    next(filler, None)
        attn_flush()
        if filler is not None:
            for _ in filler:
                pass

    def phase_attn(l, need_ctx):
        k.push()
        pools = attn_pools()
        KTp = Pool(k, "aKT", [96, TT], BF16, 2)
        KTg = Pool(k, "aKTg", [128, TT], BF16, 2)
        for t_ in KTg.t:
            k.memset(t_[64:128, :], 0.0)
        Vp = Pool(k, "aV", [128, NKC, 65], BF16, 2)
        for Vt in Vp.t:
            k.memset(Vt[:, :, 64:65], 1.0)
        allk = list(range(NKC))
        jobs = []
        for b in range(NB):
            for h in range(4):
                def kvl(KT, Vt, b=b, h=h):
                    k.dma(KT[0:64, :], mknT[h * 64:(h + 1) * 64, b, :])
                    k.dma(KT[64:96, :], mkpT[:, b, :])
                    load_V(Vt, mv, b, h * 64)
                for qg in range(8):
                    t0 = LC + qg * 512
                    jobs.append(dict(kv=("m", b, h), kvload=kvl, dk=96, chunks=allk, qsrc=mqT[h * 96:(h + 1) * 96, b, t0:t0 + 512], nq=512,
                                     scale=96 ** -0.5, odst=oT.k((b, t0, h))[h * 64:(h + 1) * 64, b, t0:t0 + 512]))
                if need_ctx:
                    jobs.append(dict(kv=("m", b, h), kvload=kvl, dk=96, chunks=[0, 1], qsrc=mqT[h * 96:(h + 1) * 96, b, 0:LC], nq=LC,
                                     scale=96 ** -0.5, odst=oT.k((b, 0, h))[h * 64:(h + 1) * 64, b, 0:LC]))
            for kv in range(2):
                def kvl(KT, Vt, b=b, kv=kv):
                    k.dma(KT[0:64, :], gkT[kv * 64:(kv + 1) * 64, b, :])
                    load_V(Vt, gv, b, kv * 64)
                for gq in range(2):
                    h = kv * 2 + gq
                    for qg in range(8):
                        t0 = LC + qg * 512
                        jobs.append(dict(kv=("g", b, kv), kvload=kvl, pad=True, dk=64, chunks=allk, qsrc=gqT[h * 64:(h + 1) * 64, b, t0:t0 + 512], nq=512,
                                         scale=0.125, odst=oT.k((b, t0, 4 + h))[256 + h * 64:256 + (h + 1) * 64, b, t0:t0 + 512]))
                    if need_ctx:
                        jobs.append(dict(kv=("g", b, kv), kvload=kvl, pad=True, dk=64, chunks=[0, 1], qsrc=gqT[h * 64:(h + 1) * 64, b, 0:LC], nq=LC,
                                         scale=0.125, odst=oT.k((b, 0, 4 + h))[256 + h * 64:256 + (h + 1) * 64, b, 0:LC]))
        filler = None
        nxt_l = [x for x in layers if x > l]
        if nxt_l:
            conv_pools(6, ("dve",))
            filler = conv_gen(nxt_l[0])
        run_attn_jobs(pools, jobs, (KTp, KTg), Vp, filler=filler, nfill=5)
        k.pop()

    def phase_na(l, need_ctx):
        k.push()
        pools = attn_pools()
        KTp = Pool(k, "nKT", [128, TT], BF16, 2)
        for t_ in KTp.t:
            k.memset(t_[64:128, :], 0.0)
        Vp = Pool(k, "nV", [128, NKC, 65], BF16, 2)
        bmp = Pool(k, "nbm", [128, 8, 512], BF16, 3)
        for Vt in Vp.t:
            k.memset(Vt[:, :, 64:65], 1.0)
        jobs = []
        for b in range(NB):
            for h in range(4):
                def kvl(KT, Vt, b=b, h=h):
                    k.dma(KT[0:64, :], nkT[h * 64:(h + 1) * 64, b, :])
                    load_V(Vt, nv, b, h * 64)
                for qg in range(8):
                    t0 = LC + qg * 512
                    pat = 0 if qg == 0 else (2 if qg == 7 else 1)
                    kr0 = min(max(8 * qg - 4, 0), 48)
                    kc0 = 2 + kr0 // 2
                    jobs.append(dict(kv=("n", b, h), kvload=kvl, pad=True, dk=64, chunks=[0, 1] + [kc0 + j for j in range(8)],
                                     qsrc=nqT[h * 64:(h + 1) * 64, b, t0:t0 + 512], nq=512, scale=1.0,
                                     odst=oT.k((b, t0, 8 + h))[512 + h * 64:512 + (h + 1) * 64, b, t0:t0 + 512],
                                     bmsrc=nabb.v(nabb.base[l, h, pat].rearrange("c p q -> p c q")),
                                     bias=lambda bm, kc0=kc0: (lambda kc: (bm[:, kc - kc0, :] if kc >= 2 else None))))
                if need_ctx:
                    jobs.append(dict(kv=("n", b, h), kvload=kvl, pad=True, dk=64, chunks=[0, 1], qsrc=nqT[h * 64:(h + 1) * 64, b, 0:LC], nq=LC, scale=1.0,
                                     odst=oT.k((b, 0, 8 + h))[512 + h * 64:512 + (h + 1) * 64, b, 0:LC]))
        run_attn_jobs(pools, jobs, KTp, Vp, bmp)
        k.pop()

    def phase_ret(l, need_ctx):
        k.push()
        lg = k.sb("lg", [128, 8], F32)
        k.dma(lg[:], RO(rdl.base[l].partition_broadcast(128)))
        k.act(lg[:], lg[:], AF.Exp, scale=-1.0)
        k.ts(lg[:], lg[:], 1.0, ALU.add)
        k.act(lg[:], lg[:], AF.Ln)
        k.ts(lg[:], lg[:], -1.0, ALU.mult)
        Dd = k.sb("Dd", [128, 2, 128], F32)
        QA = k.sb("QA", [128, 2, 128], F32)
        KA = k.sb("KA", [128, 2], F32)
        k.dma(Dd[:], RO(c_retD.base.rearrange("d p n -> p d n")))
        k.dma(QA[:], RO(c_retQA.base.rearrange("d p n -> p d n")))
        k.dma(KA[:], RO(c_retKA.base))
        decT = k.sb("decT", [128, 2, 4, 128], F32)
        qdec = k.sb("qdec", [128, 2, 4, 128], F32)
        kdec = k.sb("kdec", [128, 2, 4], F32)
        cdec = k.sb("cdec", [128, 8], F32)
        gnr = k.sb("gnr", [128, 256], F32)
        k.dma(gnr[:], RO(gnw.base[l].partition_broadcast(128)))
        for d in range(2):
            for h in range(4):
                k.act(decT[:, d, h, :], Dd[:, d, :], AF.Exp, scale=lg[:, d * 4 + h:d * 4 + h + 1])
                k.act(qdec[:, d, h, :], QA[:, d, :], AF.Exp, scale=lg[:, d * 4 + h:d * 4 + h + 1])
            k.act(kdec[:, d, :], lg[:, d * 4:(d + 1) * 4], AF.Exp, scale=KA[:, d:d + 1])
        k.act(cdec[:], lg[:], AF.Exp, scale=128.0)
        QT = k.sb("rQT", [128, 2, TT], BF16)
        KT = k.sb("rKT", [128, 2, TT], BF16)
        KVt = k.sb("rKV", [128, NKC, 512], BF16)
        G = k.sb("rG", [128, NKC, 512], BF16)
        Ys = [k.sb("rY", [128, NKC, 256], F32), k.sb("rYb", [128, NKC, 256], BF16)]
        Sts = [k.sb("rS", [128, 2, 64], F32) for _ in range(2)]
        Sbs = [k.sb("rSb", [128, 2, 64], BF16) for _ in range(2)]
        kdp = Pool(k, "rkd", [128, 256], BF16, 2)
        idp = Pool(k, "rid", [128, 128], BF16, 3)
        qdp = Pool(k, "rqd", [128, 128], BF16, 3)
        osb = Pool(k, "ros", [128, 256], F32, 2)
        sgp = Pool(k, "rsg", [128, 256], F32, 2)
        stp = Pool(k, "rst", [128, 4], F32, 4)
        ybp = Pool(k, "ryb", [128, 256], BF16, 2)
        ysp = Pool(k, "rys", [128, 2, 128], BF16, 2)
        b3 = lambda t_, ap: t_.v(ap)
        for b in range(NB):
            for c in range(2):
                k.dma(QT[:, c, :], rqT[c * 128:(c + 1) * 128, b, :])
                k.dma(KT[:, c, :], rkT[c * 128:(c + 1) * 128, b, :])
            k.dma(KVt[:], rkv.v(rkv.base[b].rearrange("(c p) f -> p c f", p=128)))
            k.dma(G[:], rg.v(rg.base[b].rearrange("(c p) f -> p c f", p=128)))
            for d in range(2):
                k.memset(Sts[d][:], 0.0)
                k.memset(Sbs[d][:], 0.0)
            orders = [list(range(NKC)), [1, 0] + list(range(NKC - 1, 1, -1))]
            for step in range(2 * NKC):
                for _once in (0,):
                    d = step % 2
                    ci = orders[d][step // 2]
                    St, Sb, Y = Sts[d], Sbs[d], Ys[d]
                    tk = slice(ci * 128, (ci + 1) * 128)
                    Kd = kdp.nxt()
                    k.tt(b3(Kd, Kd.base[:, :].rearrange("p (h e) -> p h e", h=4)),
                         b3(KVt, KVt.base[:, ci, 0:256].rearrange("p (h e) -> p h e", h=4)),
                         b3(kdec, kdec.base[:, d, :].unsqueeze(2).to_broadcast([128, 4, 64])), ALU.mult)
                    pso = psB.nxt()
                    for h in range(4):
                        c2, po = h // 2, (h % 2) * 64
                        ps1 = P()
                        k.mm(ps1[:, 0:128], KT[po:po + 64, c2, tk], QT[po:po + 64, c2, tk])
                        idT = idp.nxt()
                        k.tt(idT[:], ps1[:, 0:128], decT[:, d, h, :], ALU.mult)
                        Qd = qdp.nxt()
                        k.tt(Qd[po:po + 64, :], QT[po:po + 64, c2, tk], qdec[po:po + 64, d, h, :], ALU.mult)
                        k.mm(pso[:, h * 64:(h + 1) * 64], idT[:], KVt[:, ci, 256 + h * 64:256 + (h + 1) * 64], start=True, stop=False)
                        k.mm(pso[:, h * 64:(h + 1) * 64], Qd[po:po + 64, :], Sb[po:po + 64, c2, :], start=False, stop=True)
                    for c2 in range(2):
                        ps2 = P()
                        k.mm(ps2[:, 0:128], Kd[:, c2 * 128:(c2 + 1) * 128], KVt[:, ci, 256 + c2 * 128:256 + (c2 + 1) * 128])
                        for hh in range(2):
                            po = hh * 64
                            h = c2 * 2 + hh
                            k.stt(St[po:po + 64, c2, :], St[po:po + 64, c2, :], cdec[po:po + 64, d * 4 + h:d * 4 + h + 1],
                                  ps2[po:po + 64, po:po + 64], ALU.mult, ALU.add)
                    k.copy(Sb[:], St[:])
                    if ci < 2 and not need_ctx:
                        continue
                    o = osb.nxt()
                    o3 = b3(o, o.base[:, :].rearrange("p (h e) -> p h e", h=4))
                    k.copy(o[:], pso[:, 0:256])
                    st = stp.nxt()
                    k.reduce(st[:], o3, ALU.add)
                    k.ts(st[:], st[:], 1.0 / 64, ALU.mult)
                    k.tt(o3, o3, b3(st, st.base[:, :].unsqueeze(2).to_broadcast([128, 4, 64])), ALU.subtract)
                    sq = sgp.nxt()
                    k.tt(sq[:], o[:], o[:], ALU.mult)
                    st2 = stp.nxt()
                    k.reduce(st2[:], b3(sq, sq.base[:, :].rearrange("p (h e) -> p h e", h=4)), ALU.add)
                    k.act(st2[:], st2[:], AF.Sqrt, bias=EPS, scale=1.0 / 64)
                    k.recip(st2[:], st2[:])
                    k.tt(o3, o3, b3(st2, st2.base[:, :].unsqueeze(2).to_broadcast([128, 4, 64])), ALU.mult)
                    k.tt(o[:], o[:], gnr[:], ALU.mult)
                    sg = sgp.nxt()
                    k.act(sg[:], G[:, ci, d * 256:(d + 1) * 256], AF.Silu)
                    k.tt(Y[:, ci, :], o[:], sg[:], ALU.mult)
            for ci in (range(NKC) if need_ctx else range(2, NKC)):
                yb = ybp.nxt()
                k.tt(yb[:], Ys[0][:, ci, :], Ys[1][:, ci, :], ALU.add)
                pt = P()
                for c2 in range(2):
                    k.tr(bfview(pt, c2 * 128, (c2 + 1) * 128), yb[:, c2 * 128:(c2 + 1) * 128], ident_bf[:])
                ys = ysp.nxt()
                k.copy(b3(ys, ys.base[:, :, :].rearrange("p c t -> p (c t)")), bfview(pt, 0, 256))
                for c2 in range(2):
                    k.dma(oT.k((b, ci, 12 + c2))[768 + c2 * 128:768 + (c2 + 1) * 128, b, ci * 128:(ci + 1) * 128], ys[:, c2, :], q="pool")
        k.pop()

    def phase_merge(l, need_ctx):
        k.push()
        wbr = k.sb("wbr", [128, 4, 2, D], BF16)
        wo = k.sb("wo", [128, 8, D], BF16)
        for i in range(4):
            k.dma(wbr[:, i, :, :], wb_br.v(wb_br.base[l, i].rearrange("(kk p) m -> p kk m", p=128)))
        k.dma(wo[:], wb_out.v(wb_out.base[l].rearrange("(j p) m -> p j m", p=128)))
        osp = Pool(k, "mo", [128, 8, 512], BF16, 2)
        mgl = Pool(k, "mgl", [128, 512], BF16, 6)
        gbp = Pool(k, "mgb", [128, 512], BF16, 4)
        accp = Pool(k, "macc", [128, 512], F32, 2)
        tmpp = Pool(k, "mtmp", [128, 512], F32, 2)
        accT = Pool(k, "maT", [128, 8, 512], BF16, 2)
        xp = Pool(k, "mx", [128, D], F32, 3)
        for g in groups_for(l):
            b, t0, n, lat, col = g["b"], g["t0"], g["n"], g["lat"], g["col"]
            if not lat and not need_ctx:
                continue
            nt = n // 128
            o_sb = osp.nxt()
            k.dma(o_sb[:, :, 0:n], oT.v(oT.base[:, b, t0:t0 + n].rearrange("(kk p) t -> p kk t", p=128)))
            aT = accT.nxt()
            pend = None
            pas = {}

            def emit_acc(pd):
                m_, i_, gb_ = pd
                k.mm(pas[m_][:, 0:n], ident_bf[:], gb_[:, 0:n], start=(i_ == 0), stop=(i_ == 3))
                if i_ == 3:
                    k.copy(aT[:, m_, 0:n], pas[m_][:, 0:n], e="act")

            for m in range(8):
                pas[m] = psB.nxt()
                for i in range(4):
                    p = P()
                    for kk in range(2):
                        k.mm(p[:, 0:n], wbr[:, i, kk, m * 128:(m + 1) * 128], o_sb[:, 2 * i + kk, 0:n], start=(kk == 0), stop=(kk == 1))
                    gl = mgl.nxt()
                    k.dma(gl[:, 0:n], gT[i * 1024 + m * 128:i * 1024 + (m + 1) * 128, b, t0:t0 + n])
                    gb_ = gbp.nxt()
                    k.tt(gb_[:, 0:n], p[:, 0:n], gl[:, 0:n], ALU.mult)
                    if pend is not None:
                        emit_acc(pend)
                    pend = (m, i, gb_)
            emit_acc(pend)
            for i in range(nt):
                xt = xp.nxt()
                k.dma(xt[:], src_rows(l, g, i))
                for hf in range(2):
                    p = P()
                    for m in range(8):
                        k.mm(p[:, :], aT[:, m, i * 128:(i + 1) * 128], wo[:, m, hf * 512:(hf + 1) * 512], start=(m == 0), stop=(m == 7))
                    tp_ = tmpp.nxt()
                    k.tt(tp_[:], p[:, :], grep[:, 0, col, hf * 512:(hf + 1) * 512], ALU.mult)
                    k.tt(xt[:, hf * 512:(hf + 1) * 512], xt[:, hf * 512:(hf + 1) * 512], tp_[:], ALU.add, e="pool")
                dst = xres.k((b, g["s0"] + i * 128))[b, g["s0"] + i * 128:g["s0"] + (i + 1) * 128, :] if lat else cres.k((b, i))[b, i * 128:(i + 1) * 128, :]
                k.dma(dst, xt[:], q="pool")
        k.pop()

    def phase_peer(l, need_ctx):
        k.push()
        norm_pools()
        wq = k.sb("pwq", [128, 8, D], BF16)
        kb = k.sb("pkb", [128, 8, 256], BF16)
        iota16 = k.sb("piota", [128, 16], F32)
        k.dma(wq[:], wb_pq.v(wb_pq.base[l].rearrange("(j p) m -> p j m", p=128)))
        k.dma(kb[:], wb_keys.v(wb_keys.base[l].rearrange("h p c -> p h c")))
        k.dma(iota16[:], RO(c_iota4.base[:, 0, 0, :]))
        xp = Pool(k, "px", [128, D], F32, 2)
        hTp = Pool(k, "phT", [128, 8, 128], BF16, 2)
        qTp = Pool(k, "pqT", [128, 128], BF16, 3)
        wkp = Pool(k, "pwk", [128, 256], F32, 2)
        up = Pool(k, "pu", [128, 2 * D], BF16, 16)
        accp = Pool(k, "pacc", [128, D], F32, 1)
        jkp = Pool(k, "pjk", [128, D], BF16, 2)
        dgp = Pool(k, "pdg", [128, 128], BF16, 4)
        b3 = lambda t_, ap: t_.v(ap)
        B2 = lambda j, c: modF[:, 24 + j, c:c + 1]
        if not breg_box:
            breg_box.append(nc.gpsimd.to_reg(16383))
        breg = breg_box[0]

        class St:
            pass
        sets = []
        for i in range(2):
            s = St()
            s.h2 = k.sb("ph2", [128, D], BF16)
            s.s_sb = k.sb("ps_sb", [128, 8, 256], F32)
            s.V1 = k.sb("pV1", [128, 8, 2, 16], F32)
            s.I1 = k.sb("pI1", [128, 8, 2, 16], U32)
            s.I1f = k.sb("pI1f", [128, 8, 2, 16], F32)
            s.cand = k.sb("pcand", [128, 8, 256], F32)
            s.CS = k.sb("pCS", [128, 8, 16], F32)
            s.POS = k.sb("pPOS", [128, 8, 16], U32)
            s.AB = k.sb("pAB", [128, 2, 8, 16], U32)
            s.ABf = k.sb("pABf", [128, 2, 8, 16], F32)
            s.isel = k.sb("pisel", [128, 2, 8, 16], F32)
            s.EI = k.sb("pEI", [128, 128], I32)
            s.gsm = k.sb("pgsm", [128, 8, 16], F32)
            s.zs = k.sb("pzs", [128, 8], F32)
            s.dots = k.sb("pdots", [128, 128], F32)
            s.wg = k.sb("pwg", [128, 128], F32)
            sets.append(s)

        def top16(vals, idx, src):
            wk = wkp.nxt()
            n_ = src.ap.shape[-1]
            v0, v1 = vals
            i0_, i1_ = idx
            k.op("dve", lambda e: e.max(out=v0.ap, in_=src.ap), r=[src], w=[v0])
            k.op("dve", lambda e: e.max_index(out=i0_.ap, in_max=v0.ap, in_values=src.ap), r=[v0, src], w=[i0_])
            k.op("dve", lambda e: e.match_replace(out=wk[:, 0:n_].ap, in_to_replace=v0.ap, in_values=src.ap, imm_value=-1e30), r=[v0, src], w=[wk[:, 0:n_]])
            k.op("dve", lambda e: e.max(out=v1.ap, in_=wk[:, 0:n_].ap), r=[wk[:, 0:n_]], w=[v1])
            k.op("dve", lambda e: e.max_index(out=i1_.ap, in_max=v1.ap, in_values=wk[:, 0:n_].ap), r=[v1, wk[:, 0:n_]], w=[i1_])

        def front(s, b, ci):
            lat = ci >= 2
            s.col = b if lat else NB
            s.rv = (xres.k((b, (ci - 2) * 128)), (b, slice((ci - 2) * 128, (ci - 1) * 128), slice(None))) if lat else (cres.k((b, ci)), (b, slice(ci * 128, (ci + 1) * 128), slice(None)))
            s.xt = xp.nxt()
            k.dma(s.xt[:], s.rv[0][s.rv[1]])
            hT = hTp.nxt()
            norm_T(s.xt, hT, 0, A2, B2, s.col)
            yield
            pt = P()
            for j in range(8):
                k.tr(bfview(pt, j * 128, (j + 1) * 128), hT[:, j, :], ident_bf[:])
            k.copy(s.h2[:], bfview(pt, 0, 1024), e="act")
            yield
            for h in range(8):
                p = P()
                for j in range(8):
                    k.mm(p[:, 0:128], wq[:, j, h * 128:(h + 1) * 128], hT[:, j, :], start=(j == 0), stop=(j == 7))
                qT = qTp.nxt()
                k.copy(qT[:], p[:, 0:128], e="act")
                p2 = P()
                k.mm(p2[:, 0:256], qT[:], kb[:, h, :])
                k.copy(s.s_sb[:, h, :], p2[:, 0:256], e="act")
                yield
            for h in range(8):
                for p_ in range(2):
                    top16((s.V1[:, h, p_, 0:8], s.V1[:, h, p_, 8:16]), (s.I1[:, h, p_, 0:8], s.I1[:, h, p_, 8:16]), s.s_sb[:, h, p_ * 128:(p_ + 1) * 128])
                yield
            k.tt(b3(s.cand, s.cand.base[:, :, :].rearrange("p h (a c) -> p h a c", a=16)),
                 b3(s.V1, s.V1.base[:, :, 0, :].unsqueeze(3).to_broadcast([128, 8, 16, 16])),
                 b3(s.V1, s.V1.base[:, :, 1, :].unsqueeze(2).to_broadcast([128, 8, 16, 16])), ALU.add)
            yield
            for h in range(8):
                top16((s.CS[:, h, 0:8], s.CS[:, h, 8:16]), (s.POS[:, h, 0:8], s.POS[:, h, 8:16]), s.cand[:, h, :])
                yield
            k.op("dve", lambda e: e.tensor_single_scalar(s.AB[:, 0, :, :].ap, s.POS[:].ap, 4, ALU.logical_shift_right), r=[s.POS[:]], w=[s.AB[:]])
            k.op("dve", lambda e: e.tensor_single_scalar(s.AB[:, 1, :, :].ap, s.POS[:].ap, 15, ALU.bitwise_and), r=[s.POS[:]], w=[s.AB[:]])
            k.copy(s.ABf[:], s.AB[:])
            k.copy(s.I1f[:], s.I1[:])
            yield
            E4 = b3(s.cand, s.cand.base[:, :, :].rearrange("p h (a c) -> p h a c", a=16))
            io = b3(iota16, iota16.base[:, :].unsqueeze(1).unsqueeze(1).to_broadcast([128, 8, 16, 16]))
            for s_ in range(2):
                k.tt(E4, b3(s.ABf, s.ABf.base[:, s_, :, :].unsqueeze(3).to_broadcast([128, 8, 16, 16])), io, ALU.is_equal)
                k.tt(E4, E4, b3(s.I1f, s.I1f.base[:, :, s_, :].unsqueeze(2).to_broadcast([128, 8, 16, 16])), ALU.mult)
                k.reduce(s.isel[:, s_, :, :], E4, ALU.add)
                yield
            k.stt(s.isel[:, 0, :, :], s.isel[:, 0, :, :], 128.0, s.isel[:, 1, :, :], ALU.mult, ALU.add)
            k.copy(b3(s.EI, s.EI.base[:, :].rearrange("p (h c) -> p h c", h=8)), s.isel[:, 0, :, :])
            k.tt(s.gsm[:], s.CS[:], b3(s.CS, s.CS.base[:, :, 0:1].to_broadcast([128, 8, 16])), ALU.subtract)
            k.act(s.gsm[:], s.gsm[:], AF.Exp)
            k.reduce(s.zs[:], s.gsm[:], ALU.add)
            k.recip(s.zs[:], s.zs[:])
            k.tt(s.gsm[:], s.gsm[:], b3(s.zs, s.zs.base[:, :].unsqueeze(2).to_broadcast([128, 8, 16])), ALU.mult)
            yield

        def back(s, nxt_gen):
            acc = accp.nxt()
            pacc = (psB.nxt(), psB.nxt())
            gflat = s.gsm.base[:, :, :].rearrange("p h c -> p (h c)")
            for g4 in range(32):
                bufs = []
                for c in range(4):
                    hk = g4 * 4 + c
                    ur = up.nxt()
                    bufs.append(ur)
                    k.dma(ur[:], uvb[l][:, :], q="pool", extra_r=[s.EI[:, hk:hk + 1]],
                          fn=lambda e, ur=ur, hk=hk: e.indirect_dma_start(out=ur[:].ap, out_offset=None, in_=uvb[l].base[:, :],
                                                                         in_offset=bass.IndirectOffsetOnAxis(ap=s.EI[:, hk:hk + 1].ap, axis=0),
                                                                         bounds_check=breg, oob_is_err=False))
                for c in range(4):
                    hk = g4 * 4 + c
                    jk = jkp.nxt()
                    k.stt(jk[:], bufs[c][:, 0:D], 1.0, s.h2[:], ALU.mult, ALU.mult, accum=s.dots[:, hk:hk + 1])
                sl = slice(g4 * 4, g4 * 4 + 4)
                k.act(s.wg[:, sl], s.dots[:, sl], AF.Gelu)
                if nxt_gen is not None:
                    next(nxt_gen, None)
                k.tt(s.wg[:, sl], s.wg[:, sl], b3(s.gsm, gflat[:, sl]), ALU.mult)
                for c in range(4):
                    hk = g4 * 4 + c
                    dg = dgp.nxt()
                    k.act(dg[:], ident_bf[:], AF.Copy, scale=s.wg[:, hk:hk + 1])
                    k.mm(pacc[0][:, :], dg[:], bufs[c][:, D:D + 512], start=(hk == 0), stop=(hk == 127))
                    k.mm(pacc[1][:, :], dg[:], bufs[c][:, D + 512:2 * D], start=(hk == 0), stop=(hk == 127))
            if nxt_gen is not None:
                for _ in nxt_gen:
                    pass
            for hf in range(2):
                k.tt(acc[:, hf * 512:(hf + 1) * 512], pacc[hf][:, :], grep[:, 1, s.col, hf * 512:(hf + 1) * 512], ALU.mult)
            k.tt(s.xt[:], s.xt[:], acc[:], ALU.add)
            k.dma(s.rv[0][s.rv[1]], s.xt[:], q="sp")

        tiles = [(b, ci) for b in range(NB) for ci in range(NKC) if (ci >= 2 or need_ctx)]
        g0 = front(sets[0], *tiles[0])
        for _ in g0:
            pass
        for ti in range(len(tiles)):
            nxt = front(sets[(ti + 1) % 2], *tiles[ti + 1]) if ti + 1 < len(tiles) else None
            back(sets[ti % 2], nxt)
        k.pop()

    breg_box = []

    def phase_final():
        k.push()
        fr = k.sb("fnr", [128, D], F32)
        k.dma(fr[:], RO(fnw.base.partition_broadcast(128)))
        xp = Pool(k, "fx", [128, D], F32, 3)
        yp = Pool(k, "fy", [128, D], F32, 3)
        for b in range(NB):
            for i in range(S // 128):
                xt = xp.nxt()
                k.dma(xt[:], xres[b, i * 128:(i + 1) * 128, :])
                ss = ssp.nxt()
                y = yp.nxt()
                k.act(y[:], xt[:], AF.Square, accum=ss[:, 0:1])
                k.act(ss[:, 1:2], ss[:, 0:1], AF.Sqrt, bias=EPS, scale=1.0 / D)
                k.recip(ss[:, 1:2], ss[:, 1:2])
                k.ts(y[:], xt[:], ss[:, 1:2], ALU.mult)
                k.tt(y[:], y[:], fr[:], ALU.mult, e="pool")
                k.dma(out_d.k((b, i))[b, i * 128:(i + 1) * 128, :], y[:], q="pool")
        k.pop()

    Rq32 = k.sb("Rq32", [32, 32], BF16)
    k.dma(Rq32[:], RO(c_Rq.base[0:32, 0:32]))

    outs = []
    for l in layers:
        phase_mod(l)
        load_layer_small(l)
        if stop == "mod":
            break
        phase_proj(l)
        if stop == "proj":
            break
        need_ctx = (l < NL - 1)
        phase_attn(l, need_ctx)
        if stop == "attn":
            break
        phase_na(l, need_ctx)
        if stop == "na":
            break
        phase_ret(l, need_ctx)
        if stop == "ret":
            break
        phase_merge(l, need_ctx)
        if stop == "merge":
            break
        phase_peer(l, need_ctx)
        if stop == "peer":
            break
    if stop is None or stop == "final":
        phase_final()

    k.finish()
    return k


_CONST_CACHE = {}


def host_shared(inp):
    f = lambda a: np.ascontiguousarray(np.asarray(a, dtype=np.float32))
    sh = {}
    if "c" not in _CONST_CACHE:
        c = {}
        c.update(rope_tables())
        c.update(misc_consts())
        _CONST_CACHE["c"] = c
    sh.update(_CONST_CACHE["c"])
    sh["mod_w"] = f(inp["mod_w"])
    sh["mod_b"] = f(inp["mod_b"])
    sh["mod_bT"] = f(np.asarray(inp["mod_b"]).reshape(NL, 48, 128).transpose(0, 2, 1))
    sh["n1wT"] = f(np.asarray(inp["norm1_w"]).reshape(NL, 8, 128).transpose(0, 2, 1))
    sh["n2wT"] = f(np.asarray(inp["norm2_w"]).reshape(NL, 8, 128).transpose(0, 2, 1))
    sh["w_in"] = f(inp["w_in"])
    sh["qnT"] = f(np.asarray(inp["mla_q_norm"]).reshape(NL, 2, 128).transpose(0, 2, 1))
    sh["kvnT"] = f(np.asarray(inp["mla_kv_norm"]).reshape(NL, 128, 1))
    sh["gqnT"] = f(np.tile(np.asarray(inp["gqa_q_norm"]), (1, 2)).reshape(NL, 128, 1))
    sh["gknT"] = f(np.tile(np.asarray(inp["gqa_k_norm"]), (1, 2)).reshape(NL, 128, 1))
    sh["mla_w_uq"] = f(inp["mla_w_uq"])
    sh["mla_w_ukv"] = f(inp["mla_w_ukv"])
    sh["nabm"] = na_biasmask(np.asarray(inp["na_bias"], dtype=np.float32))
    sh["ret_decay_logit"] = f(np.asarray(inp["ret_decay_logit"]).reshape(NL, 8))
    sh["ret_gn_w"] = f(inp["ret_gn_w"])
    sh["w_branch"] = f(inp["w_branch"])
    sh["w_out"] = f(inp["w_out"])
    sh["peer_w_q"] = f(inp["peer_w_q"])
    pk = np.asarray(inp["peer_keys"], dtype=np.float32)
    kb = np.zeros((NL, 8, 128, 256), np.float32)
    kb[:, :, 0:64, 0:128] = pk[:, :, 0].transpose(0, 1, 3, 2)
    kb[:, :, 64:128, 128:256] = pk[:, :, 1].transpose(0, 1, 3, 2)
    sh["keysbd"] = kb
    for i in range(NL):
        sh["peer_u%d" % i] = f(np.asarray(inp["peer_u"])[i])
        sh["peer_v%d" % i] = f(np.asarray(inp["peer_v"])[i])
    sh["final_norm_w"] = f(inp["final_norm_w"])
    return sh


def host_core(inp, b0, NB):
    x = np.asarray(inp["x"], dtype=np.float32)
    d = {}
    d["x"] = np.ascontiguousarray(x[b0:b0 + NB])
    d["ctx"] = np.ascontiguousarray(np.asarray(inp["ctx"], dtype=np.float32)[b0:b0 + NB])
    cc = np.concatenate([np.asarray(inp["c"], dtype=np.float32)[b0:b0 + NB], np.asarray(inp["c_ctx"], dtype=np.float32)[None, :]], 0)
    d["cT3"] = np.ascontiguousarray(cc.reshape(NB + 1, 8, 128).transpose(2, 1, 0))
    return d


_PROG = {}


def kernel(**inputs):
    NB = 2
    if "k" not in _PROG:
        _PROG["k"] = build(NB=NB)
    k = _PROG["k"]
    sh = host_shared(inputs)
    in_maps = []
    for c in range(NCORES):
        m = dict(sh)
        m.update(host_core(inputs, c * NB, NB))
        in_maps.append(m)
    res = run_bass_kernel_spmd(k.nc, in_maps, core_ids=list(range(NCORES)))
    out = np.concatenate([np.asarray(r["out"]) for r in res.results], axis=0)
    return np.ascontiguousarray(out.astype(np.float32))
```
